# Optimizing a Trainium2 kernel written in Bass

```python
import jax, jax.numpy as jnp
from jax import lax
import numpy as np

D_MODEL = 1024
BATCH = 32
SEQ = 2048
DEPTH = 1

HEAD_DIM = 64
ROPE_THETA = 10000.0
EPS = 1e-6
NEG_INF = -1e30
FORCE_SCORE = 1e9
ATTN_BLOCK = 128
NSA_HEADS = 8
NSA_KV_HEADS = 2
NSA_GROUP = NSA_HEADS // NSA_KV_HEADS
CMP_BLOCK = 32
CMP_STRIDE = 16
CMP_HIDDEN = 256
SEL_BLOCK = 64
N_SEL = 8
N_LOCAL_SEL = 2
WINDOW = 512
QUERY_CHUNK = 32
DIL_PAIRS = ((128, 1), (512, 4), (2048, 16))
DIL_HEADS_PER_GROUP = 2
DIL_HEADS = DIL_HEADS_PER_GROUP * len(DIL_PAIRS)
A_Q = NSA_HEADS * HEAD_DIM
A_KV = NSA_KV_HEADS * HEAD_DIM
DIL_W = DIL_HEADS * HEAD_DIM
IN_COLS = A_Q + 6 * A_KV + 3 * NSA_HEADS + 3 * DIL_W + 2 * D_MODEL
N_GROUPS = 4
EXPERTS_PER_GROUP = 8
N_EXPERTS = N_GROUPS * EXPERTS_PER_GROUP
TOP_K_INNER = 2
EXPERT_FF = 512
MOE_BLOCK = 256

kernel_name = "nsa_dilated_hmoe_hybrid_block"


def rms_norm(x, g):
    xf = x.astype(jnp.float32)
    y = xf * lax.rsqrt(jnp.mean(xf * xf, axis=-1, keepdims=True) + EPS)
    return (y * g.astype(jnp.float32)).astype(x.dtype)


def rope_tables(pos):
    inv = ROPE_THETA ** (-jnp.arange(0, HEAD_DIM, 2, dtype=jnp.float32) / HEAD_DIM)
    ang = pos.astype(jnp.float32)[..., None] * inv
    return jnp.cos(ang)[:, None], jnp.sin(ang)[:, None]


def apply_rope(x, cos, sin):
    x1, x2 = jnp.split(x.astype(jnp.float32), 2, axis=-1)
    return jnp.concatenate([x1 * cos - x2 * sin, x1 * sin + x2 * cos], axis=-1).astype(x.dtype)


def to_heads(t, n):
    b, s, _ = t.shape
    return t.reshape(b, s, n, HEAD_DIM).transpose(0, 2, 1, 3)


def banded_attention(q, k, v, window):
    b, hk, g, L, dh = q.shape
    blk = ATTN_BLOCK
    Lp = -(-L // blk) * blk
    nback = -(-window // blk)
    pad_end = Lp - L
    nkeys = (nback + 1) * blk
    qp = jnp.pad(q, ((0, 0), (0, 0), (0, 0), (0, pad_end), (0, 0)))
    kp = jnp.pad(k, ((0, 0), (0, 0), (nback * blk, pad_end), (0, 0)))
    vp = jnp.pad(v, ((0, 0), (0, 0), (nback * blk, pad_end), (0, 0)))
    scale = dh ** -0.5
    qi = jnp.arange(blk)
    kj = jnp.arange(nkeys) - nback * blk
    dist = qi[:, None] - kj[None, :]

    def one_block(bi):
        start = bi * blk
        qb = lax.dynamic_slice_in_dim(qp, start, blk, axis=3)
        kb = lax.dynamic_slice_in_dim(kp, start, nkeys, axis=2)
        vb = lax.dynamic_slice_in_dim(vp, start, nkeys, axis=2)
        s = jnp.einsum('bkgqd,bknd->bkgqn', qb, kb, preferred_element_type=jnp.float32) * scale
        valid = (dist >= 0) & (dist <= window) & ((start + kj)[None, :] >= 0)
        s = jnp.where(valid, s, -jnp.inf)
        lse = jax.nn.logsumexp(s, axis=-1)
        p = jnp.exp(s - lse[..., None])
        o = jnp.einsum('bkgqn,bknd->bkgqd', p.astype(vb.dtype), vb)
        return o, lse

    o, lse = lax.map(one_block, jnp.arange(Lp // blk))
    o = jnp.moveaxis(o, 0, 3).reshape(b, hk, g, Lp, dh)[:, :, :, :L]
    lse = jnp.moveaxis(lse, 0, 3).reshape(b, hk, g, Lp)[..., :L]
    return o, lse


def compress_blocks(kv, pe, w1, w2):
    s = kv.shape[2]
    n_c = (s - CMP_BLOCK) // CMP_STRIDE + 1
    idx = jnp.arange(n_c)[:, None] * CMP_STRIDE + jnp.arange(CMP_BLOCK)[None, :]
    blocks = kv[:, :, idx] + pe
    flat = blocks.reshape(*blocks.shape[:3], CMP_BLOCK * HEAD_DIM)
    return jax.nn.silu(flat @ w1) @ w2


def nsa_compressed_and_selected(q, kc, vc, k_slc, v_slc):
    b, hk, g, s_len, dh = q.shape
    n_c = kc.shape[2]
    n_s = s_len // SEL_BLOCK
    n_sel = min(N_SEL, n_s)
    scale = dh ** -0.5
    cmp_start = jnp.arange(n_c) * CMP_STRIDE
    cmp_end = cmp_start + CMP_BLOCK - 1
    sel_start = jnp.arange(n_s) * SEL_BLOCK
    overlap = jnp.clip(jnp.minimum(cmp_start[:, None] + CMP_BLOCK, sel_start[None, :] + SEL_BLOCK)
                       - jnp.maximum(cmp_start[:, None], sel_start[None, :]), 0, None).astype(jnp.float32) / CMP_BLOCK
    k_blocks = k_slc.reshape(b, hk, n_s, SEL_BLOCK, dh)
    v_blocks = v_slc.reshape(b, hk, n_s, SEL_BLOCK, dh)
    b_idx = jnp.arange(b)[:, None, None, None]
    h_idx = jnp.arange(hk)[None, :, None, None]
    blk_ids = jnp.arange(n_s)
    tok_in_blk = jnp.arange(SEL_BLOCK)

    def chunk(ci):
        start = ci * QUERY_CHUNK
        t = start + jnp.arange(QUERY_CHUNK)
        qc = lax.dynamic_slice_in_dim(q, start, QUERY_CHUNK, axis=3)
        s = jnp.einsum('bkgqd,bknd->bkgqn', qc, kc, preferred_element_type=jnp.float32) * scale
        valid = cmp_end[None, :] <= t[:, None]
        p = jnp.where(valid, jax.nn.softmax(jnp.where(valid, s, NEG_INF), axis=-1), 0.0)
        o_cmp = jnp.einsum('bkgqn,bknd->bkgqd', p.astype(vc.dtype), vc)
        imp = jnp.einsum('bkgqn,ns->bkqs', p, overlap)
        rel = (t // SEL_BLOCK)[:, None] - blk_ids[None, :]
        forced = (blk_ids[None, :] == 0) | ((rel >= 0) & (rel < N_LOCAL_SEL))
        score = jnp.where(rel < 0, NEG_INF, jnp.where(forced, FORCE_SCORE, imp))
        _, idx = lax.top_k(score, n_sel)
        kg = k_blocks[b_idx, h_idx, idx]
        vg = v_blocks[b_idx, h_idx, idx]
        tok = idx[..., None] * SEL_BLOCK + tok_in_blk
        ok = tok <= t[None, None, :, None, None]
        s2 = jnp.einsum('bkgqd,bkqnld->bkgqnl', qc, kg, preferred_element_type=jnp.float32) * scale
        p2 = jax.nn.softmax(jnp.where(ok[:, :, None], s2, -jnp.inf), axis=(-2, -1))
        o_slc = jnp.einsum('bkgqnl,bkqnld->bkgqd', p2.astype(vg.dtype), vg)
        return o_cmp, o_slc

    o_cmp, o_slc = lax.map(chunk, jnp.arange(s_len // QUERY_CHUNK))
    o_cmp = jnp.moveaxis(o_cmp, 0, 3).reshape(b, hk, g, s_len, dh)
    o_slc = jnp.moveaxis(o_slc, 0, 3).reshape(b, hk, g, s_len, dh)
    return o_cmp, o_slc


def dilated_attention(q, k, v):
    b, _, s_len, dh = q.shape
    hpg = DIL_HEADS_PER_GROUP
    outs, lses = [], []
    for gi, (w, d) in enumerate(DIL_PAIRS):
        sl = slice(gi * hpg, (gi + 1) * hpg)
        L = s_len // d

        def strided(t):
            return t[:, sl].reshape(b, hpg, L, d, dh).transpose(0, 1, 3, 2, 4).reshape(b, hpg * d, L, dh)

        o, lse = banded_attention(strided(q)[:, :, None], strided(k), strided(v), w // d)
        outs.append(o[:, :, 0].reshape(b, hpg, d, L, dh).transpose(0, 1, 3, 2, 4).reshape(b, hpg, s_len, dh))
        lses.append(lse[:, :, 0].reshape(b, hpg, d, L).transpose(0, 1, 3, 2).reshape(b, hpg, s_len))
    alpha = jax.nn.softmax(jnp.stack(lses), axis=0)
    o = jnp.stack(outs) * alpha[..., None].astype(outs[0].dtype)
    return o.transpose(1, 3, 0, 2, 4).reshape(b, s_len, DIL_W)


def hybrid_mixer(h, positions, w_in, nsa_q_norm, nsa_k_norm, cmp_pe_k, cmp_w1_k, cmp_w2_k,
                 cmp_pe_v, cmp_w1_v, cmp_w2_v, dil_q_norm, dil_k_norm, w_up_a, w_up_b, w_out):
    b, s_len, _ = h.shape
    proj = h @ w_in
    c1 = A_Q
    c2 = c1 + 6 * A_KV
    c3 = c2 + 3 * NSA_HEADS
    c4 = c3 + 3 * DIL_W
    q_a, kv_a, g_a, qkv_b, g_m = jnp.split(proj, [c1, c2, c3, c4], axis=-1)
    cos, sin = rope_tables(positions)

    q = apply_rope(rms_norm(to_heads(q_a, NSA_HEADS), nsa_q_norm), cos, sin)
    q = q.reshape(b, NSA_KV_HEADS, NSA_GROUP, s_len, HEAD_DIM)
    k_cmp, v_cmp, k_slc, v_slc, k_win, v_win = [to_heads(t, NSA_KV_HEADS) for t in jnp.split(kv_a, 6, axis=-1)]
    n_c = (s_len - CMP_BLOCK) // CMP_STRIDE + 1
    cmp_pos = positions[:, jnp.arange(n_c) * CMP_STRIDE + CMP_BLOCK - 1]
    ccos, csin = rope_tables(cmp_pos)
    kc = apply_rope(rms_norm(compress_blocks(k_cmp, cmp_pe_k, cmp_w1_k, cmp_w2_k), nsa_k_norm), ccos, csin)
    vc = compress_blocks(v_cmp, cmp_pe_v, cmp_w1_v, cmp_w2_v)
    k_slc = apply_rope(rms_norm(k_slc, nsa_k_norm), cos, sin)
    k_win = apply_rope(rms_norm(k_win, nsa_k_norm), cos, sin)
    o_cmp, o_slc = nsa_compressed_and_selected(q, kc, vc, k_slc, v_slc)
    o_win, _ = banded_attention(q, k_win, v_win, WINDOW)
    gates = jax.nn.sigmoid(g_a).reshape(b, s_len, 3, NSA_KV_HEADS, NSA_GROUP).transpose(2, 0, 3, 4, 1)[..., None]
    o_a = gates[0] * o_cmp + gates[1] * o_slc + gates[2] * o_win
    o_a = o_a.transpose(0, 3, 1, 2, 4).reshape(b, s_len, A_Q)

    q_b, k_b, v_b = [to_heads(t, DIL_HEADS) for t in jnp.split(qkv_b, 3, axis=-1)]
    q_b = apply_rope(rms_norm(q_b, dil_q_norm), cos, sin)
    k_b = apply_rope(rms_norm(k_b, dil_k_norm), cos, sin)
    o_b = dilated_attention(q_b, k_b, v_b)

    gm_a, gm_b = jnp.split(jax.nn.sigmoid(g_m), 2, axis=-1)
    y = gm_a * (o_a @ w_up_a) + gm_b * (o_b @ w_up_b)
    return y @ w_out


def hierarchical_moe(h, w_group, b_group, w_router, b_router, w_gate, w_up, w_down):
    b, s_len, d = h.shape
    T = b * s_len
    xt = h.reshape(T, d)
    g_logits = jnp.matmul(xt, w_group, preferred_element_type=jnp.float32) + b_group
    g_w, g_idx = lax.top_k(jax.nn.softmax(g_logits, axis=-1), 1)
    e_logits = jnp.einsum('td,gde->tge', xt, w_router, preferred_element_type=jnp.float32) + b_router
    e_logits = jnp.take_along_axis(e_logits, g_idx[:, :, None], axis=1)[:, 0]
    e_val, e_idx = lax.top_k(e_logits, TOP_K_INNER)
    weights = g_w * jax.nn.softmax(e_val, axis=-1)
    expert = g_idx * EXPERTS_PER_GROUP + e_idx

    A = T * TOP_K_INNER
    flat_e = expert.reshape(A)
    flat_tok = jnp.arange(A, dtype=jnp.int32) // TOP_K_INNER
    order = jnp.argsort(flat_e)
    sorted_e = flat_e[order]
    counts = jnp.bincount(flat_e, length=N_EXPERTS)
    starts = jnp.cumsum(counts) - counts
    padded = (counts + MOE_BLOCK - 1) // MOE_BLOCK * MOE_BLOCK
    pad_ends = jnp.cumsum(padded)
    pad_starts = pad_ends - padded
    dest_sorted = pad_starts[sorted_e] + jnp.arange(A, dtype=jnp.int32) - starts[sorted_e]
    P = A + N_EXPERTS * MOE_BLOCK
    n_blk = P // MOE_BLOCK
    slot_tok = jnp.full((P,), T, jnp.int32).at[dest_sorted].set(flat_tok[order])
    blk_expert = jnp.minimum(jnp.searchsorted(pad_ends, jnp.arange(n_blk) * MOE_BLOCK, side='right'), N_EXPERTS - 1)
    x_pad = jnp.concatenate([xt, jnp.zeros((1, d), xt.dtype)], axis=0)

    def run_block(args):
        tok, e = args
        xb = x_pad[tok]
        hid = jax.nn.silu(xb @ w_gate[e]) * (xb @ w_up[e])
        return hid @ w_down[e]

    y_slots = lax.map(run_block, (slot_tok.reshape(n_blk, MOE_BLOCK), blk_expert)).reshape(P, d)
    dest = jnp.zeros((A,), jnp.int32).at[order].set(dest_sorted)
    y = jnp.einsum('tkd,tk->td', y_slots[dest].reshape(T, TOP_K_INNER, d), weights.astype(xt.dtype))
    return y.reshape(b, s_len, d)


def setup_inputs(seed: int = 0) -> dict:
    key = jax.random.key(seed)
    ks = jax.random.split(key, 32)
    nrm = lambda k, shape, s: jax.random.normal(k, shape, jnp.float32) * s
    L, D = DEPTH, D_MODEL
    return {
        "x": nrm(ks[0], (BATCH, SEQ, D), 1.0),
        "c": nrm(ks[1], (BATCH, D), 1.0),
        "positions": (jnp.arange(SEQ, dtype=jnp.int32)[None, :]
                      + jax.random.randint(ks[2], (BATCH, 1), 0, 1024, dtype=jnp.int32)),
        "w_ada": nrm(ks[3], (L, D, 6 * D), 0.5 * D ** -0.5),
        "b_ada": nrm(ks[4], (L, 6 * D), 0.02),
        "norm1_g": 1.0 + nrm(ks[5], (L, D), 0.02),
        "norm2_g": 1.0 + nrm(ks[6], (L, D), 0.02),
        "w_in": nrm(ks[7], (L, D, IN_COLS), D ** -0.5),
        "nsa_q_norm": 1.0 + nrm(ks[8], (L, HEAD_DIM), 0.02),
        "nsa_k_norm": 1.0 + nrm(ks[9], (L, HEAD_DIM), 0.02),
        "cmp_pe_k": nrm(ks[10], (L, CMP_BLOCK, HEAD_DIM), 0.02),
        "cmp_w1_k": nrm(ks[11], (L, CMP_BLOCK * HEAD_DIM, CMP_HIDDEN), (CMP_BLOCK * HEAD_DIM) ** -0.5),
        "cmp_w2_k": nrm(ks[12], (L, CMP_HIDDEN, HEAD_DIM), CMP_HIDDEN ** -0.5),
        "cmp_pe_v": nrm(ks[13], (L, CMP_BLOCK, HEAD_DIM), 0.02),
        "cmp_w1_v": nrm(ks[14], (L, CMP_BLOCK * HEAD_DIM, CMP_HIDDEN), (CMP_BLOCK * HEAD_DIM) ** -0.5),
        "cmp_w2_v": nrm(ks[15], (L, CMP_HIDDEN, HEAD_DIM), CMP_HIDDEN ** -0.5),
        "dil_q_norm": 1.0 + nrm(ks[16], (L, HEAD_DIM), 0.02),
        "dil_k_norm": 1.0 + nrm(ks[17], (L, HEAD_DIM), 0.02),
        "w_up_a": nrm(ks[18], (L, A_Q, D), A_Q ** -0.5),
        "w_up_b": nrm(ks[19], (L, DIL_W, D), DIL_W ** -0.5),
        "w_out": nrm(ks[20], (L, D, D), D ** -0.5),
        "w_group": nrm(ks[21], (L, D, N_GROUPS), D ** -0.5),
        "b_group": nrm(ks[22], (L, N_GROUPS), 0.01),
        "w_router": nrm(ks[23], (L, N_GROUPS, D, EXPERTS_PER_GROUP), D ** -0.5),
        "b_router": nrm(ks[24], (L, N_GROUPS, EXPERTS_PER_GROUP), 0.01),
        "w_e_gate": nrm(ks[25], (L, N_EXPERTS, D, EXPERT_FF), D ** -0.5),
        "w_e_up": nrm(ks[26], (L, N_EXPERTS, D, EXPERT_FF), D ** -0.5),
        "w_e_down": nrm(ks[27], (L, N_EXPERTS, EXPERT_FF, D), EXPERT_FF ** -0.5),
    }


def reference(x, c, positions, w_ada, b_ada, norm1_g, norm2_g, w_in, nsa_q_norm, nsa_k_norm,
              cmp_pe_k, cmp_w1_k, cmp_w2_k, cmp_pe_v, cmp_w1_v, cmp_w2_v, dil_q_norm, dil_k_norm,
              w_up_a, w_up_b, w_out, w_group, b_group, w_router, b_router, w_e_gate, w_e_up, w_e_down):
    c_act = jax.nn.silu(c)
    for l in range(DEPTH):
        mod = (c_act @ w_ada[l] + b_ada[l])[:, None, :]
        sh1, sc1, gt1, sh2, sc2, gt2 = jnp.split(mod, 6, axis=-1)
        h = rms_norm(x, norm1_g[l]) * (1.0 + sc1) + sh1
        x = x + gt1 * hybrid_mixer(h, positions, w_in[l], nsa_q_norm[l], nsa_k_norm[l],
                                   cmp_pe_k[l], cmp_w1_k[l], cmp_w2_k[l], cmp_pe_v[l], cmp_w1_v[l], cmp_w2_v[l],
                                   dil_q_norm[l], dil_k_norm[l], w_up_a[l], w_up_b[l], w_out[l])
        h = rms_norm(x, norm2_g[l]) * (1.0 + sc2) + sh2
        x = x + gt2 * hierarchical_moe(h, w_group[l], b_group[l], w_router[l], b_router[l],
                                       w_e_gate[l], w_e_up[l], w_e_down[l])
    return x
```

```python
import numpy as np
import concourse.bass as bass
import concourse.mybir as mybir

F32 = mybir.dt.float32
BF16 = mybir.dt.bfloat16
I32 = mybir.dt.int32
U32 = mybir.dt.uint32
ALU = mybir.AluOpType
AF = mybir.ActivationFunctionType
AX = mybir.AxisListType

ENG_ATTR = {'pe': 'tensor', 'act': 'scalar', 'dve': 'vector', 'pool': 'gpsimd', 'sp': 'sync'}
ENGS = ['pe', 'act', 'dve', 'pool', 'sp']
DMA_RING = 6
_ESZ = {}


def esz(dt):
    k = str(dt)
    if k not in _ESZ:
        _ESZ[k] = mybir.dt.size(dt) if hasattr(mybir.dt, 'size') else np.dtype(mybir.dt.np(dt)).itemsize
    return _ESZ[k]


def box_of(ap):
    t = ap.tensor
    name = t.name
    pat = ap.ap
    e = esz(ap.dtype)
    space = str(ap.space)
    if 'DRAM' in space.upper() or 'HBM' in space.upper():
        lo = ap.offset
        hi = lo
        for st, n in pat:
            if st >= 0:
                hi += st * (n - 1)
            else:
                lo += st * (n - 1)
        return (name, 'D', 0, 1, lo * e, (hi + 1) * e)
    pstep, pn = pat[0]
    p0 = ap.start_partition()
    p1 = p0 + ap.partition_size()
    off = ap.offset - p0 * pstep if pstep else ap.offset
    lo = off
    hi = off
    for st, n in pat[1:]:
        if st >= 0:
            hi += st * (n - 1)
        else:
            lo += st * (n - 1)
    sp = 'P' if 'PSUM' in space.upper() else 'S'
    return (name, sp, p0, p1, lo * e, (hi + 1) * e)


class Op:
    __slots__ = ('eng', 'chan', 'pos', 'emit', 'waits', 'snap', 'inc', 'dma')


class Prog:
    def __init__(self, nc):
        self.nc = nc
        self.sems = {}
        self.semcnt = {}
        self.ops = {e: [] for e in ENGS}
        self.chan_ops = {}
        self.chan_base = {}
        self.vc = {e: {} for e in ENGS}
        self.trk = {}
        self.psum_last = {}
        self.dma_n = {e: 0 for e in ENGS}
        self._oldvals = {}
        self.n_ops = 0

    def sem(self, chan):
        if chan not in self.sems:
            nm = 's_' + (chan if isinstance(chan, str) else '%s%d' % chan)
            h = self.nc.alloc_semaphore(name=nm)
            self.sems[chan] = h
            self.semcnt[chan] = 0
        return self.sems[chan]

    def _known(self, eng, chan, pos):
        return self.vc[eng].get(chan, -1) >= pos

    def _learn(self, eng, chan, pos):
        vc = self.vc[eng]
        op = self.chan_ops[chan][pos - self.chan_base.get(chan, 0)] if pos >= self.chan_base.get(chan, 0) else None
        if op is not None and op.snap:
            for c, p in op.snap.items():
                if vc.get(c, -1) < p:
                    vc[c] = p
        if vc.get(chan, -1) < pos:
            vc[chan] = pos

    def _deps_for(self, eng, reads, writes):
        deps = set()
        for ap in reads:
            bx = box_of(ap)
            name, sp = bx[0], bx[1]
            if sp == 'P':
                self._psum_deps(eng, name, deps)
                continue
            t = self.trk.get(name)
            if t is None:
                continue
            for (wb, c, p) in t['w']:
                if wb[2] < bx[3] and bx[2] < wb[3] and wb[4] < bx[5] and bx[4] < wb[5]:
                    deps.add((c, p))
        for ap in writes:
            bx = box_of(ap)
            name, sp = bx[0], bx[1]
            if sp == 'P':
                self._psum_deps(eng, name, deps)
                continue
            t = self.trk.get(name)
            if t is None:
                continue
            for (wb, c, p) in t['w']:
                if wb[2] < bx[3] and bx[2] < wb[3] and wb[4] < bx[5] and bx[4] < wb[5]:
                    deps.add((c, p))
            for (rb, c), p in t['r'].items():
                if rb[2] < bx[3] and bx[2] < rb[3] and rb[4] < bx[5] and bx[4] < rb[5]:
                    deps.add((c, p))
        return deps

    def _psum_deps(self, eng, name, deps):
        last = self.psum_last.get(name)
        if not last:
            return
        for e, cp in last.items():
            if e == eng and eng == 'pe':
                continue
            deps.add(cp)

    def _record_access(self, eng, chan, pos, reads, writes):
        for ap in reads:
            bx = box_of(ap)
            name, sp = bx[0], bx[1]
            if sp == 'P':
                self.psum_last.setdefault(name, {})[eng] = (chan, pos)
                continue
            t = self.trk.setdefault(name, {'w': [], 'r': {}})
            t['r'][(bx, chan)] = pos
        for ap in writes:
            bx = box_of(ap)
            name, sp = bx[0], bx[1]
            if sp == 'P':
                self.psum_last.setdefault(name, {})[eng] = (chan, pos)
                continue
            t = self.trk.setdefault(name, {'w': [], 'r': {}})
            neww = []
            for ent in t['w']:
                wb = ent[0]
                if bx[2] <= wb[2] and wb[3] <= bx[3] and bx[4] <= wb[4] and wb[5] <= bx[5]:
                    continue
                neww.append(ent)
            neww.append((bx, chan, pos))
            t['w'] = neww
            if t['r']:
                t['r'] = {k: v for k, v in t['r'].items()
                          if not (bx[2] <= k[0][2] and k[0][3] <= bx[3] and bx[4] <= k[0][4] and k[0][5] <= bx[5])}

    def add(self, eng, emit, reads=(), writes=(), dma=False, extra_deps=()):
        op = Op()
        op.eng = eng
        op.emit = emit
        op.dma = dma
        op.inc = dma
        deps = self._deps_for(eng, reads, writes)
        deps.update(extra_deps)
        if dma:
            slot = self.dma_n[eng] % DMA_RING
            self.dma_n[eng] += 1
            chan = (eng, slot)
            lst = self.chan_ops.setdefault(chan, [])
            base = self.chan_base.get(chan, 0)
            if lst or base:
                deps.add((chan, base + len(lst) - 1))
        else:
            chan = eng
            lst = self.chan_ops.setdefault(chan, [])
        self.sem(chan)
        pos = self.chan_base.get(chan, 0) + len(lst)
        waits = []
        for (c, p) in sorted(deps, key=lambda cp: (str(cp[0]), cp[1])):
            if c == eng and eng == 'pe':
                continue
            if self._known(eng, c, p):
                continue
            waits.append((c, p))
        best = {}
        for c, p in waits:
            if best.get(c, -1) < p:
                best[c] = p
        op.waits = list(best.items())
        for c, p in op.waits:
            cb = self.chan_base.get(c, 0)
            if p >= cb:
                self.chan_ops[c][p - cb].inc = True
            self._learn(eng, c, p)
        op.snap = dict(self.vc[eng])
        op.chan = chan
        op.pos = pos
        lst.append(op)
        self.ops[eng].append(op)
        self._record_access(eng, chan, pos, reads, writes)
        self.n_ops += 1
        return (chan, pos)

    def barrier(self):
        lasts = []
        for chan, lst in self.chan_ops.items():
            if lst:
                lst[-1].inc = True
                lasts.append((chan, self.chan_base.get(chan, 0) + len(lst) - 1))
        for e in ENGS:
            op = Op()
            op.eng = e
            op.emit = None
            op.dma = False
            op.inc = False
            op.chan = None
            op.pos = -1
            waits = []
            for (c, p) in lasts:
                if c == e and e == 'pe':
                    continue
                if self._known(e, c, p):
                    continue
                waits.append((c, p))
            op.waits = waits
            for c, p in waits:
                self._learn(e, c, p)
            op.snap = None
            self.ops[e].append(op)

    def flush(self):
        nc = self.nc
        semval = {}
        for chan, lst in self.chan_ops.items():
            v = self.semcnt[chan]
            base = self.chan_base.get(chan, 0)
            for i, op in enumerate(lst):
                if op.inc:
                    v += 16 if op.dma else 1
                semval[(chan, base + i)] = v if op.inc else None
            self.semcnt[chan] = v
        old = self._oldvals
        old.update({k: v for k, v in semval.items() if v is not None})
        ops = self.ops
        sems = self.sems

        def run(engname):
            def f(eng):
                for op in ops[engname]:
                    for (c, p) in op.waits:
                        v = old.get((c, p))
                        assert v is not None, (engname, c, p)
                        eng.wait_ge(sems[c], v)
                    if op.emit is None:
                        continue
                    inst = op.emit(eng)
                    if op.inc:
                        inst.then_inc(sems[op.chan], 16 if op.dma else 1)
            return f

        with nc.Block() as block:
            block.tensor(run('pe'))
            block.scalar(run('act'))
            block.vector(run('dve'))
            block.gpsimd(run('pool'))
            block.sync(run('sp'))
        for chan, lst in self.chan_ops.items():
            self.chan_base[chan] = self.chan_base.get(chan, 0) + len(lst)
            self.chan_ops[chan] = []
        self.ops = {e: [] for e in ENGS}

    def dma(self, out, in_, q='sp', **kw):
        return self.add(q, lambda e: e.dma_start(out=out, in_=in_, **kw), [in_], [out], dma=True)

    def mm(self, out, lhsT, rhs, start=True, stop=True, **kw):
        return self.add('pe', lambda e: e.matmul(out, lhsT, rhs, start=start, stop=stop, **kw),
                        [lhsT, rhs], [out])

    def tr(self, out, in_, ident):
        return self.add('pe', lambda e: e.transpose(out, in_, ident), [in_, ident], [out])

    def act(self, out, in_, func, bias=None, scale=None, accum_out=None, eng='act'):
        kw = {}
        rd = [in_]
        wr = [out]
        if bias is not None:
            kw['bias'] = bias
            if not isinstance(bias, (int, float)):
                rd.append(bias)
        if scale is not None:
            kw['scale'] = scale
            if not isinstance(scale, (int, float)):
                rd.append(scale)
        if accum_out is not None:
            kw['accum_out'] = accum_out
            wr.append(accum_out)
        return self.add('act', lambda e: e.activation(out, in_, func, **kw), rd, wr)

    def tt(self, out, in0, in1, op, eng='dve'):
        return self.add(eng, lambda e: e.tensor_tensor(out, in0, in1, op), [in0, in1], [out])

    def ts(self, out, in0, s1, s2, op0, op1=None, eng='dve', accum_out=None):
        rd = [in0]
        if not isinstance(s1, (int, float)) and s1 is not None:
            rd.append(s1)
        if not isinstance(s2, (int, float)) and s2 is not None:
            rd.append(s2)
        kw = {}
        wr = [out]
        if op1 is not None:
            kw['op1'] = op1
        if accum_out is not None:
            kw['accum_out'] = accum_out
            wr.append(accum_out)
        return self.add(eng, lambda e: e.tensor_scalar(out, in0, s1, s2, op0, **kw), rd, wr)

    def stt(self, out, in0, scalar, in1, op0, op1, eng='dve'):
        rd = [in0, in1]
        if not isinstance(scalar, (int, float)):
            rd.append(scalar)
        return self.add(eng, lambda e: e.scalar_tensor_tensor(out, in0, scalar, in1, op0, op1), rd, [out])

    def copy(self, out, in_, eng='dve'):
        if eng == 'act':
            return self.add('act', lambda e: e.copy(out, in_), [in_], [out])
        return self.add(eng, lambda e: e.tensor_copy(out, in_), [in_], [out])

    def reduce(self, out, in_, op, axis=AX.X, eng='dve'):
        return self.add(eng, lambda e: e.tensor_reduce(out, in_, axis, op), [in_], [out])

    def memset(self, ap, val, eng='dve'):
        return self.add(eng, lambda e: e.memset(ap, val), [], [ap])

    def recip(self, out, in_, eng='dve'):
        return self.add(eng, lambda e: e.reciprocal(out, in_), [in_], [out])

    def max8(self, out, in_):
        return self.add('dve', lambda e: e.max(out, in_), [in_], [out])
from concourse.bass_utils import run_bass_kernel_spmd
from contextlib import ExitStack

S = 2048
D = 1024
DH = 64
NT = S // 128
IN_COLS = 4504
EPS = 1e-6
BIG = 30000.0
MAGIC = 12582912.0
TWO_PI = 6.283185307179586
NEXP = 32
FF = 512
MB = 256


def host_consts():
    c = {}
    c['identf'] = np.eye(128, dtype=np.float32)
    inv = (10000.0 ** (-np.arange(0, 64, 2, dtype=np.float32) / 64)).astype(np.float32)
    c['invf'] = np.tile(inv[None, :], (128, 1)).astype(np.float32)
    k = np.arange(128)[:, None]
    q = np.arange(128)[None, :]
    tri = (k <= q).astype(np.float32)
    anti = (k >= q).astype(np.float32)
    c['tri'] = tri
    c['anti'] = anti
    c['trianti'] = np.concatenate([tri, anti], axis=1)
    c['tri4'] = np.tile(tri, (1, 4))
    t = np.arange(S)
    cv = np.zeros((128, S), np.float32)
    cv[:127] = ((np.arange(127) * 16 + 31)[:, None] <= t[None, :])
    c['cmpvalid'] = cv
    c['blkoh'] = (np.arange(32)[:, None] == (t // 64)[None, :]).astype(np.float32)
    cs = np.arange(127) * 16
    ss = np.arange(32) * 64
    ov = np.clip(np.minimum(cs[:, None] + 32, ss[None, :] + 64) - np.maximum(cs[:, None], ss[None, :]), 0, None) / 32.0
    ovp = np.zeros((128, 32), np.float32)
    ovp[:127] = ov
    c['overlap'] = ovp
    b = (t // 64)[:, None]
    s = np.arange(32)[None, :]
    cand = ((s >= 1) & (s <= b - 2)).astype(np.float32)
    forced = (((s == 0) | (s == b) | (s == b - 1)) & (s <= b)).astype(np.float32)
    tm = lambda a: np.ascontiguousarray(a.reshape(NT, 128, 32).transpose(1, 0, 2)).astype(np.float32)
    c['cand'] = tm(cand)
    c['candm1'] = tm(cand - 1.0)
    c['forced'] = tm(forced)
    c['lstrict'] = (k < q).astype(np.float32)
    c['ones'] = np.ones((128, 128), np.float32)
    return c


class KB:
    def __init__(self, NSEQ, dbg=None):
        self.NSEQ = NSEQ
        self.NTOK = NSEQ * S
        self.NTT = NSEQ * NT
        self.NBLK = (self.NTOK * 2) // MB + NEXP
        self.NSLOT = self.NBLK * MB
        self.dbg = dbg or {}
        self.nc = bass.Bass("TRN2", target_bir_lowering=False)
        self.P = Prog(self.nc)
        self.din = {}
        self.consts = host_consts()
        blk = np.arange(self.NBLK, dtype=np.float32)[:, None] * MB
        self.consts['thr'] = np.tile(np.tile(blk, (1, 32)).reshape(1, -1), (128, 1)).astype(np.float32)
        p = np.arange(128, dtype=np.float32)[:, None]
        self.consts['rowoff'] = np.concatenate([np.arange(8)[None, :] * 128 + p, np.arange(4)[None, :] * 128 + p], axis=1).astype(np.float32)

    def dram_in(self, name, shape, dt=F32):
        h = self.nc.dram_tensor(name, list(shape), dt, kind="ExternalInput")
        self.din[name] = h
        return h.ap()

    def declare(self):
        NSEQ = self.NSEQ
        nc = self.nc
        d = self.dram_in
        self.x = d('x', [self.NTOK, D])
        self.c = d('c', [NSEQ, D])
        self.pos = d('positions', [NSEQ, S], I32)
        self.w_ada = d('w_ada', [1, D, 6 * D])
        self.b_ada = d('b_ada', [1, 6 * D])
        self.norm1_g = d('norm1_g', [1, D])
        self.norm2_g = d('norm2_g', [1, D])
        self.w_in = d('w_in', [1, D, IN_COLS])
        self.nsa_q_norm = d('nsa_q_norm', [1, DH])
        self.nsa_k_norm = d('nsa_k_norm', [1, DH])
        self.cmp_pe_k = d('cmp_pe_k', [1, 32, DH])
        self.cmp_w1_k = d('cmp_w1_k', [1, 2048, 256])
        self.cmp_w2_k = d('cmp_w2_k', [1, 256, DH])
        self.cmp_pe_v = d('cmp_pe_v', [1, 32, DH])
        self.cmp_w1_v = d('cmp_w1_v', [1, 2048, 256])
        self.cmp_w2_v = d('cmp_w2_v', [1, 256, DH])
        self.dil_q_norm = d('dil_q_norm', [1, DH])
        self.dil_k_norm = d('dil_k_norm', [1, DH])
        self.w_up_a = d('w_up_a', [1, 512, D])
        self.w_up_b = d('w_up_b', [1, 384, D])
        self.w_out = d('w_out', [1, D, D])
        self.w_group = d('w_group', [1, D, 4])
        self.b_group = d('b_group', [1, 4])
        self.w_router = d('w_router', [1, 4, D, 8])
        self.b_router = d('b_router', [1, 4, 8])
        self.w_e_gate = d('w_e_gate', [1, NEXP, D, FF])
        self.w_e_up = d('w_e_up', [1, NEXP, D, FF])
        self.w_e_down = d('w_e_down', [1, NEXP, FF, D])
        self.cd = {}
        for k, v in self.consts.items():
            self.cd[k] = d('k_' + k, v.shape)
        self.out = nc.dram_tensor('out', [self.NTOK, D], F32, kind="ExternalOutput").ap()
        sc = lambda n, shp, dt: nc.dram_tensor(n, list(shp), dt, kind="Internal").ap()
        self.mod_d = sc('mod_d', [NSEQ, 6 * D], F32)
        self.x1_d = sc('x1_d', [self.NTOK, D], F32)
        self.h2_d = sc('h2_d', [self.NTOK, D], BF16)
        self.xs_d = sc('xs_d', [self.NSLOT, D], BF16)
        self.ys_d = sc('ys_d', [self.NSLOT, D], F32)
        self.wg_l = nc.dram_tensor('wg_l', [NEXP * 128, 8 * FF], BF16, kind="Internal").ap()
        self.wu_l = nc.dram_tensor('wu_l', [NEXP * 128, 8 * FF], BF16, kind="Internal").ap()
        self.wd_l = nc.dram_tensor('wd_l', [NEXP * 128, 4 * D], BF16, kind="Internal").ap()
        self.conv_list = []
        for e_ in range(NEXP):
            rows = slice(e_ * 128, (e_ + 1) * 128)
            self.conv_list.append((self.wg_l[rows, :].rearrange('p (k f) -> p k f', f=FF),
                                   self.w_e_gate[0, e_].rearrange('(k p) f -> p k f', p=128)))
            self.conv_list.append((self.wu_l[rows, :].rearrange('p (k f) -> p k f', f=FF),
                                   self.w_e_up[0, e_].rearrange('(k p) f -> p k f', p=128)))
            self.conv_list.append((self.wd_l[rows, :].rearrange('p (k f) -> p k f', f=D),
                                   self.w_e_down[0, e_].rearrange('(k p) f -> p k f', p=128)))
        self.conv_pos = 0
        self.w_nsa_d = sc('w_nsa_d', [128, 8 * 1304], BF16)
        self.w_dil_d = sc('w_dil_d', [128, 8 * 1152], BF16)
        self.w_gm_d = sc('w_gm_d', [128, 8 * 2048], BF16)
        self.w_upa_d = sc('w_upa_d', [128, 4 * D], BF16)
        self.w_upb_d = sc('w_upb_d', [128, 3 * D], BF16)
        self.w_o_d = sc('w_o_d', [128, 8 * D], BF16)
        self.dbg_out = {}
        for k, shp in self.dbg.items():
            self.dbg_out[k] = nc.dram_tensor('dbg_' + k, list(shp), F32, kind="ExternalOutput").ap()

    def T(self, st, name, shape, dt):
        self._uid = getattr(self, '_uid', 0) + 1
        return st.enter_context(self.nc.sbuf_tensor('%s_%d' % (name, self._uid), list(shape), dt))

    def emit_conv(self, n):
        for _ in range(n):
            if self.conv_pos >= len(self.conv_list):
                return
            dst, src = self.conv_list[self.conv_pos]
            self.conv_pos += 1
            self.P.dma(dst, src, q='pool')

    def dump(self, key, ap):
        if key in self.dbg_out:
            self.P.dma(self.dbg_out[key], ap, q='pool')

    def build(self, upto=99):
        nc, P = self.nc, self.P
        self.declare()
        with ExitStack() as top:
            self.top = top
            T = lambda n, s, dt: self.T(top, n, s, dt)
            self.ps = [top.enter_context(nc.psum_tensor("ps%d" % i, [128, 512], F32)) for i in range(8)]
            self.identf = T('identf', [128, 128], F32)
            self.identb = T('identb', [128, 128], BF16)
            self.invf = T('invf', [128, 32], F32)
            self.trib = T('trib', [128, 128], BF16)
            self.antib = T('antib', [128, 128], BF16)
            self.triantib = T('triantib', [128, 256], BF16)
            self.tri4b = T('tri4b', [128, 512], BF16)
            self.cmpvalid = T('cmpvalid', [128, S], BF16)
            self.cand = T('cand', [128, NT, 32], F32)
            self.candm1 = T('candm1', [128, NT, 32], F32)
            self.forced = T('forced', [128, NT, 32], F32)
            self.lstrict = T('lstrict', [128, 128], BF16)
            self.onesb = T('onesb', [128, 128], BF16)
            P.dma(self.identf[:], self.cd['identf'])
            P.dma(self.identb[:], self.cd['identf'], q='pool')
            P.dma(self.invf[:], self.cd['invf'])
            P.dma(self.trib[:], self.cd['tri'], q='pool')
            P.dma(self.antib[:], self.cd['anti'], q='pool')
            P.dma(self.triantib[:], self.cd['trianti'], q='pool')
            P.dma(self.tri4b[:], self.cd['tri4'], q='pool')
            P.dma(self.cmpvalid[:], self.cd['cmpvalid'], q='pool')
            P.dma(self.cand[:], self.cd['cand'])
            P.dma(self.candm1[:], self.cd['candm1'])
            P.dma(self.forced[:], self.cd['forced'])
            P.dma(self.lstrict[:], self.cd['lstrict'], q='pool')
            P.dma(self.onesb[:], self.cd['ones'], q='pool')
            self.modT1 = T('modT1', [128, 16, 4], F32)
            self.hT = T('hT', [128, 8, S], BF16)
            self.phase0()
            if upto >= 1:
                for b in range(self.NSEQ):
                    self.phase1(b)
                    if upto >= 2:
                        pass
            P.barrier()
            P.flush()
        return nc

    def sync_phase(self, name=None):
        self.P.barrier()
        if name is None:
            import inspect
            fr = inspect.stack()[1]
            name = '%s_%d' % (fr.function.replace('_kb_', ''), fr.lineno)
        with self.nc.named_scope(name):
            self.P.flush()

    def phase0(self):
        nc, P, NSEQ = self.nc, self.P, self.NSEQ
        ps = self.ps
        with ExitStack() as st:
            T = lambda n, s, dt: self.T(st, n, s, dt)
            cs = T('cs', [4, D], F32)
            csT = T('csT', [128, 8, 4], F32)
            wa = [T('wa%d' % i, [128, 8, 512], F32) for i in range(2)]
            modrows = T('modrows', [4, 6 * D], F32)
            bada = T('bada', [4, 6 * D], F32)
            g1b = T('g1b', [4, D], F32)
            g2b = T('g2b', [4, D], F32)
            P.dma(cs[0:NSEQ, :], self.c)
            P.dma(bada[0:NSEQ, :], self.b_ada.to_broadcast([NSEQ, 6 * D]))
            P.dma(g1b[0:NSEQ, :], self.norm1_g.to_broadcast([NSEQ, D]))
            P.dma(g2b[0:NSEQ, :], self.norm2_g.to_broadcast([NSEQ, D]))
            P.act(cs[0:NSEQ, :], cs[0:NSEQ, :], AF.Silu)
            for k in range(8):
                P.tr(ps[0][:, k * 4:k * 4 + NSEQ], cs[0:NSEQ, k * 128:(k + 1) * 128], self.identf[0:NSEQ, 0:NSEQ])
            P.copy(csT[:, :, 0:NSEQ], ps[0][:, 0:32].rearrange('p (k b) -> p k b', b=4)[:, :, 0:NSEQ])
            wv = self.w_ada[0].rearrange('(k p) c -> p k c', p=128)
            for cc in range(12):
                w = wa[cc % 2]
                P.dma(w[:], wv[:, :, cc * 512:(cc + 1) * 512], q=('sp' if cc % 2 == 0 else 'act'))
                pb = ps[1 + cc % 2]
                for k in range(8):
                    P.mm(pb[0:NSEQ, :], csT[:, k, 0:NSEQ], w[:, k, :], start=(k == 0), stop=(k == 7))
                P.tt(modrows[0:NSEQ, cc * 512:(cc + 1) * 512], pb[0:NSEQ, :], bada[0:NSEQ, cc * 512:(cc + 1) * 512], ALU.add)
            P.stt(modrows[0:NSEQ, D:2 * D], modrows[0:NSEQ, D:2 * D], 1.0, g1b[0:NSEQ, :], ALU.add, ALU.mult)
            P.stt(modrows[0:NSEQ, 4 * D:5 * D], modrows[0:NSEQ, 4 * D:5 * D], 1.0, g2b[0:NSEQ, :], ALU.add, ALU.mult)
            P.dma(self.mod_d, modrows[0:NSEQ, :])
            for ch in range(16):
                P.tr(ps[3][:, ch * 4:ch * 4 + NSEQ], modrows[0:NSEQ, ch * 128:(ch + 1) * 128], self.identf[0:NSEQ, 0:NSEQ])
            P.copy(self.modT1[:, :, 0:NSEQ], ps[3][:, 0:64].rearrange('p (k b) -> p k b', b=4)[:, :, 0:NSEQ])
            self.dump('modrows', modrows[0:NSEQ, :])
            self.sync_phase()

    def phase1(self, b):
        nc, P = self.nc, self.P
        ps = self.ps
        with ExitStack() as st:
            T = lambda n, s, dt: self.T(st, n, s, dt)
            xt = [T('xt%d' % i, [128, D], F32) for i in range(3)]
            junk = T('p1junk', [128, D], F32)
            xn = [T('xn%d' % i, [128, D], F32) for i in range(2)]
            ss = [T('p1ss%d' % i, [128, 4], F32) for i in range(2)]
            def p0(tt):
                r0 = b * S + tt * 128
                P.dma(xt[tt % 3][:], self.x[r0:r0 + 128, :], q=('sp' if tt % 2 == 0 else 'act'))

            def p1(tt):
                s_ = ss[tt % 2]
                P.act(junk[:], xt[tt % 3][:], AF.Square, accum_out=s_[:, 0:1])
                P.act(s_[:, 1:2], s_[:, 0:1], AF.Ln, scale=1.0 / D, bias=EPS)
                P.act(s_[:, 2:3], s_[:, 1:2], AF.Exp, scale=-0.5)

            def p2(tt):
                P.ts(xn[tt % 2][:], xt[tt % 3][:], ss[tt % 2][:, 2:3], None, ALU.mult)

            def p3(tt):
                for k in range(8):
                    pb = ps[(tt % 2) * 2 + k // 4]
                    P.tr(pb[:, (k % 4) * 128:(k % 4 + 1) * 128], xn[tt % 2][:, k * 128:(k + 1) * 128], self.identf[:])

            def p4(tt):
                for k in range(8):
                    pb = ps[(tt % 2) * 2 + k // 4]
                    src = pb[:, (k % 4) * 128:(k % 4 + 1) * 128]
                    dst = self.hT[:, k, tt * 128:(tt + 1) * 128]
                    if k % 2 == 0:
                        P.act(dst, src, AF.Identity, scale=self.modT1[:, 8 + k, b:b + 1], bias=self.modT1[:, k, b:b + 1])
                    else:
                        P.ts(dst, src, self.modT1[:, 8 + k, b:b + 1], self.modT1[:, k, b:b + 1], ALU.mult, ALU.add)

            pipeline(NT, [p0, p1, p2, p3, p4], reverse=True)
            if b == 0:
                for k in range(8):
                    if ('hT%d' % k) in self.dbg_out:
                        self.dump('hT%d' % k, self.hT[:, k, :])
            self.sync_phase()


PI_SAFE = 3.1415925


def _kb_rope_tables(self, st, posf, n, cos_out, sin_out, tag):
    P = self.P
    T = lambda nm, s, dt: self.T(st, tag + nm, s, dt)
    ang = T('ang', [128, n, 32], F32)
    a2 = T('a2', [128, n, 32], F32)
    kk = T('kk', [128, n, 32], F32)
    P.tt(ang[:], self.invf[:, :].unsqueeze(1).to_broadcast([128, n, 32]),
         posf.unsqueeze(2).to_broadcast([128, n, 32]), ALU.mult)
    for off, outp in ((0.0, sin_out), (np.pi / 2, cos_out)):
        if off == 0.0:
            a = ang
        else:
            P.ts(a2[:], ang[:], float(off), None, ALU.add)
            a = a2
        P.ts(kk[:], a[:], 1.0 / TWO_PI, MAGIC, ALU.mult, ALU.add)
        P.ts(kk[:], kk[:], MAGIC, None, ALU.subtract)
        P.stt(kk[:], kk[:], -TWO_PI, a[:], ALU.mult, ALU.add)
        P.ts(kk[:], kk[:], PI_SAFE, -PI_SAFE, ALU.min, ALU.max)
        P.act(outp, kk[:], AF.Sin)


KB.rope_tables = _kb_rope_tables


def _kb_setup_nsa_consts(self):
    P, top = self.P, self.top
    T = lambda n, s, dt: self.T(top, n, s, dt)
    self.kslc = T('kslc', [96, S], BF16)
    P.dma(self.kslc[64:96, :], self.cd['blkoh'], q='pool')
    self.v2 = T('v2', [128, NT, 2, 65], BF16)
    P.memset(self.v2[:].rearrange('p a b c -> p (a b c)'), 1.0)
    self.vcaug = T('vcaug', [128, 97], BF16)
    P.memset(self.vcaug[:, 64:65], 1.0)
    P.dma(self.vcaug[:, 65:97], self.cd['overlap'], q='pool')
    self.w1kv = T('w1kv', [128, 32, 256], BF16)
    P.dma(self.w1kv[0:64], self.cmp_w1_k[0].rearrange('(l d) h -> d l h', d=64), q='pool')
    P.dma(self.w1kv[64:128], self.cmp_w1_v[0].rearrange('(l d) h -> d l h', d=64), q='pool')
    self.w2kv = T('w2kv', [128, 2, 2, 64], BF16)
    P.dma(self.w2kv[:, 0], self.cmp_w2_k[0].rearrange('(c p) d -> p c d', p=128), q='pool')
    P.dma(self.w2kv[:, 1], self.cmp_w2_v[0].rearrange('(c p) d -> p c d', p=128), q='pool')
    self.ckv = T('ckv', [128, 2, 2], F32)
    self.gfull = T('gfull', [128, 6, 64], F32)
    self.gk = T('gk', [128, 64], F32)
    self.gqb = T('gqb', [128, 64], F32)
    self.gkb = T('gkb', [128, 64], F32)
    for hh in range(4):
        P.dma(self.gfull[:, hh, :], self.nsa_q_norm.to_broadcast([128, 64]))
    for hh in range(4, 6):
        P.dma(self.gfull[:, hh, :], self.nsa_k_norm.to_broadcast([128, 64]))
    P.ts(self.gfull[:, 0:4, :], self.gfull[:, 0:4, :], 0.125, None, ALU.mult)
    P.dma(self.gk[:], self.nsa_k_norm.to_broadcast([128, 64]))
    P.dma(self.gqb[:], self.dil_q_norm.to_broadcast([128, 64]))
    P.ts(self.gqb[:], self.gqb[:], 0.125, None, ALU.mult)
    P.dma(self.gkb[:], self.dil_k_norm.to_broadcast([128, 64]))
    with ExitStack() as st:
        T2 = lambda n, s, dt: self.T(st, n, s, dt)
        pekv = T2('pekv', [32, 128], F32)
        peT = T2('peT', [128, 32], BF16)
        P.dma(pekv[:, 0:64], self.cmp_pe_k[0])
        P.dma(pekv[:, 64:128], self.cmp_pe_v[0])
        P.tr(self.ps[0][:, 0:32], pekv[:, :], self.identf[0:32, 0:32])
        P.copy(peT[:], self.ps[0][:, 0:32])
        for kv in range(2):
            base = 64 * kv
            for hc in range(2):
                for l in range(32):
                    P.mm(self.ps[1 + kv][:, hc:hc + 1], self.w1kv[base:base + 64, l, hc * 128:(hc + 1) * 128],
                         peT[base:base + 64, l:l + 1], start=(hc == 0 and l == 0), stop=(l == 31), skip_group_check=True)
            P.copy(self.ckv[:, kv, :], self.ps[1 + kv][:, 0:2])
        self.sync_phase()


KB.setup_nsa_consts = _kb_setup_nsa_consts


def _kb_seq_prologue(self, st, b):
    P = self.P
    T = lambda n, s, dt: self.T(st, n, s, dt)
    self.cosT = T('cosT', [128, NT, 32], F32)
    self.sinT = T('sinT', [128, NT, 32], F32)
    self.cosC = T('cosC', [128, 1, 32], F32)
    self.sinC = T('sinC', [128, 1, 32], F32)
    with ExitStack() as s2:
        T2 = lambda n, s, dt: self.T(s2, n, s, dt)
        posi = T2('posi', [128, 2], I32)
        posi16 = T2('posi16', [16, 128], I32)
        posf16 = T2('posf16', [16, 128], F32)
        posf = T2('posf', [128, NT + 1], F32)
        P.memset(posi[:], 0)
        P.dma(posi16[:], self.pos[b].rearrange('(t p) -> t p', p=128))
        P.copy(posf16[:], posi16[:])
        P.tr(self.ps[0][:, 0:NT], posf16[:], self.identf[0:16, 0:16])
        P.copy(posf[:, 0:NT], self.ps[0][:, 0:NT])
        P.dma(posi[0:127, 0:1], self.pos[b, 31:31 + 16 * 126 + 1:16].unsqueeze(1), allow_slow_non_contiguous=True)
        P.copy(posf[:, NT:NT + 1], posi[:, 0:1])
        self.rope_tables(s2, posf[:, 0:NT], NT, self.cosT[:], self.sinT[:], 'rt')
        self.rope_tables(s2, posf[:, NT:NT + 1], 1, self.cosC[:], self.sinC[:], 'rc')
        self.sync_phase()


KB.seq_prologue = _kb_seq_prologue


class BankRound:
    def __init__(self):
        self.started = {}

    def reset(self, bank):
        self.started[bank.name] = False

    def start(self, bank):
        s = not self.started.get(bank.name, False)
        self.started[bank.name] = True
        return s


def pipeline(n, stages, delays=None, reverse=False):
    if delays is None:
        delays = list(range(len(stages)))
    order = list(range(len(stages)))
    if reverse:
        order = order[::-1]
    for step in range(n + max(delays)):
        for j in order:
            i = step - delays[j]
            if 0 <= i < n:
                stages[j](i)


def _kb_phase2_nsa(self, b, st_seq):
    nc, P, ps = self.nc, self.P, self.ps
    BR = self.br
    wv = self.w_in[0].rearrange('(k p) c -> p k c', p=128)
    with ExitStack() as st:
        T = lambda n, s, dt: self.T(st, n, s, dt)
        w_nsa = T('w_nsa', [128, 8, 1304], BF16)
        P.dma(w_nsa[:].rearrange('p k c -> p (k c)'), self.w_nsa_d)
        gates = T('gates', [128, NT, 24], F32)
        for g in range(2):
            with ExitStack() as sg:
                self.nsa_group(b, g, sg, w_nsa, gates)
                self.sync_phase()


def _kb_nsa_group(self, b, g, st, w_nsa, gates):
    nc, P, ps = self.nc, self.P, self.ps
    BR = self.br
    T = lambda n, s, dt: self.T(st, n, s, dt)
    qaug = T('qaug', [96, 4, S], BF16)
    kwin = T('kwin', [64, S], BF16)
    kvcT = T('kvcT', [128, S], BF16)
    kcT = T('kcT', [64, 128], BF16)
    kslc, v2, vcaug = self.kslc, self.v2, self.vcaug
    with ExitStack() as s2:
        T2 = lambda n, s, dt: self.T(s2, n, s, dt)
        DEP = 4
        sq = [T2('sq%d' % i, [128, 6, 64], F32) for i in range(DEP)]
        rc = [T2('rc%d' % i, [128, 6, 64], F32) for i in range(DEP)]
        rn = [T2('rn%d' % i, [128, 6, 64], F32) for i in range(2)]
        tmp = [T2('rtmp%d' % i, [128, 4, 6, 32], F32) for i in range(2)]
        rr = [T2('rr%d' % i, [128, 6, 64], BF16) for i in range(DEP)]
        st6 = [T2('st6%d' % i, [128, 3, 6], F32) for i in range(DEP)]
        for tc in range(4):
            pc = ps[6 + tc % 2]
            for k in range(8):
                P.mm(pc[:, :], w_nsa[:, k, 1024 + 128 * g:1024 + 128 * g + 128], self.hT[:, k, tc * 512:(tc + 1) * 512],
                     start=(k == 0), stop=(k == 7))
            P.copy(kvcT[:, tc * 512:(tc + 1) * 512], pc[:, :], eng='act')
        tokf = lambda tt: slice(tt * 128, (tt + 1) * 128)

        def f0(tt):
            pa = ps[tt % 3]
            for k in range(8):
                P.mm(pa[:, :], self.hT[:, k, tokf(tt)], w_nsa[:, k, g * 512:(g + 1) * 512], start=(k == 0), stop=(k == 7))
            if g == 0:
                pg = ps[5 + tt % 2]
                for k in range(8):
                    P.mm(pg[:, 0:24], self.hT[:, k, tokf(tt)], w_nsa[:, k, 1280:1304], start=(k == 0), stop=(k == 7))

        def f1(tt):
            pa = ps[tt % 3]
            R = pa[:, 0:384].rearrange('p (h d) -> p h d', d=64)
            P.act(sq[tt % DEP][:], R, AF.Square)
            P.copy(rc[tt % DEP][:], R)
            P.copy(v2[:, tt, :, 0:64], pa[:, 384:512].rearrange('p (a d) -> p a d', d=64), eng='act')
            if g == 0:
                P.copy(gates[:, tt, :], ps[5 + tt % 2][:, 0:24])

        def f2(tt):
            P.reduce(st6[tt % DEP][:, 0, :], sq[tt % DEP][:], ALU.add)

        def f3(tt):
            P.act(st6[tt % DEP][:, 1, :], st6[tt % DEP][:, 0, :], AF.Ln, scale=1.0 / DH, bias=EPS)
            P.act(st6[tt % DEP][:, 2, :], st6[tt % DEP][:, 1, :], AF.Exp, scale=-0.5)

        def f4(tt):
            i2 = tt % 2
            P.tt(rn[i2][:], rc[tt % DEP][:], st6[tt % DEP][:, 2, :].unsqueeze(2).to_broadcast([128, 6, 64]), ALU.mult)
            P.tt(rn[i2][:], rn[i2][:], self.gfull[:], ALU.mult)
            cosb = self.cosT[:, tt, :].unsqueeze(1).to_broadcast([128, 6, 32])
            sinb = self.sinT[:, tt, :].unsqueeze(1).to_broadcast([128, 6, 32])
            x1 = rn[i2][:, :, 0:32]
            x2 = rn[i2][:, :, 32:64]
            tm = tmp[i2]
            P.tt(tm[:, 0], x1, cosb, ALU.mult)
            P.tt(tm[:, 1], x2, sinb, ALU.mult)
            P.tt(tm[:, 2], x1, sinb, ALU.mult, eng='pool')
            P.tt(tm[:, 3], x2, cosb, ALU.mult, eng='pool')
            P.tt(rr[tt % DEP][:, :, 0:32], tm[:, 0], tm[:, 1], ALU.subtract)
            P.tt(rr[tt % DEP][:, :, 32:64], tm[:, 2], tm[:, 3], ALU.add, eng='pool')

        def f5(tt):
            pt_ = ps[3 + tt % 2].bitcast(BF16)
            for hh in range(6):
                P.tr(pt_[0:64, hh * 128:(hh + 1) * 128], rr[tt % DEP][:, hh, :], self.identb[:])

        def f6(tt):
            pt_ = ps[3 + tt % 2].bitcast(BF16)
            tok = tokf(tt)
            P.copy(qaug[0:64, :, tok], pt_[0:64, 0:512].rearrange('p (h t) -> p h t', t=128), eng='act')
            P.copy(kslc[0:64, tok], pt_[0:64, 512:640], eng='act')
            P.copy(kwin[0:64, tok], pt_[0:64, 640:768], eng='act')

        pipeline(NT, [f0, f1, f2, f3, f4, f5, f6])
        if g == 0:
            P.act(gates[:].rearrange('p a b -> p (a b)'), gates[:].rearrange('p a b -> p (a b)'), AF.Sigmoid)
        hid = T2('hid', [128, 2, 2, 128], BF16)
        kc4 = T2('kc4', [128, 8, 64], F32)
        kst = T2('kst', [128, 4], F32)
        kcr = T2('kcr', [128, 64], BF16)
        ktm = T2('ktm', [128, 4, 32], F32)
        for kv in range(2):
            base = 64 * kv
            pz = ps[5 + kv]
            BR.reset(pz)
            for hc in range(2):
                for l in range(32):
                    P.mm(pz[:, hc * 128:hc * 128 + 127], self.w1kv[base:base + 64, l, hc * 128:(hc + 1) * 128],
                         kvcT[base:base + 64, l:l + 16 * 126 + 1:16], start=BR.start(pz), stop=(l == 31),
                         skip_group_check=True)
            for hc in range(2):
                P.act(hid[:, kv, hc, 0:127], pz[:, hc * 128:hc * 128 + 127], AF.Silu, bias=self.ckv[:, kv, hc:hc + 1])
        p2 = ps[7]
        BR.reset(p2)
        for kv in range(2):
            for hc in range(2):
                P.mm(p2[0:127, kv * 64:(kv + 1) * 64], hid[:, kv, hc, 0:127], self.w2kv[:, kv, hc, :],
                     start=BR.start(p2), stop=(hc == 1), skip_group_check=True)
        P.copy(vcaug[0:127, 0:64], p2[0:127, 64:128], eng='act')
        P.act(kc4[0:127, 0, :], p2[0:127, 0:64], AF.Square)
        P.reduce(kst[0:127, 0:1], kc4[0:127, 0, :], ALU.add)
        P.act(kst[0:127, 1:2], kst[0:127, 0:1], AF.Ln, scale=1.0 / DH, bias=EPS)
        P.act(kst[0:127, 2:3], kst[0:127, 1:2], AF.Exp, scale=-0.5)
        P.stt(kc4[0:127, 1, :], p2[0:127, 0:64], kst[0:127, 2:3], self.gk[0:127, :], ALU.mult, ALU.mult)
        x1 = kc4[0:127, 1, 0:32]
        x2 = kc4[0:127, 1, 32:64]
        cC = self.cosC[0:127, 0, :]
        sC = self.sinC[0:127, 0, :]
        P.tt(ktm[0:127, 0], x1, cC, ALU.mult)
        P.tt(ktm[0:127, 1], x2, sC, ALU.mult)
        P.tt(ktm[0:127, 2], x1, sC, ALU.mult)
        P.tt(ktm[0:127, 3], x2, cC, ALU.mult)
        P.tt(kcr[0:127, 0:32], ktm[0:127, 0], ktm[0:127, 1], ALU.subtract)
        P.tt(kcr[0:127, 32:64], ktm[0:127, 2], ktm[0:127, 3], ALU.add)
        pk = ps[3].bitcast(BF16)
        P.tr(pk[0:64, 0:127], kcr[0:127, :], self.identb[0:127, 0:127])
        P.copy(kcT[:, 0:127], pk[0:64, 0:127])
        if b == 0:
            self.dump('qaug%d' % g, qaug[0:64].rearrange('p h s -> p (h s)'))
            self.dump('kslc%d' % g, kslc[0:64, :])
            self.dump('kwin%d' % g, kwin[:, :])
            self.dump('kcT%d' % g, kcT[:, :])
            self.dump('vc%d' % g, vcaug[:, 0:64])
        self.sync_phase()
    self.nsa_attention(b, g, st, qaug, kwin, kcT, gates)


KB.phase2_nsa = _kb_phase2_nsa
KB.nsa_group = _kb_nsa_group


def _kb_nsa_attention(self, b, g, st, qaug, kwin, kcT, gates):
    nc, P, ps = self.nc, self.P, self.ps
    BR = self.br
    self.emit_conv(-(-len(self.conv_list) // (2 * self.NSEQ)))
    kslc, v2, vcaug = self.kslc, self.v2, self.vcaug
    with ExitStack() as s3:
        T = lambda n, s, dt: self.T(s3, n, s, dt)
        ptile = [T('ptile%d' % i, [128, 512], BF16) for i in range(3)]
        oacc = T('oacc', [128, NT, 4, 64], F32)
        impacc = T('impacc', [128, NT, 32], F32)
        rz = [T('rz%d' % i, [128, 2, 4], F32) for i in range(3)]
        otmp = [T('otmp%d' % i, [128, 4, 64], F32) for i in range(2)]
        itmp = [T('itmp%d' % i, [128, 4, 32], F32) for i in range(2)]
        scw = [T('scw%d' % i, [128, 4, 32], F32) for i in range(2)]
        slw = [T('slw%d' % i, [128, 4, 32], F32) for i in range(2)]
        m8 = [T('m8%d' % i, [128, 4, 8], F32) for i in range(2)]
        biasb = [T('biasb%d' % i, [128, 4, 32], BF16) for i in range(2)]
        ob = [T('ob%d' % i, [128, 256], BF16) for i in range(2)]
        sbank = [ps[0], ps[1], ps[2]]
        pvbank = [ps[3], ps[4]]
        misc = ps[5]
        otb = [ps[6], ps[7]]
        cnt = {'s': 0, 'pv': 0, 'fin': 0}

        def finalize(pvb, hl, br, qc, ncol, first, want_imp, first_imp):
            h = 4 * g + hl
            k_ = cnt['fin']
            cnt['fin'] += 1
            r = rz[k_ % 3]
            pv3 = pvb[:, 0:4 * ncol].rearrange('p (q c) -> p q c', c=ncol)
            P.ts(r[:, 0, :], pv3[:, :, 64], 1e-30, None, ALU.max)
            P.recip(r[:, 0, :], r[:, 0, :])
            P.tt(r[:, 1, :], r[:, 0, :], gates[:, qc * 4:(qc + 1) * 4, br * 8 + h], ALU.mult)
            tgt = oacc[:, qc * 4:(qc + 1) * 4, hl, :]
            sb_ = r[:, 1, :].unsqueeze(2).to_broadcast([128, 4, 64])
            if first:
                P.tt(tgt, pv3[:, :, 0:64], sb_, ALU.mult)
            else:
                ot = otmp[k_ % 2]
                P.tt(ot[:], pv3[:, :, 0:64], sb_, ALU.mult)
                P.tt(tgt, tgt, ot[:], ALU.add)
            if want_imp:
                itg = impacc[:, qc * 4:(qc + 1) * 4, :]
                rb_ = r[:, 0, :].unsqueeze(2).to_broadcast([128, 4, 32])
                if first_imp:
                    P.tt(itg, pv3[:, :, 65:97], rb_, ALU.mult)
                else:
                    it = itmp[k_ % 2]
                    P.tt(it[:], pv3[:, :, 65:97], rb_, ALU.mult)
                    P.tt(itg, itg, it[:], ALU.add)

        def selection(qc):
            sc, sl, m_, bb = scw[qc % 2], slw[qc % 2], m8[qc % 2], biasb[qc % 2]
            tq = slice(qc * 4, (qc + 1) * 4)
            P.tt(sc[:], impacc[:, tq, :], self.cand[:, tq, :], ALU.mult)
            P.tt(sc[:], sc[:], self.candm1[:, tq, :], ALU.add)
            for qt in range(4):
                P.max8(m_[:, qt, :], sc[:, qt, :])
            P.tt(sl[:], sc[:], m_[:, :, 4].unsqueeze(2).to_broadcast([128, 4, 32]), ALU.is_ge)
            P.tt(sl[:], sl[:], self.forced[:, tq, :], ALU.max)
            P.ts(bb[:], sl[:], 1.0, BIG, ALU.subtract, ALU.mult)
            mb = misc.bitcast(BF16)
            for qt in range(4):
                P.tr(mb[0:32, qt * 128:(qt + 1) * 128], bb[:, qt, :], self.identb[:])
            P.copy(qaug[64:96, :, qc * 512:(qc + 1) * 512],
                   mb[0:32, 0:512].unsqueeze(1).to_broadcast([32, 4, 512]), eng='act')
            if b == 0:
                for qt in range(4):
                    self.dump('sel%d_%d' % (g, qc * 4 + qt), sl[:, qt, :])

        astate = {}

        def c0(i):
            qc, hl = i // 4, i % 4
            k_ = cnt['s']
            cnt['s'] += 1
            astate[i] = (sbank[k_ % 3], ptile[k_ % 3])
            P.mm(astate[i][0][0:127, :], kcT[0:64, 0:127], qaug[0:64, hl, qc * 512:(qc + 1) * 512], start=True, stop=True)

        def c1(i):
            sb, pt = astate[i]
            P.act(pt[0:127, :], sb[0:127, :], AF.Exp)

        def c2(i):
            qc = i // 4
            sb, pt = astate[i]
            P.tt(pt[0:127, :], pt[0:127, :], self.cmpvalid[0:127, qc * 512:(qc + 1) * 512], ALU.mult)

        def c3(i):
            sb, pt = astate[i]
            pvb = pvbank[cnt['pv'] % 2]
            cnt['pv'] += 1
            BR.reset(pvb)
            for qt in range(4):
                P.mm(pvb[:, qt * 97:(qt + 1) * 97], pt[0:127, qt * 128:(qt + 1) * 128], vcaug[0:127, :],
                     start=BR.start(pvb), stop=True, skip_group_check=True)
            astate[i] = pvb

        def c4(i):
            qc, hl = i // 4, i % 4
            finalize(astate.pop(i), hl, 0, qc, 97, True, True, hl == 0)
            if hl == 3:
                selection(qc)

        pipeline(16, [c0, c1, c2, c3, c4])

        steps = []
        for qc in range(4):
            for hl in range(4):
                kbs = list(range(max(0, 4 * qc - 4), 4 * qc + 4))
                for kb in kbs:
                    if kb < 4 * qc:
                        j = kb - (4 * qc - 4)
                        c0_, c1_, mask = 0, 128 * (j + 1), ('anti', 128 * j)
                    else:
                        j = kb - 4 * qc
                        c0_, c1_, mask = 128 * j, 512, ('tri', 128 * j)
                    steps.append(dict(br=2, hl=hl, kb=kb, c0=c0_, c1=c1_, mask=mask, qc=qc, firsth=(kb == kbs[0]), last=(kb == kbs[-1])))
            for hl in range(4):
                kbs = list(range(0, 4 * qc + 4))
                for kb in kbs:
                    if kb < 4 * qc:
                        c0_, c1_, mask = 0, 512, None
                    else:
                        j = kb - 4 * qc
                        c0_, c1_, mask = 128 * j, 512, ('tri', 128 * j)
                    steps.append(dict(br=1, hl=hl, kb=kb, c0=c0_, c1=c1_, mask=mask, qc=qc, firsth=(kb == kbs[0]), last=(kb == kbs[-1])))
        n = len(steps)
        state = {}

        def qk(i):
            s_ = steps[i]
            k_ = cnt['s']
            cnt['s'] += 1
            sb = sbank[k_ % 3]
            pt = ptile[k_ % 3]
            hl, kb, c0_, c1_, br = s_['hl'], s_['kb'], s_['c0'], s_['c1'], s_['br']
            q0 = s_['qc'] * 512
            if br == 1:
                lhsT = kslc[0:96, kb * 128:(kb + 1) * 128]
                rhs = qaug[0:96, hl, q0 + c0_:q0 + c1_]
            else:
                lhsT = kwin[0:64, kb * 128:(kb + 1) * 128]
                rhs = qaug[0:64, hl, q0 + c0_:q0 + c1_]
            P.mm(sb[:, c0_:c1_], lhsT, rhs, start=True, stop=True)
            P.act(pt[:, c0_:c1_], sb[:, c0_:c1_], AF.Exp)
            if s_['mask'] is not None:
                kind, col = s_['mask']
                mk = self.trib if kind == 'tri' else self.antib
                P.tt(pt[:, col:col + 128], pt[:, col:col + 128], mk[:], ALU.mult)
            state[i] = pt

        def pv(i):
            s_ = steps[i]
            pt = state.pop(i)
            hl, kb, c0_, c1_, br = s_['hl'], s_['kb'], s_['c0'], s_['c1'], s_['br']
            if s_['firsth']:
                state['pvb'] = pvbank[cnt['pv'] % 2]
                cnt['pv'] += 1
                BR.reset(state['pvb'])
            pvb = state['pvb']
            for qt in range(c0_ // 128, c1_ // 128):
                P.mm(pvb[:, qt * 65:(qt + 1) * 65], pt[:, qt * 128:(qt + 1) * 128], v2[:, kb, br - 1, :],
                     start=BR.start(pvb), stop=True, skip_group_check=True)
            if s_['last']:
                finalize(pvb, hl, br, s_['qc'], 65, False, False, False)

        AHEAD = 2
        for i in range(min(AHEAD, n)):
            qk(i)
        for i in range(n):
            if i + AHEAD < n:
                qk(i + AHEAD)
            pv(i)

        def o0(tg):
            P.copy(ob[tg % 2][:], oacc[:, tg].rearrange('p h d -> p (h d)'), eng='act')

        def o1(tg):
            pb = otb[tg % 2].bitcast(BF16)
            for pr in range(2):
                P.tr(pb[:, pr * 128:(pr + 1) * 128], ob[tg % 2][:, pr * 128:(pr + 1) * 128], self.identb[:])

        def o2(tg):
            pb = otb[tg % 2].bitcast(BF16)
            P.copy(self.o_aT[:, 2 * g:2 * g + 2, tg * 128:(tg + 1) * 128],
                   pb[:, 0:256].rearrange('p (c t) -> p c t', t=128), eng='dve')

        pipeline(NT, [o0, o1, o2])


KB.nsa_attention = _kb_nsa_attention


def _kb_prep_weights(self):
    P = self.P
    wv = self.w_in[0].rearrange('(k p) c -> p k c', p=128)
    with ExitStack() as st:
        T = lambda n, s, dt: self.T(st, n, s, dt)
        w_nsa = T('pw_nsa', [128, 8, 1304], BF16)
        for g in range(2):
            o = g * 512
            for (dst, src, n) in ((0, 256 * g, 256), (256, 768 + 64 * g, 64), (320, 1024 + 64 * g, 64),
                                  (384, 896 + 64 * g, 64), (448, 1152 + 64 * g, 64)):
                P.dma(w_nsa[:, :, o + dst:o + dst + n], wv[:, :, src:src + n], q='pool')
            P.dma(w_nsa[:, :, 1024 + 128 * g:1024 + 128 * g + 64], wv[:, :, 512 + 64 * g:512 + 64 * g + 64], q='pool')
            P.dma(w_nsa[:, :, 1024 + 128 * g + 64:1024 + 128 * g + 128], wv[:, :, 640 + 64 * g:640 + 64 * g + 64], q='pool')
        P.dma(w_nsa[:, :, 1280:1304], wv[:, :, 1280:1304], q='pool')
        P.dma(self.w_nsa_d, w_nsa[:].rearrange('p k c -> p (k c)'))
        w_dil = T('pw_dil', [128, 8, 1152], BF16)
        for gi in range(3):
            for pi, base in enumerate((1304, 1688, 2072)):
                P.dma(w_dil[:, :, gi * 384 + pi * 128:gi * 384 + (pi + 1) * 128],
                      wv[:, :, base + 128 * gi:base + 128 * gi + 128], q='pool')
        P.dma(self.w_dil_d, w_dil[:].rearrange('p k c -> p (k c)'))
        w_gm = T('pw_gm', [128, 8, 2048], BF16)
        for q4 in range(4):
            P.dma(w_gm[:, :, q4 * 512:(q4 + 1) * 512], wv[:, :, 2456 + q4 * 512:2456 + (q4 + 1) * 512], q='pool')
        P.dma(self.w_gm_d, w_gm[:].rearrange('p k c -> p (k c)'))
        w_upa = T('pw_upa', [128, 4, D], BF16)
        w_upb = T('pw_upb', [128, 3, D], BF16)
        for c in range(4):
            P.dma(w_upa[:, c, :], self.w_up_a[0, c * 128:(c + 1) * 128, :], q='pool')
        for c in range(3):
            P.dma(w_upb[:, c, :], self.w_up_b[0, c * 128:(c + 1) * 128, :], q='pool')
        P.dma(self.w_upa_d, w_upa[:].rearrange('p k c -> p (k c)'))
        P.dma(self.w_upb_d, w_upb[:].rearrange('p k c -> p (k c)'))
        w_o = T('pw_o', [128, 8, D], BF16)
        for k in range(8):
            P.dma(w_o[:, k, :], self.w_out[0, k * 128:(k + 1) * 128, :], q='pool')
        P.dma(self.w_o_d, w_o[:].rearrange('p k c -> p (k c)'))
        self.sync_phase()


KB.prep_weights = _kb_prep_weights


def _kb_build(self, upto=99):
    nc, P = self.nc, self.P
    self.declare()
    self.br = BankRound()
    with ExitStack() as top:
        self.top = top
        T = lambda n, s, dt: self.T(top, n, s, dt)
        self.ps = [top.enter_context(nc.psum_tensor("ps%d" % i, [128, 512], F32)) for i in range(8)]
        self.identf = T('identf', [128, 128], F32)
        self.identb = T('identb', [128, 128], BF16)
        self.invf = T('invf', [128, 32], F32)
        self.trib = T('trib', [128, 128], BF16)
        self.antib = T('antib', [128, 128], BF16)
        self.triantib = T('triantib', [128, 256], BF16)
        self.tri4b = T('tri4b', [128, 512], BF16)
        self.cmpvalid = T('cmpvalid', [128, S], BF16)
        self.cand = T('cand', [128, NT, 32], F32)
        self.candm1 = T('candm1', [128, NT, 32], F32)
        self.forced = T('forced', [128, NT, 32], F32)
        self.lstrict = T('lstrict', [128, 128], BF16)
        self.onesb = T('onesb', [128, 128], BF16)
        P.dma(self.identf[:], self.cd['identf'])
        P.dma(self.identb[:], self.cd['identf'], q='pool')
        P.dma(self.invf[:], self.cd['invf'])
        P.dma(self.trib[:], self.cd['tri'], q='pool')
        P.dma(self.antib[:], self.cd['anti'], q='pool')
        P.dma(self.triantib[:], self.cd['trianti'], q='pool')
        P.dma(self.tri4b[:], self.cd['tri4'], q='pool')
        P.dma(self.cmpvalid[:], self.cd['cmpvalid'], q='pool')
        P.dma(self.cand[:], self.cd['cand'])
        P.dma(self.candm1[:], self.cd['candm1'])
        P.dma(self.forced[:], self.cd['forced'])
        P.dma(self.lstrict[:], self.cd['lstrict'], q='pool')
        P.dma(self.onesb[:], self.cd['ones'], q='pool')
        self.modT1 = T('modT1', [128, 16, 4], F32)
        if upto >= 4:
            self.setup_moe_consts()
        if upto >= 2:
            self.prep_weights()
        with ExitStack() as mix:
            self.top = mix
            Tm = lambda n, s, dt: self.T(mix, n, s, dt)
            self.hT = Tm('hT', [128, 8, S], BF16)
            self.o_aT = Tm('o_aT', [128, 4, S], BF16)
            self.o_bT = Tm('o_bT', [128, 3, S], BF16)
            self.phase0()
            if upto >= 2:
                self.setup_nsa_consts()
            for b in range(self.NSEQ):
                if upto >= 1:
                    self.phase1(b)
                if upto >= 2:
                    with ExitStack() as st_seq:
                        self.seq_prologue(st_seq, b)
                        self.phase2_nsa(b, st_seq)
                        if b == 0:
                            for c in range(4):
                                self.dump('oaT%d' % c, self.o_aT[:, c, :])
                        if upto >= 3:
                            self.phase2_dil(b, st_seq)
                            if b == 0:
                                for c in range(3):
                                    self.dump('obT%d' % c, self.o_bT[:, c, :])
                        if upto >= 4:
                            self.phase3(b, st_seq)
                        self.sync_phase()
            self.sync_phase()
        self.top = top
        if upto >= 5:
            self.phase4()
        P.barrier()
        P.flush()
    return nc


KB.build = _kb_build


DIL_D = (1, 4, 16)


def _kb_phase2_dil(self, b, st_seq):
    nc, P, ps = self.nc, self.P, self.ps
    BR = self.br
    wv = self.w_in[0].rearrange('(k p) c -> p k c', p=128)
    with ExitStack() as st:
        T = lambda n, s, dt: self.T(st, n, s, dt)
        w_dil = T('w_dil', [128, 8, 1152], BF16)
        P.dma(w_dil[:].rearrange('p k c -> p (k c)'), self.w_dil_d, q='act')
        us = T('us', [128, 3, S], F32)
        ztot = T('ztot', [128, S], F32)
        gfb = T('gfb', [128, 4, 64], F32)
        for hh in range(2):
            P.copy(gfb[:, hh, :], self.gqb[:])
            P.copy(gfb[:, 2 + hh, :], self.gkb[:])
        for gi in range(3):
            d = DIL_D[gi]
            with ExitStack() as sg:
                T2 = lambda n, s, dt: self.T(sg, n, s, dt)
                qbT = T2('qbT', [64, 2, S], BF16)
                kbT = T2('kbT', [64, 2, S], BF16)
                vb = T2('vb', [128, 16, 128], BF16)
                DEP = 4
                sq = [T2('dsq%d' % i, [128, 4, 64], F32) for i in range(DEP)]
                rc = [T2('drc%d' % i, [128, 4, 64], F32) for i in range(DEP)]
                rn = [T2('drn%d' % i, [128, 4, 64], F32) for i in range(2)]
                tmp = [T2('dtmp%d' % i, [128, 4, 4, 32], F32) for i in range(2)]
                rr = [T2('drr%d' % i, [128, 4, 64], BF16) for i in range(DEP)]
                st4 = [T2('dst%d' % i, [128, 3, 4], F32) for i in range(DEP)]
                ptile = [T2('dpt%d' % i, [128, 512], BF16) for i in range(3)]
                tokf = lambda tt: slice(tt * 128, (tt + 1) * 128)

                def d0(tt):
                    pa = ps[tt % 3]
                    for k in range(8):
                        P.mm(pa[:, 0:256], self.hT[:, k, tokf(tt)], w_dil[:, k, gi * 384:gi * 384 + 256], start=(k == 0), stop=(k == 7))

                def d1(tt):
                    R = ps[tt % 3][:, 0:256].rearrange('p (h d) -> p h d', d=64)
                    P.act(sq[tt % DEP][:], R, AF.Square)
                    P.copy(rc[tt % DEP][:], R)

                def d2(tt):
                    P.reduce(st4[tt % DEP][:, 0, :], sq[tt % DEP][:], ALU.add)

                def d3(tt):
                    P.act(st4[tt % DEP][:, 1, :], st4[tt % DEP][:, 0, :], AF.Ln, scale=1.0 / DH, bias=EPS)
                    P.act(st4[tt % DEP][:, 2, :], st4[tt % DEP][:, 1, :], AF.Exp, scale=-0.5)

                def d4(tt):
                    i2 = tt % 2
                    P.tt(rn[i2][:], rc[tt % DEP][:], st4[tt % DEP][:, 2, :].unsqueeze(2).to_broadcast([128, 4, 64]), ALU.mult)
                    P.tt(rn[i2][:], rn[i2][:], gfb[:], ALU.mult)
                    cosb = self.cosT[:, tt, :].unsqueeze(1).to_broadcast([128, 4, 32])
                    sinb = self.sinT[:, tt, :].unsqueeze(1).to_broadcast([128, 4, 32])
                    x1 = rn[i2][:, :, 0:32]
                    x2 = rn[i2][:, :, 32:64]
                    tm = tmp[i2]
                    P.tt(tm[:, 0], x1, cosb, ALU.mult)
                    P.tt(tm[:, 1], x2, sinb, ALU.mult)
                    P.tt(tm[:, 2], x1, sinb, ALU.mult, eng='pool')
                    P.tt(tm[:, 3], x2, cosb, ALU.mult, eng='pool')
                    P.tt(rr[tt % DEP][:, :, 0:32], tm[:, 0], tm[:, 1], ALU.subtract)
                    P.tt(rr[tt % DEP][:, :, 32:64], tm[:, 2], tm[:, 3], ALU.add, eng='pool')

                def d5(tt):
                    pt_ = ps[3 + tt % 2].bitcast(BF16)
                    for hh in range(4):
                        P.tr(pt_[0:64, hh * 128:(hh + 1) * 128], rr[tt % DEP][:, hh, :], self.identb[:])

                def d6(tt):
                    pt_ = ps[3 + tt % 2].bitcast(BF16)
                    P.copy(qbT[:, :, tokf(tt)], pt_[0:64, 0:256].rearrange('p (h t) -> p h t', t=128), eng='act')
                    P.copy(kbT[:, :, tokf(tt)], pt_[0:64, 256:512].rearrange('p (h t) -> p h t', t=128), eng='act')

                pipeline(NT, [d0, d1, d2, d3, d4, d5, d6])
                nkb = 16 // d
                for r in range(d):
                    for kbs in range(nkb):
                        bi = r * nkb + kbs
                        pvp = ps[4 + bi % 2]
                        s0 = 128 * kbs * d + r
                        for k in range(8):
                            P.mm(pvp[:, 0:128], self.hT[:, k, s0:s0 + 127 * d + 1:d], w_dil[:, k, gi * 384 + 256:gi * 384 + 384],
                                 start=(k == 0), stop=(k == 7))
                        P.copy(vb[:, bi, :], pvp[:, 0:128], eng='act')
                rounds = []
                if d == 1:
                    for Rn_ in range(4):
                        steps = []
                        for kbs in range(max(0, 4 * Rn_ - 1), 4 * Rn_ + 4):
                            if kbs < 4 * Rn_:
                                steps.append(dict(r=0, kbs=kbs, q0=4 * Rn_, nq=1, slot=0, mask='anti'))
                            elif kbs < 4 * Rn_ + 3:
                                steps.append(dict(r=0, kbs=kbs, q0=kbs, nq=2, slot=kbs - 4 * Rn_, mask='trianti'))
                            else:
                                steps.append(dict(r=0, kbs=kbs, q0=kbs, nq=1, slot=3, mask='tri'))
                        rounds.append(dict(steps=steps, out=('contig', 512 * Rn_)))
                elif d == 4:
                    for r in range(4):
                        steps = []
                        for kbs in range(4):
                            if kbs < 3:
                                steps.append(dict(r=r, kbs=kbs, q0=kbs, nq=2, slot=kbs, mask='trianti'))
                            else:
                                steps.append(dict(r=r, kbs=kbs, q0=kbs, nq=1, slot=3, mask='tri'))
                        rounds.append(dict(steps=steps, out=('strided', r)))
                else:
                    for Rr in range(4):
                        steps = [dict(r=4 * Rr + i, kbs=0, q0=0, nq=1, slot=i, mask='tri') for i in range(4)]
                        rounds.append(dict(steps=steps, out=('res16', 4 * Rr)))
                scnt = 0
                for rd_i, rd in enumerate(rounds):
                    pu = ps[4 + (rd_i % 2) * 2]
                    pz = ps[5 + (rd_i % 2) * 2]
                    started = {}
                    for s_ in rd['steps']:
                        r, kbs, q0, nq, slot = s_['r'], s_['kbs'], s_['q0'], s_['nq'], s_['slot']
                        bi = r * nkb + kbs
                        k0 = 128 * kbs * d + r
                        qs0 = 128 * q0 * d + r
                        ncol = 128 * nq
                        c0 = slot * 128
                        for j in range(2):
                            sb = ps[scnt % 4]
                            pt = ptile[scnt % 3]
                            scnt += 1
                            P.mm(sb[:, 0:ncol], kbT[0:64, j, k0:k0 + 127 * d + 1:d],
                                 qbT[0:64, j, qs0:qs0 + (ncol - 1) * d + 1:d], start=True, stop=True)
                            P.act(pt[:, 0:ncol], sb[:, 0:ncol], AF.Exp)
                            mk = {'tri': self.trib[:], 'anti': self.antib[:], 'trianti': self.triantib[:]}[s_['mask']]
                            P.tt(pt[:, 0:ncol], pt[:, 0:ncol], mk, ALU.mult)
                            first = not started.get(j, False)
                            started[j] = True
                            P.mm(pu[64 * j:64 * j + 64, c0:c0 + ncol], vb[:, bi, 64 * j:64 * j + 64], pt[:, 0:ncol],
                                 start=first, stop=True, skip_group_check=True)
                            P.mm(pz[64 * j:64 * j + 64, c0:c0 + ncol], self.onesb[:, 0:64], pt[:, 0:ncol],
                                 start=first, stop=True, skip_group_check=True)
                    kind, o0 = rd['out']
                    if kind == 'contig':
                        uo = us[:, gi, o0:o0 + 512]
                        zo = ztot[:, o0:o0 + 512]
                        pui, pzi = pu[:, :], pz[:, :]
                    elif kind == 'strided':
                        uo = us[:, gi, o0:o0 + 511 * 4 + 1:4]
                        zo = ztot[:, o0:o0 + 511 * 4 + 1:4]
                        pui, pzi = pu[:, :], pz[:, :]
                    else:
                        uo = us[:, gi, :].rearrange('p (k r) -> p r k', r=16)[:, o0:o0 + 4, :]
                        zo = ztot[:, :].rearrange('p (k r) -> p r k', r=16)[:, o0:o0 + 4, :]
                        pui = pu[:, :].rearrange('p (s k) -> p s k', k=128)
                        pzi = pz[:, :].rearrange('p (s k) -> p s k', k=128)
                    P.copy(uo, pui, eng='act')
                    if gi == 0:
                        P.copy(zo, pzi)
                    else:
                        P.tt(zo, pzi, zo, ALU.add)
                self.sync_phase()
        P.recip(ztot[:], ztot[:])
        for gi in range(3):
            P.tt(self.o_bT[:, gi, :], us[:, gi, :], ztot[:], ALU.mult)
        self.sync_phase()


KB.phase2_dil = _kb_phase2_dil


def _kb_setup_moe_consts(self):
    P, top = self.P, self.top
    T = lambda n, s, dt: self.T(top, n, s, dt)
    NTT = self.NTT
    self.wr = T('wr', [128, 8, 36], F32)
    with self.nc.allow_non_contiguous_dma(reason="tiny router weight rows"):
        P.dma(self.wr[:, :, 0:4], self.w_group[0].rearrange('(k p) g -> p k g', p=128))
        for gg in range(4):
            P.dma(self.wr[:, :, 4 + 8 * gg:12 + 8 * gg], self.w_router[0, gg].rearrange('(k p) e -> p k e', p=128))
    self.brow = T('brow', [128, 36], F32)
    P.dma(self.brow[:, 0:4], self.b_group.to_broadcast([128, 4]))
    P.dma(self.brow[:, 4:36], self.b_router[0:1].rearrange('o g e -> o (g e)').to_broadcast([128, 32]))
    self.EH = [T('EH%d' % k, [128, NTT, 32], BF16) for k in range(2)]
    self.rank = T('rank', [128, NTT, 2], F32)
    self.wts = T('wts', [128, NTT, 2], F32)
    self.carry = T('carry', [128, 32], F32)
    P.memset(self.carry[:], 0.0)


KB.setup_moe_consts = _kb_setup_moe_consts


def _kb_phase3(self, b, st_seq):
    nc, P, ps = self.nc, self.P, self.ps
    with ExitStack() as st:
        T = lambda n, s, dt: self.T(st, n, s, dt)
        w_gm = T('w_gm', [128, 8, 2048], BF16)
        w_upa = T('w_upa', [128, 4, D], BF16)
        w_upb = T('w_upb', [128, 3, D], BF16)
        P.dma(w_upa[:].rearrange('p k c -> p (k c)'), self.w_upa_d, q='act')
        P.dma(w_upb[:].rearrange('p k c -> p (k c)'), self.w_upb_d, q='act')
        P.dma(w_gm[:].rearrange('p k c -> p (k c)'), self.w_gm_d)
        gm = [T('gm%d' % i, [128, 2, 512], F32) for i in range(2)]
        ybf = [T('ybf%d' % i, [128, 512], BF16) for i in range(2)]
        tokf = lambda tt: slice(tt * 128, (tt + 1) * 128)

        def a0(i):
            tt, hf = i // 2, i % 2
            pa_, pb_ = (ps[0], ps[1]) if i % 2 == 0 else (ps[5], ps[6])
            cs = slice(hf * 512, (hf + 1) * 512)
            for c in range(4):
                P.mm(pa_[:, :], self.o_aT[:, c, tokf(tt)], w_upa[:, c, cs], start=(c == 0), stop=(c == 3))
            for c in range(3):
                P.mm(pb_[:, :], self.o_bT[:, c, tokf(tt)], w_upb[:, c, cs], start=(c == 0), stop=(c == 2))
            for q2 in range(2):
                cg = slice(q2 * 1024 + hf * 512, q2 * 1024 + (hf + 1) * 512)
                for k in range(8):
                    P.mm(ps[2 + q2][:, :], self.hT[:, k, tokf(tt)], w_gm[:, k, cg], start=(k == 0), stop=(k == 7))

        def a1(i):
            for q2 in range(2):
                P.act(gm[i % 2][:, q2, :], ps[2 + q2][:, :], AF.Sigmoid)

        def a2(i):
            pa_, pb_ = (ps[0], ps[1]) if i % 2 == 0 else (ps[5], ps[6])
            g_ = gm[i % 2]
            P.tt(g_[:, 0, :], g_[:, 0, :], pa_[:, :], ALU.mult)
            P.tt(g_[:, 1, :], g_[:, 1, :], pb_[:, :], ALU.mult)
            P.tt(ybf[i % 2][:], g_[:, 0, :], g_[:, 1, :], ALU.add)

        def a3(i):
            pb = ps[4].bitcast(BF16)
            for k in range(4):
                P.tr(pb[:, k * 128:(k + 1) * 128], ybf[i % 2][:, k * 128:(k + 1) * 128], self.identb[:])

        def a4(i):
            tt, hf = i // 2, i % 2
            pb = ps[4].bitcast(BF16)
            P.copy(self.hT[:, 4 * hf:4 * hf + 4, tokf(tt)], pb[:, 0:512].rearrange('p (k t) -> p k t', t=128), eng='act')

        pipeline(2 * NT, [a0, a1, a2, a3, a4], reverse=True)
        self.sync_phase()
    with ExitStack() as st:
        T = lambda n, s, dt: self.T(st, n, s, dt)
        w_o = T('w_o', [128, 8, D], BF16)
        P.dma(w_o[:].rearrange('p k c -> p (k c)'), self.w_o_d)
        gt1 = T('gt1', [128, D], F32)
        A2 = T('A2', [128, D], F32)
        sh2 = T('sh2', [128, D], F32)
        P.dma(gt1[:], self.mod_d[b:b + 1, 2 * D:3 * D].to_broadcast([128, D]), q='act')
        P.dma(sh2[:], self.mod_d[b:b + 1, 3 * D:4 * D].to_broadcast([128, D]), q='act')
        P.dma(A2[:], self.mod_d[b:b + 1, 4 * D:5 * D].to_broadcast([128, D]), q='act')
        xt = [T('x3t%d' % i, [128, D], F32) for i in range(2)]
        x1 = [T('x1t%d' % i, [128, D], F32) for i in range(2)]
        h2 = [T('h2t%d' % i, [128, D], F32) for i in range(2)]
        junk = T('p3junk', [128, D], F32)
        h2T = [T('h2T%d' % i, [128, 8, 128], F32) for i in range(2)]
        sm = [T('sm%d' % i, [128, 4], F32) for i in range(2)]
        lgall = T('lgall', [128, NT, 36], F32)
        tokf = lambda tt: slice(tt * 128, (tt + 1) * 128)

        def b0(tt):
            r0 = b * S + tt * 128
            P.dma(xt[tt % 2][:], self.x[r0:r0 + 128, :], q=('sp' if tt % 2 == 0 else 'act'))

        def b1(tt):
            for hf in range(2):
                for k in range(8):
                    P.mm(ps[hf][:, :], self.hT[:, k, tokf(tt)], w_o[:, k, hf * 512:(hf + 1) * 512], start=(k == 0), stop=(k == 7))

        def b2(tt):
            i2 = tt % 2
            r0 = b * S + tt * 128
            for hf in range(2):
                sl = slice(hf * 512, (hf + 1) * 512)
                P.tt(x1[i2][:, sl], ps[hf][:, :], gt1[:, sl], ALU.mult)
                P.tt(x1[i2][:, sl], x1[i2][:, sl], xt[i2][:, sl], ALU.add)
            P.dma(self.x1_d[r0:r0 + 128, :], x1[i2][:])

        def b3(tt):
            s_ = sm[tt % 2]
            P.act(junk[:], x1[tt % 2][:], AF.Square, accum_out=s_[:, 0:1])
            P.act(s_[:, 1:2], s_[:, 0:1], AF.Ln, scale=1.0 / D, bias=EPS)
            P.act(s_[:, 2:3], s_[:, 1:2], AF.Exp, scale=-0.5)

        def b4(tt):
            i2 = tt % 2
            r0 = b * S + tt * 128
            P.stt(h2[i2][:], x1[i2][:], sm[i2][:, 2:3], A2[:], ALU.mult, ALU.mult)
            P.tt(h2[i2][:], h2[i2][:], sh2[:], ALU.add)
            P.dma(self.h2_d[r0:r0 + 128, :], h2[i2][:], q='pool')

        def b5(tt):
            for k in range(8):
                pb = ps[2 + k // 4]
                P.tr(pb[:, (k % 4) * 128:(k % 4 + 1) * 128], h2[tt % 2][:, k * 128:(k + 1) * 128], self.identf[:])

        def b6(tt):
            for hf in range(2):
                P.copy(h2T[tt % 2][:, hf * 4:(hf + 1) * 4, :], ps[2 + hf][:, :].rearrange('p (k t) -> p k t', t=128),
                       eng=('act' if hf == 0 else 'dve'))

        def b7(tt):
            for k in range(8):
                P.mm(ps[4][:, 0:36], h2T[tt % 2][:, k, :], self.wr[:, k, :], start=(k == 0), stop=(k == 7))

        def b8(tt):
            P.tt(lgall[:, tt, :], ps[4][:, 0:36], self.brow[:], ALU.add)

        pipeline(NT, [b0, b1, b2, b3, b4, b5, b6, b7, b8], reverse=True)
        T0 = b * NT
        rt = T('rt', [128, NT, 16], F32)
        gw = T('gw', [128, 6, NT], F32)
        sel4 = T('sel4', [128, NT, 4, 8], F32)
        sel = T('selr', [128, NT, 8], F32)
        m8a = T('m8a', [128, NT, 8], F32)
        oh = T('ohr', [128, 2, NT, 8], F32)
        eh12 = T('eh12a', [128, NT, 32], BF16)
        pre = T('pre', [128, NT, 32], F32)
        big = T('bigr', [128, NT, 32], F32)
        G4 = lgall[:, :, 0:4]
        P.reduce(gw[:, 0, :], G4, ALU.max)
        P.tt(rt[:, :, 0:4], G4, gw[:, 0, :].unsqueeze(2).to_broadcast([128, NT, 4]), ALU.subtract)
        P.act(rt[:, :, 4:8], rt[:, :, 0:4], AF.Exp)
        P.reduce(gw[:, 1, :], rt[:, :, 4:8], ALU.add)
        P.recip(gw[:, 1, :], gw[:, 1, :])
        P.tt(rt[:, :, 8:12], G4, gw[:, 0, :].unsqueeze(2).to_broadcast([128, NT, 4]), ALU.is_equal)
        goh = rt[:, :, 8:12]
        P.tt(sel4[:], lgall[:, :, 4:36].rearrange('p t (g e) -> p t g e', e=8),
             goh.unsqueeze(3).to_broadcast([128, NT, 4, 8]), ALU.mult)
        P.reduce(sel[:], sel4[:].rearrange('p t g e -> p t e g'), ALU.add)
        for t in range(NT):
            P.max8(m8a[:, t, :], sel[:, t, :])
        P.tt(gw[:, 2, :], m8a[:, :, 1], m8a[:, :, 0], ALU.subtract)
        P.act(gw[:, 3, :], gw[:, 2, :], AF.Exp)
        P.ts(gw[:, 4, :], gw[:, 3, :], 1.0, None, ALU.add)
        P.recip(gw[:, 4, :], gw[:, 4, :])
        P.tt(gw[:, 5, :], gw[:, 3, :], gw[:, 4, :], ALU.mult)
        P.tt(self.wts[:, T0:T0 + NT, 0], gw[:, 4, :], gw[:, 1, :], ALU.mult)
        P.tt(self.wts[:, T0:T0 + NT, 1], gw[:, 5, :], gw[:, 1, :], ALU.mult)
        for kk in range(2):
            P.tt(oh[:, kk], sel[:], m8a[:, :, kk].unsqueeze(2).to_broadcast([128, NT, 8]), ALU.is_equal)
            P.tt(self.EH[kk][:, T0:T0 + NT, :].rearrange('p t (g e) -> p t g e', e=8),
                 goh.unsqueeze(3).to_broadcast([128, NT, 4, 8]),
                 oh[:, kk].unsqueeze(2).to_broadcast([128, NT, 4, 8]), ALU.mult)
        P.tt(eh12[:], self.EH[0][:, T0:T0 + NT, :], self.EH[1][:, T0:T0 + NT, :], ALU.add)
        P.mm(ps[5][:, :], self.lstrict[:], eh12[:].rearrange('p t e -> p (t e)'), start=True, stop=True)
        P.mm(ps[6][:, :], self.onesb[:], eh12[:].rearrange('p t e -> p (t e)'), start=True, stop=True)
        P.copy(big[:].rearrange('p t e -> p (t e)'), ps[6][:, :], eng='act')
        for t in range(NT):
            P.tt(pre[:, t, :], ps[5][:, t * 32:(t + 1) * 32], self.carry[:], ALU.add)
            P.tt(self.carry[:], self.carry[:], big[:, t, :], ALU.add)
        for kk in range(2):
            P.tt(big[:], self.EH[kk][:, T0:T0 + NT, :], pre[:], ALU.mult)
            P.reduce(self.rank[:, T0:T0 + NT, kk], big[:], ALU.add)
        if b == 0:
            self.dump('lg0', lgall[:, 0, :])
        self.sync_phase()


KB.phase3 = _kb_phase3


def _kb_phase4(self):
    nc, P, ps = self.nc, self.P, self.ps
    NTT, NBLK = self.NTT, self.NBLK
    wg_v = self.w_e_gate[0].rearrange('e r f -> (e r) f')
    wu_v = self.w_e_up[0].rearrange('e r f -> (e r) f')
    wd_v = self.w_e_down[0].rearrange('e r f -> (e r) f')
    IOA = bass.IndirectOffsetOnAxis
    with ExitStack() as st:
        T = lambda n, s, dt: self.T(st, n, s, dt)
        dest = T('dest', [128, 2, NTT], I32)
        widx = T('widx', [128, NBLK], I32)
        with ExitStack() as s2:
            T2 = lambda n, s, dt: self.T(s2, n, s, dt)
            cnt = T2('cnt', [128, 6, 32], F32)
            onesf = T2('onesf', [128, 32], F32)
            big = T2('bigtmp', [128, NTT, 32], F32)
            thr = T2('thr', [128, NBLK, 32], F32)
            cmp_ = T2('cmpb', [128, NBLK, 32], F32)
            be = T2('be', [128, 2, NBLK], F32)
            rgu = T2('rgu', [128, 12], F32)
            df = T2('df', [128, 2, NTT], F32)
            P.dma(thr[:].rearrange('p a b -> p (a b)'), self.cd['thr'])
            P.memset(onesf[:], 1.0)
            P.copy(cnt[:, 0, :], self.carry[:])
            P.ts(cnt[:, 1, :], cnt[:, 0, :], float(MB - 1), 1.0 / MB, ALU.add, ALU.mult)
            P.ts(cnt[:, 1, :], cnt[:, 1, :], -0.498, MAGIC, ALU.add, ALU.add)
            P.ts(cnt[:, 1, :], cnt[:, 1, :], MAGIC, float(MB), ALU.subtract, ALU.mult)
            P.add('dve', lambda e: e.tensor_tensor_scan(cnt[:, 2, :], onesf[:], cnt[:, 1, :], 0.0, ALU.mult, ALU.add),
                  [onesf[:], cnt[:, 1, :]], [cnt[:, 2, :]])
            P.tt(cnt[:, 3, :], cnt[:, 2, :], cnt[:, 1, :], ALU.subtract)
            for kk in range(2):
                P.tt(big[:], self.EH[kk][:], cnt[:, 3, :].unsqueeze(1).to_broadcast([128, NTT, 32]), ALU.mult)
                P.reduce(df[:, kk, :], big[:], ALU.add)
                P.tt(df[:, kk, :], df[:, kk, :], self.rank[:, :, kk], ALU.add)
            P.copy(dest[:], df[:])
            P.tt(cmp_[:], cnt[:, 2, :].unsqueeze(1).to_broadcast([128, NBLK, 32]), thr[:], ALU.is_le)
            P.reduce(be[:, 0, :], cmp_[:], ALU.add)
            P.ts(be[:, 0, :], be[:, 0, :], float(NEXP - 1), None, ALU.min)
            P.dma(rgu[:], self.cd['rowoff'])
            P.ts(be[:, 1, :], be[:, 0, :], 128.0, rgu[:, 0:1], ALU.mult, ALU.add)
            P.copy(widx[:], be[:, 1, :])
            self.dump('dest', df[:].rearrange('p a b -> p (a b)'))
            self.dump('be', be[:, 0, :])
            self.dump('cnt', cnt[:].rearrange('p a b -> p (a b)'))
            self.sync_phase()
        with ExitStack() as s2:
            T2 = lambda n, s, dt: self.T(s2, n, s, dt)
            hb = [T2('h2b%d' % i, [128, D], BF16) for i in range(3)]
            for Tg in range(NTT):
                h_ = hb[Tg % 3]
                P.dma(h_[:], self.h2_d[Tg * 128:(Tg + 1) * 128, :], q='sp')
                for kk in range(2):
                    ia = dest[:, kk, Tg:Tg + 1]
                    P.add('pool', (lambda h_=h_, ia=ia: (lambda e: e.indirect_dma_start(
                        out=self.xs_d, out_offset=IOA(ap=ia, axis=0), in_=h_[:, :], in_offset=None)))(),
                        [h_[:], ia], [self.xs_d], dma=True)
            self.sync_phase()
        with ExitStack() as s2:
            T2 = lambda n, s, dt: self.T(s2, n, s, dt)
            wg = [T2('wg%d' % i, [128, 8, FF], BF16) for i in range(2)]
            wu = [T2('wu%d' % i, [128, 8, FF], BF16) for i in range(2)]
            wd = [T2('wd%d' % i, [128, 4, D], BF16) for i in range(2)]
            xsb = [T2('xsb%d' % i, [128, 2, D], BF16) for i in range(2)]
            xT = [T2('xT%d' % i, [128, 8, MB], BF16) for i in range(2)]
            sg = [T2('sg%d' % i, [128, 4, MB], F32) for i in range(2)]
            hidT = [T2('hidT%d' % i, [128, 4, MB], BF16) for i in range(2)]
            yb = [T2('yb%d' % i, [128, 2, D], F32) for i in range(2)]

            def load_w(blk, which):
                i2 = blk % 2
                ia = widx[:, blk:blk + 1]
                lst = ((wg[i2], self.wg_l), (wu[i2], self.wu_l)) if which == 0 else ((wd[i2], self.wd_l),)
                for (wt, src) in lst:
                    P.add('pool', (lambda wt=wt, src=src, ia=ia: (lambda e: e.indirect_dma_start(
                        out=wt[:].rearrange('p k f -> p (k f)'), out_offset=None, in_=src, in_offset=IOA(ap=ia, axis=0))))(),
                        [ia], [wt[:]], dma=True)

            def m0(blk):
                r0 = blk * MB
                P.dma(xsb[blk % 2][:], self.xs_d[r0:r0 + MB, :].rearrange('(t p) d -> p t d', p=128), q='act')

            def m1(blk):
                load_w(blk, 0)
                for t2 in range(2):
                    pb = ps[t2].bitcast(BF16)
                    for k in range(8):
                        P.tr(pb[:, k * 128:(k + 1) * 128], xsb[blk % 2][:, t2, k * 128:(k + 1) * 128], self.identb[:])

            def m2(blk):
                for t2 in range(2):
                    pb = ps[t2].bitcast(BF16)
                    P.copy(xT[blk % 2][:, :, t2 * 128:(t2 + 1) * 128], pb[:, :].rearrange('p (k t) -> p k t', t=128),
                           eng=('act' if t2 == 0 else 'dve'))

            def m3(blk):
                i2 = blk % 2
                load_w(blk, 1)
                for f in range(4):
                    pg_ = ps[2 + f // 2]
                    pu_ = ps[4 + f // 2]
                    cs = slice((f % 2) * MB, (f % 2 + 1) * MB)
                    for k in range(8):
                        P.mm(pg_[:, cs], wg[i2][:, k, f * 128:(f + 1) * 128], xT[i2][:, k, :], start=(k == 0 and f % 2 == 0),
                             stop=(k == 7), skip_group_check=True)
                    for k in range(8):
                        P.mm(pu_[:, cs], wu[i2][:, k, f * 128:(f + 1) * 128], xT[i2][:, k, :], start=(k == 0 and f % 2 == 0),
                             stop=(k == 7), skip_group_check=True)

            def m4(blk):
                i2 = blk % 2
                for f2 in range(2):
                    P.act(sg[i2][:, 2 * f2:2 * f2 + 2, :], ps[2 + f2][:, :].rearrange('p (f t) -> p f t', t=MB), AF.Silu)
                    P.tt(hidT[i2][:, 2 * f2:2 * f2 + 2, :], sg[i2][:, 2 * f2:2 * f2 + 2, :],
                         ps[4 + f2][:, :].rearrange('p (f t) -> p f t', t=MB), ALU.mult)

            def m5(blk):
                i2 = blk % 2
                for t2 in range(2):
                    for hf in range(2):
                        py = ps[6 + hf]
                        for f in range(4):
                            P.mm(py[:, :], hidT[i2][:, f, t2 * 128:(t2 + 1) * 128], wd[i2][:, f, hf * 512:(hf + 1) * 512],
                                 start=(f == 0), stop=(f == 3))
                        P.copy(yb[i2][:, t2, hf * 512:(hf + 1) * 512], py[:, :], eng=('act' if hf == 0 else 'dve'))

            def m6(blk):
                r0 = blk * MB
                P.dma(self.ys_d[r0:r0 + MB, :].rearrange('(t p) d -> p t d', p=128), yb[blk % 2][:], q='act')

            pipeline(NBLK, [m0, m1, m2, m3, m4, m5, m6], reverse=True)
            self.sync_phase()
        with ExitStack() as s2:
            T2 = lambda n, s, dt: self.T(s2, n, s, dt)
            y0 = [T2('y0_%d' % i, [128, D], F32) for i in range(2)]
            y1 = [T2('y1_%d' % i, [128, D], F32) for i in range(2)]
            x1 = [T2('x1f%d' % i, [128, D], F32) for i in range(2)]
            gt2 = T2('gt2', [128, D], F32)
            for Tg in range(NTT):
                i2 = Tg % 2
                b = Tg // NT
                if Tg % NT == 0:
                    P.dma(gt2[:], self.mod_d[b:b + 1, 5 * D:6 * D].to_broadcast([128, D]))
                for kk, yt in ((0, y0[i2]), (1, y1[i2])):
                    ia = dest[:, kk, Tg:Tg + 1]
                    P.add('pool', (lambda yt=yt, ia=ia: (lambda e: e.indirect_dma_start(
                        out=yt[:, :], out_offset=None, in_=self.ys_d, in_offset=IOA(ap=ia, axis=0))))(),
                        [ia, self.ys_d], [yt[:]], dma=True)
                P.dma(x1[i2][:], self.x1_d[Tg * 128:(Tg + 1) * 128, :], q='sp')
                P.ts(y0[i2][:], y0[i2][:], self.wts[:, Tg, 0:1], None, ALU.mult)
                P.stt(y0[i2][:], y1[i2][:], self.wts[:, Tg, 1:2], y0[i2][:], ALU.mult, ALU.add)
                P.tt(y0[i2][:], y0[i2][:], gt2[:], ALU.mult)
                P.tt(y0[i2][:], y0[i2][:], x1[i2][:], ALU.add)
                P.dma(self.out[Tg * 128:(Tg + 1) * 128, :], y0[i2][:], q='act')
            self.sync_phase()


KB.phase4 = _kb_phase4


N_CORES = 8
_CACHE = {}

_WEIGHT_KEYS = ['w_ada', 'b_ada', 'norm1_g', 'norm2_g', 'w_in', 'nsa_q_norm', 'nsa_k_norm', 'cmp_pe_k', 'cmp_w1_k',
                'cmp_w2_k', 'cmp_pe_v', 'cmp_w1_v', 'cmp_w2_v', 'dil_q_norm', 'dil_k_norm', 'w_up_a', 'w_up_b', 'w_out',
                'w_group', 'b_group', 'w_router', 'b_router', 'w_e_gate', 'w_e_up', 'w_e_down']


def kernel(**inputs):
    x = np.asarray(inputs['x'], dtype=np.float32)
    c = np.asarray(inputs['c'], dtype=np.float32)
    pos = np.asarray(inputs['positions'], dtype=np.int32)
    B = x.shape[0]
    nseq = B // N_CORES
    if 'kb' not in _CACHE:
        kb = KB(nseq)
        kb.build()
        _CACHE['kb'] = kb
    kb = _CACHE['kb']
    w = {k: np.ascontiguousarray(np.asarray(inputs[k], dtype=np.float32)) for k in _WEIGHT_KEYS}
    in_maps = []
    for i in range(N_CORES):
        m = dict(w)
        m['x'] = np.ascontiguousarray(x[i * nseq:(i + 1) * nseq].reshape(nseq * S, D))
        m['c'] = np.ascontiguousarray(c[i * nseq:(i + 1) * nseq])
        m['positions'] = np.ascontiguousarray(pos[i * nseq:(i + 1) * nseq])
        for k, v in kb.consts.items():
            m['k_' + k] = v
        in_maps.append(m)
    res = run_bass_kernel_spmd(kb.nc, in_maps, core_ids=list(range(N_CORES)))
    out = np.concatenate([np.asarray(r['out']).reshape(nseq, S, D) for r in res.results], axis=0)
    return out.astype(np.float32)
```

```python
import numpy as np
import concourse.bass as bass
import concourse.mybir as mybir

F32 = mybir.dt.float32
BF16 = mybir.dt.bfloat16
I32 = mybir.dt.int32
U32 = mybir.dt.uint32
ALU = mybir.AluOpType
AF = mybir.ActivationFunctionType
AX = mybir.AxisListType

ENG_ATTR = {'pe': 'tensor', 'act': 'scalar', 'dve': 'vector', 'pool': 'gpsimd', 'sp': 'sync'}
ENGS = ['pe', 'act', 'dve', 'pool', 'sp']
DMA_RING = 6
_ESZ = {}


def esz(dt):
    k = str(dt)
    if k not in _ESZ:
        _ESZ[k] = mybir.dt.size(dt) if hasattr(mybir.dt, 'size') else np.dtype(mybir.dt.np(dt)).itemsize
    return _ESZ[k]


def box_of(ap):
    t = ap.tensor
    name = t.name
    pat = ap.ap
    e = esz(ap.dtype)
    space = str(ap.space)
    if 'DRAM' in space.upper() or 'HBM' in space.upper():
        lo = ap.offset
        hi = lo
        for st, n in pat:
            if st >= 0:
                hi += st * (n - 1)
            else:
                lo += st * (n - 1)
        return (name, 'D', 0, 1, lo * e, (hi + 1) * e)
    pstep, pn = pat[0]
    p0 = ap.start_partition()
    p1 = p0 + ap.partition_size()
    off = ap.offset - p0 * pstep if pstep else ap.offset
    lo = off
    hi = off
    for st, n in pat[1:]:
        if st >= 0:
            hi += st * (n - 1)
        else:
            lo += st * (n - 1)
    sp = 'P' if 'PSUM' in space.upper() else 'S'
    return (name, sp, p0, p1, lo * e, (hi + 1) * e)


class Op:
    __slots__ = ('eng', 'chan', 'pos', 'emit', 'waits', 'snap', 'inc', 'dma')


class Prog:
    def __init__(self, nc):
        self.nc = nc
        self.sems = {}
        self.semcnt = {}
        self.ops = {e: [] for e in ENGS}
        self.chan_ops = {}
        self.chan_base = {}
        self.vc = {e: {} for e in ENGS}
        self.trk = {}
        self.psum_last = {}
        self.dma_n = {e: 0 for e in ENGS}
        self._oldvals = {}
        self.n_ops = 0

    def sem(self, chan):
        if chan not in self.sems:
            nm = 's_' + (chan if isinstance(chan, str) else '%s%d' % chan)
            h = self.nc.alloc_semaphore(name=nm)
            self.sems[chan] = h
            self.semcnt[chan] = 0
        return self.sems[chan]

    def _known(self, eng, chan, pos):
        return self.vc[eng].get(chan, -1) >= pos

    def _learn(self, eng, chan, pos):
        vc = self.vc[eng]
        op = self.chan_ops[chan][pos - self.chan_base.get(chan, 0)] if pos >= self.chan_base.get(chan, 0) else None
        if op is not None and op.snap:
            for c, p in op.snap.items():
                if vc.get(c, -1) < p:
                    vc[c] = p
        if vc.get(chan, -1) < pos:
            vc[chan] = pos

    def _deps_for(self, eng, reads, writes):
        deps = set()
        for ap in reads:
            bx = box_of(ap)
            name, sp = bx[0], bx[1]
            if sp == 'P':
                self._psum_deps(eng, name, deps)
                continue
            t = self.trk.get(name)
            if t is None:
                continue
            for (wb, c, p) in t['w']:
                if wb[2] < bx[3] and bx[2] < wb[3] and wb[4] < bx[5] and bx[4] < wb[5]:
                    deps.add((c, p))
        for ap in writes:
            bx = box_of(ap)
            name, sp = bx[0], bx[1]
            if sp == 'P':
                self._psum_deps(eng, name, deps)
                continue
            t = self.trk.get(name)
            if t is None:
                continue
            for (wb, c, p) in t['w']:
                if wb[2] < bx[3] and bx[2] < wb[3] and wb[4] < bx[5] and bx[4] < wb[5]:
                    deps.add((c, p))
            for (rb, c), p in t['r'].items():
                if rb[2] < bx[3] and bx[2] < rb[3] and rb[4] < bx[5] and bx[4] < rb[5]:
                    deps.add((c, p))
        return deps

    def _psum_deps(self, eng, name, deps):
        last = self.psum_last.get(name)
        if not last:
            return
        for e, cp in last.items():
            if e == eng and eng == 'pe':
                continue
            deps.add(cp)

    def _record_access(self, eng, chan, pos, reads, writes):
        for ap in reads:
            bx = box_of(ap)
            name, sp = bx[0], bx[1]
            if sp == 'P':
                self.psum_last.setdefault(name, {})[eng] = (chan, pos)
                continue
            t = self.trk.setdefault(name, {'w': [], 'r': {}})
            t['r'][(bx, chan)] = pos
        for ap in writes:
            bx = box_of(ap)
            name, sp = bx[0], bx[1]
            if sp == 'P':
                self.psum_last.setdefault(name, {})[eng] = (chan, pos)
                continue
            t = self.trk.setdefault(name, {'w': [], 'r': {}})
            neww = []
            for ent in t['w']:
                wb = ent[0]
                if bx[2] <= wb[2] and wb[3] <= bx[3] and bx[4] <= wb[4] and wb[5] <= bx[5]:
                    continue
                neww.append(ent)
            neww.append((bx, chan, pos))
            t['w'] = neww
            if t['r']:
                t['r'] = {k: v for k, v in t['r'].items()
                          if not (bx[2] <= k[0][2] and k[0][3] <= bx[3] and bx[4] <= k[0][4] and k[0][5] <= bx[5])}

    def add(self, eng, emit, reads=(), writes=(), dma=False, extra_deps=()):
        op = Op()
        op.eng = eng
        op.emit = emit
        op.dma = dma
        op.inc = dma
        deps = self._deps_for(eng, reads, writes)
        deps.update(extra_deps)
        if dma:
            slot = self.dma_n[eng] % DMA_RING
            self.dma_n[eng] += 1
            chan = (eng, slot)
            lst = self.chan_ops.setdefault(chan, [])
            base = self.chan_base.get(chan, 0)
            if lst or base:
                deps.add((chan, base + len(lst) - 1))
        else:
            chan = eng
            lst = self.chan_ops.setdefault(chan, [])
        self.sem(chan)
        pos = self.chan_base.get(chan, 0) + len(lst)
        waits = []
        for (c, p) in sorted(deps, key=lambda cp: (str(cp[0]), cp[1])):
            if c == eng and eng == 'pe':
                continue
            if self._known(eng, c, p):
                continue
            waits.append((c, p))
        best = {}
        for c, p in waits:
            if best.get(c, -1) < p:
                best[c] = p
        op.waits = list(best.items())
        for c, p in op.waits:
            cb = self.chan_base.get(c, 0)
            if p >= cb:
                self.chan_ops[c][p - cb].inc = True
            self._learn(eng, c, p)
        op.snap = dict(self.vc[eng])
        op.chan = chan
        op.pos = pos
        lst.append(op)
        self.ops[eng].append(op)
        self._record_access(eng, chan, pos, reads, writes)
        self.n_ops += 1
        return (chan, pos)

    def barrier(self):
        lasts = []
        for chan, lst in self.chan_ops.items():
            if lst:
                lst[-1].inc = True
                lasts.append((chan, self.chan_base.get(chan, 0) + len(lst) - 1))
        for e in ENGS:
            op = Op()
            op.eng = e
            op.emit = None
            op.dma = False
            op.inc = False
            op.chan = None
            op.pos = -1
            waits = []
            for (c, p) in lasts:
                if c == e and e == 'pe':
                    continue
                if self._known(e, c, p):
                    continue
                waits.append((c, p))
            op.waits = waits
            for c, p in waits:
                self._learn(e, c, p)
            op.snap = None
            self.ops[e].append(op)

    def flush(self):
        nc = self.nc
        semval = {}
        for chan, lst in self.chan_ops.items():
            v = self.semcnt[chan]
            base = self.chan_base.get(chan, 0)
            for i, op in enumerate(lst):
                if op.inc:
                    v += 16 if op.dma else 1
                semval[(chan, base + i)] = v if op.inc else None
            self.semcnt[chan] = v
        old = self._oldvals
        old.update({k: v for k, v in semval.items() if v is not None})
        ops = self.ops
        sems = self.sems

        def run(engname):
            def f(eng):
                for op in ops[engname]:
                    for (c, p) in op.waits:
                        v = old.get((c, p))
                        assert v is not None, (engname, c, p)
                        eng.wait_ge(sems[c], v)
                    if op.emit is None:
                        continue
                    inst = op.emit(eng)
                    if op.inc:
                        inst.then_inc(sems[op.chan], 16 if op.dma else 1)
            return f

        with nc.Block() as block:
            block.tensor(run('pe'))
            block.scalar(run('act'))
            block.vector(run('dve'))
            block.gpsimd(run('pool'))
            block.sync(run('sp'))
        for chan, lst in self.chan_ops.items():
            self.chan_base[chan] = self.chan_base.get(chan, 0) + len(lst)
            self.chan_ops[chan] = []
        self.ops = {e: [] for e in ENGS}

    def dma(self, out, in_, q='sp', **kw):
        return self.add(q, lambda e: e.dma_start(out=out, in_=in_, **kw), [in_], [out], dma=True)

    def mm(self, out, lhsT, rhs, start=True, stop=True, **kw):
        return self.add('pe', lambda e: e.matmul(out, lhsT, rhs, start=start, stop=stop, **kw),
                        [lhsT, rhs], [out])

    def tr(self, out, in_, ident):
        return self.add('pe', lambda e: e.transpose(out, in_, ident), [in_, ident], [out])

    def act(self, out, in_, func, bias=None, scale=None, accum_out=None, eng='act'):
        kw = {}
        rd = [in_]
        wr = [out]
        if bias is not None:
            kw['bias'] = bias
            if not isinstance(bias, (int, float)):
                rd.append(bias)
        if scale is not None:
            kw['scale'] = scale
            if not isinstance(scale, (int, float)):
                rd.append(scale)
        if accum_out is not None:
            kw['accum_out'] = accum_out
            wr.append(accum_out)
        return self.add('act', lambda e: e.activation(out, in_, func, **kw), rd, wr)

    def tt(self, out, in0, in1, op, eng='dve'):
        return self.add(eng, lambda e: e.tensor_tensor(out, in0, in1, op), [in0, in1], [out])

    def ts(self, out, in0, s1, s2, op0, op1=None, eng='dve', accum_out=None):
        rd = [in0]
        if not isinstance(s1, (int, float)) and s1 is not None:
            rd.append(s1)
        if not isinstance(s2, (int, float)) and s2 is not None:
            rd.append(s2)
        kw = {}
        wr = [out]
        if op1 is not None:
            kw['op1'] = op1
        if accum_out is not None:
            kw['accum_out'] = accum_out
            wr.append(accum_out)
        return self.add(eng, lambda e: e.tensor_scalar(out, in0, s1, s2, op0, **kw), rd, wr)

    def stt(self, out, in0, scalar, in1, op0, op1, eng='dve'):
        rd = [in0, in1]
        if not isinstance(scalar, (int, float)):
            rd.append(scalar)
        return self.add(eng, lambda e: e.scalar_tensor_tensor(out, in0, scalar, in1, op0, op1), rd, [out])

    def copy(self, out, in_, eng='dve'):
        if eng == 'act':
            return self.add('act', lambda e: e.copy(out, in_), [in_], [out])
        return self.add(eng, lambda e: e.tensor_copy(out, in_), [in_], [out])

    def reduce(self, out, in_, op, axis=AX.X, eng='dve'):
        return self.add(eng, lambda e: e.tensor_reduce(out, in_, axis, op), [in_], [out])

    def memset(self, ap, val, eng='dve'):
        return self.add(eng, lambda e: e.memset(ap, val), [], [ap])

    def recip(self, out, in_, eng='dve'):
        return self.add(eng, lambda e: e.reciprocal(out, in_), [in_], [out])

    def max8(self, out, in_):
        return self.add('dve', lambda e: e.max(out, in_), [in_], [out])
from concourse.bass_utils import run_bass_kernel_spmd
from contextlib import ExitStack

S = 2048
D = 1024
DH = 64
NT = S // 128
IN_COLS = 4504
EPS = 1e-6
BIG = 30000.0
MAGIC = 12582912.0
TWO_PI = 6.283185307179586
NEXP = 32
FF = 512
MB = 256


def host_consts():
    c = {}
    c['identf'] = np.eye(128, dtype=np.float32)
    inv = (10000.0 ** (-np.arange(0, 64, 2, dtype=np.float32) / 64)).astype(np.float32)
    c['invf'] = np.tile(inv[None, :], (128, 1)).astype(np.float32)
    k = np.arange(128)[:, None]
    q = np.arange(128)[None, :]
    tri = (k <= q).astype(np.float32)
    anti = (k >= q).astype(np.float32)
    c['tri'] = tri
    c['anti'] = anti
    c['trianti'] = np.concatenate([tri, anti], axis=1)
    c['tri4'] = np.tile(tri, (1, 4))
    t = np.arange(S)
    cv = np.zeros((128, S), np.float32)
    cv[:127] = ((np.arange(127) * 16 + 31)[:, None] <= t[None, :])
    c['cmpvalid'] = cv
    c['blkoh'] = (np.arange(32)[:, None] == (t // 64)[None, :]).astype(np.float32)
    cs = np.arange(127) * 16
    ss = np.arange(32) * 64
    ov = np.clip(np.minimum(cs[:, None] + 32, ss[None, :] + 64) - np.maximum(cs[:, None], ss[None, :]), 0, None) / 32.0
    ovp = np.zeros((128, 32), np.float32)
    ovp[:127] = ov
    c['overlap'] = ovp
    b = (t // 64)[:, None]
    s = np.arange(32)[None, :]
    cand = ((s >= 1) & (s <= b - 2)).astype(np.float32)
    forced = (((s == 0) | (s == b) | (s == b - 1)) & (s <= b)).astype(np.float32)
    tm = lambda a: np.ascontiguousarray(a.reshape(NT, 128, 32).transpose(1, 0, 2)).astype(np.float32)
    c['cand'] = tm(cand)
    c['candm1'] = tm(cand - 1.0)
    c['forced'] = tm(forced)
    c['lstrict'] = (k < q).astype(np.float32)
    c['ones'] = np.ones((128, 128), np.float32)
    return c


class KB:
    def __init__(self, NSEQ, dbg=None):
        self.NSEQ = NSEQ
        self.NTOK = NSEQ * S
        self.NTT = NSEQ * NT
        self.NBLK = (self.NTOK * 2) // MB + NEXP
        self.NSLOT = self.NBLK * MB
        self.dbg = dbg or {}
        self.nc = bass.Bass("TRN2", target_bir_lowering=False)
        self.P = Prog(self.nc)
        self.din = {}
        self.consts = host_consts()
        blk = np.arange(self.NBLK, dtype=np.float32)[:, None] * MB
        self.consts['thr'] = np.tile(np.tile(blk, (1, 32)).reshape(1, -1), (128, 1)).astype(np.float32)
        p = np.arange(128, dtype=np.float32)[:, None]
        self.consts['rowoff'] = np.concatenate([np.arange(8)[None, :] * 128 + p, np.arange(4)[None, :] * 128 + p], axis=1).astype(np.float32)

    def dram_in(self, name, shape, dt=F32):
        h = self.nc.dram_tensor(name, list(shape), dt, kind="ExternalInput")
        self.din[name] = h
        return h.ap()

    def declare(self):
        NSEQ = self.NSEQ
        nc = self.nc
        d = self.dram_in
        self.x = d('x', [self.NTOK, D])
        self.c = d('c', [NSEQ, D])
        self.pos = d('positions', [NSEQ, S], I32)
        self.w_ada = d('w_ada', [1, D, 6 * D])
        self.b_ada = d('b_ada', [1, 6 * D])
        self.norm1_g = d('norm1_g', [1, D])
        self.norm2_g = d('norm2_g', [1, D])
        self.w_in = d('w_in', [1, D, IN_COLS])
        self.nsa_q_norm = d('nsa_q_norm', [1, DH])
        self.nsa_k_norm = d('nsa_k_norm', [1, DH])
        self.cmp_pe_k = d('cmp_pe_k', [1, 32, DH])
        self.cmp_w1_k = d('cmp_w1_k', [1, 2048, 256])
        self.cmp_w2_k = d('cmp_w2_k', [1, 256, DH])
        self.cmp_pe_v = d('cmp_pe_v', [1, 32, DH])
        self.cmp_w1_v = d('cmp_w1_v', [1, 2048, 256])
        self.cmp_w2_v = d('cmp_w2_v', [1, 256, DH])
        self.dil_q_norm = d('dil_q_norm', [1, DH])
        self.dil_k_norm = d('dil_k_norm', [1, DH])
        self.w_up_a = d('w_up_a', [1, 512, D])
        self.w_up_b = d('w_up_b', [1, 384, D])
        self.w_out = d('w_out', [1, D, D])
        self.w_group = d('w_group', [1, D, 4])
        self.b_group = d('b_group', [1, 4])
        self.w_router = d('w_router', [1, 4, D, 8])
        self.b_router = d('b_router', [1, 4, 8])
        self.w_e_gate = d('w_e_gate', [1, NEXP, D, FF])
        self.w_e_up = d('w_e_up', [1, NEXP, D, FF])
        self.w_e_down = d('w_e_down', [1, NEXP, FF, D])
        self.cd = {}
        for k, v in self.consts.items():
            self.cd[k] = d('k_' + k, v.shape)
        self.out = nc.dram_tensor('out', [self.NTOK, D], F32, kind="ExternalOutput").ap()
        sc = lambda n, shp, dt: nc.dram_tensor(n, list(shp), dt, kind="Internal").ap()
        self.mod_d = sc('mod_d', [NSEQ, 6 * D], F32)
        self.x1_d = sc('x1_d', [self.NTOK, D], F32)
        self.h2_d = sc('h2_d', [self.NTOK, D], BF16)
        self.xs_d = sc('xs_d', [self.NSLOT, D], BF16)
        self.ys_d = sc('ys_d', [self.NSLOT, D], BF16)
        self.wg_l = nc.dram_tensor('wg_l', [NEXP * 128, 8 * FF], BF16, kind="Internal").ap()
        self.wu_l = nc.dram_tensor('wu_l', [NEXP * 128, 8 * FF], BF16, kind="Internal").ap()
        self.wd_l = nc.dram_tensor('wd_l', [NEXP * 128, 4 * D], BF16, kind="Internal").ap()
        self.conv_list = []
        for e_ in range(NEXP):
            rows = slice(e_ * 128, (e_ + 1) * 128)
            self.conv_list.append((self.wg_l[rows, :].rearrange('p (k f) -> p k f', f=FF),
                                   self.w_e_gate[0, e_].rearrange('(k p) f -> p k f', p=128)))
            self.conv_list.append((self.wu_l[rows, :].rearrange('p (k f) -> p k f', f=FF),
                                   self.w_e_up[0, e_].rearrange('(k p) f -> p k f', p=128)))
            self.conv_list.append((self.wd_l[rows, :].rearrange('p (k f) -> p k f', f=D),
                                   self.w_e_down[0, e_].rearrange('(k p) f -> p k f', p=128)))
        self.conv_pos = 0
        self.w_nsa_d = sc('w_nsa_d', [128, 8 * 1304], BF16)
        self.w_dil_d = sc('w_dil_d', [128, 8 * 1152], BF16)
        self.w_gm_d = sc('w_gm_d', [128, 8 * 2048], BF16)
        self.w_upa_d = sc('w_upa_d', [128, 4 * D], BF16)
        self.w_upb_d = sc('w_upb_d', [128, 3 * D], BF16)
        self.w_o_d = sc('w_o_d', [128, 8 * D], BF16)
        self.dbg_out = {}
        for k, shp in self.dbg.items():
            self.dbg_out[k] = nc.dram_tensor('dbg_' + k, list(shp), F32, kind="ExternalOutput").ap()

    def T(self, st, name, shape, dt):
        self._uid = getattr(self, '_uid', 0) + 1
        return st.enter_context(self.nc.sbuf_tensor('%s_%d' % (name, self._uid), list(shape), dt))

    def emit_conv(self, n):
        for _ in range(n):
            if self.conv_pos >= len(self.conv_list):
                return
            dst, src = self.conv_list[self.conv_pos]
            self.conv_pos += 1
            self.P.dma(dst, src, q='pool')

    def dump(self, key, ap):
        if key in self.dbg_out:
            self.P.dma(self.dbg_out[key], ap, q='pool')

    def build(self, upto=99):
        nc, P = self.nc, self.P
        self.declare()
        with ExitStack() as top:
            self.top = top
            T = lambda n, s, dt: self.T(top, n, s, dt)
            self.ps = [top.enter_context(nc.psum_tensor("ps%d" % i, [128, 512], F32)) for i in range(8)]
            self.identf = T('identf', [128, 128], F32)
            self.identb = T('identb', [128, 128], BF16)
            self.invf = T('invf', [128, 32], F32)
            self.trib = T('trib', [128, 128], BF16)
            self.antib = T('antib', [128, 128], BF16)
            self.triantib = T('triantib', [128, 256], BF16)
            self.tri4b = T('tri4b', [128, 512], BF16)
            self.cmpvalid = T('cmpvalid', [128, S], BF16)
            self.cand = T('cand', [128, NT, 32], F32)
            self.candm1 = T('candm1', [128, NT, 32], F32)
            self.forced = T('forced', [128, NT, 32], F32)
            self.lstrict = T('lstrict', [128, 128], BF16)
            self.onesb = T('onesb', [128, 128], BF16)
            P.dma(self.identf[:], self.cd['identf'])
            P.dma(self.identb[:], self.cd['identf'], q='pool')
            P.dma(self.invf[:], self.cd['invf'])
            P.dma(self.trib[:], self.cd['tri'], q='pool')
            P.dma(self.antib[:], self.cd['anti'], q='pool')
            P.dma(self.triantib[:], self.cd['trianti'], q='pool')
            P.dma(self.tri4b[:], self.cd['tri4'], q='pool')
            P.dma(self.cmpvalid[:], self.cd['cmpvalid'], q='pool')
            P.dma(self.cand[:], self.cd['cand'])
            P.dma(self.candm1[:], self.cd['candm1'])
            P.dma(self.forced[:], self.cd['forced'])
            P.dma(self.lstrict[:], self.cd['lstrict'], q='pool')
            P.dma(self.onesb[:], self.cd['ones'], q='pool')
            self.modT1 = T('modT1', [128, 16, 4], F32)
            self.hT = T('hT', [128, 8, S], BF16)
            self.phase0()
            if upto >= 1:
                for b in range(self.NSEQ):
                    self.phase1(b)
                    if upto >= 2:
                        pass
            P.barrier()
            P.flush()
        return nc

    def sync_phase(self, name=None):
        self.P.barrier()
        if name is None:
            import inspect
            fr = inspect.stack()[1]
            name = '%s_%d' % (fr.function.replace('_kb_', ''), fr.lineno)
        with self.nc.named_scope(name):
            self.P.flush()

    def phase0(self):
        nc, P, NSEQ = self.nc, self.P, self.NSEQ
        ps = self.ps
        with ExitStack() as st:
            T = lambda n, s, dt: self.T(st, n, s, dt)
            cs = T('cs', [4, D], F32)
            csT = T('csT', [128, 8, 4], F32)
            wa = [T('wa%d' % i, [128, 8, 512], F32) for i in range(2)]
            modrows = T('modrows', [4, 6 * D], F32)
            bada = T('bada', [4, 6 * D], F32)
            g1b = T('g1b', [4, D], F32)
            g2b = T('g2b', [4, D], F32)
            P.dma(cs[0:NSEQ, :], self.c)
            P.dma(bada[0:NSEQ, :], self.b_ada.to_broadcast([NSEQ, 6 * D]))
            P.dma(g1b[0:NSEQ, :], self.norm1_g.to_broadcast([NSEQ, D]))
            P.dma(g2b[0:NSEQ, :], self.norm2_g.to_broadcast([NSEQ, D]))
            P.act(cs[0:NSEQ, :], cs[0:NSEQ, :], AF.Silu)
            for k in range(8):
                P.tr(ps[0][:, k * 4:k * 4 + NSEQ], cs[0:NSEQ, k * 128:(k + 1) * 128], self.identf[0:NSEQ, 0:NSEQ])
            P.copy(csT[:, :, 0:NSEQ], ps[0][:, 0:32].rearrange('p (k b) -> p k b', b=4)[:, :, 0:NSEQ])
            wv = self.w_ada[0].rearrange('(k p) c -> p k c', p=128)
            for cc in range(12):
                w = wa[cc % 2]
                P.dma(w[:], wv[:, :, cc * 512:(cc + 1) * 512], q=('sp' if cc % 2 == 0 else 'act'))
                pb = ps[1 + cc % 2]
                for k in range(8):
                    P.mm(pb[0:NSEQ, :], csT[:, k, 0:NSEQ], w[:, k, :], start=(k == 0), stop=(k == 7))
                P.tt(modrows[0:NSEQ, cc * 512:(cc + 1) * 512], pb[0:NSEQ, :], bada[0:NSEQ, cc * 512:(cc + 1) * 512], ALU.add)
            P.stt(modrows[0:NSEQ, D:2 * D], modrows[0:NSEQ, D:2 * D], 1.0, g1b[0:NSEQ, :], ALU.add, ALU.mult)
            P.stt(modrows[0:NSEQ, 4 * D:5 * D], modrows[0:NSEQ, 4 * D:5 * D], 1.0, g2b[0:NSEQ, :], ALU.add, ALU.mult)
            P.dma(self.mod_d, modrows[0:NSEQ, :])
            for ch in range(16):
                P.tr(ps[3][:, ch * 4:ch * 4 + NSEQ], modrows[0:NSEQ, ch * 128:(ch + 1) * 128], self.identf[0:NSEQ, 0:NSEQ])
            P.copy(self.modT1[:, :, 0:NSEQ], ps[3][:, 0:64].rearrange('p (k b) -> p k b', b=4)[:, :, 0:NSEQ])
            self.dump('modrows', modrows[0:NSEQ, :])
            self.sync_phase()

    def phase1(self, b):
        nc, P = self.nc, self.P
        ps = self.ps
        with ExitStack() as st:
            T = lambda n, s, dt: self.T(st, n, s, dt)
            xt = [T('xt%d' % i, [128, D], F32) for i in range(3)]
            junk = T('p1junk', [128, D], F32)
            xn = [T('xn%d' % i, [128, D], F32) for i in range(2)]
            ss = [T('p1ss%d' % i, [128, 4], F32) for i in range(2)]
            def p0(tt):
                r0 = b * S + tt * 128
                P.dma(xt[tt % 3][:], self.x[r0:r0 + 128, :], q=('sp' if tt % 2 == 0 else 'act'))

            def p1(tt):
                s_ = ss[tt % 2]
                P.act(junk[:], xt[tt % 3][:], AF.Square, accum_out=s_[:, 0:1])
                P.act(s_[:, 1:2], s_[:, 0:1], AF.Ln, scale=1.0 / D, bias=EPS)
                P.act(s_[:, 2:3], s_[:, 1:2], AF.Exp, scale=-0.5)

            def p2(tt):
                P.ts(xn[tt % 2][:], xt[tt % 3][:], ss[tt % 2][:, 2:3], None, ALU.mult)

            def p3(tt):
                for k in range(8):
                    pb = ps[(tt % 2) * 2 + k // 4]
                    P.tr(pb[:, (k % 4) * 128:(k % 4 + 1) * 128], xn[tt % 2][:, k * 128:(k + 1) * 128], self.identf[:])

            def p4(tt):
                for k in range(8):
                    pb = ps[(tt % 2) * 2 + k // 4]
                    src = pb[:, (k % 4) * 128:(k % 4 + 1) * 128]
                    dst = self.hT[:, k, tt * 128:(tt + 1) * 128]
                    if k % 2 == 0:
                        P.act(dst, src, AF.Identity, scale=self.modT1[:, 8 + k, b:b + 1], bias=self.modT1[:, k, b:b + 1])
                    else:
                        P.ts(dst, src, self.modT1[:, 8 + k, b:b + 1], self.modT1[:, k, b:b + 1], ALU.mult, ALU.add)

            pipeline(NT, [p0, p1, p2, p3, p4], reverse=True)
            if b == 0:
                for k in range(8):
                    if ('hT%d' % k) in self.dbg_out:
                        self.dump('hT%d' % k, self.hT[:, k, :])
            self.sync_phase()


PI_SAFE = 3.1415925


def _kb_rope_tables(self, st, posf, n, cos_out, sin_out, tag):
    P = self.P
    T = lambda nm, s, dt: self.T(st, tag + nm, s, dt)
    ang = T('ang', [128, n, 32], F32)
    a2 = T('a2', [128, n, 32], F32)
    kk = T('kk', [128, n, 32], F32)
    P.tt(ang[:], self.invf[:, :].unsqueeze(1).to_broadcast([128, n, 32]),
         posf.unsqueeze(2).to_broadcast([128, n, 32]), ALU.mult)
    for off, outp in ((0.0, sin_out), (np.pi / 2, cos_out)):
        if off == 0.0:
            a = ang
        else:
            P.ts(a2[:], ang[:], float(off), None, ALU.add)
            a = a2
        P.ts(kk[:], a[:], 1.0 / TWO_PI, MAGIC, ALU.mult, ALU.add)
        P.ts(kk[:], kk[:], MAGIC, None, ALU.subtract)
        P.stt(kk[:], kk[:], -TWO_PI, a[:], ALU.mult, ALU.add)
        P.ts(kk[:], kk[:], PI_SAFE, -PI_SAFE, ALU.min, ALU.max)
        P.act(outp, kk[:], AF.Sin)


KB.rope_tables = _kb_rope_tables


def _kb_setup_nsa_consts(self):
    P, top = self.P, self.top
    T = lambda n, s, dt: self.T(top, n, s, dt)
    self.kslc = T('kslc', [96, S], BF16)
    P.dma(self.kslc[64:96, :], self.cd['blkoh'], q='pool')
    self.v2 = T('v2', [128, NT, 2, 65], BF16)
    P.memset(self.v2[:].rearrange('p a b c -> p (a b c)'), 1.0)
    self.vcaug = T('vcaug', [128, 97], BF16)
    P.memset(self.vcaug[:, 64:65], 1.0)
    P.dma(self.vcaug[:, 65:97], self.cd['overlap'], q='pool')
    self.w1kv = T('w1kv', [128, 32, 256], BF16)
    P.dma(self.w1kv[0:64], self.cmp_w1_k[0].rearrange('(l d) h -> d l h', d=64), q='pool')
    P.dma(self.w1kv[64:128], self.cmp_w1_v[0].rearrange('(l d) h -> d l h', d=64), q='pool')
    self.w2kv = T('w2kv', [128, 2, 2, 64], BF16)
    P.dma(self.w2kv[:, 0], self.cmp_w2_k[0].rearrange('(c p) d -> p c d', p=128), q='pool')
    P.dma(self.w2kv[:, 1], self.cmp_w2_v[0].rearrange('(c p) d -> p c d', p=128), q='pool')
    self.ckv = T('ckv', [128, 2, 2], F32)
    self.gfull = T('gfull', [128, 6, 64], F32)
    self.gk = T('gk', [128, 64], F32)
    self.gqb = T('gqb', [128, 64], F32)
    self.gkb = T('gkb', [128, 64], F32)
    for hh in range(4):
        P.dma(self.gfull[:, hh, :], self.nsa_q_norm.to_broadcast([128, 64]))
    for hh in range(4, 6):
        P.dma(self.gfull[:, hh, :], self.nsa_k_norm.to_broadcast([128, 64]))
    P.ts(self.gfull[:, 0:4, :], self.gfull[:, 0:4, :], 0.125, None, ALU.mult)
    P.dma(self.gk[:], self.nsa_k_norm.to_broadcast([128, 64]))
    P.dma(self.gqb[:], self.dil_q_norm.to_broadcast([128, 64]))
    P.ts(self.gqb[:], self.gqb[:], 0.125, None, ALU.mult)
    P.dma(self.gkb[:], self.dil_k_norm.to_broadcast([128, 64]))
    with ExitStack() as st:
        T2 = lambda n, s, dt: self.T(st, n, s, dt)
        pekv = T2('pekv', [32, 128], F32)
        peT = T2('peT', [128, 32], BF16)
        P.dma(pekv[:, 0:64], self.cmp_pe_k[0])
        P.dma(pekv[:, 64:128], self.cmp_pe_v[0])
        P.tr(self.ps[0][:, 0:32], pekv[:, :], self.identf[0:32, 0:32])
        P.copy(peT[:], self.ps[0][:, 0:32])
        for kv in range(2):
            base = 64 * kv
            for hc in range(2):
                for l in range(32):
                    P.mm(self.ps[1 + kv][:, hc:hc + 1], self.w1kv[base:base + 64, l, hc * 128:(hc + 1) * 128],
                         peT[base:base + 64, l:l + 1], start=(hc == 0 and l == 0), stop=(l == 31), skip_group_check=True)
            P.copy(self.ckv[:, kv, :], self.ps[1 + kv][:, 0:2])
        self.sync_phase()


KB.setup_nsa_consts = _kb_setup_nsa_consts


def _kb_seq_prologue(self, st, b):
    P = self.P
    T = lambda n, s, dt: self.T(st, n, s, dt)
    self.cosT = T('cosT', [128, NT, 32], F32)
    self.sinT = T('sinT', [128, NT, 32], F32)
    self.cosC = T('cosC', [128, 1, 32], F32)
    self.sinC = T('sinC', [128, 1, 32], F32)
    with ExitStack() as s2:
        T2 = lambda n, s, dt: self.T(s2, n, s, dt)
        posi = T2('posi', [128, 2], I32)
        posi16 = T2('posi16', [16, 128], I32)
        posf16 = T2('posf16', [16, 128], F32)
        posf = T2('posf', [128, NT + 1], F32)
        P.memset(posi[:], 0)
        P.dma(posi16[:], self.pos[b].rearrange('(t p) -> t p', p=128))
        P.copy(posf16[:], posi16[:])
        P.tr(self.ps[0][:, 0:NT], posf16[:], self.identf[0:16, 0:16])
        P.copy(posf[:, 0:NT], self.ps[0][:, 0:NT])
        P.dma(posi[0:127, 0:1], self.pos[b, 31:31 + 16 * 126 + 1:16].unsqueeze(1), allow_slow_non_contiguous=True)
        P.copy(posf[:, NT:NT + 1], posi[:, 0:1])
        self.rope_tables(s2, posf[:, 0:NT], NT, self.cosT[:], self.sinT[:], 'rt')
        self.rope_tables(s2, posf[:, NT:NT + 1], 1, self.cosC[:], self.sinC[:], 'rc')
        self.sync_phase()


KB.seq_prologue = _kb_seq_prologue


class BankRound:
    def __init__(self):
        self.started = {}

    def reset(self, bank):
        self.started[bank.name] = False

    def start(self, bank):
        s = not self.started.get(bank.name, False)
        self.started[bank.name] = True
        return s


def pipeline(n, stages, delays=None, reverse=False):
    if delays is None:
        delays = list(range(len(stages)))
    order = list(range(len(stages)))
    if reverse:
        order = order[::-1]
    for step in range(n + max(delays)):
        for j in order:
            i = step - delays[j]
            if 0 <= i < n:
                stages[j](i)


def _kb_phase2_nsa(self, b, st_seq):
    nc, P, ps = self.nc, self.P, self.ps
    BR = self.br
    wv = self.w_in[0].rearrange('(k p) c -> p k c', p=128)
    with ExitStack() as st:
        T = lambda n, s, dt: self.T(st, n, s, dt)
        w_nsa = T('w_nsa', [128, 8, 1304], BF16)
        P.dma(w_nsa[:].rearrange('p k c -> p (k c)'), self.w_nsa_d)
        gates = T('gates', [128, NT, 24], F32)
        for g in range(2):
            with ExitStack() as sg:
                self.nsa_group(b, g, sg, w_nsa, gates)
                self.sync_phase()


def _kb_nsa_group(self, b, g, st, w_nsa, gates):
    nc, P, ps = self.nc, self.P, self.ps
    BR = self.br
    T = lambda n, s, dt: self.T(st, n, s, dt)
    qaug = T('qaug', [96, 4, S], BF16)
    kwin = T('kwin', [64, S], BF16)
    kvcT = T('kvcT', [128, S], BF16)
    kcT = T('kcT', [64, 128], BF16)
    kslc, v2, vcaug = self.kslc, self.v2, self.vcaug
    with ExitStack() as s2:
        T2 = lambda n, s, dt: self.T(s2, n, s, dt)
        NP = NT // 2
        sq = [T2('sq%d' % i, [128, 2, 6, 64], F32) for i in range(2)]
        rc = [T2('rc%d' % i, [128, 2, 6, 64], F32) for i in range(4)]
        rn = [T2('rn%d' % i, [128, 2, 6, 64], F32) for i in range(2)]
        tmp = [T2('rtmp%d' % i, [128, 4, 2, 6, 32], F32) for i in range(2)]
        rr = [T2('rr%d' % i, [128, 2, 6, 64], BF16) for i in range(2)]
        st6 = [T2('st6%d' % i, [128, 3, 12], F32) for i in range(2)]
        for tc in range(4):
            pc = ps[6 + tc % 2]
            for k in range(8):
                P.mm(pc[:, :], w_nsa[:, k, 1024 + 128 * g:1024 + 128 * g + 128], self.hT[:, k, tc * 512:(tc + 1) * 512],
                     start=(k == 0), stop=(k == 7))
            P.copy(kvcT[:, tc * 512:(tc + 1) * 512], pc[:, :], eng='act')
        tokf = lambda tt: slice(tt * 128, (tt + 1) * 128)

        def f0(i):
            for u in range(2):
                tt = 2 * i + u
                pa = ps[(i % 2) * 2 + u]
                for k in range(8):
                    P.mm(pa[:, :], self.hT[:, k, tokf(tt)], w_nsa[:, k, g * 512:(g + 1) * 512], start=(k == 0), stop=(k == 7))
                if g == 0:
                    for k in range(8):
                        P.mm(ps[6][:, u * 24:(u + 1) * 24], self.hT[:, k, tokf(tt)], w_nsa[:, k, 1280:1304],
                             start=(k == 0 and u == 0), stop=(k == 7), skip_group_check=True)

        def f1(i):
            for u in range(2):
                tt = 2 * i + u
                pa = ps[(i % 2) * 2 + u]
                R = pa[:, 0:384].rearrange('p (h d) -> p h d', d=64)
                P.act(sq[i % 2][:, u], R, AF.Square)
                P.copy(rc[i % 4][:, u], R, eng='act')
                P.copy(v2[:, tt, :, 0:64], pa[:, 384:512].rearrange('p (a d) -> p a d', d=64), eng='act')
            if g == 0:
                P.copy(gates[:, 2 * i:2 * i + 2, :], ps[6][:, 0:48].rearrange('p (u c) -> p u c', c=24))

        def f2(i):
            P.reduce(st6[i % 2][:, 0, :], sq[i % 2][:].rearrange('p u h d -> p (u h) d'), ALU.add)

        def f3(i):
            P.act(st6[i % 2][:, 1, :], st6[i % 2][:, 0, :], AF.Ln, scale=1.0 / DH, bias=EPS)
            P.act(st6[i % 2][:, 2, :], st6[i % 2][:, 1, :], AF.Exp, scale=-0.5)

        def f4(i):
            i2 = i % 2
            rnv = rn[i2][:].rearrange('p u h d -> p (u h) d')
            P.tt(rnv, rc[i % 4][:].rearrange('p u h d -> p (u h) d'),
                 st6[i2][:, 2, :].unsqueeze(2).to_broadcast([128, 12, 64]), ALU.mult)
            P.tt(rn[i2][:], rn[i2][:], self.gfull[:].unsqueeze(1).to_broadcast([128, 2, 6, 64]), ALU.mult)
            cosb = self.cosT[:, 2 * i:2 * i + 2, :].unsqueeze(2).to_broadcast([128, 2, 6, 32])
            sinb = self.sinT[:, 2 * i:2 * i + 2, :].unsqueeze(2).to_broadcast([128, 2, 6, 32])
            x1 = rn[i2][:, :, :, 0:32]
            x2 = rn[i2][:, :, :, 32:64]
            tm = tmp[i2]
            P.tt(tm[:, 0], x1, cosb, ALU.mult)
            P.tt(tm[:, 1], x2, sinb, ALU.mult)
            P.tt(tm[:, 2], x1, sinb, ALU.mult, eng='pool')
            P.tt(tm[:, 3], x2, cosb, ALU.mult, eng='pool')
            P.tt(rr[i2][:, :, :, 0:32], tm[:, 0], tm[:, 1], ALU.subtract)
            P.tt(rr[i2][:, :, :, 32:64], tm[:, 2], tm[:, 3], ALU.add, eng='pool')

        def f5(i):
            for u in range(2):
                pt_ = ps[4 + u].bitcast(BF16)
                for hh in range(6):
                    P.tr(pt_[0:64, hh * 128:(hh + 1) * 128], rr[i % 2][:, u, hh, :], self.identb[:])

        def f6(i):
            for u in range(2):
                tt = 2 * i + u
                pt_ = ps[4 + u].bitcast(BF16)
                tok = tokf(tt)
                P.copy(qaug[0:64, :, tok], pt_[0:64, 0:512].rearrange('p (h t) -> p h t', t=128), eng='act')
                P.copy(kslc[0:64, tok], pt_[0:64, 512:640], eng=('act' if u == 0 else 'dve'))
                P.copy(kwin[0:64, tok], pt_[0:64, 640:768], eng=('act' if u == 0 else 'dve'))

        pipeline(NP, [f0, f1, f2, f3, f4, f5, f6], reverse=True)
        if g == 0:
            P.act(gates[:].rearrange('p a b -> p (a b)'), gates[:].rearrange('p a b -> p (a b)'), AF.Sigmoid)
        hid = T2('hid', [128, 2, 2, 128], BF16)
        kc4 = T2('kc4', [128, 8, 64], F32)
        kst = T2('kst', [128, 4], F32)
        kcr = T2('kcr', [128, 64], BF16)
        ktm = T2('ktm', [128, 4, 32], F32)
        for kv in range(2):
            base = 64 * kv
            pz = ps[5 + kv]
            BR.reset(pz)
            for hc in range(2):
                for l in range(32):
                    P.mm(pz[:, hc * 128:hc * 128 + 127], self.w1kv[base:base + 64, l, hc * 128:(hc + 1) * 128],
                         kvcT[base:base + 64, l:l + 16 * 126 + 1:16], start=BR.start(pz), stop=(l == 31),
                         skip_group_check=True)
            for hc in range(2):
                P.act(hid[:, kv, hc, 0:127], pz[:, hc * 128:hc * 128 + 127], AF.Silu, bias=self.ckv[:, kv, hc:hc + 1])
        p2 = ps[7]
        BR.reset(p2)
        for kv in range(2):
            for hc in range(2):
                P.mm(p2[0:127, kv * 64:(kv + 1) * 64], hid[:, kv, hc, 0:127], self.w2kv[:, kv, hc, :],
                     start=BR.start(p2), stop=(hc == 1), skip_group_check=True)
        P.copy(vcaug[0:127, 0:64], p2[0:127, 64:128], eng='act')
        P.act(kc4[0:127, 0, :], p2[0:127, 0:64], AF.Square)
        P.reduce(kst[0:127, 0:1], kc4[0:127, 0, :], ALU.add)
        P.act(kst[0:127, 1:2], kst[0:127, 0:1], AF.Ln, scale=1.0 / DH, bias=EPS)
        P.act(kst[0:127, 2:3], kst[0:127, 1:2], AF.Exp, scale=-0.5)
        P.stt(kc4[0:127, 1, :], p2[0:127, 0:64], kst[0:127, 2:3], self.gk[0:127, :], ALU.mult, ALU.mult)
        x1 = kc4[0:127, 1, 0:32]
        x2 = kc4[0:127, 1, 32:64]
        cC = self.cosC[0:127, 0, :]
        sC = self.sinC[0:127, 0, :]
        P.tt(ktm[0:127, 0], x1, cC, ALU.mult)
        P.tt(ktm[0:127, 1], x2, sC, ALU.mult)
        P.tt(ktm[0:127, 2], x1, sC, ALU.mult)
        P.tt(ktm[0:127, 3], x2, cC, ALU.mult)
        P.tt(kcr[0:127, 0:32], ktm[0:127, 0], ktm[0:127, 1], ALU.subtract)
        P.tt(kcr[0:127, 32:64], ktm[0:127, 2], ktm[0:127, 3], ALU.add)
        pk = ps[3].bitcast(BF16)
        P.tr(pk[0:64, 0:127], kcr[0:127, :], self.identb[0:127, 0:127])
        P.copy(kcT[:, 0:127], pk[0:64, 0:127])
        if b == 0:
            self.dump('qaug%d' % g, qaug[0:64].rearrange('p h s -> p (h s)'))
            self.dump('kslc%d' % g, kslc[0:64, :])
            self.dump('kwin%d' % g, kwin[:, :])
            self.dump('kcT%d' % g, kcT[:, :])
            self.dump('vc%d' % g, vcaug[:, 0:64])
        self.sync_phase()
    self.nsa_attention(b, g, st, qaug, kwin, kcT, gates)


KB.phase2_nsa = _kb_phase2_nsa
KB.nsa_group = _kb_nsa_group


def _kb_nsa_attention(self, b, g, st, qaug, kwin, kcT, gates):
    nc, P, ps = self.nc, self.P, self.ps
    BR = self.br
    self.emit_conv(-(-len(self.conv_list) // (2 * self.NSEQ)))
    kslc, v2, vcaug = self.kslc, self.v2, self.vcaug
    with ExitStack() as s3:
        T = lambda n, s, dt: self.T(s3, n, s, dt)
        ptile = [T('ptile%d' % i, [128, 512], BF16) for i in range(3)]
        oacc = T('oacc', [128, NT, 4, 64], F32)
        impacc = T('impacc', [128, NT, 32], F32)
        rz = [T('rz%d' % i, [128, 2, 4], F32) for i in range(3)]
        otmp = [T('otmp%d' % i, [128, 4, 64], F32) for i in range(2)]
        itmp = [T('itmp%d' % i, [128, 4, 32], F32) for i in range(2)]
        scw = [T('scw%d' % i, [128, 4, 32], F32) for i in range(2)]
        slw = [T('slw%d' % i, [128, 4, 32], F32) for i in range(2)]
        m8 = [T('m8%d' % i, [128, 4, 8], F32) for i in range(2)]
        biasb = [T('biasb%d' % i, [128, 4, 32], BF16) for i in range(2)]
        ob = [T('ob%d' % i, [128, 256], BF16) for i in range(2)]
        sbank = [ps[0], ps[1], ps[2]]
        pvbank = [ps[3], ps[4]]
        misc = ps[5]
        otb = [ps[6], ps[7]]
        cnt = {'s': 0, 'pv': 0, 'fin': 0}

        def finalize(pvb, hl, br, qc, ncol, first, want_imp, first_imp):
            h = 4 * g + hl
            k_ = cnt['fin']
            cnt['fin'] += 1
            r = rz[k_ % 3]
            pv3 = pvb[:, 0:4 * ncol].rearrange('p (q c) -> p q c', c=ncol)
            P.ts(r[:, 0, :], pv3[:, :, 64], 1e-30, None, ALU.max)
            P.recip(r[:, 0, :], r[:, 0, :])
            P.tt(r[:, 1, :], r[:, 0, :], gates[:, qc * 4:(qc + 1) * 4, br * 8 + h], ALU.mult)
            tgt = oacc[:, qc * 4:(qc + 1) * 4, hl, :]
            sb_ = r[:, 1, :].unsqueeze(2).to_broadcast([128, 4, 64])
            if first:
                P.tt(tgt, pv3[:, :, 0:64], sb_, ALU.mult)
            else:
                ot = otmp[k_ % 2]
                P.tt(ot[:], pv3[:, :, 0:64], sb_, ALU.mult)
                P.tt(tgt, tgt, ot[:], ALU.add)
            if want_imp:
                itg = impacc[:, qc * 4:(qc + 1) * 4, :]
                rb_ = r[:, 0, :].unsqueeze(2).to_broadcast([128, 4, 32])
                if first_imp:
                    P.tt(itg, pv3[:, :, 65:97], rb_, ALU.mult)
                else:
                    it = itmp[k_ % 2]
                    P.tt(it[:], pv3[:, :, 65:97], rb_, ALU.mult)
                    P.tt(itg, itg, it[:], ALU.add)

        def selection(qc):
            sc, sl, m_, bb = scw[qc % 2], slw[qc % 2], m8[qc % 2], biasb[qc % 2]
            tq = slice(qc * 4, (qc + 1) * 4)
            P.tt(sc[:], impacc[:, tq, :], self.cand[:, tq, :], ALU.mult)
            P.tt(sc[:], sc[:], self.candm1[:, tq, :], ALU.add)
            for qt in range(4):
                P.max8(m_[:, qt, :], sc[:, qt, :])
            P.tt(sl[:], sc[:], m_[:, :, 4].unsqueeze(2).to_broadcast([128, 4, 32]), ALU.is_ge)
            P.tt(sl[:], sl[:], self.forced[:, tq, :], ALU.max)
            P.ts(bb[:], sl[:], 1.0, BIG, ALU.subtract, ALU.mult)
            mb = misc.bitcast(BF16)
            for qt in range(4):
                P.tr(mb[0:32, qt * 128:(qt + 1) * 128], bb[:, qt, :], self.identb[:])
            P.copy(qaug[64:96, :, qc * 512:(qc + 1) * 512],
                   mb[0:32, 0:512].unsqueeze(1).to_broadcast([32, 4, 512]), eng='act')
            if b == 0:
                for qt in range(4):
                    self.dump('sel%d_%d' % (g, qc * 4 + qt), sl[:, qt, :])

        astate = {}

        def c0(i):
            qc, hl = i // 4, i % 4
            k_ = cnt['s']
            cnt['s'] += 1
            astate[i] = (sbank[k_ % 3], ptile[k_ % 3])
            P.mm(astate[i][0][0:127, :], kcT[0:64, 0:127], qaug[0:64, hl, qc * 512:(qc + 1) * 512], start=True, stop=True)

        def c1(i):
            sb, pt = astate[i]
            P.act(pt[0:127, :], sb[0:127, :], AF.Exp)

        def c2(i):
            qc = i // 4
            sb, pt = astate[i]
            P.tt(pt[0:127, :], pt[0:127, :], self.cmpvalid[0:127, qc * 512:(qc + 1) * 512], ALU.mult)

        def c3(i):
            sb, pt = astate[i]
            pvb = pvbank[cnt['pv'] % 2]
            cnt['pv'] += 1
            BR.reset(pvb)
            for qt in range(4):
                P.mm(pvb[:, qt * 97:(qt + 1) * 97], pt[0:127, qt * 128:(qt + 1) * 128], vcaug[0:127, :],
                     start=BR.start(pvb), stop=True, skip_group_check=True)
            astate[i] = pvb

        def c4(i):
            qc, hl = i // 4, i % 4
            finalize(astate.pop(i), hl, 0, qc, 97, True, True, hl == 0)
            if hl == 3:
                selection(qc)

        pipeline(16, [c0, c1, c2, c3, c4])

        steps = []
        for qc in range(4):
            for hl in range(4):
                kbs = list(range(max(0, 4 * qc - 4), 4 * qc + 4))
                for kb in kbs:
                    if kb < 4 * qc:
                        j = kb - (4 * qc - 4)
                        c0_, c1_, mask = 0, 128 * (j + 1), ('anti', 128 * j)
                    else:
                        j = kb - 4 * qc
                        c0_, c1_, mask = 128 * j, 512, ('tri', 128 * j)
                    steps.append(dict(br=2, hl=hl, kb=kb, c0=c0_, c1=c1_, mask=mask, qc=qc, firsth=(kb == kbs[0]), last=(kb == kbs[-1])))
            for hl in range(4):
                kbs = list(range(0, 4 * qc + 4))
                for kb in kbs:
                    if kb < 4 * qc:
                        c0_, c1_, mask = 0, 512, None
                    else:
                        j = kb - 4 * qc
                        c0_, c1_, mask = 128 * j, 512, ('tri', 128 * j)
                    steps.append(dict(br=1, hl=hl, kb=kb, c0=c0_, c1=c1_, mask=mask, qc=qc, firsth=(kb == kbs[0]), last=(kb == kbs[-1])))
        n = len(steps)
        state = {}

        def qk(i):
            s_ = steps[i]
            k_ = cnt['s']
            cnt['s'] += 1
            sb = sbank[k_ % 3]
            pt = ptile[k_ % 3]
            hl, kb, c0_, c1_, br = s_['hl'], s_['kb'], s_['c0'], s_['c1'], s_['br']
            q0 = s_['qc'] * 512
            if br == 1:
                lhsT = kslc[0:96, kb * 128:(kb + 1) * 128]
                rhs = qaug[0:96, hl, q0 + c0_:q0 + c1_]
            else:
                lhsT = kwin[0:64, kb * 128:(kb + 1) * 128]
                rhs = qaug[0:64, hl, q0 + c0_:q0 + c1_]
            P.mm(sb[:, c0_:c1_], lhsT, rhs, start=True, stop=True)
            P.act(pt[:, c0_:c1_], sb[:, c0_:c1_], AF.Exp)
            if s_['mask'] is not None:
                kind, col = s_['mask']
                mk = self.trib if kind == 'tri' else self.antib
                P.tt(pt[:, col:col + 128], pt[:, col:col + 128], mk[:], ALU.mult)
            state[i] = pt

        def pv(i):
            s_ = steps[i]
            pt = state.pop(i)
            hl, kb, c0_, c1_, br = s_['hl'], s_['kb'], s_['c0'], s_['c1'], s_['br']
            if s_['firsth']:
                state['pvb'] = pvbank[cnt['pv'] % 2]
                cnt['pv'] += 1
                BR.reset(state['pvb'])
            pvb = state['pvb']
            for qt in range(c0_ // 128, c1_ // 128):
                P.mm(pvb[:, qt * 65:(qt + 1) * 65], pt[:, qt * 128:(qt + 1) * 128], v2[:, kb, br - 1, :],
                     start=BR.start(pvb), stop=True, skip_group_check=True)
            if s_['last']:
                finalize(pvb, hl, br, s_['qc'], 65, False, False, False)

        AHEAD = 2
        for i in range(min(AHEAD, n)):
            qk(i)
        for i in range(n):
            if i + AHEAD < n:
                qk(i + AHEAD)
            pv(i)

        def o0(tg):
            P.copy(ob[tg % 2][:], oacc[:, tg].rearrange('p h d -> p (h d)'), eng='act')

        def o1(tg):
            pb = otb[tg % 2].bitcast(BF16)
            for pr in range(2):
                P.tr(pb[:, pr * 128:(pr + 1) * 128], ob[tg % 2][:, pr * 128:(pr + 1) * 128], self.identb[:])

        def o2(tg):
            pb = otb[tg % 2].bitcast(BF16)
            P.copy(self.o_aT[:, 2 * g:2 * g + 2, tg * 128:(tg + 1) * 128],
                   pb[:, 0:256].rearrange('p (c t) -> p c t', t=128), eng='dve')

        pipeline(NT, [o0, o1, o2])


KB.nsa_attention = _kb_nsa_attention


def _kb_prep_weights(self):
    P = self.P
    wv = self.w_in[0].rearrange('(k p) c -> p k c', p=128)
    with ExitStack() as st:
        T = lambda n, s, dt: self.T(st, n, s, dt)
        w_nsa = T('pw_nsa', [128, 8, 1304], BF16)
        for g in range(2):
            o = g * 512
            for (dst, src, n) in ((0, 256 * g, 256), (256, 768 + 64 * g, 64), (320, 1024 + 64 * g, 64),
                                  (384, 896 + 64 * g, 64), (448, 1152 + 64 * g, 64)):
                P.dma(w_nsa[:, :, o + dst:o + dst + n], wv[:, :, src:src + n], q='pool')
            P.dma(w_nsa[:, :, 1024 + 128 * g:1024 + 128 * g + 64], wv[:, :, 512 + 64 * g:512 + 64 * g + 64], q='pool')
            P.dma(w_nsa[:, :, 1024 + 128 * g + 64:1024 + 128 * g + 128], wv[:, :, 640 + 64 * g:640 + 64 * g + 64], q='pool')
        P.dma(w_nsa[:, :, 1280:1304], wv[:, :, 1280:1304], q='pool')
        P.dma(self.w_nsa_d, w_nsa[:].rearrange('p k c -> p (k c)'))
        w_dil = T('pw_dil', [128, 8, 1152], BF16)
        for gi in range(3):
            for pi, base in enumerate((1304, 1688, 2072)):
                P.dma(w_dil[:, :, gi * 384 + pi * 128:gi * 384 + (pi + 1) * 128],
                      wv[:, :, base + 128 * gi:base + 128 * gi + 128], q='pool')
        P.dma(self.w_dil_d, w_dil[:].rearrange('p k c -> p (k c)'))
        w_gm = T('pw_gm', [128, 8, 2048], BF16)
        for q4 in range(4):
            P.dma(w_gm[:, :, q4 * 512:(q4 + 1) * 512], wv[:, :, 2456 + q4 * 512:2456 + (q4 + 1) * 512], q='pool')
        P.dma(self.w_gm_d, w_gm[:].rearrange('p k c -> p (k c)'))
        w_upa = T('pw_upa', [128, 4, D], BF16)
        w_upb = T('pw_upb', [128, 3, D], BF16)
        for c in range(4):
            P.dma(w_upa[:, c, :], self.w_up_a[0, c * 128:(c + 1) * 128, :], q='pool')
        for c in range(3):
            P.dma(w_upb[:, c, :], self.w_up_b[0, c * 128:(c + 1) * 128, :], q='pool')
        P.dma(self.w_upa_d, w_upa[:].rearrange('p k c -> p (k c)'))
        P.dma(self.w_upb_d, w_upb[:].rearrange('p k c -> p (k c)'))
        w_o = T('pw_o', [128, 8, D], BF16)
        for k in range(8):
            P.dma(w_o[:, k, :], self.w_out[0, k * 128:(k + 1) * 128, :], q='pool')
        P.dma(self.w_o_d, w_o[:].rearrange('p k c -> p (k c)'))
        self.sync_phase()


KB.prep_weights = _kb_prep_weights


def _kb_build(self, upto=99):
    nc, P = self.nc, self.P
    self.declare()
    self.br = BankRound()
    with ExitStack() as top:
        self.top = top
        T = lambda n, s, dt: self.T(top, n, s, dt)
        self.ps = [top.enter_context(nc.psum_tensor("ps%d" % i, [128, 512], F32)) for i in range(8)]
        self.identf = T('identf', [128, 128], F32)
        self.identb = T('identb', [128, 128], BF16)
        self.invf = T('invf', [128, 32], F32)
        self.trib = T('trib', [128, 128], BF16)
        self.antib = T('antib', [128, 128], BF16)
        self.triantib = T('triantib', [128, 256], BF16)
        self.tri4b = T('tri4b', [128, 512], BF16)
        self.cmpvalid = T('cmpvalid', [128, S], BF16)
        self.cand = T('cand', [128, NT, 32], F32)
        self.candm1 = T('candm1', [128, NT, 32], F32)
        self.forced = T('forced', [128, NT, 32], F32)
        self.lstrict = T('lstrict', [128, 128], BF16)
        self.onesb = T('onesb', [128, 128], BF16)
        P.dma(self.identf[:], self.cd['identf'])
        P.dma(self.identb[:], self.cd['identf'], q='pool')
        P.dma(self.invf[:], self.cd['invf'])
        P.dma(self.trib[:], self.cd['tri'], q='pool')
        P.dma(self.antib[:], self.cd['anti'], q='pool')
        P.dma(self.triantib[:], self.cd['trianti'], q='pool')
        P.dma(self.tri4b[:], self.cd['tri4'], q='pool')
        P.dma(self.cmpvalid[:], self.cd['cmpvalid'], q='pool')
        P.dma(self.cand[:], self.cd['cand'])
        P.dma(self.candm1[:], self.cd['candm1'])
        P.dma(self.forced[:], self.cd['forced'])
        P.dma(self.lstrict[:], self.cd['lstrict'], q='pool')
        P.dma(self.onesb[:], self.cd['ones'], q='pool')
        self.modT1 = T('modT1', [128, 16, 4], F32)
        if upto >= 4:
            self.setup_moe_consts()
        if upto >= 2:
            self.prep_weights()
        with ExitStack() as mix:
            self.top = mix
            Tm = lambda n, s, dt: self.T(mix, n, s, dt)
            self.hT = Tm('hT', [128, 8, S], BF16)
            self.o_aT = Tm('o_aT', [128, 4, S], BF16)
            self.o_bT = Tm('o_bT', [128, 3, S], BF16)
            self.phase0()
            if upto >= 2:
                self.setup_nsa_consts()
            for b in range(self.NSEQ):
                if upto >= 1:
                    self.phase1(b)
                if upto >= 2:
                    with ExitStack() as st_seq:
                        self.seq_prologue(st_seq, b)
                        self.phase2_nsa(b, st_seq)
                        if b == 0:
                            for c in range(4):
                                self.dump('oaT%d' % c, self.o_aT[:, c, :])
                        if upto >= 3:
                            self.phase2_dil(b, st_seq)
                            if b == 0:
                                for c in range(3):
                                    self.dump('obT%d' % c, self.o_bT[:, c, :])
                        if upto >= 4:
                            self.phase3(b, st_seq)
                        self.sync_phase()
            self.sync_phase()
        self.top = top
        if upto >= 5:
            self.phase4()
        P.barrier()
        P.flush()
    return nc


KB.build = _kb_build


DIL_D = (1, 4, 16)


def _kb_phase2_dil(self, b, st_seq):
    nc, P, ps = self.nc, self.P, self.ps
    BR = self.br
    wv = self.w_in[0].rearrange('(k p) c -> p k c', p=128)
    with ExitStack() as st:
        T = lambda n, s, dt: self.T(st, n, s, dt)
        w_dil = T('w_dil', [128, 8, 1152], BF16)
        P.dma(w_dil[:].rearrange('p k c -> p (k c)'), self.w_dil_d, q='act')
        us = T('us', [128, 3, S], F32)
        ztot = T('ztot', [128, S], F32)
        gfb = T('gfb', [128, 4, 64], F32)
        for hh in range(2):
            P.copy(gfb[:, hh, :], self.gqb[:])
            P.copy(gfb[:, 2 + hh, :], self.gkb[:])
        for gi in range(3):
            d = DIL_D[gi]
            with ExitStack() as sg:
                T2 = lambda n, s, dt: self.T(sg, n, s, dt)
                qbT = T2('qbT', [64, 2, S], BF16)
                kbT = T2('kbT', [64, 2, S], BF16)
                vb = T2('vb', [128, 16, 128], BF16)
                DEP = 4
                sq = [T2('dsq%d' % i, [128, 4, 64], F32) for i in range(DEP)]
                rc = [T2('drc%d' % i, [128, 4, 64], F32) for i in range(DEP)]
                rn = [T2('drn%d' % i, [128, 4, 64], F32) for i in range(2)]
                tmp = [T2('dtmp%d' % i, [128, 4, 4, 32], F32) for i in range(2)]
                rr = [T2('drr%d' % i, [128, 4, 64], BF16) for i in range(DEP)]
                st4 = [T2('dst%d' % i, [128, 3, 4], F32) for i in range(DEP)]
                ptile = [T2('dpt%d' % i, [128, 512], BF16) for i in range(3)]
                tokf = lambda tt: slice(tt * 128, (tt + 1) * 128)

                def d0(tt):
                    pa = ps[tt % 3]
                    for k in range(8):
                        P.mm(pa[:, 0:256], self.hT[:, k, tokf(tt)], w_dil[:, k, gi * 384:gi * 384 + 256], start=(k == 0), stop=(k == 7))

                def d1(tt):
                    R = ps[tt % 3][:, 0:256].rearrange('p (h d) -> p h d', d=64)
                    P.act(sq[tt % DEP][:], R, AF.Square)
                    P.copy(rc[tt % DEP][:], R)

                def d2(tt):
                    P.reduce(st4[tt % DEP][:, 0, :], sq[tt % DEP][:], ALU.add)

                def d3(tt):
                    P.act(st4[tt % DEP][:, 1, :], st4[tt % DEP][:, 0, :], AF.Ln, scale=1.0 / DH, bias=EPS)
                    P.act(st4[tt % DEP][:, 2, :], st4[tt % DEP][:, 1, :], AF.Exp, scale=-0.5)

                def d4(tt):
                    i2 = tt % 2
                    P.tt(rn[i2][:], rc[tt % DEP][:], st4[tt % DEP][:, 2, :].unsqueeze(2).to_broadcast([128, 4, 64]), ALU.mult)
                    P.tt(rn[i2][:], rn[i2][:], gfb[:], ALU.mult)
                    cosb = self.cosT[:, tt, :].unsqueeze(1).to_broadcast([128, 4, 32])
                    sinb = self.sinT[:, tt, :].unsqueeze(1).to_broadcast([128, 4, 32])
                    x1 = rn[i2][:, :, 0:32]
                    x2 = rn[i2][:, :, 32:64]
                    tm = tmp[i2]
                    P.tt(tm[:, 0], x1, cosb, ALU.mult)
                    P.tt(tm[:, 1], x2, sinb, ALU.mult)
                    P.tt(tm[:, 2], x1, sinb, ALU.mult, eng='pool')
                    P.tt(tm[:, 3], x2, cosb, ALU.mult, eng='pool')
                    P.tt(rr[tt % DEP][:, :, 0:32], tm[:, 0], tm[:, 1], ALU.subtract)
                    P.tt(rr[tt % DEP][:, :, 32:64], tm[:, 2], tm[:, 3], ALU.add, eng='pool')

                def d5(tt):
                    pt_ = ps[3 + tt % 2].bitcast(BF16)
                    for hh in range(4):
                        P.tr(pt_[0:64, hh * 128:(hh + 1) * 128], rr[tt % DEP][:, hh, :], self.identb[:])

                def d6(tt):
                    pt_ = ps[3 + tt % 2].bitcast(BF16)
                    P.copy(qbT[:, :, tokf(tt)], pt_[0:64, 0:256].rearrange('p (h t) -> p h t', t=128), eng='act')
                    P.copy(kbT[:, :, tokf(tt)], pt_[0:64, 256:512].rearrange('p (h t) -> p h t', t=128), eng='act')

                pipeline(NT, [d0, d1, d2, d3, d4, d5, d6])
                nkb = 16 // d
                for r in range(d):
                    for kbs in range(nkb):
                        bi = r * nkb + kbs
                        pvp = ps[4 + bi % 2]
                        s0 = 128 * kbs * d + r
                        for k in range(8):
                            P.mm(pvp[:, 0:128], self.hT[:, k, s0:s0 + 127 * d + 1:d], w_dil[:, k, gi * 384 + 256:gi * 384 + 384],
                                 start=(k == 0), stop=(k == 7))
                        P.copy(vb[:, bi, :], pvp[:, 0:128], eng='act')
                rounds = []
                if d == 1:
                    for Rn_ in range(4):
                        steps = []
                        for kbs in range(max(0, 4 * Rn_ - 1), 4 * Rn_ + 4):
                            if kbs < 4 * Rn_:
                                steps.append(dict(r=0, kbs=kbs, q0=4 * Rn_, nq=1, slot=0, mask='anti'))
                            elif kbs < 4 * Rn_ + 3:
                                steps.append(dict(r=0, kbs=kbs, q0=kbs, nq=2, slot=kbs - 4 * Rn_, mask='trianti'))
                            else:
                                steps.append(dict(r=0, kbs=kbs, q0=kbs, nq=1, slot=3, mask='tri'))
                        rounds.append(dict(steps=steps, out=('contig', 512 * Rn_)))
                elif d == 4:
                    for r in range(4):
                        steps = []
                        for kbs in range(4):
                            if kbs < 3:
                                steps.append(dict(r=r, kbs=kbs, q0=kbs, nq=2, slot=kbs, mask='trianti'))
                            else:
                                steps.append(dict(r=r, kbs=kbs, q0=kbs, nq=1, slot=3, mask='tri'))
                        rounds.append(dict(steps=steps, out=('strided', r)))
                else:
                    for Rr in range(4):
                        steps = [dict(r=4 * Rr + i, kbs=0, q0=0, nq=1, slot=i, mask='tri') for i in range(4)]
                        rounds.append(dict(steps=steps, out=('res16', 4 * Rr)))
                items = []
                for rd_i, rd in enumerate(rounds):
                    for si, s_ in enumerate(rd['steps']):
                        for j in range(2):
                            items.append((rd_i, s_, j, si == len(rd['steps']) - 1 and j == 1))
                dstate = {}
                dcnt = {'s': 0}
                dstarted = {}

                def dqk(ii):
                    rd_i, s_, j, _ = items[ii]
                    r, kbs, q0, nq = s_['r'], s_['kbs'], s_['q0'], s_['nq']
                    k0 = 128 * kbs * d + r
                    qs0 = 128 * q0 * d + r
                    ncol = 128 * nq
                    sb = ps[dcnt['s'] % 4]
                    pt = ptile[dcnt['s'] % 3]
                    dcnt['s'] += 1
                    P.mm(sb[:, 0:ncol], kbT[0:64, j, k0:k0 + 127 * d + 1:d],
                         qbT[0:64, j, qs0:qs0 + (ncol - 1) * d + 1:d], start=True, stop=True)
                    P.act(pt[:, 0:ncol], sb[:, 0:ncol], AF.Exp)
                    mk = {'tri': self.trib[:], 'anti': self.antib[:], 'trianti': self.triantib[:]}[s_['mask']]
                    P.tt(pt[:, 0:ncol], pt[:, 0:ncol], mk, ALU.mult)
                    dstate[ii] = pt

                def dpv(ii):
                    rd_i, s_, j, last = items[ii]
                    rd = rounds[rd_i]
                    pt = dstate.pop(ii)
                    r, kbs, nq, slot = s_['r'], s_['kbs'], s_['nq'], s_['slot']
                    bi = r * nkb + kbs
                    ncol = 128 * nq
                    c0 = slot * 128
                    pu = ps[4 + (rd_i % 2) * 2]
                    pz = ps[5 + (rd_i % 2) * 2]
                    first = not dstarted.get((rd_i, j), False)
                    dstarted[(rd_i, j)] = True
                    P.mm(pu[64 * j:64 * j + 64, c0:c0 + ncol], vb[:, bi, 64 * j:64 * j + 64], pt[:, 0:ncol],
                         start=first, stop=True, skip_group_check=True)
                    P.mm(pz[64 * j:64 * j + 64, c0:c0 + ncol], self.onesb[:, 0:64], pt[:, 0:ncol],
                         start=first, stop=True, skip_group_check=True)
                    if last:
                        kind, o0 = rd['out']
                        if kind == 'contig':
                            uo = us[:, gi, o0:o0 + 512]
                            zo = ztot[:, o0:o0 + 512]
                            pui, pzi = pu[:, :], pz[:, :]
                        elif kind == 'strided':
                            uo = us[:, gi, o0:o0 + 511 * 4 + 1:4]
                            zo = ztot[:, o0:o0 + 511 * 4 + 1:4]
                            pui, pzi = pu[:, :], pz[:, :]
                        else:
                            uo = us[:, gi, :].rearrange('p (k r) -> p r k', r=16)[:, o0:o0 + 4, :]
                            zo = ztot[:, :].rearrange('p (k r) -> p r k', r=16)[:, o0:o0 + 4, :]
                            pui = pu[:, :].rearrange('p (s k) -> p s k', k=128)
                            pzi = pz[:, :].rearrange('p (s k) -> p s k', k=128)
                        P.copy(uo, pui, eng='act')
                        if gi == 0:
                            P.copy(zo, pzi)
                        else:
                            P.tt(zo, pzi, zo, ALU.add)

                AH = 2
                nit = len(items)
                for ii in range(min(AH, nit)):
                    dqk(ii)
                for ii in range(nit):
                    if ii + AH < nit:
                        dqk(ii + AH)
                    dpv(ii)
                self.sync_phase()
        P.act(ztot[:], ztot[:], AF.Ln)
        P.act(ztot[:], ztot[:], AF.Exp, scale=-1.0)
        for gi in range(3):
            P.tt(self.o_bT[:, gi, :], us[:, gi, :], ztot[:], ALU.mult)
        self.sync_phase()


KB.phase2_dil = _kb_phase2_dil


def _kb_setup_moe_consts(self):
    P, top = self.P, self.top
    T = lambda n, s, dt: self.T(top, n, s, dt)
    NTT = self.NTT
    self.wr = T('wr', [128, 8, 36], F32)
    with self.nc.allow_non_contiguous_dma(reason="tiny router weight rows"):
        P.dma(self.wr[:, :, 0:4], self.w_group[0].rearrange('(k p) g -> p k g', p=128))
        for gg in range(4):
            P.dma(self.wr[:, :, 4 + 8 * gg:12 + 8 * gg], self.w_router[0, gg].rearrange('(k p) e -> p k e', p=128))
    self.brow = T('brow', [128, 36], F32)
    P.dma(self.brow[:, 0:4], self.b_group.to_broadcast([128, 4]))
    P.dma(self.brow[:, 4:36], self.b_router[0:1].rearrange('o g e -> o (g e)').to_broadcast([128, 32]))
    self.EH = [T('EH%d' % k, [128, NTT, 32], BF16) for k in range(2)]
    self.rank = T('rank', [128, NTT, 2], F32)
    self.wts = T('wts', [128, NTT, 2], F32)
    self.carry = T('carry', [128, 32], F32)
    P.memset(self.carry[:], 0.0)


KB.setup_moe_consts = _kb_setup_moe_consts


def _kb_phase3(self, b, st_seq):
    nc, P, ps = self.nc, self.P, self.ps
    with ExitStack() as st:
        T = lambda n, s, dt: self.T(st, n, s, dt)
        w_gm = T('w_gm', [128, 8, 2048], BF16)
        w_upa = T('w_upa', [128, 4, D], BF16)
        w_upb = T('w_upb', [128, 3, D], BF16)
        P.dma(w_upa[:].rearrange('p k c -> p (k c)'), self.w_upa_d, q='act')
        P.dma(w_upb[:].rearrange('p k c -> p (k c)'), self.w_upb_d, q='act')
        P.dma(w_gm[:].rearrange('p k c -> p (k c)'), self.w_gm_d)
        gm = [T('gm%d' % i, [128, 2, 512], F32) for i in range(2)]
        ybf = [T('ybf%d' % i, [128, 512], BF16) for i in range(2)]
        tokf = lambda tt: slice(tt * 128, (tt + 1) * 128)

        def a0(i):
            tt, hf = i // 2, i % 2
            pa_, pb_ = (ps[0], ps[1]) if i % 2 == 0 else (ps[5], ps[6])
            cs = slice(hf * 512, (hf + 1) * 512)
            for c in range(4):
                P.mm(pa_[:, :], self.o_aT[:, c, tokf(tt)], w_upa[:, c, cs], start=(c == 0), stop=(c == 3))
            for c in range(3):
                P.mm(pb_[:, :], self.o_bT[:, c, tokf(tt)], w_upb[:, c, cs], start=(c == 0), stop=(c == 2))
            for q2 in range(2):
                cg = slice(q2 * 1024 + hf * 512, q2 * 1024 + (hf + 1) * 512)
                for k in range(8):
                    P.mm(ps[2 + q2][:, :], self.hT[:, k, tokf(tt)], w_gm[:, k, cg], start=(k == 0), stop=(k == 7))

        def a1(i):
            for q2 in range(2):
                P.act(gm[i % 2][:, q2, :], ps[2 + q2][:, :], AF.Sigmoid)

        def a2(i):
            pa_, pb_ = (ps[0], ps[1]) if i % 2 == 0 else (ps[5], ps[6])
            g_ = gm[i % 2]
            P.tt(g_[:, 0, :], g_[:, 0, :], pa_[:, :], ALU.mult)
            P.tt(g_[:, 1, :], g_[:, 1, :], pb_[:, :], ALU.mult)
            P.tt(ybf[i % 2][:], g_[:, 0, :], g_[:, 1, :], ALU.add)

        def a3(i):
            pb = ps[4].bitcast(BF16)
            for k in range(4):
                P.tr(pb[:, k * 128:(k + 1) * 128], ybf[i % 2][:, k * 128:(k + 1) * 128], self.identb[:])

        def a4(i):
            tt, hf = i // 2, i % 2
            pb = ps[4].bitcast(BF16)
            P.copy(self.hT[:, 4 * hf:4 * hf + 4, tokf(tt)], pb[:, 0:512].rearrange('p (k t) -> p k t', t=128), eng='act')

        pipeline(2 * NT, [a0, a1, a2, a3, a4], reverse=True)
        self.sync_phase()
    with ExitStack() as st:
        T = lambda n, s, dt: self.T(st, n, s, dt)
        w_o = T('w_o', [128, 8, D], BF16)
        P.dma(w_o[:].rearrange('p k c -> p (k c)'), self.w_o_d)
        gt1 = T('gt1', [128, D], F32)
        A2 = T('A2', [128, D], F32)
        sh2 = T('sh2', [128, D], F32)
        P.dma(gt1[:], self.mod_d[b:b + 1, 2 * D:3 * D].to_broadcast([128, D]), q='act')
        P.dma(sh2[:], self.mod_d[b:b + 1, 3 * D:4 * D].to_broadcast([128, D]), q='act')
        P.dma(A2[:], self.mod_d[b:b + 1, 4 * D:5 * D].to_broadcast([128, D]), q='act')
        xt = [T('x3t%d' % i, [128, D], F32) for i in range(2)]
        x1 = [T('x1t%d' % i, [128, D], F32) for i in range(2)]
        h2 = [T('h2t%d' % i, [128, D], F32) for i in range(2)]
        junk = T('p3junk', [128, D], F32)
        h2T = [T('h2T%d' % i, [128, 8, 128], F32) for i in range(2)]
        sm = [T('sm%d' % i, [128, 4], F32) for i in range(2)]
        lgall = T('lgall', [128, NT, 36], F32)
        tokf = lambda tt: slice(tt * 128, (tt + 1) * 128)

        def b0(tt):
            r0 = b * S + tt * 128
            P.dma(xt[tt % 2][:], self.x[r0:r0 + 128, :], q=('sp' if tt % 2 == 0 else 'act'))

        def b1(tt):
            for hf in range(2):
                for k in range(8):
                    P.mm(ps[hf][:, :], self.hT[:, k, tokf(tt)], w_o[:, k, hf * 512:(hf + 1) * 512], start=(k == 0), stop=(k == 7))

        def b2(tt):
            i2 = tt % 2
            r0 = b * S + tt * 128
            for hf in range(2):
                sl = slice(hf * 512, (hf + 1) * 512)
                P.tt(x1[i2][:, sl], ps[hf][:, :], gt1[:, sl], ALU.mult)
                P.tt(x1[i2][:, sl], x1[i2][:, sl], xt[i2][:, sl], ALU.add)
            P.dma(self.x1_d[r0:r0 + 128, :], x1[i2][:])

        def b3(tt):
            s_ = sm[tt % 2]
            P.act(junk[:], x1[tt % 2][:], AF.Square, accum_out=s_[:, 0:1])
            P.act(s_[:, 1:2], s_[:, 0:1], AF.Ln, scale=1.0 / D, bias=EPS)
            P.act(s_[:, 2:3], s_[:, 1:2], AF.Exp, scale=-0.5)

        def b4(tt):
            i2 = tt % 2
            r0 = b * S + tt * 128
            P.stt(h2[i2][:], x1[i2][:], sm[i2][:, 2:3], A2[:], ALU.mult, ALU.mult)
            P.tt(h2[i2][:], h2[i2][:], sh2[:], ALU.add)
            P.dma(self.h2_d[r0:r0 + 128, :], h2[i2][:], q='pool')

        def b5(tt):
            for k in range(8):
                pb = ps[2 + k // 4]
                P.tr(pb[:, (k % 4) * 128:(k % 4 + 1) * 128], h2[tt % 2][:, k * 128:(k + 1) * 128], self.identf[:])

        def b6(tt):
            for hf in range(2):
                P.copy(h2T[tt % 2][:, hf * 4:(hf + 1) * 4, :], ps[2 + hf][:, :].rearrange('p (k t) -> p k t', t=128),
                       eng=('act' if hf == 0 else 'dve'))

        def b7(tt):
            for k in range(8):
                P.mm(ps[4][:, 0:36], h2T[tt % 2][:, k, :], self.wr[:, k, :], start=(k == 0), stop=(k == 7))

        def b8(tt):
            P.tt(lgall[:, tt, :], ps[4][:, 0:36], self.brow[:], ALU.add)

        pipeline(NT, [b0, b1, b2, b3, b4, b5, b6, b7, b8], reverse=True)
        T0 = b * NT
        rt = T('rt', [128, NT, 16], F32)
        gw = T('gw', [128, 6, NT], F32)
        sel4 = T('sel4', [128, NT, 4, 8], F32)
        sel = T('selr', [128, NT, 8], F32)
        m8a = T('m8a', [128, NT, 8], F32)
        oh = T('ohr', [128, 2, NT, 8], F32)
        eh12 = T('eh12a', [128, NT, 32], BF16)
        pre = T('pre', [128, NT, 32], F32)
        big = T('bigr', [128, NT, 32], F32)
        G4 = lgall[:, :, 0:4]
        P.reduce(gw[:, 0, :], G4, ALU.max)
        P.tt(rt[:, :, 0:4], G4, gw[:, 0, :].unsqueeze(2).to_broadcast([128, NT, 4]), ALU.subtract)
        P.act(rt[:, :, 4:8], rt[:, :, 0:4], AF.Exp)
        P.reduce(gw[:, 1, :], rt[:, :, 4:8], ALU.add)
        P.recip(gw[:, 1, :], gw[:, 1, :])
        P.tt(rt[:, :, 8:12], G4, gw[:, 0, :].unsqueeze(2).to_broadcast([128, NT, 4]), ALU.is_equal)
        goh = rt[:, :, 8:12]
        P.tt(sel4[:], lgall[:, :, 4:36].rearrange('p t (g e) -> p t g e', e=8),
             goh.unsqueeze(3).to_broadcast([128, NT, 4, 8]), ALU.mult)
        P.reduce(sel[:], sel4[:].rearrange('p t g e -> p t e g'), ALU.add)
        for t in range(NT):
            P.max8(m8a[:, t, :], sel[:, t, :])
        P.tt(gw[:, 2, :], m8a[:, :, 1], m8a[:, :, 0], ALU.subtract)
        P.act(gw[:, 3, :], gw[:, 2, :], AF.Exp)
        P.ts(gw[:, 4, :], gw[:, 3, :], 1.0, None, ALU.add)
        P.recip(gw[:, 4, :], gw[:, 4, :])
        P.tt(gw[:, 5, :], gw[:, 3, :], gw[:, 4, :], ALU.mult)
        P.tt(self.wts[:, T0:T0 + NT, 0], gw[:, 4, :], gw[:, 1, :], ALU.mult)
        P.tt(self.wts[:, T0:T0 + NT, 1], gw[:, 5, :], gw[:, 1, :], ALU.mult)
        for kk in range(2):
            P.tt(oh[:, kk], sel[:], m8a[:, :, kk].unsqueeze(2).to_broadcast([128, NT, 8]), ALU.is_equal)
            P.tt(self.EH[kk][:, T0:T0 + NT, :].rearrange('p t (g e) -> p t g e', e=8),
                 goh.unsqueeze(3).to_broadcast([128, NT, 4, 8]),
                 oh[:, kk].unsqueeze(2).to_broadcast([128, NT, 4, 8]), ALU.mult)
        P.tt(eh12[:], self.EH[0][:, T0:T0 + NT, :], self.EH[1][:, T0:T0 + NT, :], ALU.add)
        P.mm(ps[5][:, :], self.lstrict[:], eh12[:].rearrange('p t e -> p (t e)'), start=True, stop=True)
        P.mm(ps[6][:, :], self.onesb[:], eh12[:].rearrange('p t e -> p (t e)'), start=True, stop=True)
        P.copy(big[:].rearrange('p t e -> p (t e)'), ps[6][:, :], eng='act')
        for t in range(NT):
            P.tt(pre[:, t, :], ps[5][:, t * 32:(t + 1) * 32], self.carry[:], ALU.add)
            P.tt(self.carry[:], self.carry[:], big[:, t, :], ALU.add)
        for kk in range(2):
            P.tt(big[:], self.EH[kk][:, T0:T0 + NT, :], pre[:], ALU.mult)
            P.reduce(self.rank[:, T0:T0 + NT, kk], big[:], ALU.add)
        if b == 0:
            self.dump('lg0', lgall[:, 0, :])
        self.sync_phase()


KB.phase3 = _kb_phase3


def _kb_phase4(self):
    nc, P, ps = self.nc, self.P, self.ps
    NTT, NBLK = self.NTT, self.NBLK
    wg_v = self.w_e_gate[0].rearrange('e r f -> (e r) f')
    wu_v = self.w_e_up[0].rearrange('e r f -> (e r) f')
    wd_v = self.w_e_down[0].rearrange('e r f -> (e r) f')
    IOA = bass.IndirectOffsetOnAxis
    with ExitStack() as st:
        T = lambda n, s, dt: self.T(st, n, s, dt)
        dest = T('dest', [128, 2, NTT], I32)
        widx = T('widx', [128, NBLK], I32)
        with ExitStack() as s2:
            T2 = lambda n, s, dt: self.T(s2, n, s, dt)
            cnt = T2('cnt', [128, 6, 32], F32)
            onesf = T2('onesf', [128, 32], F32)
            big = T2('bigtmp', [128, NTT, 32], F32)
            thr = T2('thr', [128, NBLK, 32], F32)
            cmp_ = T2('cmpb', [128, NBLK, 32], F32)
            be = T2('be', [128, 2, NBLK], F32)
            rgu = T2('rgu', [128, 12], F32)
            df = T2('df', [128, 2, NTT], F32)
            P.dma(thr[:].rearrange('p a b -> p (a b)'), self.cd['thr'])
            P.memset(onesf[:], 1.0)
            P.copy(cnt[:, 0, :], self.carry[:])
            P.ts(cnt[:, 1, :], cnt[:, 0, :], float(MB - 1), 1.0 / MB, ALU.add, ALU.mult)
            P.ts(cnt[:, 1, :], cnt[:, 1, :], -0.498, MAGIC, ALU.add, ALU.add)
            P.ts(cnt[:, 1, :], cnt[:, 1, :], MAGIC, float(MB), ALU.subtract, ALU.mult)
            P.add('dve', lambda e: e.tensor_tensor_scan(cnt[:, 2, :], onesf[:], cnt[:, 1, :], 0.0, ALU.mult, ALU.add),
                  [onesf[:], cnt[:, 1, :]], [cnt[:, 2, :]])
            P.tt(cnt[:, 3, :], cnt[:, 2, :], cnt[:, 1, :], ALU.subtract)
            for kk in range(2):
                P.tt(big[:], self.EH[kk][:], cnt[:, 3, :].unsqueeze(1).to_broadcast([128, NTT, 32]), ALU.mult)
                P.reduce(df[:, kk, :], big[:], ALU.add)
                P.tt(df[:, kk, :], df[:, kk, :], self.rank[:, :, kk], ALU.add)
            P.copy(dest[:], df[:])
            P.tt(cmp_[:], cnt[:, 2, :].unsqueeze(1).to_broadcast([128, NBLK, 32]), thr[:], ALU.is_le)
            P.reduce(be[:, 0, :], cmp_[:], ALU.add)
            P.ts(be[:, 0, :], be[:, 0, :], float(NEXP - 1), None, ALU.min)
            P.dma(rgu[:], self.cd['rowoff'])
            P.ts(be[:, 1, :], be[:, 0, :], 128.0, rgu[:, 0:1], ALU.mult, ALU.add)
            P.copy(widx[:], be[:, 1, :])
            self.dump('dest', df[:].rearrange('p a b -> p (a b)'))
            self.dump('be', be[:, 0, :])
            self.dump('cnt', cnt[:].rearrange('p a b -> p (a b)'))
            self.sync_phase()
        with ExitStack() as s2:
            T2 = lambda n, s, dt: self.T(s2, n, s, dt)
            hb = [T2('h2b%d' % i, [128, D], BF16) for i in range(3)]
            for Tg in range(NTT):
                h_ = hb[Tg % 3]
                P.dma(h_[:], self.h2_d[Tg * 128:(Tg + 1) * 128, :], q='sp')
                for kk in range(2):
                    ia = dest[:, kk, Tg:Tg + 1]
                    P.add('pool', (lambda h_=h_, ia=ia: (lambda e: e.indirect_dma_start(
                        out=self.xs_d, out_offset=IOA(ap=ia, axis=0), in_=h_[:, :], in_offset=None)))(),
                        [h_[:], ia], [], dma=True)
            self.sync_phase()
        with ExitStack() as s2:
            T2 = lambda n, s, dt: self.T(s2, n, s, dt)
            wg = [T2('wg%d' % i, [128, 8, FF], BF16) for i in range(2)]
            wu = [T2('wu%d' % i, [128, 8, FF], BF16) for i in range(2)]
            wd = [T2('wd%d' % i, [128, 4, D], BF16) for i in range(2)]
            xsb = [T2('xsb%d' % i, [128, 2, D], BF16) for i in range(2)]
            xT = [T2('xT%d' % i, [128, 8, MB], BF16) for i in range(2)]
            sg = [T2('sg%d' % i, [128, 4, MB], F32) for i in range(2)]
            hidT = [T2('hidT%d' % i, [128, 4, MB], BF16) for i in range(2)]
            yb = [T2('yb%d' % i, [128, 2, D], BF16) for i in range(2)]

            def load_w(blk, which):
                i2 = blk % 2
                ia = widx[:, blk:blk + 1]
                lst = ((wg[i2], self.wg_l), (wu[i2], self.wu_l)) if which == 0 else ((wd[i2], self.wd_l),)
                for (wt, src) in lst:
                    P.add('pool', (lambda wt=wt, src=src, ia=ia: (lambda e: e.indirect_dma_start(
                        out=wt[:].rearrange('p k f -> p (k f)'), out_offset=None, in_=src, in_offset=IOA(ap=ia, axis=0))))(),
                        [ia], [wt[:]], dma=True)

            def m0(blk):
                r0 = blk * MB
                P.dma(xsb[blk % 2][:], self.xs_d[r0:r0 + MB, :].rearrange('(t p) d -> p t d', p=128), q='act')

            def m1(blk):
                load_w(blk, 0)
                for t2 in range(2):
                    pb = ps[t2].bitcast(BF16)
                    for k in range(8):
                        P.tr(pb[:, k * 128:(k + 1) * 128], xsb[blk % 2][:, t2, k * 128:(k + 1) * 128], self.identb[:])

            def m2(blk):
                for t2 in range(2):
                    pb = ps[t2].bitcast(BF16)
                    P.copy(xT[blk % 2][:, :, t2 * 128:(t2 + 1) * 128], pb[:, :].rearrange('p (k t) -> p k t', t=128),
                           eng=('act' if t2 == 0 else 'dve'))

            def m3(blk):
                i2 = blk % 2
                load_w(blk, 1)
                for f in range(4):
                    pg_ = ps[2 + f // 2]
                    pu_ = ps[4 + f // 2]
                    cs = slice((f % 2) * MB, (f % 2 + 1) * MB)
                    for k in range(8):
                        P.mm(pg_[:, cs], wg[i2][:, k, f * 128:(f + 1) * 128], xT[i2][:, k, :], start=(k == 0 and f % 2 == 0),
                             stop=(k == 7), skip_group_check=True)
                    for k in range(8):
                        P.mm(pu_[:, cs], wu[i2][:, k, f * 128:(f + 1) * 128], xT[i2][:, k, :], start=(k == 0 and f % 2 == 0),
                             stop=(k == 7), skip_group_check=True)

            def m4(blk):
                i2 = blk % 2
                for f2 in range(2):
                    P.act(sg[i2][:, 2 * f2:2 * f2 + 2, :], ps[2 + f2][:, :].rearrange('p (f t) -> p f t', t=MB), AF.Silu)
                    P.tt(hidT[i2][:, 2 * f2:2 * f2 + 2, :], sg[i2][:, 2 * f2:2 * f2 + 2, :],
                         ps[4 + f2][:, :].rearrange('p (f t) -> p f t', t=MB), ALU.mult)

            def m5(blk):
                i2 = blk % 2
                for t2 in range(2):
                    for hf in range(2):
                        py = ps[6 + hf]
                        for f in range(4):
                            P.mm(py[:, :], hidT[i2][:, f, t2 * 128:(t2 + 1) * 128], wd[i2][:, f, hf * 512:(hf + 1) * 512],
                                 start=(f == 0), stop=(f == 3))
                        P.copy(yb[i2][:, t2, hf * 512:(hf + 1) * 512], py[:, :], eng=('act' if hf == 0 else 'dve'))

            def m6(blk):
                r0 = blk * MB
                P.dma(self.ys_d[r0:r0 + MB, :].rearrange('(t p) d -> p t d', p=128), yb[blk % 2][:], q='act')

            pipeline(NBLK, [m0, m1, m2, m3, m4, m5, m6], reverse=True)
            self.sync_phase()
        with ExitStack() as s2:
            T2 = lambda n, s, dt: self.T(s2, n, s, dt)
            ND = 3
            y0 = [T2('y0_%d' % i, [128, D], BF16) for i in range(ND)]
            y1 = [T2('y1_%d' % i, [128, D], BF16) for i in range(ND)]
            yo = [T2('yo_%d' % i, [128, D], F32) for i in range(ND)]
            x1 = [T2('x1f%d' % i, [128, D], F32) for i in range(ND)]
            gt2 = [T2('gt2_%d' % i, [128, D], F32) for i in range(2)]
            for Tg in range(NTT):
                i2 = Tg % ND
                b = Tg // NT
                if Tg % NT == 0:
                    P.dma(gt2[b % 2][:], self.mod_d[b:b + 1, 5 * D:6 * D].to_broadcast([128, D]))
                for kk, yt in ((0, y0[i2]), (1, y1[i2])):
                    ia = dest[:, kk, Tg:Tg + 1]
                    P.add('pool', (lambda yt=yt, ia=ia: (lambda e: e.indirect_dma_start(
                        out=yt[:, :], out_offset=None, in_=self.ys_d, in_offset=IOA(ap=ia, axis=0))))(),
                        [ia, self.ys_d], [yt[:]], dma=True)
                P.dma(x1[i2][:], self.x1_d[Tg * 128:(Tg + 1) * 128, :], q='sp')
                P.ts(yo[i2][:], y0[i2][:], self.wts[:, Tg, 0:1], None, ALU.mult)
                P.stt(yo[i2][:], y1[i2][:], self.wts[:, Tg, 1:2], yo[i2][:], ALU.mult, ALU.add)
                P.tt(yo[i2][:], yo[i2][:], gt2[b % 2][:], ALU.mult)
                P.tt(yo[i2][:], yo[i2][:], x1[i2][:], ALU.add)
                P.dma(self.out[Tg * 128:(Tg + 1) * 128, :], yo[i2][:], q='act')
            self.sync_phase()


KB.phase4 = _kb_phase4


N_CORES = 8
_CACHE = {}

_WEIGHT_KEYS = ['w_ada', 'b_ada', 'norm1_g', 'norm2_g', 'w_in', 'nsa_q_norm', 'nsa_k_norm', 'cmp_pe_k', 'cmp_w1_k',
                'cmp_w2_k', 'cmp_pe_v', 'cmp_w1_v', 'cmp_w2_v', 'dil_q_norm', 'dil_k_norm', 'w_up_a', 'w_up_b', 'w_out',
                'w_group', 'b_group', 'w_router', 'b_router', 'w_e_gate', 'w_e_up', 'w_e_down']


def kernel(**inputs):
    x = np.asarray(inputs['x'], dtype=np.float32)
    c = np.asarray(inputs['c'], dtype=np.float32)
    pos = np.asarray(inputs['positions'], dtype=np.int32)
    B = x.shape[0]
    nseq = B // N_CORES
    if 'kb' not in _CACHE:
        kb = KB(nseq)
        kb.build()
        _CACHE['kb'] = kb
    kb = _CACHE['kb']
    w = {k: np.ascontiguousarray(np.asarray(inputs[k], dtype=np.float32)) for k in _WEIGHT_KEYS}
    in_maps = []
    for i in range(N_CORES):
        m = dict(w)
        m['x'] = np.ascontiguousarray(x[i * nseq:(i + 1) * nseq].reshape(nseq * S, D))
        m['c'] = np.ascontiguousarray(c[i * nseq:(i + 1) * nseq])
        m['positions'] = np.ascontiguousarray(pos[i * nseq:(i + 1) * nseq])
        for k, v in kb.consts.items():
            m['k_' + k] = v
        in_maps.append(m)
    res = run_bass_kernel_spmd(kb.nc, in_maps, core_ids=list(range(N_CORES)))
    out = np.concatenate([np.asarray(r['out']).reshape(nseq, S, D) for r in res.results], axis=0)
    return out.astype(np.float32)
```

```python
import numpy as np
import concourse.bass as bass
import concourse.mybir as mybir

F32 = mybir.dt.float32
BF16 = mybir.dt.bfloat16
I32 = mybir.dt.int32
U32 = mybir.dt.uint32
ALU = mybir.AluOpType
AF = mybir.ActivationFunctionType
AX = mybir.AxisListType

ENG_ATTR = {'pe': 'tensor', 'act': 'scalar', 'dve': 'vector', 'pool': 'gpsimd', 'sp': 'sync'}
ENGS = ['pe', 'act', 'dve', 'pool', 'sp']
DMA_RING = 6
_ESZ = {}


def esz(dt):
    k = str(dt)
    if k not in _ESZ:
        _ESZ[k] = mybir.dt.size(dt) if hasattr(mybir.dt, 'size') else np.dtype(mybir.dt.np(dt)).itemsize
    return _ESZ[k]


def box_of(ap):
    t = ap.tensor
    name = t.name
    pat = ap.ap
    e = esz(ap.dtype)
    space = str(ap.space)
    if 'DRAM' in space.upper() or 'HBM' in space.upper():
        lo = ap.offset
        hi = lo
        for st, n in pat:
            if st >= 0:
                hi += st * (n - 1)
            else:
                lo += st * (n - 1)
        return (name, 'D', 0, 1, lo * e, (hi + 1) * e)
    pstep, pn = pat[0]
    p0 = ap.start_partition()
    p1 = p0 + ap.partition_size()
    off = ap.offset - p0 * pstep if pstep else ap.offset
    lo = off
    hi = off
    for st, n in pat[1:]:
        if st >= 0:
            hi += st * (n - 1)
        else:
            lo += st * (n - 1)
    sp = 'P' if 'PSUM' in space.upper() else 'S'
    return (name, sp, p0, p1, lo * e, (hi + 1) * e)


class Op:
    __slots__ = ('eng', 'chan', 'pos', 'emit', 'waits', 'snap', 'inc', 'dma')


class Prog:
    def __init__(self, nc):
        self.nc = nc
        self.sems = {}
        self.semcnt = {}
        self.ops = {e: [] for e in ENGS}
        self.chan_ops = {}
        self.chan_base = {}
        self.vc = {e: {} for e in ENGS}
        self.trk = {}
        self.psum_last = {}
        self.dma_n = {e: 0 for e in ENGS}
        self._oldvals = {}
        self.n_ops = 0

    def sem(self, chan):
        if chan not in self.sems:
            nm = 's_' + (chan if isinstance(chan, str) else '%s%d' % chan)
            h = self.nc.alloc_semaphore(name=nm)
            self.sems[chan] = h
            self.semcnt[chan] = 0
        return self.sems[chan]

    def _known(self, eng, chan, pos):
        return self.vc[eng].get(chan, -1) >= pos

    def _learn(self, eng, chan, pos):
        vc = self.vc[eng]
        op = self.chan_ops[chan][pos - self.chan_base.get(chan, 0)] if pos >= self.chan_base.get(chan, 0) else None
        if op is not None and op.snap:
            for c, p in op.snap.items():
                if vc.get(c, -1) < p:
                    vc[c] = p
        if vc.get(chan, -1) < pos:
            vc[chan] = pos

    def _deps_for(self, eng, reads, writes):
        deps = set()
        for ap in reads:
            bx = box_of(ap)
            name, sp = bx[0], bx[1]
            if sp == 'P':
                self._psum_deps(eng, name, deps)
                continue
            t = self.trk.get(name)
            if t is None:
                continue
            for (wb, c, p) in t['w']:
                if wb[2] < bx[3] and bx[2] < wb[3] and wb[4] < bx[5] and bx[4] < wb[5]:
                    deps.add((c, p))
        for ap in writes:
            bx = box_of(ap)
            name, sp = bx[0], bx[1]
            if sp == 'P':
                self._psum_deps(eng, name, deps)
                continue
            t = self.trk.get(name)
            if t is None:
                continue
            for (wb, c, p) in t['w']:
                if wb[2] < bx[3] and bx[2] < wb[3] and wb[4] < bx[5] and bx[4] < wb[5]:
                    deps.add((c, p))
            for (rb, c), p in t['r'].items():
                if rb[2] < bx[3] and bx[2] < rb[3] and rb[4] < bx[5] and bx[4] < rb[5]:
                    deps.add((c, p))
        return deps

    def _psum_deps(self, eng, name, deps):
        last = self.psum_last.get(name)
        if not last:
            return
        for e, cp in last.items():
            if e == eng and eng == 'pe':
                continue
            deps.add(cp)

    def _record_access(self, eng, chan, pos, reads, writes):
        for ap in reads:
            bx = box_of(ap)
            name, sp = bx[0], bx[1]
            if sp == 'P':
                self.psum_last.setdefault(name, {})[eng] = (chan, pos)
                continue
            t = self.trk.setdefault(name, {'w': [], 'r': {}})
            t['r'][(bx, chan)] = pos
        for ap in writes:
            bx = box_of(ap)
            name, sp = bx[0], bx[1]
            if sp == 'P':
                self.psum_last.setdefault(name, {})[eng] = (chan, pos)
                continue
            t = self.trk.setdefault(name, {'w': [], 'r': {}})
            neww = []
            for ent in t['w']:
                wb = ent[0]
                if bx[2] <= wb[2] and wb[3] <= bx[3] and bx[4] <= wb[4] and wb[5] <= bx[5]:
                    continue
                neww.append(ent)
            neww.append((bx, chan, pos))
            t['w'] = neww
            if t['r']:
                t['r'] = {k: v for k, v in t['r'].items()
                          if not (bx[2] <= k[0][2] and k[0][3] <= bx[3] and bx[4] <= k[0][4] and k[0][5] <= bx[5])}

    def add(self, eng, emit, reads=(), writes=(), dma=False, extra_deps=()):
        op = Op()
        op.eng = eng
        op.emit = emit
        op.dma = dma
        op.inc = dma
        deps = self._deps_for(eng, reads, writes)
        deps.update(extra_deps)
        if dma:
            slot = self.dma_n[eng] % DMA_RING
            self.dma_n[eng] += 1
            chan = (eng, slot)
            lst = self.chan_ops.setdefault(chan, [])
            base = self.chan_base.get(chan, 0)
            if lst or base:
                deps.add((chan, base + len(lst) - 1))
        else:
            chan = eng
            lst = self.chan_ops.setdefault(chan, [])
        self.sem(chan)
        pos = self.chan_base.get(chan, 0) + len(lst)
        waits = []
        for (c, p) in sorted(deps, key=lambda cp: (str(cp[0]), cp[1])):
            if c == eng and eng == 'pe':
                continue
            if self._known(eng, c, p):
                continue
            waits.append((c, p))
        best = {}
        for c, p in waits:
            if best.get(c, -1) < p:
                best[c] = p
        op.waits = list(best.items())
        for c, p in op.waits:
            cb = self.chan_base.get(c, 0)
            if p >= cb:
                self.chan_ops[c][p - cb].inc = True
            self._learn(eng, c, p)
        op.snap = dict(self.vc[eng])
        op.chan = chan
        op.pos = pos
        lst.append(op)
        self.ops[eng].append(op)
        self._record_access(eng, chan, pos, reads, writes)
        self.n_ops += 1
        return (chan, pos)

    def barrier(self):
        lasts = []
        for chan, lst in self.chan_ops.items():
            if lst:
                lst[-1].inc = True
                lasts.append((chan, self.chan_base.get(chan, 0) + len(lst) - 1))
        for e in ENGS:
            op = Op()
            op.eng = e
            op.emit = None
            op.dma = False
            op.inc = False
            op.chan = None
            op.pos = -1
            waits = []
            for (c, p) in lasts:
                if c == e and e == 'pe':
                    continue
                if self._known(e, c, p):
                    continue
                waits.append((c, p))
            op.waits = waits
            for c, p in waits:
                self._learn(e, c, p)
            op.snap = None
            self.ops[e].append(op)

    def flush(self):
        nc = self.nc
        semval = {}
        for chan, lst in self.chan_ops.items():
            v = self.semcnt[chan]
            base = self.chan_base.get(chan, 0)
            for i, op in enumerate(lst):
                if op.inc:
                    v += 16 if op.dma else 1
                semval[(chan, base + i)] = v if op.inc else None
            self.semcnt[chan] = v
        old = self._oldvals
        old.update({k: v for k, v in semval.items() if v is not None})
        ops = self.ops
        sems = self.sems

        def run(engname):
            def f(eng):
                for op in ops[engname]:
                    for (c, p) in op.waits:
                        v = old.get((c, p))
                        assert v is not None, (engname, c, p)
                        eng.wait_ge(sems[c], v)
                    if op.emit is None:
                        continue
                    inst = op.emit(eng)
                    if op.inc:
                        inst.then_inc(sems[op.chan], 16 if op.dma else 1)
            return f

        with nc.Block() as block:
            block.tensor(run('pe'))
            block.scalar(run('act'))
            block.vector(run('dve'))
            block.gpsimd(run('pool'))
            block.sync(run('sp'))
        for chan, lst in self.chan_ops.items():
            self.chan_base[chan] = self.chan_base.get(chan, 0) + len(lst)
            self.chan_ops[chan] = []
        self.ops = {e: [] for e in ENGS}

    def dma(self, out, in_, q='sp', **kw):
        return self.add(q, lambda e: e.dma_start(out=out, in_=in_, **kw), [in_], [out], dma=True)

    def mm(self, out, lhsT, rhs, start=True, stop=True, **kw):
        return self.add('pe', lambda e: e.matmul(out, lhsT, rhs, start=start, stop=stop, **kw),
                        [lhsT, rhs], [out])

    def tr(self, out, in_, ident):
        return self.add('pe', lambda e: e.transpose(out, in_, ident), [in_, ident], [out])

    def act(self, out, in_, func, bias=None, scale=None, accum_out=None, eng='act'):
        kw = {}
        rd = [in_]
        wr = [out]
        if bias is not None:
            kw['bias'] = bias
            if not isinstance(bias, (int, float)):
                rd.append(bias)
        if scale is not None:
            kw['scale'] = scale
            if not isinstance(scale, (int, float)):
                rd.append(scale)
        if accum_out is not None:
            kw['accum_out'] = accum_out
            wr.append(accum_out)
        return self.add('act', lambda e: e.activation(out, in_, func, **kw), rd, wr)

    def tt(self, out, in0, in1, op, eng='dve'):
        return self.add(eng, lambda e: e.tensor_tensor(out, in0, in1, op), [in0, in1], [out])

    def ts(self, out, in0, s1, s2, op0, op1=None, eng='dve', accum_out=None):
        rd = [in0]
        if not isinstance(s1, (int, float)) and s1 is not None:
            rd.append(s1)
        if not isinstance(s2, (int, float)) and s2 is not None:
            rd.append(s2)
        kw = {}
        wr = [out]
        if op1 is not None:
            kw['op1'] = op1
        if accum_out is not None:
            kw['accum_out'] = accum_out
            wr.append(accum_out)
        return self.add(eng, lambda e: e.tensor_scalar(out, in0, s1, s2, op0, **kw), rd, wr)

    def stt(self, out, in0, scalar, in1, op0, op1, eng='dve'):
        rd = [in0, in1]
        if not isinstance(scalar, (int, float)):
            rd.append(scalar)
        return self.add(eng, lambda e: e.scalar_tensor_tensor(out, in0, scalar, in1, op0, op1), rd, [out])

    def copy(self, out, in_, eng='dve'):
        if eng == 'act':
            return self.add('act', lambda e: e.copy(out, in_), [in_], [out])
        return self.add(eng, lambda e: e.tensor_copy(out, in_), [in_], [out])

    def reduce(self, out, in_, op, axis=AX.X, eng='dve'):
        return self.add(eng, lambda e: e.tensor_reduce(out, in_, axis, op), [in_], [out])

    def memset(self, ap, val, eng='dve'):
        return self.add(eng, lambda e: e.memset(ap, val), [], [ap])

    def recip(self, out, in_, eng='dve'):
        return self.add(eng, lambda e: e.reciprocal(out, in_), [in_], [out])

    def max8(self, out, in_):
        return self.add('dve', lambda e: e.max(out, in_), [in_], [out])
from concourse.bass_utils import run_bass_kernel_spmd
from contextlib import ExitStack

S = 2048
D = 1024
DH = 64
NT = S // 128
IN_COLS = 4504
EPS = 1e-6
BIG = 30000.0
MAGIC = 12582912.0
TWO_PI = 6.283185307179586
NEXP = 32
FF = 512
MB = 256


def host_consts():
    c = {}
    c['identf'] = np.eye(128, dtype=np.float32)
    inv = (10000.0 ** (-np.arange(0, 64, 2, dtype=np.float32) / 64)).astype(np.float32)
    c['invf'] = np.tile(inv[None, :], (128, 1)).astype(np.float32)
    k = np.arange(128)[:, None]
    q = np.arange(128)[None, :]
    tri = (k <= q).astype(np.float32)
    anti = (k >= q).astype(np.float32)
    c['tri'] = tri
    c['anti'] = anti
    c['trianti'] = np.concatenate([tri, anti], axis=1)
    c['tri4'] = np.tile(tri, (1, 4))
    t = np.arange(S)
    cv = np.zeros((128, S), np.float32)
    cv[:127] = ((np.arange(127) * 16 + 31)[:, None] <= t[None, :])
    c['cmpvalid'] = cv
    c['blkoh'] = (np.arange(32)[:, None] == (t // 64)[None, :]).astype(np.float32)
    cs = np.arange(127) * 16
    ss = np.arange(32) * 64
    ov = np.clip(np.minimum(cs[:, None] + 32, ss[None, :] + 64) - np.maximum(cs[:, None], ss[None, :]), 0, None) / 32.0
    ovp = np.zeros((128, 32), np.float32)
    ovp[:127] = ov
    c['overlap'] = ovp
    b = (t // 64)[:, None]
    s = np.arange(32)[None, :]
    cand = ((s >= 1) & (s <= b - 2)).astype(np.float32)
    forced = (((s == 0) | (s == b) | (s == b - 1)) & (s <= b)).astype(np.float32)
    tm = lambda a: np.ascontiguousarray(a.reshape(NT, 128, 32).transpose(1, 0, 2)).astype(np.float32)
    c['cand'] = tm(cand)
    c['candm1'] = tm(cand - 1.0)
    c['forced'] = tm(forced)
    c['lstrict'] = (k < q).astype(np.float32)
    c['ones'] = np.ones((128, 128), np.float32)
    return c


class KB:
    def __init__(self, NSEQ, dbg=None):
        self.NSEQ = NSEQ
        self.NTOK = NSEQ * S
        self.NTT = NSEQ * NT
        self.NBLK = (self.NTOK * 2) // MB + NEXP
        self.NSLOT = self.NBLK * MB
        self.dbg = dbg or {}
        self.nc = bass.Bass("TRN2", target_bir_lowering=False)
        self.P = Prog(self.nc)
        self.din = {}
        self.consts = host_consts()
        blk = np.arange(self.NBLK, dtype=np.float32)[:, None] * MB
        self.consts['thr'] = np.tile(np.tile(blk, (1, 32)).reshape(1, -1), (128, 1)).astype(np.float32)
        p = np.arange(128, dtype=np.float32)[:, None]
        self.consts['rowoff'] = np.concatenate([np.arange(8)[None, :] * 128 + p, np.arange(4)[None, :] * 128 + p], axis=1).astype(np.float32)

    def dram_in(self, name, shape, dt=F32):
        h = self.nc.dram_tensor(name, list(shape), dt, kind="ExternalInput")
        self.din[name] = h
        return h.ap()

    def declare(self):
        NSEQ = self.NSEQ
        nc = self.nc
        d = self.dram_in
        self.x = d('x', [self.NTOK, D])
        self.c = d('c', [NSEQ, D])
        self.pos = d('positions', [NSEQ, S], I32)
        self.w_ada = d('w_ada', [1, D, 6 * D])
        self.b_ada = d('b_ada', [1, 6 * D])
        self.norm1_g = d('norm1_g', [1, D])
        self.norm2_g = d('norm2_g', [1, D])
        self.w_in = d('w_in', [1, D, IN_COLS])
        self.nsa_q_norm = d('nsa_q_norm', [1, DH])
        self.nsa_k_norm = d('nsa_k_norm', [1, DH])
        self.cmp_pe_k = d('cmp_pe_k', [1, 32, DH])
        self.cmp_w1_k = d('cmp_w1_k', [1, 2048, 256])
        self.cmp_w2_k = d('cmp_w2_k', [1, 256, DH])
        self.cmp_pe_v = d('cmp_pe_v', [1, 32, DH])
        self.cmp_w1_v = d('cmp_w1_v', [1, 2048, 256])
        self.cmp_w2_v = d('cmp_w2_v', [1, 256, DH])
        self.dil_q_norm = d('dil_q_norm', [1, DH])
        self.dil_k_norm = d('dil_k_norm', [1, DH])
        self.w_up_a = d('w_up_a', [1, 512, D])
        self.w_up_b = d('w_up_b', [1, 384, D])
        self.w_out = d('w_out', [1, D, D])
        self.w_group = d('w_group', [1, D, 4])
        self.b_group = d('b_group', [1, 4])
        self.w_router = d('w_router', [1, 4, D, 8])
        self.b_router = d('b_router', [1, 4, 8])
        self.w_e_gate = d('w_e_gate', [1, NEXP, D, FF])
        self.w_e_up = d('w_e_up', [1, NEXP, D, FF])
        self.w_e_down = d('w_e_down', [1, NEXP, FF, D])
        self.cd = {}
        for k, v in self.consts.items():
            self.cd[k] = d('k_' + k, v.shape)
        self.out = nc.dram_tensor('out', [self.NTOK, D], F32, kind="ExternalOutput").ap()
        sc = lambda n, shp, dt: nc.dram_tensor(n, list(shp), dt, kind="Internal").ap()
        self.mod_d = sc('mod_d', [NSEQ, 6 * D], F32)
        self.x1_d = sc('x1_d', [self.NTOK, D], F32)
        self.h2_d = sc('h2_d', [self.NTOK, D], BF16)
        self.xs_d = sc('xs_d', [self.NSLOT, D], BF16)
        self.ys_d = sc('ys_d', [self.NSLOT, D], BF16)
        self.wg_l = nc.dram_tensor('wg_l', [NEXP * 128, 8 * FF], BF16, kind="Internal").ap()
        self.wu_l = nc.dram_tensor('wu_l', [NEXP * 128, 8 * FF], BF16, kind="Internal").ap()
        self.wd_l = nc.dram_tensor('wd_l', [NEXP * 128, 4 * D], BF16, kind="Internal").ap()
        self.conv_list = []
        for e_ in range(NEXP):
            rows = slice(e_ * 128, (e_ + 1) * 128)
            self.conv_list.append((self.wg_l[rows, :].rearrange('p (k f) -> p k f', f=FF),
                                   self.w_e_gate[0, e_].rearrange('(k p) f -> p k f', p=128)))
            self.conv_list.append((self.wu_l[rows, :].rearrange('p (k f) -> p k f', f=FF),
                                   self.w_e_up[0, e_].rearrange('(k p) f -> p k f', p=128)))
            self.conv_list.append((self.wd_l[rows, :].rearrange('p (k f) -> p k f', f=D),
                                   self.w_e_down[0, e_].rearrange('(k p) f -> p k f', p=128)))
        self.conv_pos = 0
        self.w_nsa_d = sc('w_nsa_d', [128, 8 * 1304], BF16)
        self.w_dil_d = sc('w_dil_d', [128, 8 * 1152], BF16)
        self.w_gm_d = sc('w_gm_d', [128, 8 * 2048], BF16)
        self.w_upa_d = sc('w_upa_d', [128, 4 * D], BF16)
        self.w_upb_d = sc('w_upb_d', [128, 3 * D], BF16)
        self.w_o_d = sc('w_o_d', [128, 8 * D], BF16)
        self.dbg_out = {}
        for k, shp in self.dbg.items():
            self.dbg_out[k] = nc.dram_tensor('dbg_' + k, list(shp), F32, kind="ExternalOutput").ap()

    def T(self, st, name, shape, dt):
        self._uid = getattr(self, '_uid', 0) + 1
        return st.enter_context(self.nc.sbuf_tensor('%s_%d' % (name, self._uid), list(shape), dt))

    def drain_pending(self, n=None):
        k = len(self.pending) if n is None else min(n, len(self.pending))
        for _ in range(k):
            self.pending.pop(0)()

    def emit_conv(self, n):
        for _ in range(n):
            if self.conv_pos >= len(self.conv_list):
                return
            dst, src = self.conv_list[self.conv_pos]
            self.conv_pos += 1
            self.P.dma(dst, src, q='pool')

    def dump(self, key, ap):
        if key in self.dbg_out:
            self.P.dma(self.dbg_out[key], ap, q='pool')

    def sync_phase(self, name=None):
        self.P.barrier()
        if name is None:
            import inspect
            fr = inspect.stack()[1]
            name = '%s_%d' % (fr.function.replace('_kb_', ''), fr.lineno)
        with self.nc.named_scope(name):
            self.P.flush()

    def phase0(self, prep=False):
        nc, P, NSEQ = self.nc, self.P, self.NSEQ
        ps = self.ps
        with ExitStack() as st:
            T = lambda n, s, dt: self.T(st, n, s, dt)
            if prep:
                self.prep_weights(st)
            cs = T('cs', [4, D], F32)
            csT = T('csT', [128, 8, 4], F32)
            wa = [T('wa%d' % i, [128, 8, 512], F32) for i in range(2)]
            modrows = T('modrows', [4, 6 * D], F32)
            bada = T('bada', [4, 6 * D], F32)
            g1b = T('g1b', [4, D], F32)
            g2b = T('g2b', [4, D], F32)
            P.dma(cs[0:NSEQ, :], self.c)
            P.dma(bada[0:NSEQ, :], self.b_ada.to_broadcast([NSEQ, 6 * D]))
            P.dma(g1b[0:NSEQ, :], self.norm1_g.to_broadcast([NSEQ, D]))
            P.dma(g2b[0:NSEQ, :], self.norm2_g.to_broadcast([NSEQ, D]))
            P.act(cs[0:NSEQ, :], cs[0:NSEQ, :], AF.Silu)
            for k in range(8):
                P.tr(ps[0][:, k * 4:k * 4 + NSEQ], cs[0:NSEQ, k * 128:(k + 1) * 128], self.identf[0:NSEQ, 0:NSEQ])
            P.copy(csT[:, :, 0:NSEQ], ps[0][:, 0:32].rearrange('p (k b) -> p k b', b=4)[:, :, 0:NSEQ])
            wv = self.w_ada[0].rearrange('(k p) c -> p k c', p=128)
            for cc in range(12):
                w = wa[cc % 2]
                P.dma(w[:], wv[:, :, cc * 512:(cc + 1) * 512], q=('sp' if cc % 2 == 0 else 'act'))
                pb = ps[1 + cc % 2]
                for k in range(8):
                    P.mm(pb[0:NSEQ, :], csT[:, k, 0:NSEQ], w[:, k, :], start=(k == 0), stop=(k == 7))
                P.tt(modrows[0:NSEQ, cc * 512:(cc + 1) * 512], pb[0:NSEQ, :], bada[0:NSEQ, cc * 512:(cc + 1) * 512], ALU.add)
            P.stt(modrows[0:NSEQ, D:2 * D], modrows[0:NSEQ, D:2 * D], 1.0, g1b[0:NSEQ, :], ALU.add, ALU.mult)
            P.stt(modrows[0:NSEQ, 4 * D:5 * D], modrows[0:NSEQ, 4 * D:5 * D], 1.0, g2b[0:NSEQ, :], ALU.add, ALU.mult)
            P.dma(self.mod_d, modrows[0:NSEQ, :])
            for ch in range(16):
                P.tr(ps[3][:, ch * 4:ch * 4 + NSEQ], modrows[0:NSEQ, ch * 128:(ch + 1) * 128], self.identf[0:NSEQ, 0:NSEQ])
            P.copy(self.modT1[:, :, 0:NSEQ], ps[3][:, 0:64].rearrange('p (k b) -> p k b', b=4)[:, :, 0:NSEQ])
            self.dump('modrows', modrows[0:NSEQ, :])
            self.sync_phase()

    def phase1(self, b):
        nc, P = self.nc, self.P
        ps = self.ps
        with ExitStack() as st:
            T = lambda n, s, dt: self.T(st, n, s, dt)
            xt = [T('xt%d' % i, [128, D], F32) for i in range(3)]
            junk = T('p1junk', [128, D], F32)
            xn = [T('xn%d' % i, [128, D], F32) for i in range(2)]
            ss = [T('p1ss%d' % i, [128, 4], F32) for i in range(2)]
            def p0(tt):
                r0 = b * S + tt * 128
                P.dma(xt[tt % 3][:], self.x[r0:r0 + 128, :], q=('sp' if tt % 2 == 0 else 'act'))

            def p1(tt):
                s_ = ss[tt % 2]
                P.act(junk[:], xt[tt % 3][:], AF.Square, accum_out=s_[:, 0:1])
                P.act(s_[:, 1:2], s_[:, 0:1], AF.Ln, scale=1.0 / D, bias=EPS)
                P.act(s_[:, 2:3], s_[:, 1:2], AF.Exp, scale=-0.5)

            npend = -(-len(getattr(self, 'pending', [])) // NT)

            def p2(tt):
                P.ts(xn[tt % 2][:], xt[tt % 3][:], ss[tt % 2][:, 2:3], None, ALU.mult)
                self.drain_pending(npend)

            def p3(tt):
                for k in range(8):
                    pb = ps[(tt % 2) * 2 + k // 4]
                    P.tr(pb[:, (k % 4) * 128:(k % 4 + 1) * 128], xn[tt % 2][:, k * 128:(k + 1) * 128], self.identf[:])

            def p4(tt):
                for k in range(8):
                    pb = ps[(tt % 2) * 2 + k // 4]
                    src = pb[:, (k % 4) * 128:(k % 4 + 1) * 128]
                    dst = self.hT[:, k, tt * 128:(tt + 1) * 128]
                    if k < 4:
                        P.act(dst, src, AF.Identity, scale=self.modT1[:, 8 + k, b:b + 1], bias=self.modT1[:, k, b:b + 1])
                    else:
                        P.ts(dst, src, self.modT1[:, 8 + k, b:b + 1], self.modT1[:, k, b:b + 1], ALU.mult, ALU.add)

            pipeline(NT, [p0, p1, p2, p3, p4], reverse=True)
            self.drain_pending()
            if b == 0:
                for k in range(8):
                    if ('hT%d' % k) in self.dbg_out:
                        self.dump('hT%d' % k, self.hT[:, k, :])
            self.sync_phase()


PI_SAFE = 3.1415925


def _kb_rope_tables(self, st, posf, n, cos_out, sin_out, tag):
    P = self.P
    T = lambda nm, s, dt: self.T(st, tag + nm, s, dt)
    ang = T('ang', [128, n, 32], F32)
    a2 = T('a2', [128, n, 32], F32)
    kk = T('kk', [128, n, 32], F32)
    P.tt(ang[:], self.invf[:, :].unsqueeze(1).to_broadcast([128, n, 32]),
         posf.unsqueeze(2).to_broadcast([128, n, 32]), ALU.mult)
    for off, outp in ((0.0, sin_out), (np.pi / 2, cos_out)):
        if off == 0.0:
            a = ang
        else:
            P.ts(a2[:], ang[:], float(off), None, ALU.add)
            a = a2
        P.ts(kk[:], a[:], 1.0 / TWO_PI, MAGIC, ALU.mult, ALU.add)
        P.ts(kk[:], kk[:], MAGIC, None, ALU.subtract)
        P.stt(kk[:], kk[:], -TWO_PI, a[:], ALU.mult, ALU.add)
        P.ts(kk[:], kk[:], PI_SAFE, -PI_SAFE, ALU.min, ALU.max)
        P.act(outp, kk[:], AF.Sin)


KB.rope_tables = _kb_rope_tables


def _kb_setup_nsa_consts(self):
    P, top = self.P, self.top
    T = lambda n, s, dt: self.T(top, n, s, dt)
    self.kslc = T('kslc', [96, S], BF16)
    P.dma(self.kslc[64:96, :], self.cd['blkoh'], q='pool')
    self.v2 = T('v2', [128, NT, 2, 65], BF16)
    P.memset(self.v2[:].rearrange('p a b c -> p (a b c)'), 1.0)
    self.vcaug = T('vcaug', [128, 97], BF16)
    P.memset(self.vcaug[:, 64:65], 1.0)
    P.dma(self.vcaug[:, 65:97], self.cd['overlap'], q='pool')
    self.w1kv = T('w1kv', [128, 32, 256], BF16)
    P.dma(self.w1kv[0:64], self.cmp_w1_k[0].rearrange('(l d) h -> d l h', d=64), q='pool')
    P.dma(self.w1kv[64:128], self.cmp_w1_v[0].rearrange('(l d) h -> d l h', d=64), q='pool')
    self.w2kv = T('w2kv', [128, 2, 2, 64], BF16)
    P.dma(self.w2kv[:, 0], self.cmp_w2_k[0].rearrange('(c p) d -> p c d', p=128), q='pool')
    P.dma(self.w2kv[:, 1], self.cmp_w2_v[0].rearrange('(c p) d -> p c d', p=128), q='pool')
    self.ckv = T('ckv', [128, 2, 2], F32)
    self.gfull = T('gfull', [128, 6, 64], F32)
    self.gk = T('gk', [128, 64], F32)
    self.gqb = T('gqb', [128, 64], F32)
    self.gkb = T('gkb', [128, 64], F32)
    for hh in range(4):
        P.dma(self.gfull[:, hh, :], self.nsa_q_norm.to_broadcast([128, 64]))
    for hh in range(4, 6):
        P.dma(self.gfull[:, hh, :], self.nsa_k_norm.to_broadcast([128, 64]))
    P.ts(self.gfull[:, 0:4, :], self.gfull[:, 0:4, :], 0.125, None, ALU.mult)
    P.dma(self.gk[:], self.nsa_k_norm.to_broadcast([128, 64]))
    P.dma(self.gqb[:], self.dil_q_norm.to_broadcast([128, 64]))
    P.ts(self.gqb[:], self.gqb[:], 0.125, None, ALU.mult)
    P.dma(self.gkb[:], self.dil_k_norm.to_broadcast([128, 64]))
    with ExitStack() as st:
        T2 = lambda n, s, dt: self.T(st, n, s, dt)
        pekv = T2('pekv', [32, 128], F32)
        peT = T2('peT', [128, 32], BF16)
        P.dma(pekv[:, 0:64], self.cmp_pe_k[0])
        P.dma(pekv[:, 64:128], self.cmp_pe_v[0])
        P.tr(self.ps[0][:, 0:32], pekv[:, :], self.identf[0:32, 0:32])
        P.copy(peT[:], self.ps[0][:, 0:32])
        for kv in range(2):
            base = 64 * kv
            for hc in range(2):
                for l in range(32):
                    P.mm(self.ps[1 + kv][:, hc:hc + 1], self.w1kv[base:base + 64, l, hc * 128:(hc + 1) * 128],
                         peT[base:base + 64, l:l + 1], start=(hc == 0 and l == 0), stop=(l == 31), skip_group_check=True)
            P.copy(self.ckv[:, kv, :], self.ps[1 + kv][:, 0:2])
        self.sync_phase()


KB.setup_nsa_consts = _kb_setup_nsa_consts


def _kb_seq_prologue(self, st, b):
    P = self.P
    T = lambda n, s, dt: self.T(st, n, s, dt)
    self.cosT = T('cosT', [128, NT, 32], F32)
    self.sinT = T('sinT', [128, NT, 32], F32)
    self.cosC = T('cosC', [128, 1, 32], F32)
    self.sinC = T('sinC', [128, 1, 32], F32)
    with ExitStack() as s2:
        T2 = lambda n, s, dt: self.T(s2, n, s, dt)
        posi = T2('posi', [128, 2], I32)
        posi16 = T2('posi16', [16, 128], I32)
        posf16 = T2('posf16', [16, 128], F32)
        posf = T2('posf', [128, NT + 1], F32)
        P.memset(posi[:], 0)
        P.dma(posi16[:], self.pos[b].rearrange('(t p) -> t p', p=128))
        P.copy(posf16[:], posi16[:])
        P.tr(self.ps[0][:, 0:NT], posf16[:], self.identf[0:16, 0:16])
        P.copy(posf[:, 0:NT], self.ps[0][:, 0:NT])
        P.dma(posi[0:127, 0:1], self.pos[b, 31:31 + 16 * 126 + 1:16].unsqueeze(1), allow_slow_non_contiguous=True)
        P.copy(posf[:, NT:NT + 1], posi[:, 0:1])
        self.rope_tables(s2, posf[:, 0:NT], NT, self.cosT[:], self.sinT[:], 'rt')
        self.rope_tables(s2, posf[:, NT:NT + 1], 1, self.cosC[:], self.sinC[:], 'rc')
        self.sync_phase()


KB.seq_prologue = _kb_seq_prologue


class BankRound:
    def __init__(self):
        self.started = {}

    def reset(self, bank):
        self.started[bank.name] = False

    def start(self, bank):
        s = not self.started.get(bank.name, False)
        self.started[bank.name] = True
        return s


def pipeline(n, stages, delays=None, reverse=False):
    if delays is None:
        delays = list(range(len(stages)))
    order = list(range(len(stages)))
    if reverse:
        order = order[::-1]
    for step in range(n + max(delays)):
        for j in order:
            i = step - delays[j]
            if 0 <= i < n:
                stages[j](i)


def _kb_phase2_nsa(self, b, st_seq):
    nc, P, ps = self.nc, self.P, self.ps
    BR = self.br
    wv = self.w_in[0].rearrange('(k p) c -> p k c', p=128)
    with ExitStack() as st:
        T = lambda n, s, dt: self.T(st, n, s, dt)
        w_nsa = T('w_nsa', [128, 8, 1304], BF16)
        P.dma(w_nsa[:].rearrange('p k c -> p (k c)'), self.w_nsa_d)
        gates = T('gates', [128, NT, 24], F32)
        for g in range(2):
            with ExitStack() as sg:
                self.nsa_group(b, g, sg, w_nsa, gates)
                self.sync_phase()


def _kb_nsa_group(self, b, g, st, w_nsa, gates):
    nc, P, ps = self.nc, self.P, self.ps
    BR = self.br
    T = lambda n, s, dt: self.T(st, n, s, dt)
    qaug = T('qaug', [96, 4, S], BF16)
    kwin = T('kwin', [64, S], BF16)
    kvcT = T('kvcT', [128, S], BF16)
    kcT = T('kcT', [64, 128], BF16)
    kslc, v2, vcaug = self.kslc, self.v2, self.vcaug
    with ExitStack() as s2:
        T2 = lambda n, s, dt: self.T(s2, n, s, dt)
        NP = NT // 2
        sq = [T2('sq%d' % i, [128, 2, 6, 64], F32) for i in range(2)]
        rc = [T2('rc%d' % i, [128, 2, 6, 64], F32) for i in range(3)]
        rn = [T2('rn%d' % i, [128, 2, 6, 64], F32) for i in range(1)] * 2
        tmp = [T2('rtmp%d' % i, [128, 4, 2, 6, 32], F32) for i in range(1)] * 2
        rr = [T2('rr%d' % i, [128, 2, 6, 64], BF16) for i in range(2)]
        st6 = [T2('st6%d' % i, [128, 3, 12], F32) for i in range(2)]
        for tc in range(4):
            pc = ps[6 + tc % 2]
            for k in range(8):
                P.mm(pc[:, :], w_nsa[:, k, 1024 + 128 * g:1024 + 128 * g + 128], self.hT[:, k, tc * 512:(tc + 1) * 512],
                     start=(k == 0), stop=(k == 7))
            P.copy(kvcT[:, tc * 512:(tc + 1) * 512], pc[:, :], eng='act')
        tokf = lambda tt: slice(tt * 128, (tt + 1) * 128)

        def f0(i):
            for u in range(2):
                tt = 2 * i + u
                pa = ps[(i % 2) * 2 + u]
                for k in range(8):
                    P.mm(pa[:, :], self.hT[:, k, tokf(tt)], w_nsa[:, k, g * 512:(g + 1) * 512], start=(k == 0), stop=(k == 7))
                if g == 0:
                    for k in range(8):
                        P.mm(ps[6][:, u * 24:(u + 1) * 24], self.hT[:, k, tokf(tt)], w_nsa[:, k, 1280:1304],
                             start=(k == 0 and u == 0), stop=(k == 7), skip_group_check=True)

        def f1(i):
            for u in range(2):
                tt = 2 * i + u
                pa = ps[(i % 2) * 2 + u]
                R = pa[:, 0:384].rearrange('p (h d) -> p h d', d=64)
                P.act(sq[i % 2][:, u], R, AF.Square)
                P.copy(rc[i % 3][:, u], R, eng='act')
                P.copy(v2[:, tt, :, 0:64], pa[:, 384:512].rearrange('p (a d) -> p a d', d=64), eng='act')
            if g == 0:
                P.copy(gates[:, 2 * i:2 * i + 2, :], ps[6][:, 0:48].rearrange('p (u c) -> p u c', c=24))

        def f2(i):
            P.reduce(st6[i % 2][:, 0, :], sq[i % 2][:].rearrange('p u h d -> p (u h) d'), ALU.add)

        def f3(i):
            P.act(st6[i % 2][:, 1, :], st6[i % 2][:, 0, :], AF.Ln, scale=1.0 / DH, bias=EPS)
            P.act(st6[i % 2][:, 2, :], st6[i % 2][:, 1, :], AF.Exp, scale=-0.5)

        def f4(i):
            i2 = i % 2
            rnv = rn[i2][:].rearrange('p u h d -> p (u h) d')
            P.tt(rnv, rc[i % 3][:].rearrange('p u h d -> p (u h) d'),
                 st6[i2][:, 2, :].unsqueeze(2).to_broadcast([128, 12, 64]), ALU.mult)
            P.tt(rn[i2][:], rn[i2][:], self.gfull[:].unsqueeze(1).to_broadcast([128, 2, 6, 64]), ALU.mult)
            cosb = self.cosT[:, 2 * i:2 * i + 2, :].unsqueeze(2).to_broadcast([128, 2, 6, 32])
            sinb = self.sinT[:, 2 * i:2 * i + 2, :].unsqueeze(2).to_broadcast([128, 2, 6, 32])
            x1 = rn[i2][:, :, :, 0:32]
            x2 = rn[i2][:, :, :, 32:64]
            tm = tmp[i2]
            P.tt(tm[:, 0], x1, cosb, ALU.mult)
            P.tt(tm[:, 1], x2, sinb, ALU.mult)
            P.tt(tm[:, 2], x1, sinb, ALU.mult, eng='pool')
            P.tt(tm[:, 3], x2, cosb, ALU.mult, eng='pool')
            P.tt(rr[i2][:, :, :, 0:32], tm[:, 0], tm[:, 1], ALU.subtract)
            P.tt(rr[i2][:, :, :, 32:64], tm[:, 2], tm[:, 3], ALU.add, eng='pool')

        def f5(i):
            for u in range(2):
                pt_ = ps[4 + u].bitcast(BF16)
                for hh in range(6):
                    P.tr(pt_[0:64, hh * 128:(hh + 1) * 128], rr[i % 2][:, u, hh, :], self.identb[:])

        def f6(i):
            for u in range(2):
                tt = 2 * i + u
                pt_ = ps[4 + u].bitcast(BF16)
                tok = tokf(tt)
                P.copy(qaug[0:64, :, tok], pt_[0:64, 0:512].rearrange('p (h t) -> p h t', t=128), eng='act')
                P.copy(kslc[0:64, tok], pt_[0:64, 512:640], eng='act')
                P.copy(kwin[0:64, tok], pt_[0:64, 640:768], eng='act')

        pipeline(NP, [f0, f1, f2, f3, f4, f5, f6], reverse=True)
        if g == 0:
            P.act(gates[:].rearrange('p a b -> p (a b)'), gates[:].rearrange('p a b -> p (a b)'), AF.Sigmoid)
        hid = T2('hid', [128, 2, 2, 128], BF16)
        kc4 = T2('kc4', [128, 8, 64], F32)
        kst = T2('kst', [128, 4], F32)
        kcr = T2('kcr', [128, 64], BF16)
        ktm = T2('ktm', [128, 4, 32], F32)
        for kv in range(2):
            base = 64 * kv
            pz = ps[5 + kv]
            BR.reset(pz)
            for hc in range(2):
                for l in range(32):
                    P.mm(pz[:, hc * 128:hc * 128 + 127], self.w1kv[base:base + 64, l, hc * 128:(hc + 1) * 128],
                         kvcT[base:base + 64, l:l + 16 * 126 + 1:16], start=BR.start(pz), stop=(l == 31),
                         skip_group_check=True)
            for hc in range(2):
                P.act(hid[:, kv, hc, 0:127], pz[:, hc * 128:hc * 128 + 127], AF.Silu, bias=self.ckv[:, kv, hc:hc + 1])
        p2 = ps[7]
        BR.reset(p2)
        for kv in range(2):
            for hc in range(2):
                P.mm(p2[0:127, kv * 64:(kv + 1) * 64], hid[:, kv, hc, 0:127], self.w2kv[:, kv, hc, :],
                     start=BR.start(p2), stop=(hc == 1), skip_group_check=True)
        P.copy(vcaug[0:127, 0:64], p2[0:127, 64:128], eng='act')
        P.act(kc4[0:127, 0, :], p2[0:127, 0:64], AF.Square)
        P.reduce(kst[0:127, 0:1], kc4[0:127, 0, :], ALU.add)
        P.act(kst[0:127, 1:2], kst[0:127, 0:1], AF.Ln, scale=1.0 / DH, bias=EPS)
        P.act(kst[0:127, 2:3], kst[0:127, 1:2], AF.Exp, scale=-0.5)
        P.stt(kc4[0:127, 1, :], p2[0:127, 0:64], kst[0:127, 2:3], self.gk[0:127, :], ALU.mult, ALU.mult)
        x1 = kc4[0:127, 1, 0:32]
        x2 = kc4[0:127, 1, 32:64]
        cC = self.cosC[0:127, 0, :]
        sC = self.sinC[0:127, 0, :]
        P.tt(ktm[0:127, 0], x1, cC, ALU.mult)
        P.tt(ktm[0:127, 1], x2, sC, ALU.mult)
        P.tt(ktm[0:127, 2], x1, sC, ALU.mult)
        P.tt(ktm[0:127, 3], x2, cC, ALU.mult)
        P.tt(kcr[0:127, 0:32], ktm[0:127, 0], ktm[0:127, 1], ALU.subtract)
        P.tt(kcr[0:127, 32:64], ktm[0:127, 2], ktm[0:127, 3], ALU.add)
        pk = ps[3].bitcast(BF16)
        P.tr(pk[0:64, 0:127], kcr[0:127, :], self.identb[0:127, 0:127])
        P.copy(kcT[:, 0:127], pk[0:64, 0:127])
        if b == 0:
            self.dump('qaug%d' % g, qaug[0:64].rearrange('p h s -> p (h s)'))
            self.dump('kslc%d' % g, kslc[0:64, :])
            self.dump('kwin%d' % g, kwin[:, :])
            self.dump('kcT%d' % g, kcT[:, :])
            self.dump('vc%d' % g, vcaug[:, 0:64])
        self.sync_phase()
    self.nsa_attention(b, g, st, qaug, kwin, kcT, gates)


KB.phase2_nsa = _kb_phase2_nsa
KB.nsa_group = _kb_nsa_group


def _kb_nsa_attention(self, b, g, st, qaug, kwin, kcT, gates):
    nc, P, ps = self.nc, self.P, self.ps
    BR = self.br
    self.emit_conv(-(-len(self.conv_list) // (2 * self.NSEQ)))
    kslc, v2, vcaug = self.kslc, self.v2, self.vcaug
    with ExitStack() as s3:
        T = lambda n, s, dt: self.T(s3, n, s, dt)
        ptile = [T('ptile%d' % i, [128, 512], BF16) for i in range(3)]
        oacc = T('oacc', [128, NT, 4, 64], F32)
        impacc = T('impacc', [128, NT, 32], F32)
        rz = [T('rz%d' % i, [128, 2, 4], F32) for i in range(3)]
        otmp = [T('otmp%d' % i, [128, 4, 64], F32) for i in range(2)]
        itmp = [T('itmp%d' % i, [128, 4, 32], F32) for i in range(2)]
        scw = [T('scw%d' % i, [128, 4, 32], F32) for i in range(2)]
        slw = [T('slw%d' % i, [128, 4, 32], F32) for i in range(2)]
        m8 = [T('m8%d' % i, [128, 4, 8], F32) for i in range(2)]
        biasb = [T('biasb%d' % i, [128, 4, 32], BF16) for i in range(2)]
        ob = [T('ob%d' % i, [128, 256], BF16) for i in range(2)]
        sbank = [ps[0], ps[1], ps[2]]
        pvbank = [ps[3], ps[4]]
        misc = ps[5]
        otb = [ps[6], ps[7]]
        cnt = {'s': 0, 'pv': 0, 'fin': 0}

        def finalize(pvb, hl, br, qc, ncol, first, want_imp, first_imp):
            h = 4 * g + hl
            k_ = cnt['fin']
            cnt['fin'] += 1
            r = rz[k_ % 3]
            pv3 = pvb[:, 0:4 * ncol].rearrange('p (q c) -> p q c', c=ncol)
            P.ts(r[:, 0, :], pv3[:, :, 64], 1e-30, None, ALU.max)
            P.recip(r[:, 0, :], r[:, 0, :])
            P.tt(r[:, 1, :], r[:, 0, :], gates[:, qc * 4:(qc + 1) * 4, br * 8 + h], ALU.mult)
            tgt = oacc[:, qc * 4:(qc + 1) * 4, hl, :]
            sb_ = r[:, 1, :].unsqueeze(2).to_broadcast([128, 4, 64])
            if first:
                P.tt(tgt, pv3[:, :, 0:64], sb_, ALU.mult)
            else:
                ot = otmp[k_ % 2]
                P.tt(ot[:], pv3[:, :, 0:64], sb_, ALU.mult)
                P.tt(tgt, tgt, ot[:], ALU.add)
            if want_imp:
                itg = impacc[:, qc * 4:(qc + 1) * 4, :]
                rb_ = r[:, 0, :].unsqueeze(2).to_broadcast([128, 4, 32])
                if first_imp:
                    P.tt(itg, pv3[:, :, 65:97], rb_, ALU.mult)
                else:
                    it = itmp[k_ % 2]
                    P.tt(it[:], pv3[:, :, 65:97], rb_, ALU.mult)
                    P.tt(itg, itg, it[:], ALU.add)

        def selection(qc):
            sc, sl, m_, bb = scw[qc % 2], slw[qc % 2], m8[qc % 2], biasb[qc % 2]
            tq = slice(qc * 4, (qc + 1) * 4)
            P.tt(sc[:], impacc[:, tq, :], self.cand[:, tq, :], ALU.mult)
            P.tt(sc[:], sc[:], self.candm1[:, tq, :], ALU.add)
            for qt in range(4):
                P.max8(m_[:, qt, :], sc[:, qt, :])
            P.tt(sl[:], sc[:], m_[:, :, 4].unsqueeze(2).to_broadcast([128, 4, 32]), ALU.is_ge)
            P.tt(sl[:], sl[:], self.forced[:, tq, :], ALU.max)
            P.ts(bb[:], sl[:], 1.0, BIG, ALU.subtract, ALU.mult)
            mb = misc.bitcast(BF16)
            for qt in range(4):
                P.tr(mb[0:32, qt * 128:(qt + 1) * 128], bb[:, qt, :], self.identb[:])
            P.copy(qaug[64:96, :, qc * 512:(qc + 1) * 512],
                   mb[0:32, 0:512].unsqueeze(1).to_broadcast([32, 4, 512]), eng='act')
            if b == 0:
                for qt in range(4):
                    self.dump('sel%d_%d' % (g, qc * 4 + qt), sl[:, qt, :])

        astate = {}

        def c0(i):
            qc, hl = i // 4, i % 4
            k_ = cnt['s']
            cnt['s'] += 1
            astate[i] = (sbank[k_ % 3], ptile[k_ % 3])
            P.mm(astate[i][0][0:127, :], kcT[0:64, 0:127], qaug[0:64, hl, qc * 512:(qc + 1) * 512], start=True, stop=True)

        def c1(i):
            sb, pt = astate[i]
            P.act(pt[0:127, :], sb[0:127, :], AF.Exp)

        def c2(i):
            qc = i // 4
            sb, pt = astate[i]
            P.tt(pt[0:127, :], pt[0:127, :], self.cmpvalid[0:127, qc * 512:(qc + 1) * 512], ALU.mult)

        def c3(i):
            sb, pt = astate[i]
            pvb = pvbank[cnt['pv'] % 2]
            cnt['pv'] += 1
            BR.reset(pvb)
            for qt in range(4):
                P.mm(pvb[:, qt * 97:(qt + 1) * 97], pt[0:127, qt * 128:(qt + 1) * 128], vcaug[0:127, :],
                     start=BR.start(pvb), stop=True, skip_group_check=True)
            astate[i] = pvb

        def c4(i):
            qc, hl = i // 4, i % 4
            finalize(astate.pop(i), hl, 0, qc, 97, True, True, hl == 0)
            if hl == 3:
                selection(qc)

        pipeline(16, [c0, c1, c2, c3, c4])

        steps = []
        for qc in range(4):
            for hl in range(4):
                kbs = list(range(max(0, 4 * qc - 4), 4 * qc + 4))
                for kb in kbs:
                    if kb < 4 * qc:
                        j = kb - (4 * qc - 4)
                        c0_, c1_, mask = 0, 128 * (j + 1), ('anti', 128 * j)
                    else:
                        j = kb - 4 * qc
                        c0_, c1_, mask = 128 * j, 512, ('tri', 128 * j)
                    steps.append(dict(br=2, hl=hl, kb=kb, c0=c0_, c1=c1_, mask=mask, qc=qc, firsth=(kb == kbs[0]), last=(kb == kbs[-1])))
            for hl in range(4):
                kbs = list(range(0, 4 * qc + 4))
                for kb in kbs:
                    if kb < 4 * qc:
                        c0_, c1_, mask = 0, 512, None
                    else:
                        j = kb - 4 * qc
                        c0_, c1_, mask = 128 * j, 512, ('tri', 128 * j)
                    steps.append(dict(br=1, hl=hl, kb=kb, c0=c0_, c1=c1_, mask=mask, qc=qc, firsth=(kb == kbs[0]), last=(kb == kbs[-1])))
        n = len(steps)
        state = {}

        def qk(i):
            s_ = steps[i]
            k_ = cnt['s']
            cnt['s'] += 1
            sb = sbank[k_ % 3]
            pt = ptile[k_ % 3]
            hl, kb, c0_, c1_, br = s_['hl'], s_['kb'], s_['c0'], s_['c1'], s_['br']
            q0 = s_['qc'] * 512
            if br == 1:
                lhsT = kslc[0:96, kb * 128:(kb + 1) * 128]
                rhs = qaug[0:96, hl, q0 + c0_:q0 + c1_]
            else:
                lhsT = kwin[0:64, kb * 128:(kb + 1) * 128]
                rhs = qaug[0:64, hl, q0 + c0_:q0 + c1_]
            P.mm(sb[:, c0_:c1_], lhsT, rhs, start=True, stop=True)
            P.act(pt[:, c0_:c1_], sb[:, c0_:c1_], AF.Exp)
            if s_['mask'] is not None:
                kind, col = s_['mask']
                mk = self.trib if kind == 'tri' else self.antib
                P.tt(pt[:, col:col + 128], pt[:, col:col + 128], mk[:], ALU.mult)
            state[i] = pt

        def pv(i):
            s_ = steps[i]
            pt = state.pop(i)
            hl, kb, c0_, c1_, br = s_['hl'], s_['kb'], s_['c0'], s_['c1'], s_['br']
            if s_['firsth']:
                state['pvb'] = pvbank[cnt['pv'] % 2]
                cnt['pv'] += 1
                BR.reset(state['pvb'])
            pvb = state['pvb']
            for qt in range(c0_ // 128, c1_ // 128):
                P.mm(pvb[:, qt * 65:(qt + 1) * 65], pt[:, qt * 128:(qt + 1) * 128], v2[:, kb, br - 1, :],
                     start=BR.start(pvb), stop=True, skip_group_check=True)
            if s_['last']:
                finalize(pvb, hl, br, s_['qc'], 65, False, False, False)

        AHEAD = 2
        for i in range(min(AHEAD, n)):
            qk(i)
        for i in range(n):
            if i + AHEAD < n:
                qk(i + AHEAD)
            pv(i)

        def o0(tg):
            P.copy(ob[tg % 2][:], oacc[:, tg].rearrange('p h d -> p (h d)'), eng='act')

        def o1(tg):
            pb = otb[tg % 2].bitcast(BF16)
            for pr in range(2):
                P.tr(pb[:, pr * 128:(pr + 1) * 128], ob[tg % 2][:, pr * 128:(pr + 1) * 128], self.identb[:])

        def o2(tg):
            pb = otb[tg % 2].bitcast(BF16)
            P.copy(self.o_aT[:, 2 * g:2 * g + 2, tg * 128:(tg + 1) * 128],
                   pb[:, 0:256].rearrange('p (c t) -> p c t', t=128), eng='dve')

        pipeline(NT, [o0, o1, o2])


KB.nsa_attention = _kb_nsa_attention


def _kb_prep_weights(self, st):
    P = self.P
    wv = self.w_in[0].rearrange('(k p) c -> p k c', p=128)
    T = lambda n, s, dt: self.T(st, n, s, dt)
    stg = [T('pw_stage%d' % i, [128, 8 * 1304], BF16) for i in range(2)]
    w_nsa = stg[0][:, 0:8 * 1304].rearrange('p (k c) -> p k c', c=1304)
    for g in range(2):
        o = g * 512
        for (dst, src, n) in ((0, 256 * g, 256), (256, 768 + 64 * g, 64), (320, 1024 + 64 * g, 64),
                              (384, 896 + 64 * g, 64), (448, 1152 + 64 * g, 64)):
            P.dma(w_nsa[:, :, o + dst:o + dst + n], wv[:, :, src:src + n], q='pool')
        P.dma(w_nsa[:, :, 1024 + 128 * g:1024 + 128 * g + 64], wv[:, :, 512 + 64 * g:512 + 64 * g + 64], q='pool')
        P.dma(w_nsa[:, :, 1024 + 128 * g + 64:1024 + 128 * g + 128], wv[:, :, 640 + 64 * g:640 + 64 * g + 64], q='pool')
    P.dma(w_nsa[:, :, 1280:1304], wv[:, :, 1280:1304], q='pool')
    P.dma(self.w_nsa_d, stg[0][:, 0:8 * 1304])
    w_dil = stg[1][:, 0:8 * 1152].rearrange('p (k c) -> p k c', c=1152)
    for gi in range(3):
        for pi, base in enumerate((1304, 1688, 2072)):
            P.dma(w_dil[:, :, gi * 384 + pi * 128:gi * 384 + (pi + 1) * 128],
                  wv[:, :, base + 128 * gi:base + 128 * gi + 128], q='pool')
    P.dma(self.w_dil_d, stg[1][:, 0:8 * 1152])
    gm_d = self.w_gm_d.rearrange('p (k c) -> p k c', c=2048)
    for hf in range(2):
        buf = stg[hf][:, 0:8 * 1024].rearrange('p (k c) -> p k c', c=1024)
        for q2 in range(2):
            c0 = 2456 + hf * 1024 + q2 * 512
            P.dma(buf[:, :, q2 * 512:(q2 + 1) * 512], wv[:, :, c0:c0 + 512], q='pool')
        P.dma(gm_d[:, :, hf * 1024:(hf + 1) * 1024], buf)
    bufa = stg[0][:, 0:4 * D].rearrange('p (k c) -> p k c', c=D)
    bufb = stg[0][:, 4 * D:7 * D].rearrange('p (k c) -> p k c', c=D)
    for c in range(4):
        P.dma(bufa[:, c, :], self.w_up_a[0, c * 128:(c + 1) * 128, :], q='pool')
    for c in range(3):
        P.dma(bufb[:, c, :], self.w_up_b[0, c * 128:(c + 1) * 128, :], q='pool')
    P.dma(self.w_upa_d, stg[0][:, 0:4 * D])
    P.dma(self.w_upb_d, stg[0][:, 4 * D:7 * D])
    bufo = stg[1][:, 0:8 * D].rearrange('p (k c) -> p k c', c=D)
    for k in range(8):
        P.dma(bufo[:, k, :], self.w_out[0, k * 128:(k + 1) * 128, :], q='pool')
    P.dma(self.w_o_d, stg[1][:, 0:8 * D])


KB.prep_weights = _kb_prep_weights


def _kb_build(self, upto=99):
    nc, P = self.nc, self.P
    self.declare()
    self.br = BankRound()
    with ExitStack() as top:
        self.top = top
        T = lambda n, s, dt: self.T(top, n, s, dt)
        self.ps = [top.enter_context(nc.psum_tensor("ps%d" % i, [128, 512], F32)) for i in range(8)]
        self.identf = T('identf', [128, 128], F32)
        self.identb = T('identb', [128, 128], BF16)
        self.invf = T('invf', [128, 32], F32)
        self.trib = T('trib', [128, 128], BF16)
        self.antib = T('antib', [128, 128], BF16)
        self.triantib = T('triantib', [128, 256], BF16)
        self.cmpvalid = T('cmpvalid', [128, S], BF16)
        self.cand = T('cand', [128, NT, 32], F32)
        self.candm1 = T('candm1', [128, NT, 32], F32)
        self.forced = T('forced', [128, NT, 32], F32)
        self.lstrict = T('lstrict', [128, 128], BF16)
        self.onesb = T('onesb', [128, 128], BF16)
        P.dma(self.identf[:], self.cd['identf'])
        P.dma(self.identb[:], self.cd['identf'], q='pool')
        P.dma(self.invf[:], self.cd['invf'])
        P.dma(self.trib[:], self.cd['tri'], q='pool')
        P.dma(self.antib[:], self.cd['anti'], q='pool')
        P.dma(self.triantib[:], self.cd['trianti'], q='pool')
        P.dma(self.cmpvalid[:], self.cd['cmpvalid'], q='pool')
        P.dma(self.cand[:], self.cd['cand'])
        P.dma(self.candm1[:], self.cd['candm1'])
        P.dma(self.forced[:], self.cd['forced'])
        P.dma(self.lstrict[:], self.cd['lstrict'], q='pool')
        P.dma(self.onesb[:], self.cd['ones'], q='pool')
        self.modT1 = T('modT1', [128, 16, 4], F32)
        if upto >= 4:
            self.setup_moe_consts()
        self.phase0(prep=(upto >= 2))
        with ExitStack() as mix:
            self.top = mix
            Tm = lambda n, s, dt: self.T(mix, n, s, dt)
            self.hT = Tm('hT', [128, 8, S], BF16)
            self.o_aT = Tm('o_aT', [128, 4, S], BF16)
            self.o_bT = Tm('o_bT', [128, 3, S], BF16)
            if upto >= 2:
                self.setup_nsa_consts()
            for b in range(self.NSEQ):
                if upto >= 1:
                    self.phase1(b)
                if upto >= 2:
                    with ExitStack() as st_seq:
                        self.seq_prologue(st_seq, b)
                        self.phase2_nsa(b, st_seq)
                        if b == 0:
                            for c in range(4):
                                self.dump('oaT%d' % c, self.o_aT[:, c, :])
                        if upto >= 3:
                            self.phase2_dil(b, st_seq)
                            if b == 0:
                                for c in range(3):
                                    self.dump('obT%d' % c, self.o_bT[:, c, :])
                        if upto >= 4:
                            self.phase3(b, st_seq)
                        self.sync_phase()
            self.sync_phase()
        self.top = top
        if upto >= 5:
            self.phase4()
        P.barrier()
        P.flush()
    return nc


KB.build = _kb_build


DIL_D = (1, 4, 16)


def _kb_phase2_dil(self, b, st_seq):
    nc, P, ps = self.nc, self.P, self.ps
    BR = self.br
    wv = self.w_in[0].rearrange('(k p) c -> p k c', p=128)
    with ExitStack() as st:
        T = lambda n, s, dt: self.T(st, n, s, dt)
        w_dil = T('w_dil', [128, 8, 1152], BF16)
        P.dma(w_dil[:].rearrange('p k c -> p (k c)'), self.w_dil_d, q='act')
        us = T('us', [128, 3, S], BF16)
        ztot = T('ztot', [128, S], F32)
        gfb = T('gfb', [128, 4, 64], F32)
        for hh in range(2):
            P.copy(gfb[:, hh, :], self.gqb[:])
            P.copy(gfb[:, 2 + hh, :], self.gkb[:])
        for gi in range(3):
            d = DIL_D[gi]
            with ExitStack() as sg:
                T2 = lambda n, s, dt: self.T(sg, n, s, dt)
                qbT = T2('qbT', [64, 2, S], BF16)
                kbT = T2('kbT', [64, 2, S], BF16)
                vb = T2('vb', [128, 16, 128], BF16)
                DEP = 4
                sq = [T2('dsq%d' % i, [128, 4, 64], F32) for i in range(DEP)]
                rc = [T2('drc%d' % i, [128, 4, 64], F32) for i in range(DEP)]
                rn = [T2('drn%d' % i, [128, 4, 64], F32) for i in range(2)]
                tmp = [T2('dtmp%d' % i, [128, 4, 4, 32], F32) for i in range(2)]
                rr = [T2('drr%d' % i, [128, 4, 64], BF16) for i in range(DEP)]
                st4 = [T2('dst%d' % i, [128, 3, 4], F32) for i in range(DEP)]
                ptile = [T2('dpt%d' % i, [128, 512], BF16) for i in range(3)]
                tokf = lambda tt: slice(tt * 128, (tt + 1) * 128)

                def d0(tt):
                    pa = ps[tt % 3]
                    for k in range(8):
                        P.mm(pa[:, 0:256], self.hT[:, k, tokf(tt)], w_dil[:, k, gi * 384:gi * 384 + 256], start=(k == 0), stop=(k == 7))

                def d1(tt):
                    R = ps[tt % 3][:, 0:256].rearrange('p (h d) -> p h d', d=64)
                    P.act(sq[tt % DEP][:], R, AF.Square)
                    P.copy(rc[tt % DEP][:], R, eng='act')

                def d2(tt):
                    P.reduce(st4[tt % DEP][:, 0, :], sq[tt % DEP][:], ALU.add)

                def d3(tt):
                    P.act(st4[tt % DEP][:, 1, :], st4[tt % DEP][:, 0, :], AF.Ln, scale=1.0 / DH, bias=EPS)
                    P.act(st4[tt % DEP][:, 2, :], st4[tt % DEP][:, 1, :], AF.Exp, scale=-0.5)

                def d4(tt):
                    i2 = tt % 2
                    P.tt(rn[i2][:], rc[tt % DEP][:], st4[tt % DEP][:, 2, :].unsqueeze(2).to_broadcast([128, 4, 64]), ALU.mult)
                    P.tt(rn[i2][:], rn[i2][:], gfb[:], ALU.mult)
                    cosb = self.cosT[:, tt, :].unsqueeze(1).to_broadcast([128, 4, 32])
                    sinb = self.sinT[:, tt, :].unsqueeze(1).to_broadcast([128, 4, 32])
                    x1 = rn[i2][:, :, 0:32]
                    x2 = rn[i2][:, :, 32:64]
                    tm = tmp[i2]
                    P.tt(tm[:, 0], x1, cosb, ALU.mult)
                    P.tt(tm[:, 1], x2, sinb, ALU.mult)
                    P.tt(tm[:, 2], x1, sinb, ALU.mult, eng='pool')
                    P.tt(tm[:, 3], x2, cosb, ALU.mult, eng='pool')
                    P.tt(rr[tt % DEP][:, :, 0:32], tm[:, 0], tm[:, 1], ALU.subtract)
                    P.tt(rr[tt % DEP][:, :, 32:64], tm[:, 2], tm[:, 3], ALU.add, eng='pool')

                def d5(tt):
                    pt_ = ps[3 + tt % 2].bitcast(BF16)
                    for hh in range(4):
                        P.tr(pt_[0:64, hh * 128:(hh + 1) * 128], rr[tt % DEP][:, hh, :], self.identb[:])

                def d6(tt):
                    pt_ = ps[3 + tt % 2].bitcast(BF16)
                    P.copy(qbT[:, :, tokf(tt)], pt_[0:64, 0:256].rearrange('p (h t) -> p h t', t=128), eng='act')
                    P.copy(kbT[:, :, tokf(tt)], pt_[0:64, 256:512].rearrange('p (h t) -> p h t', t=128), eng='act')

                pipeline(NT, [d0, d1, d2, d3, d4, d5, d6])
                nkb = 16 // d
                for r in range(d):
                    for kbs in range(nkb):
                        bi = r * nkb + kbs
                        pvp = ps[4 + bi % 2]
                        s0 = 128 * kbs * d + r
                        for k in range(8):
                            P.mm(pvp[:, 0:128], self.hT[:, k, s0:s0 + 127 * d + 1:d], w_dil[:, k, gi * 384 + 256:gi * 384 + 384],
                                 start=(k == 0), stop=(k == 7))
                        P.copy(vb[:, bi, :], pvp[:, 0:128], eng='act')
                rounds = []
                if d == 1:
                    for Rn_ in range(4):
                        steps = []
                        for kbs in range(max(0, 4 * Rn_ - 1), 4 * Rn_ + 4):
                            if kbs < 4 * Rn_:
                                steps.append(dict(r=0, kbs=kbs, q0=4 * Rn_, nq=1, slot=0, mask='anti'))
                            elif kbs < 4 * Rn_ + 3:
                                steps.append(dict(r=0, kbs=kbs, q0=kbs, nq=2, slot=kbs - 4 * Rn_, mask='trianti'))
                            else:
                                steps.append(dict(r=0, kbs=kbs, q0=kbs, nq=1, slot=3, mask='tri'))
                        rounds.append(dict(steps=steps, out=('contig', 512 * Rn_)))
                elif d == 4:
                    for r in range(4):
                        steps = []
                        for kbs in range(4):
                            if kbs < 3:
                                steps.append(dict(r=r, kbs=kbs, q0=kbs, nq=2, slot=kbs, mask='trianti'))
                            else:
                                steps.append(dict(r=r, kbs=kbs, q0=kbs, nq=1, slot=3, mask='tri'))
                        rounds.append(dict(steps=steps, out=('strided', r)))
                else:
                    for Rr in range(4):
                        steps = [dict(r=4 * Rr + i, kbs=0, q0=0, nq=1, slot=i, mask='tri') for i in range(4)]
                        rounds.append(dict(steps=steps, out=('res16', 4 * Rr)))
                items = []
                for rd_i, rd in enumerate(rounds):
                    for si, s_ in enumerate(rd['steps']):
                        for j in range(2):
                            items.append((rd_i, s_, j, si == len(rd['steps']) - 1 and j == 1))
                dstate = {}
                dcnt = {'s': 0}
                dstarted = {}

                def dqk(ii):
                    rd_i, s_, j, _ = items[ii]
                    r, kbs, q0, nq = s_['r'], s_['kbs'], s_['q0'], s_['nq']
                    k0 = 128 * kbs * d + r
                    qs0 = 128 * q0 * d + r
                    ncol = 128 * nq
                    sb = ps[dcnt['s'] % 4]
                    pt = ptile[dcnt['s'] % 3]
                    dcnt['s'] += 1
                    P.mm(sb[:, 0:ncol], kbT[0:64, j, k0:k0 + 127 * d + 1:d],
                         qbT[0:64, j, qs0:qs0 + (ncol - 1) * d + 1:d], start=True, stop=True)
                    P.act(pt[:, 0:ncol], sb[:, 0:ncol], AF.Exp)
                    mk = {'tri': self.trib[:], 'anti': self.antib[:], 'trianti': self.triantib[:]}[s_['mask']]
                    P.tt(pt[:, 0:ncol], pt[:, 0:ncol], mk, ALU.mult)
                    dstate[ii] = pt

                def dpv(ii):
                    rd_i, s_, j, last = items[ii]
                    rd = rounds[rd_i]
                    pt = dstate.pop(ii)
                    r, kbs, nq, slot = s_['r'], s_['kbs'], s_['nq'], s_['slot']
                    bi = r * nkb + kbs
                    ncol = 128 * nq
                    c0 = slot * 128
                    pu = ps[4 + (rd_i % 2) * 2]
                    pz = ps[5 + (rd_i % 2) * 2]
                    first = not dstarted.get((rd_i, j), False)
                    dstarted[(rd_i, j)] = True
                    P.mm(pu[64 * j:64 * j + 64, c0:c0 + ncol], vb[:, bi, 64 * j:64 * j + 64], pt[:, 0:ncol],
                         start=first, stop=True, skip_group_check=True)
                    P.mm(pz[64 * j:64 * j + 64, c0:c0 + ncol], self.onesb[:, 0:64], pt[:, 0:ncol],
                         start=first, stop=True, skip_group_check=True)
                    if last:
                        kind, o0 = rd['out']
                        if kind == 'contig':
                            uo = us[:, gi, o0:o0 + 512]
                            zo = ztot[:, o0:o0 + 512]
                            pui, pzi = pu[:, :], pz[:, :]
                        elif kind == 'strided':
                            uo = us[:, gi, o0:o0 + 511 * 4 + 1:4]
                            zo = ztot[:, o0:o0 + 511 * 4 + 1:4]
                            pui, pzi = pu[:, :], pz[:, :]
                        else:
                            uo = us[:, gi, :].rearrange('p (k r) -> p r k', r=16)[:, o0:o0 + 4, :]
                            zo = ztot[:, :].rearrange('p (k r) -> p r k', r=16)[:, o0:o0 + 4, :]
                            pui = pu[:, :].rearrange('p (s k) -> p s k', k=128)
                            pzi = pz[:, :].rearrange('p (s k) -> p s k', k=128)
                        P.copy(uo, pui, eng='act')
                        if gi == 0:
                            P.copy(zo, pzi)
                        else:
                            P.tt(zo, pzi, zo, ALU.add)

                AH = 2
                nit = len(items)
                for ii in range(min(AH, nit)):
                    dqk(ii)
                for ii in range(nit):
                    if ii + AH < nit:
                        dqk(ii + AH)
                    dpv(ii)
                self.sync_phase()
        P.act(ztot[:], ztot[:], AF.Ln)
        P.act(ztot[:], ztot[:], AF.Exp, scale=-1.0)
        for gi in range(3):
            P.tt(self.o_bT[:, gi, :], us[:, gi, :], ztot[:], ALU.mult)
        self.sync_phase()


KB.phase2_dil = _kb_phase2_dil


def _kb_setup_moe_consts(self):
    P, top = self.P, self.top
    T = lambda n, s, dt: self.T(top, n, s, dt)
    NTT = self.NTT
    self.wr = T('wr', [128, 8, 36], F32)
    with self.nc.allow_non_contiguous_dma(reason="tiny router weight rows"):
        P.dma(self.wr[:, :, 0:4], self.w_group[0].rearrange('(k p) g -> p k g', p=128))
        for gg in range(4):
            P.dma(self.wr[:, :, 4 + 8 * gg:12 + 8 * gg], self.w_router[0, gg].rearrange('(k p) e -> p k e', p=128))
    self.brow = T('brow', [128, 36], F32)
    P.dma(self.brow[:, 0:4], self.b_group.to_broadcast([128, 4]))
    P.dma(self.brow[:, 4:36], self.b_router[0:1].rearrange('o g e -> o (g e)').to_broadcast([128, 32]))
    self.EH = [T('EH%d' % k, [128, NTT, 32], BF16) for k in range(2)]
    self.rank = T('rank', [128, NTT, 2], F32)
    self.wts = T('wts', [128, NTT, 2], F32)
    self.lgall = [T('lgall%d' % i, [128, NT, 36], F32) for i in range(2)]
    self.rt_tiles = dict(rt=T('rt', [128, NT, 16], F32), gw=T('gw', [128, 6, NT], F32), sel4=T('sel4', [128, NT, 4, 8], F32),
                         sel=T('selr', [128, NT, 8], F32), m8a=T('m8a', [128, NT, 8], F32), oh=T('ohr', [128, 2, NT, 8], F32),
                         eh12=T('eh12a', [128, NT, 32], BF16), pre=T('pre', [128, NT, 32], F32), big=T('bigr', [128, NT, 32], F32))
    self.pending = []
    self.carry = T('carry', [128, 32], F32)
    P.memset(self.carry[:], 0.0)


KB.setup_moe_consts = _kb_setup_moe_consts


def _kb_phase3(self, b, st_seq):
    nc, P, ps = self.nc, self.P, self.ps
    with ExitStack() as st:
        T = lambda n, s, dt: self.T(st, n, s, dt)
        w_gm = T('w_gm', [128, 8, 2048], BF16)
        w_upa = T('w_upa', [128, 4, D], BF16)
        w_upb = T('w_upb', [128, 3, D], BF16)
        P.dma(w_upa[:].rearrange('p k c -> p (k c)'), self.w_upa_d, q='act')
        P.dma(w_upb[:].rearrange('p k c -> p (k c)'), self.w_upb_d, q='act')
        P.dma(w_gm[:].rearrange('p k c -> p (k c)'), self.w_gm_d)
        gm = [T('gm%d' % i, [128, 2, 512], F32) for i in range(2)]
        ybf = [T('ybf%d' % i, [128, 512], BF16) for i in range(2)]
        tokf = lambda tt: slice(tt * 128, (tt + 1) * 128)

        def a0(i):
            tt, hf = i // 2, i % 2
            pa_, pb_ = (ps[0], ps[1]) if i % 2 == 0 else (ps[5], ps[6])
            cs = slice(hf * 512, (hf + 1) * 512)
            for c in range(4):
                P.mm(pa_[:, :], self.o_aT[:, c, tokf(tt)], w_upa[:, c, cs], start=(c == 0), stop=(c == 3))
            for c in range(3):
                P.mm(pb_[:, :], self.o_bT[:, c, tokf(tt)], w_upb[:, c, cs], start=(c == 0), stop=(c == 2))
            for q2 in range(2):
                cg = slice(q2 * 1024 + hf * 512, q2 * 1024 + (hf + 1) * 512)
                for k in range(8):
                    P.mm(ps[2 + q2][:, :], self.hT[:, k, tokf(tt)], w_gm[:, k, cg], start=(k == 0), stop=(k == 7))

        def a1(i):
            for q2 in range(2):
                P.act(gm[i % 2][:, q2, :], ps[2 + q2][:, :], AF.Sigmoid)

        def a2(i):
            pa_, pb_ = (ps[0], ps[1]) if i % 2 == 0 else (ps[5], ps[6])
            g_ = gm[i % 2]
            P.tt(g_[:, 0, :], g_[:, 0, :], pa_[:, :], ALU.mult)
            P.tt(g_[:, 1, :], g_[:, 1, :], pb_[:, :], ALU.mult)
            P.tt(ybf[i % 2][:], g_[:, 0, :], g_[:, 1, :], ALU.add)

        def a3(i):
            pb = ps[4].bitcast(BF16)
            for k in range(4):
                P.tr(pb[:, k * 128:(k + 1) * 128], ybf[i % 2][:, k * 128:(k + 1) * 128], self.identb[:])

        def a4(i):
            tt, hf = i // 2, i % 2
            pb = ps[4].bitcast(BF16)
            P.copy(self.hT[:, 4 * hf:4 * hf + 4, tokf(tt)], pb[:, 0:512].rearrange('p (k t) -> p k t', t=128), eng='act')

        pipeline(2 * NT, [a0, a1, a2, a3, a4], reverse=True)
        self.sync_phase()
    with ExitStack() as st:
        T = lambda n, s, dt: self.T(st, n, s, dt)
        w_o = T('w_o', [128, 8, D], BF16)
        P.dma(w_o[:].rearrange('p k c -> p (k c)'), self.w_o_d)
        gt1 = T('gt1', [128, D], F32)
        A2 = T('A2', [128, D], F32)
        sh2 = T('sh2', [128, D], F32)
        P.dma(gt1[:], self.mod_d[b:b + 1, 2 * D:3 * D].to_broadcast([128, D]), q='act')
        P.dma(sh2[:], self.mod_d[b:b + 1, 3 * D:4 * D].to_broadcast([128, D]), q='act')
        P.dma(A2[:], self.mod_d[b:b + 1, 4 * D:5 * D].to_broadcast([128, D]), q='act')
        xt = [T('x3t%d' % i, [128, D], F32) for i in range(2)]
        x1 = [T('x1t%d' % i, [128, D], F32) for i in range(2)]
        h2 = [T('h2t%d' % i, [128, D], F32) for i in range(2)]
        junk = T('p3junk', [128, D], F32)
        h2T = [T('h2T%d' % i, [128, 8, 128], F32) for i in range(2)]
        sm = [T('sm%d' % i, [128, 4], F32) for i in range(2)]
        lgall = self.lgall[b % 2]
        tokf = lambda tt: slice(tt * 128, (tt + 1) * 128)

        def b0(tt):
            r0 = b * S + tt * 128
            P.dma(xt[tt % 2][:], self.x[r0:r0 + 128, :], q=('sp' if tt % 2 == 0 else 'act'))

        def b1(tt):
            for hf in range(2):
                for k in range(8):
                    P.mm(ps[hf][:, :], self.hT[:, k, tokf(tt)], w_o[:, k, hf * 512:(hf + 1) * 512], start=(k == 0), stop=(k == 7))

        def b2(tt):
            i2 = tt % 2
            r0 = b * S + tt * 128
            for hf in range(2):
                sl = slice(hf * 512, (hf + 1) * 512)
                P.tt(x1[i2][:, sl], ps[hf][:, :], gt1[:, sl], ALU.mult)
                P.tt(x1[i2][:, sl], x1[i2][:, sl], xt[i2][:, sl], ALU.add)
            P.dma(self.x1_d[r0:r0 + 128, :], x1[i2][:])

        def b3(tt):
            s_ = sm[tt % 2]
            P.act(junk[:], x1[tt % 2][:], AF.Square, accum_out=s_[:, 0:1])
            P.act(s_[:, 1:2], s_[:, 0:1], AF.Ln, scale=1.0 / D, bias=EPS)
            P.act(s_[:, 2:3], s_[:, 1:2], AF.Exp, scale=-0.5)

        def b4(tt):
            i2 = tt % 2
            r0 = b * S + tt * 128
            P.stt(h2[i2][:], x1[i2][:], sm[i2][:, 2:3], A2[:], ALU.mult, ALU.mult)
            P.tt(h2[i2][:], h2[i2][:], sh2[:], ALU.add)
            P.dma(self.h2_d[r0:r0 + 128, :], h2[i2][:], q='pool')

        def b5(tt):
            for k in range(8):
                pb = ps[2 + k // 4]
                P.tr(pb[:, (k % 4) * 128:(k % 4 + 1) * 128], h2[tt % 2][:, k * 128:(k + 1) * 128], self.identf[:])

        def b6(tt):
            for hf in range(2):
                P.copy(h2T[tt % 2][:, hf * 4:(hf + 1) * 4, :], ps[2 + hf][:, :].rearrange('p (k t) -> p k t', t=128),
                       eng=('act' if hf == 0 else 'dve'))

        def b7(tt):
            for k in range(8):
                P.mm(ps[4][:, 0:36], h2T[tt % 2][:, k, :], self.wr[:, k, :], start=(k == 0), stop=(k == 7))

        def b8(tt):
            P.tt(lgall[:, tt, :], ps[4][:, 0:36], self.brow[:], ALU.add)

        pipeline(NT, [b0, b1, b2, b3, b4, b5, b6, b7, b8], reverse=True)
        T0 = b * NT
        R_ = self.rt_tiles
        rt, gw, sel4, sel, m8a, oh, eh12, pre, big = (R_['rt'], R_['gw'], R_['sel4'], R_['sel'], R_['m8a'], R_['oh'],
                                                      R_['eh12'], R_['pre'], R_['big'])
        lgs = self.lgall[b % 2]
        th = []
        A = th.append
        G4 = lgs[:, :, 0:4]
        A(lambda: P.reduce(gw[:, 0, :], G4, ALU.max))
        A(lambda: P.tt(rt[:, :, 0:4], G4, gw[:, 0, :].unsqueeze(2).to_broadcast([128, NT, 4]), ALU.subtract))
        A(lambda: P.act(rt[:, :, 4:8], rt[:, :, 0:4], AF.Exp))
        A(lambda: P.reduce(gw[:, 1, :], rt[:, :, 4:8], ALU.add))
        A(lambda: P.recip(gw[:, 1, :], gw[:, 1, :]))
        A(lambda: P.tt(rt[:, :, 8:12], G4, gw[:, 0, :].unsqueeze(2).to_broadcast([128, NT, 4]), ALU.is_equal))
        goh = rt[:, :, 8:12]
        A(lambda: P.tt(sel4[:], lgs[:, :, 4:36].rearrange('p t (g e) -> p t g e', e=8),
                       goh.unsqueeze(3).to_broadcast([128, NT, 4, 8]), ALU.mult))
        A(lambda: P.reduce(sel[:], sel4[:].rearrange('p t g e -> p t e g'), ALU.add))
        for t in range(NT):
            A(lambda t=t: P.max8(m8a[:, t, :], sel[:, t, :]))
        A(lambda: P.tt(gw[:, 2, :], m8a[:, :, 1], m8a[:, :, 0], ALU.subtract))
        A(lambda: P.act(gw[:, 3, :], gw[:, 2, :], AF.Exp))
        A(lambda: P.ts(gw[:, 4, :], gw[:, 3, :], 1.0, None, ALU.add))
        A(lambda: P.recip(gw[:, 4, :], gw[:, 4, :]))
        A(lambda: P.tt(gw[:, 5, :], gw[:, 3, :], gw[:, 4, :], ALU.mult))
        A(lambda: P.tt(self.wts[:, T0:T0 + NT, 0], gw[:, 4, :], gw[:, 1, :], ALU.mult))
        A(lambda: P.tt(self.wts[:, T0:T0 + NT, 1], gw[:, 5, :], gw[:, 1, :], ALU.mult))
        for kk in range(2):
            A(lambda kk=kk: P.tt(oh[:, kk], sel[:], m8a[:, :, kk].unsqueeze(2).to_broadcast([128, NT, 8]), ALU.is_equal))
            A(lambda kk=kk: P.tt(self.EH[kk][:, T0:T0 + NT, :].rearrange('p t (g e) -> p t g e', e=8),
                                 goh.unsqueeze(3).to_broadcast([128, NT, 4, 8]),
                                 oh[:, kk].unsqueeze(2).to_broadcast([128, NT, 4, 8]), ALU.mult))
        A(lambda: P.tt(eh12[:], self.EH[0][:, T0:T0 + NT, :], self.EH[1][:, T0:T0 + NT, :], ALU.add))
        A(lambda: P.mm(ps[7][:, :], self.lstrict[:], eh12[:].rearrange('p t e -> p (t e)'), start=True, stop=True))
        A(lambda: P.copy(pre[:].rearrange('p t e -> p (t e)'), ps[7][:, :]))
        A(lambda: P.mm(ps[7][:, :], self.onesb[:], eh12[:].rearrange('p t e -> p (t e)'), start=True, stop=True))
        A(lambda: P.copy(big[:].rearrange('p t e -> p (t e)'), ps[7][:, :]))
        for t in range(NT):
            A(lambda t=t: P.tt(pre[:, t, :], pre[:, t, :], self.carry[:], ALU.add))
            A(lambda t=t: P.tt(self.carry[:], self.carry[:], big[:, t, :], ALU.add))
        for kk in range(2):
            A(lambda kk=kk: P.tt(big[:], self.EH[kk][:, T0:T0 + NT, :], pre[:], ALU.mult))
            A(lambda kk=kk: P.reduce(self.rank[:, T0:T0 + NT, kk], big[:], ALU.add))
        self.pending = th
        if b == 0:
            self.dump('lg0', lgall[:, 0, :])
        self.sync_phase()


KB.phase3 = _kb_phase3


def _kb_phase4(self):
    nc, P, ps = self.nc, self.P, self.ps
    NTT, NBLK = self.NTT, self.NBLK
    wg_v = self.w_e_gate[0].rearrange('e r f -> (e r) f')
    wu_v = self.w_e_up[0].rearrange('e r f -> (e r) f')
    wd_v = self.w_e_down[0].rearrange('e r f -> (e r) f')
    IOA = bass.IndirectOffsetOnAxis
    with ExitStack() as st:
        T = lambda n, s, dt: self.T(st, n, s, dt)
        self.drain_pending()
        dest = T('dest', [128, 2, NTT], I32)
        widx = T('widx', [128, NBLK], I32)
        with ExitStack() as s2:
            T2 = lambda n, s, dt: self.T(s2, n, s, dt)
            cnt = T2('cnt', [128, 6, 32], F32)
            onesf = T2('onesf', [128, 32], F32)
            big = T2('bigtmp', [128, NTT, 32], F32)
            thr = T2('thr', [128, NBLK, 32], F32)
            cmp_ = T2('cmpb', [128, NBLK, 32], F32)
            be = T2('be', [128, 2, NBLK], F32)
            rgu = T2('rgu', [128, 12], F32)
            df = T2('df', [128, 2, NTT], F32)
            P.dma(thr[:].rearrange('p a b -> p (a b)'), self.cd['thr'])
            P.memset(onesf[:], 1.0)
            P.copy(cnt[:, 0, :], self.carry[:])
            P.ts(cnt[:, 1, :], cnt[:, 0, :], float(MB - 1), 1.0 / MB, ALU.add, ALU.mult)
            P.ts(cnt[:, 1, :], cnt[:, 1, :], -0.498, MAGIC, ALU.add, ALU.add)
            P.ts(cnt[:, 1, :], cnt[:, 1, :], MAGIC, float(MB), ALU.subtract, ALU.mult)
            P.add('dve', lambda e: e.tensor_tensor_scan(cnt[:, 2, :], onesf[:], cnt[:, 1, :], 0.0, ALU.mult, ALU.add),
                  [onesf[:], cnt[:, 1, :]], [cnt[:, 2, :]])
            P.tt(cnt[:, 3, :], cnt[:, 2, :], cnt[:, 1, :], ALU.subtract)
            for kk in range(2):
                P.tt(big[:], self.EH[kk][:], cnt[:, 3, :].unsqueeze(1).to_broadcast([128, NTT, 32]), ALU.mult)
                P.reduce(df[:, kk, :], big[:], ALU.add)
                P.tt(df[:, kk, :], df[:, kk, :], self.rank[:, :, kk], ALU.add)
            P.copy(dest[:], df[:])
            P.tt(cmp_[:], cnt[:, 2, :].unsqueeze(1).to_broadcast([128, NBLK, 32]), thr[:], ALU.is_le)
            P.reduce(be[:, 0, :], cmp_[:], ALU.add)
            P.ts(be[:, 0, :], be[:, 0, :], float(NEXP - 1), None, ALU.min)
            P.dma(rgu[:], self.cd['rowoff'])
            P.ts(be[:, 1, :], be[:, 0, :], 128.0, rgu[:, 0:1], ALU.mult, ALU.add)
            P.copy(widx[:], be[:, 1, :])
            self.dump('dest', df[:].rearrange('p a b -> p (a b)'))
            self.dump('be', be[:, 0, :])
            self.dump('cnt', cnt[:].rearrange('p a b -> p (a b)'))
            self.sync_phase()
        with ExitStack() as s2:
            T2 = lambda n, s, dt: self.T(s2, n, s, dt)
            hb = [T2('h2b%d' % i, [128, D], BF16) for i in range(3)]
            for Tg in range(NTT):
                h_ = hb[Tg % 3]
                P.dma(h_[:], self.h2_d[Tg * 128:(Tg + 1) * 128, :], q='sp')
                for kk in range(2):
                    ia = dest[:, kk, Tg:Tg + 1]
                    P.add('pool', (lambda h_=h_, ia=ia: (lambda e: e.indirect_dma_start(
                        out=self.xs_d, out_offset=IOA(ap=ia, axis=0), in_=h_[:, :], in_offset=None)))(),
                        [h_[:], ia], [], dma=True)
            self.sync_phase()
        with ExitStack() as s2:
            T2 = lambda n, s, dt: self.T(s2, n, s, dt)
            wg = [T2('wg%d' % i, [128, 8, FF], BF16) for i in range(2)]
            wu = [T2('wu%d' % i, [128, 8, FF], BF16) for i in range(2)]
            wd = [T2('wd%d' % i, [128, 4, D], BF16) for i in range(2)]
            xsb = [T2('xsb%d' % i, [128, 2, D], BF16) for i in range(2)]
            xT = [T2('xT%d' % i, [128, 8, MB], BF16) for i in range(2)]
            sg = [T2('sg%d' % i, [128, 4, MB], F32) for i in range(2)]
            hidT = [T2('hidT%d' % i, [128, 4, MB], BF16) for i in range(2)]
            yb = [T2('yb%d' % i, [128, 2, D], BF16) for i in range(2)]

            def load_w(blk, which):
                i2 = blk % 2
                ia = widx[:, blk:blk + 1]
                lst = ((wg[i2], self.wg_l), (wu[i2], self.wu_l)) if which == 0 else ((wd[i2], self.wd_l),)
                for (wt, src) in lst:
                    P.add('pool', (lambda wt=wt, src=src, ia=ia: (lambda e: e.indirect_dma_start(
                        out=wt[:].rearrange('p k f -> p (k f)'), out_offset=None, in_=src, in_offset=IOA(ap=ia, axis=0))))(),
                        [ia], [wt[:]], dma=True)

            def m0(blk):
                r0 = blk * MB
                P.dma(xsb[blk % 2][:], self.xs_d[r0:r0 + MB, :].rearrange('(t p) d -> p t d', p=128), q='act')

            def m1(blk):
                load_w(blk, 0)
                for t2 in range(2):
                    pb = ps[t2].bitcast(BF16)
                    for k in range(8):
                        P.tr(pb[:, k * 128:(k + 1) * 128], xsb[blk % 2][:, t2, k * 128:(k + 1) * 128], self.identb[:])

            def m2(blk):
                for t2 in range(2):
                    pb = ps[t2].bitcast(BF16)
                    P.copy(xT[blk % 2][:, :, t2 * 128:(t2 + 1) * 128], pb[:, :].rearrange('p (k t) -> p k t', t=128),
                           eng=('act' if t2 == 0 else 'dve'))

            def m3(blk):
                i2 = blk % 2
                load_w(blk, 1)
                for f in range(4):
                    pg_ = ps[2 + f // 2]
                    pu_ = ps[4 + f // 2]
                    cs = slice((f % 2) * MB, (f % 2 + 1) * MB)
                    for k in range(8):
                        P.mm(pg_[:, cs], wg[i2][:, k, f * 128:(f + 1) * 128], xT[i2][:, k, :], start=(k == 0 and f % 2 == 0),
                             stop=(k == 7), skip_group_check=True)
                    for k in range(8):
                        P.mm(pu_[:, cs], wu[i2][:, k, f * 128:(f + 1) * 128], xT[i2][:, k, :], start=(k == 0 and f % 2 == 0),
                             stop=(k == 7), skip_group_check=True)

            def m4(blk):
                i2 = blk % 2
                for f2 in range(2):
                    P.act(sg[i2][:, 2 * f2:2 * f2 + 2, :], ps[2 + f2][:, :].rearrange('p (f t) -> p f t', t=MB), AF.Silu)
                    P.tt(hidT[i2][:, 2 * f2:2 * f2 + 2, :], sg[i2][:, 2 * f2:2 * f2 + 2, :],
                         ps[4 + f2][:, :].rearrange('p (f t) -> p f t', t=MB), ALU.mult)

            def m5(blk):
                i2 = blk % 2
                for t2 in range(2):
                    for hf in range(2):
                        py = ps[6 + hf]
                        for f in range(4):
                            P.mm(py[:, :], hidT[i2][:, f, t2 * 128:(t2 + 1) * 128], wd[i2][:, f, hf * 512:(hf + 1) * 512],
                                 start=(f == 0), stop=(f == 3))
                        P.copy(yb[i2][:, t2, hf * 512:(hf + 1) * 512], py[:, :], eng=('act' if hf == 0 else 'dve'))

            def m6(blk):
                r0 = blk * MB
                P.dma(self.ys_d[r0:r0 + MB, :].rearrange('(t p) d -> p t d', p=128), yb[blk % 2][:], q='act')

            pipeline(NBLK, [m0, m1, m2, m3, m4, m5, m6], reverse=True)
            self.sync_phase()
        with ExitStack() as s2:
            T2 = lambda n, s, dt: self.T(s2, n, s, dt)
            ND = 3
            y0 = [T2('y0_%d' % i, [128, D], BF16) for i in range(ND)]
            y1 = [T2('y1_%d' % i, [128, D], BF16) for i in range(ND)]
            yo = [T2('yo_%d' % i, [128, D], F32) for i in range(ND)]
            x1 = [T2('x1f%d' % i, [128, D], F32) for i in range(ND)]
            gt2 = [T2('gt2_%d' % i, [128, D], F32) for i in range(2)]
            for Tg in range(NTT):
                i2 = Tg % ND
                b = Tg // NT
                if Tg % NT == 0:
                    P.dma(gt2[b % 2][:], self.mod_d[b:b + 1, 5 * D:6 * D].to_broadcast([128, D]))
                for kk, yt in ((0, y0[i2]), (1, y1[i2])):
                    ia = dest[:, kk, Tg:Tg + 1]
                    P.add('pool', (lambda yt=yt, ia=ia: (lambda e: e.indirect_dma_start(
                        out=yt[:, :], out_offset=None, in_=self.ys_d, in_offset=IOA(ap=ia, axis=0))))(),
                        [ia, self.ys_d], [yt[:]], dma=True)
                P.dma(x1[i2][:], self.x1_d[Tg * 128:(Tg + 1) * 128, :], q='sp')
                P.ts(yo[i2][:], y0[i2][:], self.wts[:, Tg, 0:1], None, ALU.mult)
                P.stt(yo[i2][:], y1[i2][:], self.wts[:, Tg, 1:2], yo[i2][:], ALU.mult, ALU.add)
                P.tt(yo[i2][:], yo[i2][:], gt2[b % 2][:], ALU.mult)
                P.tt(yo[i2][:], yo[i2][:], x1[i2][:], ALU.add)
                P.dma(self.out[Tg * 128:(Tg + 1) * 128, :], yo[i2][:], q='act')
            self.sync_phase()


KB.phase4 = _kb_phase4


N_CORES = 8
_CACHE = {}

_WEIGHT_KEYS = ['w_ada', 'b_ada', 'norm1_g', 'norm2_g', 'w_in', 'nsa_q_norm', 'nsa_k_norm', 'cmp_pe_k', 'cmp_w1_k',
                'cmp_w2_k', 'cmp_pe_v', 'cmp_w1_v', 'cmp_w2_v', 'dil_q_norm', 'dil_k_norm', 'w_up_a', 'w_up_b', 'w_out',
                'w_group', 'b_group', 'w_router', 'b_router', 'w_e_gate', 'w_e_up', 'w_e_down']


def kernel(**inputs):
    x = np.asarray(inputs['x'], dtype=np.float32)
    c = np.asarray(inputs['c'], dtype=np.float32)
    pos = np.asarray(inputs['positions'], dtype=np.int32)
    B = x.shape[0]
    nseq = B // N_CORES
    if 'kb' not in _CACHE:
        kb = KB(nseq)
        kb.build()
        _CACHE['kb'] = kb
    kb = _CACHE['kb']
    w = {k: np.ascontiguousarray(np.asarray(inputs[k], dtype=np.float32)) for k in _WEIGHT_KEYS}
    in_maps = []
    for i in range(N_CORES):
        m = dict(w)
        m['x'] = np.ascontiguousarray(x[i * nseq:(i + 1) * nseq].reshape(nseq * S, D))
        m['c'] = np.ascontiguousarray(c[i * nseq:(i + 1) * nseq])
        m['positions'] = np.ascontiguousarray(pos[i * nseq:(i + 1) * nseq])
        for k, v in kb.consts.items():
            m['k_' + k] = v
        in_maps.append(m)
    res = run_bass_kernel_spmd(kb.nc, in_maps, core_ids=list(range(N_CORES)))
    out = np.concatenate([np.asarray(r['out']).reshape(nseq, S, D) for r in res.results], axis=0)
    return out.astype(np.float32)
```

```python
import numpy as np
import concourse.bass as bass
import concourse.mybir as mybir

F32 = mybir.dt.float32
BF16 = mybir.dt.bfloat16
I32 = mybir.dt.int32
U32 = mybir.dt.uint32
ALU = mybir.AluOpType
AF = mybir.ActivationFunctionType
AX = mybir.AxisListType

ENG_ATTR = {'pe': 'tensor', 'act': 'scalar', 'dve': 'vector', 'pool': 'gpsimd', 'sp': 'sync'}
ENGS = ['pe', 'act', 'dve', 'pool', 'sp']
DMA_RING = 6
_ESZ = {}


def esz(dt):
    k = str(dt)
    if k not in _ESZ:
        _ESZ[k] = mybir.dt.size(dt) if hasattr(mybir.dt, 'size') else np.dtype(mybir.dt.np(dt)).itemsize
    return _ESZ[k]


def box_of(ap):
    t = ap.tensor
    name = t.name
    pat = ap.ap
    e = esz(ap.dtype)
    space = str(ap.space)
    if 'DRAM' in space.upper() or 'HBM' in space.upper():
        lo = ap.offset
        hi = lo
        for st, n in pat:
            if st >= 0:
                hi += st * (n - 1)
            else:
                lo += st * (n - 1)
        return (name, 'D', 0, 1, lo * e, (hi + 1) * e)
    pstep, pn = pat[0]
    p0 = ap.start_partition()
    p1 = p0 + ap.partition_size()
    off = ap.offset - p0 * pstep if pstep else ap.offset
    lo = off
    hi = off
    for st, n in pat[1:]:
        if st >= 0:
            hi += st * (n - 1)
        else:
            lo += st * (n - 1)
    sp = 'P' if 'PSUM' in space.upper() else 'S'
    return (name, sp, p0, p1, lo * e, (hi + 1) * e)


class Op:
    __slots__ = ('eng', 'chan', 'pos', 'emit', 'waits', 'snap', 'inc', 'dma')


class Prog:
    def __init__(self, nc):
        self.nc = nc
        self.sems = {}
        self.semcnt = {}
        self.ops = {e: [] for e in ENGS}
        self.chan_ops = {}
        self.chan_base = {}
        self.vc = {e: {} for e in ENGS}
        self.trk = {}
        self.psum_last = {}
        self.dma_n = {e: 0 for e in ENGS}
        self._oldvals = {}
        self.n_ops = 0

    def sem(self, chan):
        if chan not in self.sems:
            nm = 's_' + (chan if isinstance(chan, str) else '%s%d' % chan)
            h = self.nc.alloc_semaphore(name=nm)
            self.sems[chan] = h
            self.semcnt[chan] = 0
        return self.sems[chan]

    def _known(self, eng, chan, pos):
        return self.vc[eng].get(chan, -1) >= pos

    def _learn(self, eng, chan, pos):
        vc = self.vc[eng]
        op = self.chan_ops[chan][pos - self.chan_base.get(chan, 0)] if pos >= self.chan_base.get(chan, 0) else None
        if op is not None and op.snap:
            for c, p in op.snap.items():
                if vc.get(c, -1) < p:
                    vc[c] = p
        if vc.get(chan, -1) < pos:
            vc[chan] = pos

    def _deps_for(self, eng, reads, writes):
        deps = set()
        for ap in reads:
            bx = box_of(ap)
            name, sp = bx[0], bx[1]
            if sp == 'P':
                self._psum_deps(eng, name, deps)
                continue
            t = self.trk.get(name)
            if t is None:
                continue
            for (wb, c, p) in t['w']:
                if wb[2] < bx[3] and bx[2] < wb[3] and wb[4] < bx[5] and bx[4] < wb[5]:
                    deps.add((c, p))
        for ap in writes:
            bx = box_of(ap)
            name, sp = bx[0], bx[1]
            if sp == 'P':
                self._psum_deps(eng, name, deps)
                continue
            t = self.trk.get(name)
            if t is None:
                continue
            for (wb, c, p) in t['w']:
                if wb[2] < bx[3] and bx[2] < wb[3] and wb[4] < bx[5] and bx[4] < wb[5]:
                    deps.add((c, p))
            for (rb, c), p in t['r'].items():
                if rb[2] < bx[3] and bx[2] < rb[3] and rb[4] < bx[5] and bx[4] < rb[5]:
                    deps.add((c, p))
        return deps

    def _psum_deps(self, eng, name, deps):
        last = self.psum_last.get(name)
        if not last:
            return
        for e, cp in last.items():
            if e == eng and eng == 'pe':
                continue
            deps.add(cp)

    def _record_access(self, eng, chan, pos, reads, writes):
        for ap in reads:
            bx = box_of(ap)
            name, sp = bx[0], bx[1]
            if sp == 'P':
                self.psum_last.setdefault(name, {})[eng] = (chan, pos)
                continue
            t = self.trk.setdefault(name, {'w': [], 'r': {}})
            t['r'][(bx, chan)] = pos
        for ap in writes:
            bx = box_of(ap)
            name, sp = bx[0], bx[1]
            if sp == 'P':
                self.psum_last.setdefault(name, {})[eng] = (chan, pos)
                continue
            t = self.trk.setdefault(name, {'w': [], 'r': {}})
            neww = []
            for ent in t['w']:
                wb = ent[0]
                if bx[2] <= wb[2] and wb[3] <= bx[3] and bx[4] <= wb[4] and wb[5] <= bx[5]:
                    continue
                neww.append(ent)
            neww.append((bx, chan, pos))
            t['w'] = neww
            if t['r']:
                t['r'] = {k: v for k, v in t['r'].items()
                          if not (bx[2] <= k[0][2] and k[0][3] <= bx[3] and bx[4] <= k[0][4] and k[0][5] <= bx[5])}

    def add(self, eng, emit, reads=(), writes=(), dma=False, extra_deps=()):
        op = Op()
        op.eng = eng
        op.emit = emit
        op.dma = dma
        op.inc = dma
        deps = self._deps_for(eng, reads, writes)
        deps.update(extra_deps)
        if dma:
            slot = self.dma_n[eng] % DMA_RING
            self.dma_n[eng] += 1
            chan = (eng, slot)
            lst = self.chan_ops.setdefault(chan, [])
            base = self.chan_base.get(chan, 0)
            if lst or base:
                deps.add((chan, base + len(lst) - 1))
        else:
            chan = eng
            lst = self.chan_ops.setdefault(chan, [])
        self.sem(chan)
        pos = self.chan_base.get(chan, 0) + len(lst)
        waits = []
        for (c, p) in sorted(deps, key=lambda cp: (str(cp[0]), cp[1])):
            if c == eng and eng == 'pe':
                continue
            if self._known(eng, c, p):
                continue
            waits.append((c, p))
        best = {}
        for c, p in waits:
            if best.get(c, -1) < p:
                best[c] = p
        op.waits = list(best.items())
        for c, p in op.waits:
            cb = self.chan_base.get(c, 0)
            if p >= cb:
                self.chan_ops[c][p - cb].inc = True
            self._learn(eng, c, p)
        op.snap = dict(self.vc[eng])
        op.chan = chan
        op.pos = pos
        lst.append(op)
        self.ops[eng].append(op)
        self._record_access(eng, chan, pos, reads, writes)
        self.n_ops += 1
        return (chan, pos)

    def barrier(self):
        lasts = []
        for chan, lst in self.chan_ops.items():
            if lst:
                lst[-1].inc = True
                lasts.append((chan, self.chan_base.get(chan, 0) + len(lst) - 1))
        for e in ENGS:
            op = Op()
            op.eng = e
            op.emit = None
            op.dma = False
            op.inc = False
            op.chan = None
            op.pos = -1
            waits = []
            for (c, p) in lasts:
                if c == e and e == 'pe':
                    continue
                if self._known(e, c, p):
                    continue
                waits.append((c, p))
            op.waits = waits
            for c, p in waits:
                self._learn(e, c, p)
            op.snap = None
            self.ops[e].append(op)

    def flush(self):
        nc = self.nc
        semval = {}
        for chan, lst in self.chan_ops.items():
            v = self.semcnt[chan]
            base = self.chan_base.get(chan, 0)
            for i, op in enumerate(lst):
                if op.inc:
                    v += 16 if op.dma else 1
                semval[(chan, base + i)] = v if op.inc else None
            self.semcnt[chan] = v
        old = self._oldvals
        old.update({k: v for k, v in semval.items() if v is not None})
        ops = self.ops
        sems = self.sems

        def run(engname):
            def f(eng):
                for op in ops[engname]:
                    for (c, p) in op.waits:
                        v = old.get((c, p))
                        assert v is not None, (engname, c, p)
                        eng.wait_ge(sems[c], v)
                    if op.emit is None:
                        continue
                    inst = op.emit(eng)
                    if op.inc:
                        inst.then_inc(sems[op.chan], 16 if op.dma else 1)
            return f

        with nc.Block() as block:
            block.tensor(run('pe'))
            block.scalar(run('act'))
            block.vector(run('dve'))
            block.gpsimd(run('pool'))
            block.sync(run('sp'))
        for chan, lst in self.chan_ops.items():
            self.chan_base[chan] = self.chan_base.get(chan, 0) + len(lst)
            self.chan_ops[chan] = []
        self.ops = {e: [] for e in ENGS}

    def dma(self, out, in_, q='sp', **kw):
        return self.add(q, lambda e: e.dma_start(out=out, in_=in_, **kw), [in_], [out], dma=True)

    def mm(self, out, lhsT, rhs, start=True, stop=True, **kw):
        return self.add('pe', lambda e: e.matmul(out, lhsT, rhs, start=start, stop=stop, **kw),
                        [lhsT, rhs], [out])

    def tr(self, out, in_, ident):
        return self.add('pe', lambda e: e.transpose(out, in_, ident), [in_, ident], [out])

    def act(self, out, in_, func, bias=None, scale=None, accum_out=None, eng='act'):
        kw = {}
        rd = [in_]
        wr = [out]
        if bias is not None:
            kw['bias'] = bias
            if not isinstance(bias, (int, float)):
                rd.append(bias)
        if scale is not None:
            kw['scale'] = scale
            if not isinstance(scale, (int, float)):
                rd.append(scale)
        if accum_out is not None:
            kw['accum_out'] = accum_out
            wr.append(accum_out)
        return self.add('act', lambda e: e.activation(out, in_, func, **kw), rd, wr)

    def tt(self, out, in0, in1, op, eng='dve'):
        return self.add(eng, lambda e: e.tensor_tensor(out, in0, in1, op), [in0, in1], [out])

    def ts(self, out, in0, s1, s2, op0, op1=None, eng='dve', accum_out=None):
        rd = [in0]
        if not isinstance(s1, (int, float)) and s1 is not None:
            rd.append(s1)
        if not isinstance(s2, (int, float)) and s2 is not None:
            rd.append(s2)
        kw = {}
        wr = [out]
        if op1 is not None:
            kw['op1'] = op1
        if accum_out is not None:
            kw['accum_out'] = accum_out
            wr.append(accum_out)
        return self.add(eng, lambda e: e.tensor_scalar(out, in0, s1, s2, op0, **kw), rd, wr)

    def stt(self, out, in0, scalar, in1, op0, op1, eng='dve'):
        rd = [in0, in1]
        if not isinstance(scalar, (int, float)):
            rd.append(scalar)
        return self.add(eng, lambda e: e.scalar_tensor_tensor(out, in0, scalar, in1, op0, op1), rd, [out])

    def copy(self, out, in_, eng='dve'):
        if eng == 'act':
            return self.add('act', lambda e: e.copy(out, in_), [in_], [out])
        return self.add(eng, lambda e: e.tensor_copy(out, in_), [in_], [out])

    def reduce(self, out, in_, op, axis=AX.X, eng='dve'):
        return self.add(eng, lambda e: e.tensor_reduce(out, in_, axis, op), [in_], [out])

    def memset(self, ap, val, eng='dve'):
        return self.add(eng, lambda e: e.memset(ap, val), [], [ap])

    def recip(self, out, in_, eng='dve'):
        return self.add(eng, lambda e: e.reciprocal(out, in_), [in_], [out])

    def max8(self, out, in_):
        return self.add('dve', lambda e: e.max(out, in_), [in_], [out])
from concourse.bass_utils import run_bass_kernel_spmd
from contextlib import ExitStack

S = 2048
D = 1024
DH = 64
NT = S // 128
IN_COLS = 4504
EPS = 1e-6
BIG = 30000.0
MAGIC = 12582912.0
TWO_PI = 6.283185307179586
NEXP = 32
FF = 512
MB = 256


def host_consts():
    c = {}
    c['identf'] = np.eye(128, dtype=np.float32)
    inv = (10000.0 ** (-np.arange(0, 64, 2, dtype=np.float32) / 64)).astype(np.float32)
    c['invf'] = np.tile(inv[None, :], (128, 1)).astype(np.float32)
    k = np.arange(128)[:, None]
    q = np.arange(128)[None, :]
    tri = (k <= q).astype(np.float32)
    anti = (k >= q).astype(np.float32)
    c['tri'] = tri
    c['anti'] = anti
    c['trianti'] = np.concatenate([tri, anti], axis=1)
    c['tri4'] = np.tile(tri, (1, 4))
    t = np.arange(S)
    cv = np.zeros((128, S), np.float32)
    cv[:127] = ((np.arange(127) * 16 + 31)[:, None] <= t[None, :])
    c['cmpvalid'] = cv
    c['blkoh'] = (np.arange(32)[:, None] == (t // 64)[None, :]).astype(np.float32)
    cs = np.arange(127) * 16
    ss = np.arange(32) * 64
    ov = np.clip(np.minimum(cs[:, None] + 32, ss[None, :] + 64) - np.maximum(cs[:, None], ss[None, :]), 0, None) / 32.0
    ovp = np.zeros((128, 32), np.float32)
    ovp[:127] = ov
    c['overlap'] = ovp
    b = (t // 64)[:, None]
    s = np.arange(32)[None, :]
    cand = ((s >= 1) & (s <= b - 2)).astype(np.float32)
    forced = (((s == 0) | (s == b) | (s == b - 1)) & (s <= b)).astype(np.float32)
    tm = lambda a: np.ascontiguousarray(a.reshape(NT, 128, 32).transpose(1, 0, 2)).astype(np.float32)
    c['cand'] = tm(cand)
    c['candm1'] = tm(cand - 1.0)
    c['forced'] = tm(forced)
    c['lstrict'] = (k < q).astype(np.float32)
    c['ones'] = np.ones((128, 128), np.float32)
    return c


class KB:
    def __init__(self, NSEQ, dbg=None):
        self.NSEQ = NSEQ
        self.NTOK = NSEQ * S
        self.NTT = NSEQ * NT
        self.NBLK = (self.NTOK * 2) // MB + NEXP
        self.NSLOT = self.NBLK * MB
        self.dbg = dbg or {}
        self.nc = bass.Bass("TRN2", target_bir_lowering=False)
        self.P = Prog(self.nc)
        self.din = {}
        self.consts = host_consts()
        blk = np.arange(self.NBLK, dtype=np.float32)[:, None] * MB
        self.consts['thr'] = np.tile(np.tile(blk, (1, 32)).reshape(1, -1), (128, 1)).astype(np.float32)
        p = np.arange(128, dtype=np.float32)[:, None]
        self.consts['rowoff'] = np.concatenate([np.arange(8)[None, :] * 128 + p, np.arange(4)[None, :] * 128 + p], axis=1).astype(np.float32)

    def dram_in(self, name, shape, dt=F32):
        h = self.nc.dram_tensor(name, list(shape), dt, kind="ExternalInput")
        self.din[name] = h
        return h.ap()

    def declare(self):
        NSEQ = self.NSEQ
        nc = self.nc
        d = self.dram_in
        self.x = d('x', [self.NTOK, D])
        self.c = d('c', [NSEQ, D])
        self.pos = d('positions', [NSEQ, S], I32)
        self.w_ada = d('w_ada', [1, D, 6 * D])
        self.b_ada = d('b_ada', [1, 6 * D])
        self.norm1_g = d('norm1_g', [1, D])
        self.norm2_g = d('norm2_g', [1, D])
        self.w_in = d('w_in', [1, D, IN_COLS])
        self.nsa_q_norm = d('nsa_q_norm', [1, DH])
        self.nsa_k_norm = d('nsa_k_norm', [1, DH])
        self.cmp_pe_k = d('cmp_pe_k', [1, 32, DH])
        self.cmp_w1_k = d('cmp_w1_k', [1, 2048, 256])
        self.cmp_w2_k = d('cmp_w2_k', [1, 256, DH])
        self.cmp_pe_v = d('cmp_pe_v', [1, 32, DH])
        self.cmp_w1_v = d('cmp_w1_v', [1, 2048, 256])
        self.cmp_w2_v = d('cmp_w2_v', [1, 256, DH])
        self.dil_q_norm = d('dil_q_norm', [1, DH])
        self.dil_k_norm = d('dil_k_norm', [1, DH])
        self.w_up_a = d('w_up_a', [1, 512, D])
        self.w_up_b = d('w_up_b', [1, 384, D])
        self.w_out = d('w_out', [1, D, D])
        self.w_group = d('w_group', [1, D, 4])
        self.b_group = d('b_group', [1, 4])
        self.w_router = d('w_router', [1, 4, D, 8])
        self.b_router = d('b_router', [1, 4, 8])
        self.w_e_gate = d('w_e_gate', [1, NEXP, D, FF])
        self.w_e_up = d('w_e_up', [1, NEXP, D, FF])
        self.w_e_down = d('w_e_down', [1, NEXP, FF, D])
        self.cd = {}
        for k, v in self.consts.items():
            self.cd[k] = d('k_' + k, v.shape)
        self.out = nc.dram_tensor('out', [self.NTOK, D], F32, kind="ExternalOutput").ap()
        sc = lambda n, shp, dt: nc.dram_tensor(n, list(shp), dt, kind="Internal").ap()
        self.mod_d = sc('mod_d', [NSEQ, 6 * D], F32)
        self.x1_d = sc('x1_d', [self.NTOK, D], F32)
        self.h2_d = sc('h2_d', [self.NTOK, D], BF16)
        self.xs_d = sc('xs_d', [self.NSLOT, D], BF16)
        self.ys_d = sc('ys_d', [self.NSLOT, D], BF16)
        self.wg_l = nc.dram_tensor('wg_l', [NEXP * 128, 8 * FF], BF16, kind="Internal").ap()
        self.wu_l = nc.dram_tensor('wu_l', [NEXP * 128, 8 * FF], BF16, kind="Internal").ap()
        self.wd_l = nc.dram_tensor('wd_l', [NEXP * 128, 4 * D], BF16, kind="Internal").ap()
        self.conv_list = []
        for e_ in range(NEXP):
            rows = slice(e_ * 128, (e_ + 1) * 128)
            self.conv_list.append((self.wg_l[rows, :].rearrange('p (k f) -> p k f', f=FF),
                                   self.w_e_gate[0, e_].rearrange('(k p) f -> p k f', p=128)))
            self.conv_list.append((self.wu_l[rows, :].rearrange('p (k f) -> p k f', f=FF),
                                   self.w_e_up[0, e_].rearrange('(k p) f -> p k f', p=128)))
            self.conv_list.append((self.wd_l[rows, :].rearrange('p (k f) -> p k f', f=D),
                                   self.w_e_down[0, e_].rearrange('(k p) f -> p k f', p=128)))
        self.conv_pos = 0
        self.w_nsa_d = sc('w_nsa_d', [128, 8 * 1304], BF16)
        self.w_dil_d = sc('w_dil_d', [128, 8 * 1152], BF16)
        self.w_gm_d = sc('w_gm_d', [128, 8 * 2048], BF16)
        self.w_upa_d = sc('w_upa_d', [128, 4 * D], BF16)
        self.w_upb_d = sc('w_upb_d', [128, 3 * D], BF16)
        self.w_o_d = sc('w_o_d', [128, 8 * D], BF16)
        self.dbg_out = {}
        for k, shp in self.dbg.items():
            self.dbg_out[k] = nc.dram_tensor('dbg_' + k, list(shp), F32, kind="ExternalOutput").ap()

    def T(self, st, name, shape, dt):
        self._uid = getattr(self, '_uid', 0) + 1
        return st.enter_context(self.nc.sbuf_tensor('%s_%d' % (name, self._uid), list(shape), dt))

    def drain_pending(self, n=None):
        if not hasattr(self, 'pending'):
            return
        k = len(self.pending) if n is None else min(n, len(self.pending))
        for _ in range(k):
            self.pending.pop(0)()

    def emit_fill(self, n):
        if not hasattr(self, 'zrow'):
            return
        for _ in range(n):
            if self.fill_pos >= self.NSLOT // 128:
                return
            r0 = self.fill_pos * 128
            self.fill_pos += 1
            self.P.dma(self.xs_d[r0:r0 + 128, :], self.zrow[:], q='sp')

    def emit_conv(self, n):
        for _ in range(n):
            if self.conv_pos >= len(self.conv_list):
                return
            dst, src = self.conv_list[self.conv_pos]
            self.conv_pos += 1
            self.P.dma(dst, src, q='pool')

    def dump(self, key, ap):
        if key in self.dbg_out:
            self.P.dma(self.dbg_out[key], ap, q='pool')

    def sync_phase(self, name=None):
        self.P.barrier()
        if name is None:
            import inspect
            fr = inspect.stack()[1]
            name = '%s_%d' % (fr.function.replace('_kb_', ''), fr.lineno)
        with self.nc.named_scope(name):
            self.P.flush()

    def phase0(self, prep=False):
        nc, P, NSEQ = self.nc, self.P, self.NSEQ
        ps = self.ps
        with ExitStack() as st:
            T = lambda n, s, dt: self.T(st, n, s, dt)
            if prep:
                self.prep_weights(st)
            cs = T('cs', [4, D], F32)
            csT = T('csT', [128, 8, 4], F32)
            wa = [T('wa%d' % i, [128, 8, 512], F32) for i in range(2)]
            modrows = T('modrows', [4, 6 * D], F32)
            bada = T('bada', [4, 6 * D], F32)
            g1b = T('g1b', [4, D], F32)
            g2b = T('g2b', [4, D], F32)
            P.dma(cs[0:NSEQ, :], self.c)
            P.dma(bada[0:NSEQ, :], self.b_ada.to_broadcast([NSEQ, 6 * D]))
            P.dma(g1b[0:NSEQ, :], self.norm1_g.to_broadcast([NSEQ, D]))
            P.dma(g2b[0:NSEQ, :], self.norm2_g.to_broadcast([NSEQ, D]))
            P.act(cs[0:NSEQ, :], cs[0:NSEQ, :], AF.Silu)
            for k in range(8):
                P.tr(ps[0][:, k * 4:k * 4 + NSEQ], cs[0:NSEQ, k * 128:(k + 1) * 128], self.identf[0:NSEQ, 0:NSEQ])
            P.copy(csT[:, :, 0:NSEQ], ps[0][:, 0:32].rearrange('p (k b) -> p k b', b=4)[:, :, 0:NSEQ])
            wv = self.w_ada[0].rearrange('(k p) c -> p k c', p=128)
            for cc in range(12):
                w = wa[cc % 2]
                P.dma(w[:], wv[:, :, cc * 512:(cc + 1) * 512], q=('sp' if cc % 2 == 0 else 'act'))
                pb = ps[1 + cc % 2]
                for k in range(8):
                    P.mm(pb[0:NSEQ, :], csT[:, k, 0:NSEQ], w[:, k, :], start=(k == 0), stop=(k == 7))
                P.tt(modrows[0:NSEQ, cc * 512:(cc + 1) * 512], pb[0:NSEQ, :], bada[0:NSEQ, cc * 512:(cc + 1) * 512], ALU.add)
            P.stt(modrows[0:NSEQ, D:2 * D], modrows[0:NSEQ, D:2 * D], 1.0, g1b[0:NSEQ, :], ALU.add, ALU.mult)
            P.stt(modrows[0:NSEQ, 4 * D:5 * D], modrows[0:NSEQ, 4 * D:5 * D], 1.0, g2b[0:NSEQ, :], ALU.add, ALU.mult)
            P.dma(self.mod_d, modrows[0:NSEQ, :])
            for ch in range(16):
                P.tr(ps[3][:, ch * 4:ch * 4 + NSEQ], modrows[0:NSEQ, ch * 128:(ch + 1) * 128], self.identf[0:NSEQ, 0:NSEQ])
            P.copy(self.modT1[:, :, 0:NSEQ], ps[3][:, 0:64].rearrange('p (k b) -> p k b', b=4)[:, :, 0:NSEQ])
            self.dump('modrows', modrows[0:NSEQ, :])
            self.sync_phase()

    def phase1(self, b):
        nc, P = self.nc, self.P
        ps = self.ps
        with ExitStack() as st:
            T = lambda n, s, dt: self.T(st, n, s, dt)
            xt = [T('xt%d' % i, [128, D], F32) for i in range(3)]
            junk = T('p1junk', [128, D], F32)
            xn = [T('xn%d' % i, [128, D], F32) for i in range(2)]
            ss = [T('p1ss%d' % i, [128, 4], F32) for i in range(2)]
            def p0(tt):
                r0 = b * S + tt * 128
                P.dma(xt[tt % 3][:], self.x[r0:r0 + 128, :], q=('sp' if tt % 2 == 0 else 'act'))

            def p1(tt):
                s_ = ss[tt % 2]
                P.act(junk[:], xt[tt % 3][:], AF.Square, accum_out=s_[:, 0:1])
                P.act(s_[:, 1:2], s_[:, 0:1], AF.Ln, scale=1.0 / D, bias=EPS)
                P.act(s_[:, 2:3], s_[:, 1:2], AF.Exp, scale=-0.5)

            npend = -(-len(getattr(self, 'pending', [])) // NT)

            def p2(tt):
                P.ts(xn[tt % 2][:], xt[tt % 3][:], ss[tt % 2][:, 2:3], None, ALU.mult)
                self.drain_pending(npend)

            def p3(tt):
                for k in range(8):
                    pb = ps[(tt % 2) * 2 + k // 4]
                    P.tr(pb[:, (k % 4) * 128:(k % 4 + 1) * 128], xn[tt % 2][:, k * 128:(k + 1) * 128], self.identf[:])

            def p4(tt):
                for k in range(8):
                    pb = ps[(tt % 2) * 2 + k // 4]
                    src = pb[:, (k % 4) * 128:(k % 4 + 1) * 128]
                    dst = self.hT[:, k, tt * 128:(tt + 1) * 128]
                    if k < 4:
                        P.act(dst, src, AF.Identity, scale=self.modT1[:, 8 + k, b:b + 1], bias=self.modT1[:, k, b:b + 1])
                    else:
                        P.ts(dst, src, self.modT1[:, 8 + k, b:b + 1], self.modT1[:, k, b:b + 1], ALU.mult, ALU.add)

            pipeline(NT, [p0, p1, p2, p3, p4], reverse=True)
            self.drain_pending()
            if b == 0:
                for k in range(8):
                    if ('hT%d' % k) in self.dbg_out:
                        self.dump('hT%d' % k, self.hT[:, k, :])
            self.sync_phase()


PI_SAFE = 3.1415925


def _kb_rope_tables(self, st, posf, n, cos_out, sin_out, tag):
    P = self.P
    T = lambda nm, s, dt: self.T(st, tag + nm, s, dt)
    ang = T('ang', [128, n, 32], F32)
    a2 = T('a2', [128, n, 32], F32)
    kk = T('kk', [128, n, 32], F32)
    P.tt(ang[:], self.invf[:, :].unsqueeze(1).to_broadcast([128, n, 32]),
         posf.unsqueeze(2).to_broadcast([128, n, 32]), ALU.mult)
    for off, outp in ((0.0, sin_out), (np.pi / 2, cos_out)):
        if off == 0.0:
            a = ang
        else:
            P.ts(a2[:], ang[:], float(off), None, ALU.add)
            a = a2
        P.ts(kk[:], a[:], 1.0 / TWO_PI, MAGIC, ALU.mult, ALU.add)
        P.ts(kk[:], kk[:], MAGIC, None, ALU.subtract)
        P.stt(kk[:], kk[:], -TWO_PI, a[:], ALU.mult, ALU.add)
        P.ts(kk[:], kk[:], PI_SAFE, -PI_SAFE, ALU.min, ALU.max)
        P.act(outp, kk[:], AF.Sin)


KB.rope_tables = _kb_rope_tables


def _kb_setup_nsa_consts(self):
    P, top = self.P, self.top
    T = lambda n, s, dt: self.T(top, n, s, dt)
    self.kslc = T('kslc', [96, S], BF16)
    P.dma(self.kslc[64:96, :], self.cd['blkoh'], q='pool')
    self.v2 = T('v2', [128, NT, 2, 65], BF16)
    P.memset(self.v2[:].rearrange('p a b c -> p (a b c)'), 1.0)
    self.vcaug = T('vcaug', [128, 97], BF16)
    P.memset(self.vcaug[:, 64:65], 1.0)
    P.dma(self.vcaug[:, 65:97], self.cd['overlap'], q='pool')
    self.w1kv = T('w1kv', [128, 32, 256], BF16)
    P.dma(self.w1kv[0:64], self.cmp_w1_k[0].rearrange('(l d) h -> d l h', d=64), q='pool')
    P.dma(self.w1kv[64:128], self.cmp_w1_v[0].rearrange('(l d) h -> d l h', d=64), q='pool')
    self.w2kv = T('w2kv', [128, 2, 2, 64], BF16)
    P.dma(self.w2kv[:, 0], self.cmp_w2_k[0].rearrange('(c p) d -> p c d', p=128), q='pool')
    P.dma(self.w2kv[:, 1], self.cmp_w2_v[0].rearrange('(c p) d -> p c d', p=128), q='pool')
    self.ckv = T('ckv', [128, 2, 2], F32)
    self.gfull = T('gfull', [128, 6, 64], F32)
    self.gk = T('gk', [128, 64], F32)
    self.gqb = T('gqb', [128, 64], F32)
    self.gkb = T('gkb', [128, 64], F32)
    for hh in range(4):
        P.dma(self.gfull[:, hh, :], self.nsa_q_norm.to_broadcast([128, 64]))
    for hh in range(4, 6):
        P.dma(self.gfull[:, hh, :], self.nsa_k_norm.to_broadcast([128, 64]))
    P.ts(self.gfull[:, 0:4, :], self.gfull[:, 0:4, :], 0.125, None, ALU.mult)
    P.dma(self.gk[:], self.nsa_k_norm.to_broadcast([128, 64]))
    P.dma(self.gqb[:], self.dil_q_norm.to_broadcast([128, 64]))
    P.ts(self.gqb[:], self.gqb[:], 0.125, None, ALU.mult)
    P.dma(self.gkb[:], self.dil_k_norm.to_broadcast([128, 64]))
    with ExitStack() as st:
        T2 = lambda n, s, dt: self.T(st, n, s, dt)
        pekv = T2('pekv', [32, 128], F32)
        peT = T2('peT', [128, 32], BF16)
        P.dma(pekv[:, 0:64], self.cmp_pe_k[0])
        P.dma(pekv[:, 64:128], self.cmp_pe_v[0])
        P.tr(self.ps[0][:, 0:32], pekv[:, :], self.identf[0:32, 0:32])
        P.copy(peT[:], self.ps[0][:, 0:32])
        for kv in range(2):
            base = 64 * kv
            for hc in range(2):
                for l in range(32):
                    P.mm(self.ps[1 + kv][:, hc:hc + 1], self.w1kv[base:base + 64, l, hc * 128:(hc + 1) * 128],
                         peT[base:base + 64, l:l + 1], start=(hc == 0 and l == 0), stop=(l == 31), skip_group_check=True)
            P.copy(self.ckv[:, kv, :], self.ps[1 + kv][:, 0:2])
        self.sync_phase()


KB.setup_nsa_consts = _kb_setup_nsa_consts


def _kb_seq_prologue(self, st, b):
    P = self.P
    T = lambda n, s, dt: self.T(st, n, s, dt)
    self.cosT = T('cosT', [128, NT, 32], F32)
    self.sinT = T('sinT', [128, NT, 32], F32)
    self.cosC = T('cosC', [128, 1, 32], F32)
    self.sinC = T('sinC', [128, 1, 32], F32)
    with ExitStack() as s2:
        T2 = lambda n, s, dt: self.T(s2, n, s, dt)
        posi = T2('posi', [128, 2], I32)
        posi16 = T2('posi16', [16, 128], I32)
        posf16 = T2('posf16', [16, 128], F32)
        posf = T2('posf', [128, NT + 1], F32)
        P.memset(posi[:], 0)
        P.dma(posi16[:], self.pos[b].rearrange('(t p) -> t p', p=128))
        P.copy(posf16[:], posi16[:])
        P.tr(self.ps[0][:, 0:NT], posf16[:], self.identf[0:16, 0:16])
        P.copy(posf[:, 0:NT], self.ps[0][:, 0:NT])
        P.dma(posi[0:127, 0:1], self.pos[b, 31:31 + 16 * 126 + 1:16].unsqueeze(1), allow_slow_non_contiguous=True)
        P.copy(posf[:, NT:NT + 1], posi[:, 0:1])
        self.rope_tables(s2, posf[:, 0:NT], NT, self.cosT[:], self.sinT[:], 'rt')
        self.rope_tables(s2, posf[:, NT:NT + 1], 1, self.cosC[:], self.sinC[:], 'rc')
        self.sync_phase()


KB.seq_prologue = _kb_seq_prologue


class BankRound:
    def __init__(self):
        self.started = {}

    def reset(self, bank):
        self.started[bank.name] = False

    def start(self, bank):
        s = not self.started.get(bank.name, False)
        self.started[bank.name] = True
        return s


def pipeline(n, stages, delays=None, reverse=False):
    if delays is None:
        delays = list(range(len(stages)))
    order = list(range(len(stages)))
    if reverse:
        order = order[::-1]
    for step in range(n + max(delays)):
        for j in order:
            i = step - delays[j]
            if 0 <= i < n:
                stages[j](i)


def _kb_phase2_nsa(self, b, st_seq):
    nc, P, ps = self.nc, self.P, self.ps
    BR = self.br
    wv = self.w_in[0].rearrange('(k p) c -> p k c', p=128)
    with ExitStack() as st:
        T = lambda n, s, dt: self.T(st, n, s, dt)
        w_nsa = T('w_nsa', [128, 8, 1304], BF16)
        P.dma(w_nsa[:].rearrange('p k c -> p (k c)'), self.w_nsa_d)
        gates = T('gates', [128, NT, 24], F32)
        for g in range(2):
            with ExitStack() as sg:
                self.nsa_group(b, g, sg, w_nsa, gates)
                self.sync_phase()


def _kb_nsa_group(self, b, g, st, w_nsa, gates):
    nc, P, ps = self.nc, self.P, self.ps
    BR = self.br
    T = lambda n, s, dt: self.T(st, n, s, dt)
    qaug = T('qaug', [96, 4, S], BF16)
    kwin = T('kwin', [64, S], BF16)
    kvcT = T('kvcT', [128, S], BF16)
    kcT = T('kcT', [64, 128], BF16)
    kslc, v2, vcaug = self.kslc, self.v2, self.vcaug
    with ExitStack() as s2:
        T2 = lambda n, s, dt: self.T(s2, n, s, dt)
        NP = NT // 2
        sq = [T2('sq%d' % i, [128, 2, 6, 64], F32) for i in range(1)] * 2
        rc = [T2('rc%d' % i, [128, 2, 6, 64], F32) for i in range(3)]
        rn = [T2('rn%d' % i, [128, 2, 6, 64], F32) for i in range(1)] * 2
        tmp = [T2('rtmp%d' % i, [128, 4, 2, 6, 32], F32) for i in range(1)] * 2
        rr = [T2('rr%d' % i, [128, 2, 6, 64], BF16) for i in range(2)]
        st6 = [T2('st6%d' % i, [128, 3, 12], F32) for i in range(2)]
        for tc in range(4):
            pc = ps[6 + tc % 2]
            for k in range(8):
                P.mm(pc[:, :], w_nsa[:, k, 1024 + 128 * g:1024 + 128 * g + 128], self.hT[:, k, tc * 512:(tc + 1) * 512],
                     start=(k == 0), stop=(k == 7))
            P.copy(kvcT[:, tc * 512:(tc + 1) * 512], pc[:, :], eng='act')
        tokf = lambda tt: slice(tt * 128, (tt + 1) * 128)

        def f0(i):
            for u in range(2):
                tt = 2 * i + u
                pa = ps[(i % 2) * 2 + u]
                for k in range(8):
                    P.mm(pa[:, :], self.hT[:, k, tokf(tt)], w_nsa[:, k, g * 512:(g + 1) * 512], start=(k == 0), stop=(k == 7))
                if g == 0:
                    for k in range(8):
                        P.mm(ps[6][:, u * 24:(u + 1) * 24], self.hT[:, k, tokf(tt)], w_nsa[:, k, 1280:1304],
                             start=(k == 0 and u == 0), stop=(k == 7), skip_group_check=True)

        def f1(i):
            for u in range(2):
                tt = 2 * i + u
                pa = ps[(i % 2) * 2 + u]
                R = pa[:, 0:384].rearrange('p (h d) -> p h d', d=64)
                P.act(sq[i % 2][:, u], R, AF.Square)
                P.copy(rc[i % 3][:, u], R, eng='act')
                P.copy(v2[:, tt, :, 0:64], pa[:, 384:512].rearrange('p (a d) -> p a d', d=64), eng='act')
            if g == 0:
                P.copy(gates[:, 2 * i:2 * i + 2, :], ps[6][:, 0:48].rearrange('p (u c) -> p u c', c=24))

        def f2(i):
            P.reduce(st6[i % 2][:, 0, :], sq[i % 2][:].rearrange('p u h d -> p (u h) d'), ALU.add)

        def f3(i):
            P.act(st6[i % 2][:, 1, :], st6[i % 2][:, 0, :], AF.Ln, scale=1.0 / DH, bias=EPS)
            P.act(st6[i % 2][:, 2, :], st6[i % 2][:, 1, :], AF.Exp, scale=-0.5)

        def f4(i):
            i2 = i % 2
            rnv = rn[i2][:].rearrange('p u h d -> p (u h) d')
            P.tt(rnv, rc[i % 3][:].rearrange('p u h d -> p (u h) d'),
                 st6[i2][:, 2, :].unsqueeze(2).to_broadcast([128, 12, 64]), ALU.mult)
            P.tt(rn[i2][:], rn[i2][:], self.gfull[:].unsqueeze(1).to_broadcast([128, 2, 6, 64]), ALU.mult)
            cosb = self.cosT[:, 2 * i:2 * i + 2, :].unsqueeze(2).to_broadcast([128, 2, 6, 32])
            sinb = self.sinT[:, 2 * i:2 * i + 2, :].unsqueeze(2).to_broadcast([128, 2, 6, 32])
            x1 = rn[i2][:, :, :, 0:32]
            x2 = rn[i2][:, :, :, 32:64]
            tm = tmp[i2]
            P.tt(tm[:, 0], x1, cosb, ALU.mult)
            P.tt(tm[:, 1], x2, sinb, ALU.mult)
            P.tt(tm[:, 2], x1, sinb, ALU.mult, eng='pool')
            P.tt(tm[:, 3], x2, cosb, ALU.mult, eng='pool')
            P.tt(rr[i2][:, :, :, 0:32], tm[:, 0], tm[:, 1], ALU.subtract)
            P.tt(rr[i2][:, :, :, 32:64], tm[:, 2], tm[:, 3], ALU.add, eng='pool')

        def f5(i):
            for u in range(2):
                pt_ = ps[4 + u].bitcast(BF16)
                for hh in range(6):
                    P.tr(pt_[0:64, hh * 128:(hh + 1) * 128], rr[i % 2][:, u, hh, :], self.identb[:])

        def f6(i):
            for u in range(2):
                tt = 2 * i + u
                pt_ = ps[4 + u].bitcast(BF16)
                tok = tokf(tt)
                P.copy(qaug[0:64, :, tok], pt_[0:64, 0:512].rearrange('p (h t) -> p h t', t=128), eng='act')
                P.copy(kslc[0:64, tok], pt_[0:64, 512:640], eng='act')
                P.copy(kwin[0:64, tok], pt_[0:64, 640:768], eng='act')

        pipeline(NP, [f0, f1, f2, f3, f4, f5, f6], reverse=True)
        if g == 0:
            P.act(gates[:].rearrange('p a b -> p (a b)'), gates[:].rearrange('p a b -> p (a b)'), AF.Sigmoid)
        hid = T2('hid', [128, 2, 2, 128], BF16)
        kc4 = T2('kc4', [128, 8, 64], F32)
        kst = T2('kst', [128, 4], F32)
        kcr = T2('kcr', [128, 64], BF16)
        ktm = T2('ktm', [128, 4, 32], F32)
        for kv in range(2):
            base = 64 * kv
            pz = ps[5 + kv]
            BR.reset(pz)
            for hc in range(2):
                for l in range(32):
                    P.mm(pz[:, hc * 128:hc * 128 + 127], self.w1kv[base:base + 64, l, hc * 128:(hc + 1) * 128],
                         kvcT[base:base + 64, l:l + 16 * 126 + 1:16], start=BR.start(pz), stop=(l == 31),
                         skip_group_check=True)
            for hc in range(2):
                P.act(hid[:, kv, hc, 0:127], pz[:, hc * 128:hc * 128 + 127], AF.Silu, bias=self.ckv[:, kv, hc:hc + 1])
        p2 = ps[7]
        BR.reset(p2)
        for kv in range(2):
            for hc in range(2):
                P.mm(p2[0:127, kv * 64:(kv + 1) * 64], hid[:, kv, hc, 0:127], self.w2kv[:, kv, hc, :],
                     start=BR.start(p2), stop=(hc == 1), skip_group_check=True)
        P.copy(vcaug[0:127, 0:64], p2[0:127, 64:128], eng='act')
        P.act(kc4[0:127, 0, :], p2[0:127, 0:64], AF.Square)
        P.reduce(kst[0:127, 0:1], kc4[0:127, 0, :], ALU.add)
        P.act(kst[0:127, 1:2], kst[0:127, 0:1], AF.Ln, scale=1.0 / DH, bias=EPS)
        P.act(kst[0:127, 2:3], kst[0:127, 1:2], AF.Exp, scale=-0.5)
        P.stt(kc4[0:127, 1, :], p2[0:127, 0:64], kst[0:127, 2:3], self.gk[0:127, :], ALU.mult, ALU.mult)
        x1 = kc4[0:127, 1, 0:32]
        x2 = kc4[0:127, 1, 32:64]
        cC = self.cosC[0:127, 0, :]
        sC = self.sinC[0:127, 0, :]
        P.tt(ktm[0:127, 0], x1, cC, ALU.mult)
        P.tt(ktm[0:127, 1], x2, sC, ALU.mult)
        P.tt(ktm[0:127, 2], x1, sC, ALU.mult)
        P.tt(ktm[0:127, 3], x2, cC, ALU.mult)
        P.tt(kcr[0:127, 0:32], ktm[0:127, 0], ktm[0:127, 1], ALU.subtract)
        P.tt(kcr[0:127, 32:64], ktm[0:127, 2], ktm[0:127, 3], ALU.add)
        pk = ps[3].bitcast(BF16)
        P.tr(pk[0:64, 0:127], kcr[0:127, :], self.identb[0:127, 0:127])
        P.copy(kcT[:, 0:127], pk[0:64, 0:127])
        if b == 0:
            self.dump('qaug%d' % g, qaug[0:64].rearrange('p h s -> p (h s)'))
            self.dump('kslc%d' % g, kslc[0:64, :])
            self.dump('kwin%d' % g, kwin[:, :])
            self.dump('kcT%d' % g, kcT[:, :])
            self.dump('vc%d' % g, vcaug[:, 0:64])
        self.sync_phase()
    self.nsa_attention(b, g, st, qaug, kwin, kcT, gates)


KB.phase2_nsa = _kb_phase2_nsa
KB.nsa_group = _kb_nsa_group


def _kb_nsa_attention(self, b, g, st, qaug, kwin, kcT, gates):
    nc, P, ps = self.nc, self.P, self.ps
    BR = self.br
    self.emit_conv(-(-len(self.conv_list) // (2 * self.NSEQ)))
    self.emit_fill(-(-(self.NSLOT // 128) // (2 * self.NSEQ)))
    kslc, v2, vcaug = self.kslc, self.v2, self.vcaug
    with ExitStack() as s3:
        T = lambda n, s, dt: self.T(s3, n, s, dt)
        ptile = [T('ptile%d' % i, [128, 512], BF16) for i in range(4)]
        oacc = T('oacc', [128, NT, 4, 64], F32)
        impacc = T('impacc', [128, NT, 32], F32)
        rz = [T('rz%d' % i, [128, 2, 4], F32) for i in range(3)]
        otmp = [T('otmp%d' % i, [128, 4, 64], F32) for i in range(2)]
        itmp = [T('itmp%d' % i, [128, 4, 32], F32) for i in range(2)]
        scw = [T('scw%d' % i, [128, 4, 32], F32) for i in range(2)]
        slw = [T('slw%d' % i, [128, 4, 32], F32) for i in range(2)]
        m8 = [T('m8%d' % i, [128, 4, 8], F32) for i in range(2)]
        biasb = [T('biasb%d' % i, [128, 4, 32], BF16) for i in range(2)]
        ob = [T('ob%d' % i, [128, 256], BF16) for i in range(2)]
        sbank = [ps[0], ps[1], ps[2]]
        pvbank = [ps[3], ps[4]]
        misc = ps[5]
        otb = [ps[6], ps[7]]
        cnt = {'s': 0, 'pv': 0, 'fin': 0}

        def finalize(pvb, hl, br, qc, ncol, first, want_imp, first_imp):
            h = 4 * g + hl
            k_ = cnt['fin']
            cnt['fin'] += 1
            r = rz[k_ % 3]
            pv3 = pvb[:, 0:4 * ncol].rearrange('p (q c) -> p q c', c=ncol)
            P.ts(r[:, 0, :], pv3[:, :, 64], 1e-30, None, ALU.max)
            P.recip(r[:, 0, :], r[:, 0, :])
            P.tt(r[:, 1, :], r[:, 0, :], gates[:, qc * 4:(qc + 1) * 4, br * 8 + h], ALU.mult)
            tgt = oacc[:, qc * 4:(qc + 1) * 4, hl, :]
            sb_ = r[:, 1, :].unsqueeze(2).to_broadcast([128, 4, 64])
            if first:
                P.tt(tgt, pv3[:, :, 0:64], sb_, ALU.mult)
            else:
                ot = otmp[k_ % 2]
                P.tt(ot[:], pv3[:, :, 0:64], sb_, ALU.mult)
                P.tt(tgt, tgt, ot[:], ALU.add)
            if want_imp:
                itg = impacc[:, qc * 4:(qc + 1) * 4, :]
                rb_ = r[:, 0, :].unsqueeze(2).to_broadcast([128, 4, 32])
                if first_imp:
                    P.tt(itg, pv3[:, :, 65:97], rb_, ALU.mult)
                else:
                    it = itmp[k_ % 2]
                    P.tt(it[:], pv3[:, :, 65:97], rb_, ALU.mult)
                    P.tt(itg, itg, it[:], ALU.add)

        def selection(qc):
            sc, sl, m_, bb = scw[qc % 2], slw[qc % 2], m8[qc % 2], biasb[qc % 2]
            tq = slice(qc * 4, (qc + 1) * 4)
            P.tt(sc[:], impacc[:, tq, :], self.cand[:, tq, :], ALU.mult)
            P.tt(sc[:], sc[:], self.candm1[:, tq, :], ALU.add)
            for qt in range(4):
                P.max8(m_[:, qt, :], sc[:, qt, :])
            P.tt(sl[:], sc[:], m_[:, :, 4].unsqueeze(2).to_broadcast([128, 4, 32]), ALU.is_ge)
            P.tt(sl[:], sl[:], self.forced[:, tq, :], ALU.max)
            P.ts(bb[:], sl[:], 1.0, BIG, ALU.subtract, ALU.mult)
            mb = misc.bitcast(BF16)
            for qt in range(4):
                P.tr(mb[0:32, qt * 128:(qt + 1) * 128], bb[:, qt, :], self.identb[:])
            P.copy(qaug[64:96, :, qc * 512:(qc + 1) * 512],
                   mb[0:32, 0:512].unsqueeze(1).to_broadcast([32, 4, 512]), eng='act')
            if b == 0:
                for qt in range(4):
                    self.dump('sel%d_%d' % (g, qc * 4 + qt), sl[:, qt, :])

        astate = {}

        def c0(i):
            qc, hl = i // 4, i % 4
            k_ = cnt['s']
            cnt['s'] += 1
            astate[i] = (sbank[k_ % 3], ptile[k_ % 3])
            P.mm(astate[i][0][0:127, :], kcT[0:64, 0:127], qaug[0:64, hl, qc * 512:(qc + 1) * 512], start=True, stop=True)

        def c1(i):
            sb, pt = astate[i]
            P.act(pt[0:127, :], sb[0:127, :], AF.Exp)

        def c2(i):
            qc = i // 4
            sb, pt = astate[i]
            P.tt(pt[0:127, :], pt[0:127, :], self.cmpvalid[0:127, qc * 512:(qc + 1) * 512], ALU.mult)

        def c3(i):
            sb, pt = astate[i]
            pvb = pvbank[cnt['pv'] % 2]
            cnt['pv'] += 1
            BR.reset(pvb)
            for qt in range(4):
                P.mm(pvb[:, qt * 97:(qt + 1) * 97], pt[0:127, qt * 128:(qt + 1) * 128], vcaug[0:127, :],
                     start=BR.start(pvb), stop=True, skip_group_check=True)
            astate[i] = pvb

        def c4(i):
            qc, hl = i // 4, i % 4
            finalize(astate.pop(i), hl, 0, qc, 97, True, True, hl == 0)
            if hl == 3:
                selection(qc)

        pipeline(16, [c0, c1, c2, c3, c4])

        steps = []
        for qc in range(4):
            for hl in range(4):
                kbs = list(range(max(0, 4 * qc - 4), 4 * qc + 4))
                for kb in kbs:
                    if kb < 4 * qc:
                        j = kb - (4 * qc - 4)
                        c0_, c1_, mask = 0, 128 * (j + 1), ('anti', 128 * j)
                    else:
                        j = kb - 4 * qc
                        c0_, c1_, mask = 128 * j, 512, ('tri', 128 * j)
                    steps.append(dict(br=2, hl=hl, kb=kb, c0=c0_, c1=c1_, mask=mask, qc=qc, firsth=(kb == kbs[0]), last=(kb == kbs[-1])))
            for hl in range(4):
                kbs = list(range(0, 4 * qc + 4))
                for kb in kbs:
                    if kb < 4 * qc:
                        c0_, c1_, mask = 0, 512, None
                    else:
                        j = kb - 4 * qc
                        c0_, c1_, mask = 128 * j, 512, ('tri', 128 * j)
                    steps.append(dict(br=1, hl=hl, kb=kb, c0=c0_, c1=c1_, mask=mask, qc=qc, firsth=(kb == kbs[0]), last=(kb == kbs[-1])))
        n = len(steps)
        state = {}

        def qk(i):
            s_ = steps[i]
            k_ = cnt['s']
            cnt['s'] += 1
            sb = sbank[k_ % 3]
            pt = ptile[k_ % 4]
            hl, kb, c0_, c1_, br = s_['hl'], s_['kb'], s_['c0'], s_['c1'], s_['br']
            q0 = s_['qc'] * 512
            if br == 1:
                lhsT = kslc[0:96, kb * 128:(kb + 1) * 128]
                rhs = qaug[0:96, hl, q0 + c0_:q0 + c1_]
            else:
                lhsT = kwin[0:64, kb * 128:(kb + 1) * 128]
                rhs = qaug[0:64, hl, q0 + c0_:q0 + c1_]
            P.mm(sb[:, c0_:c1_], lhsT, rhs, start=True, stop=True)
            P.act(pt[:, c0_:c1_], sb[:, c0_:c1_], AF.Exp)
            if s_['mask'] is not None:
                kind, col = s_['mask']
                mk = self.trib if kind == 'tri' else self.antib
                P.tt(pt[:, col:col + 128], pt[:, col:col + 128], mk[:], ALU.mult)
            state[i] = pt

        def pv(i):
            s_ = steps[i]
            pt = state.pop(i)
            hl, kb, c0_, c1_, br = s_['hl'], s_['kb'], s_['c0'], s_['c1'], s_['br']
            if s_['firsth']:
                state['pvb'] = pvbank[cnt['pv'] % 2]
                cnt['pv'] += 1
                BR.reset(state['pvb'])
            pvb = state['pvb']
            for qt in range(c0_ // 128, c1_ // 128):
                P.mm(pvb[:, qt * 65:(qt + 1) * 65], pt[:, qt * 128:(qt + 1) * 128], v2[:, kb, br - 1, :],
                     start=BR.start(pvb), stop=True, skip_group_check=True)
            if s_['last']:
                finalize(pvb, hl, br, s_['qc'], 65, False, False, False)

        AHEAD = 3
        for i in range(min(AHEAD, n)):
            qk(i)
        for i in range(n):
            if i + AHEAD < n:
                qk(i + AHEAD)
            pv(i)

        def o0(tg):
            P.copy(ob[tg % 2][:], oacc[:, tg].rearrange('p h d -> p (h d)'), eng='act')

        def o1(tg):
            pb = otb[tg % 2].bitcast(BF16)
            for pr in range(2):
                P.tr(pb[:, pr * 128:(pr + 1) * 128], ob[tg % 2][:, pr * 128:(pr + 1) * 128], self.identb[:])

        def o2(tg):
            pb = otb[tg % 2].bitcast(BF16)
            P.copy(self.o_aT[:, 2 * g:2 * g + 2, tg * 128:(tg + 1) * 128],
                   pb[:, 0:256].rearrange('p (c t) -> p c t', t=128), eng='dve')

        pipeline(NT, [o0, o1, o2])


KB.nsa_attention = _kb_nsa_attention


def _kb_prep_weights(self, st):
    P = self.P
    wv = self.w_in[0].rearrange('(k p) c -> p k c', p=128)
    T = lambda n, s, dt: self.T(st, n, s, dt)
    stg = [T('pw_stage%d' % i, [128, 8 * 1304], BF16) for i in range(2)]
    w_nsa = stg[0][:, 0:8 * 1304].rearrange('p (k c) -> p k c', c=1304)
    for g in range(2):
        o = g * 512
        for (dst, src, n) in ((0, 256 * g, 256), (256, 768 + 64 * g, 64), (320, 1024 + 64 * g, 64),
                              (384, 896 + 64 * g, 64), (448, 1152 + 64 * g, 64)):
            P.dma(w_nsa[:, :, o + dst:o + dst + n], wv[:, :, src:src + n], q='pool')
        P.dma(w_nsa[:, :, 1024 + 128 * g:1024 + 128 * g + 64], wv[:, :, 512 + 64 * g:512 + 64 * g + 64], q='pool')
        P.dma(w_nsa[:, :, 1024 + 128 * g + 64:1024 + 128 * g + 128], wv[:, :, 640 + 64 * g:640 + 64 * g + 64], q='pool')
    P.dma(w_nsa[:, :, 1280:1304], wv[:, :, 1280:1304], q='pool')
    P.dma(self.w_nsa_d, stg[0][:, 0:8 * 1304])
    w_dil = stg[1][:, 0:8 * 1152].rearrange('p (k c) -> p k c', c=1152)
    for gi in range(3):
        for pi, base in enumerate((1304, 1688, 2072)):
            P.dma(w_dil[:, :, gi * 384 + pi * 128:gi * 384 + (pi + 1) * 128],
                  wv[:, :, base + 128 * gi:base + 128 * gi + 128], q='pool')
    P.dma(self.w_dil_d, stg[1][:, 0:8 * 1152])
    gm_d = self.w_gm_d.rearrange('p (k c) -> p k c', c=2048)
    for hf in range(2):
        buf = stg[hf][:, 0:8 * 1024].rearrange('p (k c) -> p k c', c=1024)
        for q2 in range(2):
            c0 = 2456 + hf * 1024 + q2 * 512
            P.dma(buf[:, :, q2 * 512:(q2 + 1) * 512], wv[:, :, c0:c0 + 512], q='pool')
        P.dma(gm_d[:, :, hf * 1024:(hf + 1) * 1024], buf)
    bufa = stg[0][:, 0:4 * D].rearrange('p (k c) -> p k c', c=D)
    bufb = stg[0][:, 4 * D:7 * D].rearrange('p (k c) -> p k c', c=D)
    for c in range(4):
        P.dma(bufa[:, c, :], self.w_up_a[0, c * 128:(c + 1) * 128, :], q='pool')
    for c in range(3):
        P.dma(bufb[:, c, :], self.w_up_b[0, c * 128:(c + 1) * 128, :], q='pool')
    P.dma(self.w_upa_d, stg[0][:, 0:4 * D])
    P.dma(self.w_upb_d, stg[0][:, 4 * D:7 * D])
    bufo = stg[1][:, 0:8 * D].rearrange('p (k c) -> p k c', c=D)
    for k in range(8):
        P.dma(bufo[:, k, :], self.w_out[0, k * 128:(k + 1) * 128, :], q='pool')
    P.dma(self.w_o_d, stg[1][:, 0:8 * D])


KB.prep_weights = _kb_prep_weights


def _kb_build(self, upto=99):
    nc, P = self.nc, self.P
    self.declare()
    self.br = BankRound()
    with ExitStack() as top:
        self.top = top
        T = lambda n, s, dt: self.T(top, n, s, dt)
        self.ps = [top.enter_context(nc.psum_tensor("ps%d" % i, [128, 512], F32)) for i in range(8)]
        self.identf = T('identf', [128, 128], F32)
        self.identb = T('identb', [128, 128], BF16)
        self.invf = T('invf', [128, 32], F32)
        self.trib = T('trib', [128, 128], BF16)
        self.antib = T('antib', [128, 128], BF16)
        self.triantib = T('triantib', [128, 256], BF16)
        self.cmpvalid = T('cmpvalid', [128, S], BF16)
        self.cand = T('cand', [128, NT, 32], BF16)
        self.candm1 = T('candm1', [128, NT, 32], BF16)
        self.forced = T('forced', [128, NT, 32], BF16)
        self.lstrict = T('lstrict', [128, 128], BF16)
        self.onesb = T('onesb', [128, 128], BF16)
        P.dma(self.identf[:], self.cd['identf'])
        P.dma(self.identb[:], self.cd['identf'], q='pool')
        P.dma(self.invf[:], self.cd['invf'])
        P.dma(self.trib[:], self.cd['tri'], q='pool')
        P.dma(self.antib[:], self.cd['anti'], q='pool')
        P.dma(self.triantib[:], self.cd['trianti'], q='pool')
        P.dma(self.cmpvalid[:], self.cd['cmpvalid'], q='pool')
        P.dma(self.cand[:], self.cd['cand'], q='pool')
        P.dma(self.candm1[:], self.cd['candm1'], q='pool')
        P.dma(self.forced[:], self.cd['forced'], q='pool')
        P.dma(self.lstrict[:], self.cd['lstrict'], q='pool')
        P.dma(self.onesb[:], self.cd['ones'], q='pool')
        self.modT1 = T('modT1', [128, 16, 4], F32)
        if upto >= 4:
            self.setup_moe_consts()
        self.phase0(prep=(upto >= 2))
        with ExitStack() as mix:
            self.top = mix
            Tm = lambda n, s, dt: self.T(mix, n, s, dt)
            self.hT = Tm('hT', [128, 8, S], BF16)
            self.o_aT = Tm('o_aT', [128, 4, S], BF16)
            self.o_bT = Tm('o_bT', [128, 3, S], BF16)
            if upto >= 2:
                self.setup_nsa_consts()
            for b in range(self.NSEQ):
                if upto >= 1:
                    self.phase1(b)
                if upto >= 2:
                    with ExitStack() as st_seq:
                        self.seq_prologue(st_seq, b)
                        self.phase2_nsa(b, st_seq)
                        if b == 0:
                            for c in range(4):
                                self.dump('oaT%d' % c, self.o_aT[:, c, :])
                        if upto >= 3:
                            self.phase2_dil(b, st_seq)
                            if b == 0:
                                for c in range(3):
                                    self.dump('obT%d' % c, self.o_bT[:, c, :])
                        if upto >= 4:
                            self.phase3(b, st_seq)
                        self.sync_phase()
            self.sync_phase()
        self.top = top
        if upto >= 5:
            self.phase4()
        P.barrier()
        P.flush()
    return nc


KB.build = _kb_build


DIL_D = (1, 4, 16)


def _kb_phase2_dil(self, b, st_seq):
    nc, P, ps = self.nc, self.P, self.ps
    BR = self.br
    wv = self.w_in[0].rearrange('(k p) c -> p k c', p=128)
    with ExitStack() as st:
        T = lambda n, s, dt: self.T(st, n, s, dt)
        w_dil = T('w_dil', [128, 8, 1152], BF16)
        P.dma(w_dil[:].rearrange('p k c -> p (k c)'), self.w_dil_d, q='act')
        us = T('us', [128, 3, S], BF16)
        ztot = T('ztot', [128, S], F32)
        gfb = T('gfb', [128, 4, 64], F32)
        for hh in range(2):
            P.copy(gfb[:, hh, :], self.gqb[:])
            P.copy(gfb[:, 2 + hh, :], self.gkb[:])
        for gi in range(3):
            d = DIL_D[gi]
            with ExitStack() as sg:
                T2 = lambda n, s, dt: self.T(sg, n, s, dt)
                qbT = T2('qbT', [64, 2, S], BF16)
                kbT = T2('kbT', [64, 2, S], BF16)
                vb = T2('vb', [128, 16, 128], BF16)
                sq = [T2('dsq%d' % i, [128, 2, 4, 64], F32) for i in range(1)] * 2
                rc = [T2('drc%d' % i, [128, 2, 4, 64], F32) for i in range(3)]
                rn = T2('drn', [128, 2, 4, 64], F32)
                tmp = T2('dtmp', [128, 4, 2, 4, 32], F32)
                rr = [T2('drr%d' % i, [128, 2, 4, 64], BF16) for i in range(2)]
                st4 = [T2('dst%d' % i, [128, 3, 8], F32) for i in range(2)]
                ptile = [T2('dpt%d' % i, [128, 512], BF16) for i in range(3)]
                tokf = lambda tt: slice(tt * 128, (tt + 1) * 128)

                def d0(i):
                    for u in range(2):
                        tt = 2 * i + u
                        pa = ps[(i % 2) * 2 + u]
                        for k in range(8):
                            P.mm(pa[:, 0:256], self.hT[:, k, tokf(tt)], w_dil[:, k, gi * 384:gi * 384 + 256], start=(k == 0), stop=(k == 7))

                def d1(i):
                    for u in range(2):
                        R = ps[(i % 2) * 2 + u][:, 0:256].rearrange('p (h d) -> p h d', d=64)
                        P.act(sq[i % 2][:, u], R, AF.Square)
                        P.copy(rc[i % 3][:, u], R, eng='act')

                def d2(i):
                    P.reduce(st4[i % 2][:, 0, :], sq[i % 2][:].rearrange('p u h d -> p (u h) d'), ALU.add)

                def d3(i):
                    P.act(st4[i % 2][:, 1, :], st4[i % 2][:, 0, :], AF.Ln, scale=1.0 / DH, bias=EPS)
                    P.act(st4[i % 2][:, 2, :], st4[i % 2][:, 1, :], AF.Exp, scale=-0.5)

                def d4(i):
                    P.tt(rn[:].rearrange('p u h d -> p (u h) d'), rc[i % 3][:].rearrange('p u h d -> p (u h) d'),
                         st4[i % 2][:, 2, :].unsqueeze(2).to_broadcast([128, 8, 64]), ALU.mult)
                    P.tt(rn[:], rn[:], gfb[:].unsqueeze(1).to_broadcast([128, 2, 4, 64]), ALU.mult)
                    cosb = self.cosT[:, 2 * i:2 * i + 2, :].unsqueeze(2).to_broadcast([128, 2, 4, 32])
                    sinb = self.sinT[:, 2 * i:2 * i + 2, :].unsqueeze(2).to_broadcast([128, 2, 4, 32])
                    x1 = rn[:, :, :, 0:32]
                    x2 = rn[:, :, :, 32:64]
                    P.tt(tmp[:, 0], x1, cosb, ALU.mult)
                    P.tt(tmp[:, 1], x2, sinb, ALU.mult)
                    P.tt(tmp[:, 2], x1, sinb, ALU.mult, eng='pool')
                    P.tt(tmp[:, 3], x2, cosb, ALU.mult, eng='pool')
                    P.tt(rr[i % 2][:, :, :, 0:32], tmp[:, 0], tmp[:, 1], ALU.subtract)
                    P.tt(rr[i % 2][:, :, :, 32:64], tmp[:, 2], tmp[:, 3], ALU.add, eng='pool')

                def d5(i):
                    pt_ = ps[4].bitcast(BF16)
                    for u in range(2):
                        for hh in range(4):
                            P.tr(pt_[0:64, (u * 4 + hh) * 128:(u * 4 + hh + 1) * 128], rr[i % 2][:, u, hh, :], self.identb[:])

                def d6(i):
                    pt_ = ps[4].bitcast(BF16)
                    for u in range(2):
                        tt = 2 * i + u
                        P.copy(qbT[:, :, tokf(tt)], pt_[0:64, u * 512:u * 512 + 256].rearrange('p (h t) -> p h t', t=128), eng='act')
                        P.copy(kbT[:, :, tokf(tt)], pt_[0:64, u * 512 + 256:u * 512 + 512].rearrange('p (h t) -> p h t', t=128), eng='act')

                pipeline(NT // 2, [d0, d1, d2, d3, d4, d5, d6], reverse=True)
                nkb = 16 // d
                for r in range(d):
                    for kbs in range(nkb):
                        bi = r * nkb + kbs
                        pvp = ps[4 + bi % 2]
                        s0 = 128 * kbs * d + r
                        for k in range(8):
                            P.mm(pvp[:, 0:128], self.hT[:, k, s0:s0 + 127 * d + 1:d], w_dil[:, k, gi * 384 + 256:gi * 384 + 384],
                                 start=(k == 0), stop=(k == 7))
                        P.copy(vb[:, bi, :], pvp[:, 0:128], eng='act')
                rounds = []
                if d == 1:
                    for Rn_ in range(4):
                        steps = []
                        for kbs in range(max(0, 4 * Rn_ - 1), 4 * Rn_ + 4):
                            if kbs < 4 * Rn_:
                                steps.append(dict(r=0, kbs=kbs, q0=4 * Rn_, nq=1, slot=0, mask='anti'))
                            elif kbs < 4 * Rn_ + 3:
                                steps.append(dict(r=0, kbs=kbs, q0=kbs, nq=2, slot=kbs - 4 * Rn_, mask='trianti'))
                            else:
                                steps.append(dict(r=0, kbs=kbs, q0=kbs, nq=1, slot=3, mask='tri'))
                        rounds.append(dict(steps=steps, out=('contig', 512 * Rn_)))
                elif d == 4:
                    for r in range(4):
                        steps = []
                        for kbs in range(4):
                            if kbs < 3:
                                steps.append(dict(r=r, kbs=kbs, q0=kbs, nq=2, slot=kbs, mask='trianti'))
                            else:
                                steps.append(dict(r=r, kbs=kbs, q0=kbs, nq=1, slot=3, mask='tri'))
                        rounds.append(dict(steps=steps, out=('strided', r)))
                else:
                    for Rr in range(4):
                        steps = [dict(r=4 * Rr + i, kbs=0, q0=0, nq=1, slot=i, mask='tri') for i in range(4)]
                        rounds.append(dict(steps=steps, out=('res16', 4 * Rr)))
                items = []
                for rd_i, rd in enumerate(rounds):
                    for si, s_ in enumerate(rd['steps']):
                        for j in range(2):
                            items.append((rd_i, s_, j, si == len(rd['steps']) - 1 and j == 1))
                dstate = {}
                dcnt = {'s': 0}
                dstarted = {}

                def dqk(ii):
                    rd_i, s_, j, _ = items[ii]
                    r, kbs, q0, nq = s_['r'], s_['kbs'], s_['q0'], s_['nq']
                    k0 = 128 * kbs * d + r
                    qs0 = 128 * q0 * d + r
                    ncol = 128 * nq
                    sb = ps[dcnt['s'] % 4]
                    pt = ptile[dcnt['s'] % 3]
                    dcnt['s'] += 1
                    P.mm(sb[:, 0:ncol], kbT[0:64, j, k0:k0 + 127 * d + 1:d],
                         qbT[0:64, j, qs0:qs0 + (ncol - 1) * d + 1:d], start=True, stop=True)
                    P.act(pt[:, 0:ncol], sb[:, 0:ncol], AF.Exp)
                    mk = {'tri': self.trib[:], 'anti': self.antib[:], 'trianti': self.triantib[:]}[s_['mask']]
                    P.tt(pt[:, 0:ncol], pt[:, 0:ncol], mk, ALU.mult)
                    dstate[ii] = pt

                def dpv(ii):
                    rd_i, s_, j, last = items[ii]
                    rd = rounds[rd_i]
                    pt = dstate.pop(ii)
                    r, kbs, nq, slot = s_['r'], s_['kbs'], s_['nq'], s_['slot']
                    bi = r * nkb + kbs
                    ncol = 128 * nq
                    c0 = slot * 128
                    pu = ps[4 + (rd_i % 2) * 2]
                    pz = ps[5 + (rd_i % 2) * 2]
                    first = not dstarted.get((rd_i, j), False)
                    dstarted[(rd_i, j)] = True
                    P.mm(pu[64 * j:64 * j + 64, c0:c0 + ncol], vb[:, bi, 64 * j:64 * j + 64], pt[:, 0:ncol],
                         start=first, stop=True, skip_group_check=True)
                    P.mm(pz[64 * j:64 * j + 64, c0:c0 + ncol], self.onesb[:, 0:64], pt[:, 0:ncol],
                         start=first, stop=True, skip_group_check=True)
                    if last:
                        kind, o0 = rd['out']
                        if kind == 'contig':
                            uo = us[:, gi, o0:o0 + 512]
                            zo = ztot[:, o0:o0 + 512]
                            pui, pzi = pu[:, :], pz[:, :]
                        elif kind == 'strided':
                            uo = us[:, gi, o0:o0 + 511 * 4 + 1:4]
                            zo = ztot[:, o0:o0 + 511 * 4 + 1:4]
                            pui, pzi = pu[:, :], pz[:, :]
                        else:
                            uo = us[:, gi, :].rearrange('p (k r) -> p r k', r=16)[:, o0:o0 + 4, :]
                            zo = ztot[:, :].rearrange('p (k r) -> p r k', r=16)[:, o0:o0 + 4, :]
                            pui = pu[:, :].rearrange('p (s k) -> p s k', k=128)
                            pzi = pz[:, :].rearrange('p (s k) -> p s k', k=128)
                        P.copy(uo, pui, eng='act')
                        if gi == 0:
                            P.copy(zo, pzi)
                        else:
                            P.tt(zo, pzi, zo, ALU.add)

                AH = 2
                nit = len(items)
                for ii in range(min(AH, nit)):
                    dqk(ii)
                for ii in range(nit):
                    if ii + AH < nit:
                        dqk(ii + AH)
                    dpv(ii)
                self.sync_phase()
        P.act(ztot[:], ztot[:], AF.Ln)
        P.act(ztot[:], ztot[:], AF.Exp, scale=-1.0)
        for gi in range(3):
            P.tt(self.o_bT[:, gi, :], us[:, gi, :], ztot[:], ALU.mult)
        self.sync_phase()


KB.phase2_dil = _kb_phase2_dil


def _kb_setup_moe_consts(self):
    P, top = self.P, self.top
    T = lambda n, s, dt: self.T(top, n, s, dt)
    NTT = self.NTT
    self.wr = T('wr', [128, 8, 36], F32)
    with self.nc.allow_non_contiguous_dma(reason="tiny router weight rows"):
        P.dma(self.wr[:, :, 0:4], self.w_group[0].rearrange('(k p) g -> p k g', p=128))
        for gg in range(4):
            P.dma(self.wr[:, :, 4 + 8 * gg:12 + 8 * gg], self.w_router[0, gg].rearrange('(k p) e -> p k e', p=128))
    self.brow = T('brow', [128, 36], F32)
    P.dma(self.brow[:, 0:4], self.b_group.to_broadcast([128, 4]))
    P.dma(self.brow[:, 4:36], self.b_router[0:1].rearrange('o g e -> o (g e)').to_broadcast([128, 32]))
    self.EH = [T('EH%d' % k, [128, NTT, 32], BF16) for k in range(2)]
    self.rank = T('rank', [128, NTT, 2], F32)
    self.wts = T('wts', [128, NTT, 2], F32)
    self.lgall = [T('lgall%d' % i, [128, NT, 36], F32) for i in range(2)]
    self.rt_tiles = dict(rt=T('rt', [128, NT, 16], F32), gw=T('gw', [128, 6, NT], F32), sel4=T('sel4', [128, NT, 4, 8], F32),
                         sel=T('selr', [128, NT, 8], F32), m8a=T('m8a', [128, NT, 8], F32), oh=T('ohr', [128, 2, NT, 8], F32),
                         eh12=T('eh12a', [128, NT, 32], BF16), pre=T('pre', [128, NT, 32], F32), big=T('bigr', [128, NT, 32], F32))
    self.pending = []
    self.zrow = T('zrow', [128, D], BF16)
    P.memset(self.zrow[:], 0.0)
    self.fill_pos = 0
    self.carry = T('carry', [128, 32], F32)
    P.memset(self.carry[:], 0.0)


KB.setup_moe_consts = _kb_setup_moe_consts


def _kb_phase3(self, b, st_seq):
    nc, P, ps = self.nc, self.P, self.ps
    with ExitStack() as st:
        T = lambda n, s, dt: self.T(st, n, s, dt)
        w_gm = T('w_gm', [128, 8, 2048], BF16)
        w_upa = T('w_upa', [128, 4, D], BF16)
        w_upb = T('w_upb', [128, 3, D], BF16)
        P.dma(w_upa[:].rearrange('p k c -> p (k c)'), self.w_upa_d, q='act')
        P.dma(w_upb[:].rearrange('p k c -> p (k c)'), self.w_upb_d, q='act')
        P.dma(w_gm[:].rearrange('p k c -> p (k c)'), self.w_gm_d)
        gm = [T('gm%d' % i, [128, 2, 512], F32) for i in range(2)]
        ybf = [T('ybf%d' % i, [128, 512], BF16) for i in range(2)]
        tokf = lambda tt: slice(tt * 128, (tt + 1) * 128)

        def a0(i):
            tt, hf = i // 2, i % 2
            pa_, pb_ = (ps[0], ps[1]) if i % 2 == 0 else (ps[5], ps[6])
            cs = slice(hf * 512, (hf + 1) * 512)
            for c in range(4):
                P.mm(pa_[:, :], self.o_aT[:, c, tokf(tt)], w_upa[:, c, cs], start=(c == 0), stop=(c == 3))
            for c in range(3):
                P.mm(pb_[:, :], self.o_bT[:, c, tokf(tt)], w_upb[:, c, cs], start=(c == 0), stop=(c == 2))
            for q2 in range(2):
                cg = slice(q2 * 1024 + hf * 512, q2 * 1024 + (hf + 1) * 512)
                for k in range(8):
                    P.mm(ps[2 + q2][:, :], self.hT[:, k, tokf(tt)], w_gm[:, k, cg], start=(k == 0), stop=(k == 7))

        def a1(i):
            for q2 in range(2):
                P.act(gm[i % 2][:, q2, :], ps[2 + q2][:, :], AF.Sigmoid)

        def a2(i):
            pa_, pb_ = (ps[0], ps[1]) if i % 2 == 0 else (ps[5], ps[6])
            g_ = gm[i % 2]
            P.tt(g_[:, 0, :], g_[:, 0, :], pa_[:, :], ALU.mult)
            P.tt(g_[:, 1, :], g_[:, 1, :], pb_[:, :], ALU.mult)
            P.tt(ybf[i % 2][:], g_[:, 0, :], g_[:, 1, :], ALU.add)

        def a3(i):
            pb = ps[4].bitcast(BF16)
            for k in range(4):
                P.tr(pb[:, k * 128:(k + 1) * 128], ybf[i % 2][:, k * 128:(k + 1) * 128], self.identb[:])

        def a4(i):
            tt, hf = i // 2, i % 2
            pb = ps[4].bitcast(BF16)
            P.copy(self.hT[:, 4 * hf:4 * hf + 4, tokf(tt)], pb[:, 0:512].rearrange('p (k t) -> p k t', t=128), eng='act')

        pipeline(2 * NT, [a0, a1, a2, a3, a4], reverse=True)
        self.sync_phase()
    with ExitStack() as st:
        T = lambda n, s, dt: self.T(st, n, s, dt)
        w_o = T('w_o', [128, 8, D], BF16)
        P.dma(w_o[:].rearrange('p k c -> p (k c)'), self.w_o_d)
        gt1 = T('gt1', [128, D], F32)
        A2 = T('A2', [128, D], F32)
        sh2 = T('sh2', [128, D], F32)
        P.dma(gt1[:], self.mod_d[b:b + 1, 2 * D:3 * D].to_broadcast([128, D]), q='act')
        P.dma(sh2[:], self.mod_d[b:b + 1, 3 * D:4 * D].to_broadcast([128, D]), q='act')
        P.dma(A2[:], self.mod_d[b:b + 1, 4 * D:5 * D].to_broadcast([128, D]), q='act')
        xt = [T('x3t%d' % i, [128, D], F32) for i in range(2)]
        x1 = [T('x1t%d' % i, [128, D], F32) for i in range(2)]
        h2 = [T('h2t%d' % i, [128, D], F32) for i in range(2)]
        junk = T('p3junk', [128, D], F32)
        h2T = [T('h2T%d' % i, [128, 8, 128], F32) for i in range(2)]
        sm = [T('sm%d' % i, [128, 4], F32) for i in range(2)]
        lgall = self.lgall[b % 2]
        tokf = lambda tt: slice(tt * 128, (tt + 1) * 128)

        def b0(tt):
            r0 = b * S + tt * 128
            P.dma(xt[tt % 2][:], self.x[r0:r0 + 128, :], q=('sp' if tt % 2 == 0 else 'act'))

        def b1(tt):
            for hf in range(2):
                for k in range(8):
                    P.mm(ps[hf][:, :], self.hT[:, k, tokf(tt)], w_o[:, k, hf * 512:(hf + 1) * 512], start=(k == 0), stop=(k == 7))

        def b2(tt):
            i2 = tt % 2
            r0 = b * S + tt * 128
            for hf in range(2):
                sl = slice(hf * 512, (hf + 1) * 512)
                P.tt(x1[i2][:, sl], ps[hf][:, :], gt1[:, sl], ALU.mult)
                P.tt(x1[i2][:, sl], x1[i2][:, sl], xt[i2][:, sl], ALU.add)
            P.dma(self.x1_d[r0:r0 + 128, :], x1[i2][:])

        def b3(tt):
            s_ = sm[tt % 2]
            P.act(junk[:], x1[tt % 2][:], AF.Square, accum_out=s_[:, 0:1])
            P.act(s_[:, 1:2], s_[:, 0:1], AF.Ln, scale=1.0 / D, bias=EPS)
            P.act(s_[:, 2:3], s_[:, 1:2], AF.Exp, scale=-0.5)

        def b4(tt):
            i2 = tt % 2
            r0 = b * S + tt * 128
            P.stt(h2[i2][:], x1[i2][:], sm[i2][:, 2:3], A2[:], ALU.mult, ALU.mult)
            P.tt(h2[i2][:], h2[i2][:], sh2[:], ALU.add)
            P.dma(self.h2_d[r0:r0 + 128, :], h2[i2][:], q='pool')

        def b5(tt):
            for k in range(8):
                pb = ps[2 + k // 4]
                P.tr(pb[:, (k % 4) * 128:(k % 4 + 1) * 128], h2[tt % 2][:, k * 128:(k + 1) * 128], self.identf[:])

        def b6(tt):
            for hf in range(2):
                P.copy(h2T[tt % 2][:, hf * 4:(hf + 1) * 4, :], ps[2 + hf][:, :].rearrange('p (k t) -> p k t', t=128),
                       eng=('act' if hf == 0 else 'dve'))

        def b7(tt):
            for k in range(8):
                P.mm(ps[4][:, 0:36], h2T[tt % 2][:, k, :], self.wr[:, k, :], start=(k == 0), stop=(k == 7))

        def b8(tt):
            P.tt(lgall[:, tt, :], ps[4][:, 0:36], self.brow[:], ALU.add)

        pipeline(NT, [b0, b1, b2, b3, b4, b5, b6, b7, b8], reverse=True)
        T0 = b * NT
        R_ = self.rt_tiles
        rt, gw, sel4, sel, m8a, oh, eh12, pre, big = (R_['rt'], R_['gw'], R_['sel4'], R_['sel'], R_['m8a'], R_['oh'],
                                                      R_['eh12'], R_['pre'], R_['big'])
        lgs = self.lgall[b % 2]
        th = []
        A = th.append
        G4 = lgs[:, :, 0:4]
        A(lambda: P.reduce(gw[:, 0, :], G4, ALU.max))
        A(lambda: P.tt(rt[:, :, 0:4], G4, gw[:, 0, :].unsqueeze(2).to_broadcast([128, NT, 4]), ALU.subtract))
        A(lambda: P.act(rt[:, :, 4:8], rt[:, :, 0:4], AF.Exp))
        A(lambda: P.reduce(gw[:, 1, :], rt[:, :, 4:8], ALU.add))
        A(lambda: P.recip(gw[:, 1, :], gw[:, 1, :]))
        A(lambda: P.tt(rt[:, :, 8:12], G4, gw[:, 0, :].unsqueeze(2).to_broadcast([128, NT, 4]), ALU.is_equal))
        goh = rt[:, :, 8:12]
        A(lambda: P.tt(sel4[:], lgs[:, :, 4:36].rearrange('p t (g e) -> p t g e', e=8),
                       goh.unsqueeze(3).to_broadcast([128, NT, 4, 8]), ALU.mult))
        A(lambda: P.reduce(sel[:], sel4[:].rearrange('p t g e -> p t e g'), ALU.add))
        for t in range(NT):
            A(lambda t=t: P.max8(m8a[:, t, :], sel[:, t, :]))
        A(lambda: P.tt(gw[:, 2, :], m8a[:, :, 1], m8a[:, :, 0], ALU.subtract))
        A(lambda: P.act(gw[:, 3, :], gw[:, 2, :], AF.Exp))
        A(lambda: P.ts(gw[:, 4, :], gw[:, 3, :], 1.0, None, ALU.add))
        A(lambda: P.recip(gw[:, 4, :], gw[:, 4, :]))
        A(lambda: P.tt(gw[:, 5, :], gw[:, 3, :], gw[:, 4, :], ALU.mult))
        A(lambda: P.tt(self.wts[:, T0:T0 + NT, 0], gw[:, 4, :], gw[:, 1, :], ALU.mult))
        A(lambda: P.tt(self.wts[:, T0:T0 + NT, 1], gw[:, 5, :], gw[:, 1, :], ALU.mult))
        for kk in range(2):
            A(lambda kk=kk: P.tt(oh[:, kk], sel[:], m8a[:, :, kk].unsqueeze(2).to_broadcast([128, NT, 8]), ALU.is_equal))
            A(lambda kk=kk: P.tt(self.EH[kk][:, T0:T0 + NT, :].rearrange('p t (g e) -> p t g e', e=8),
                                 goh.unsqueeze(3).to_broadcast([128, NT, 4, 8]),
                                 oh[:, kk].unsqueeze(2).to_broadcast([128, NT, 4, 8]), ALU.mult))
        A(lambda: P.tt(eh12[:], self.EH[0][:, T0:T0 + NT, :], self.EH[1][:, T0:T0 + NT, :], ALU.add))
        A(lambda: P.mm(ps[7][:, :], self.lstrict[:], eh12[:].rearrange('p t e -> p (t e)'), start=True, stop=True))
        A(lambda: P.copy(pre[:].rearrange('p t e -> p (t e)'), ps[7][:, :]))
        A(lambda: P.mm(ps[7][:, :], self.onesb[:], eh12[:].rearrange('p t e -> p (t e)'), start=True, stop=True))
        A(lambda: P.copy(big[:].rearrange('p t e -> p (t e)'), ps[7][:, :]))
        for t in range(NT):
            A(lambda t=t: P.tt(pre[:, t, :], pre[:, t, :], self.carry[:], ALU.add))
            A(lambda t=t: P.tt(self.carry[:], self.carry[:], big[:, t, :], ALU.add))
        for kk in range(2):
            A(lambda kk=kk: P.tt(big[:], self.EH[kk][:, T0:T0 + NT, :], pre[:], ALU.mult))
            A(lambda kk=kk: P.reduce(self.rank[:, T0:T0 + NT, kk], big[:], ALU.add))
        self.pending = th
        if b == 0:
            self.dump('lg0', lgall[:, 0, :])
        self.sync_phase()


KB.phase3 = _kb_phase3


def _kb_phase4(self):
    nc, P, ps = self.nc, self.P, self.ps
    NTT, NBLK = self.NTT, self.NBLK
    wg_v = self.w_e_gate[0].rearrange('e r f -> (e r) f')
    wu_v = self.w_e_up[0].rearrange('e r f -> (e r) f')
    wd_v = self.w_e_down[0].rearrange('e r f -> (e r) f')
    IOA = bass.IndirectOffsetOnAxis
    with ExitStack() as st:
        T = lambda n, s, dt: self.T(st, n, s, dt)
        self.drain_pending()
        dest = T('dest', [128, 2, NTT], I32)
        widx = T('widx', [128, NBLK], I32)
        with ExitStack() as s2:
            T2 = lambda n, s, dt: self.T(s2, n, s, dt)
            cnt = T2('cnt', [128, 6, 32], F32)
            onesf = T2('onesf', [128, 32], F32)
            big = T2('bigtmp', [128, NTT, 32], F32)
            thr = T2('thr', [128, NBLK, 32], F32)
            cmp_ = T2('cmpb', [128, NBLK, 32], F32)
            be = T2('be', [128, 2, NBLK], F32)
            rgu = T2('rgu', [128, 12], F32)
            df = T2('df', [128, 2, NTT], F32)
            P.dma(thr[:].rearrange('p a b -> p (a b)'), self.cd['thr'])
            P.memset(onesf[:], 1.0)
            P.copy(cnt[:, 0, :], self.carry[:])
            P.ts(cnt[:, 1, :], cnt[:, 0, :], float(MB - 1), 1.0 / MB, ALU.add, ALU.mult)
            P.ts(cnt[:, 1, :], cnt[:, 1, :], -0.498, MAGIC, ALU.add, ALU.add)
            P.ts(cnt[:, 1, :], cnt[:, 1, :], MAGIC, float(MB), ALU.subtract, ALU.mult)
            P.add('dve', lambda e: e.tensor_tensor_scan(cnt[:, 2, :], onesf[:], cnt[:, 1, :], 0.0, ALU.mult, ALU.add),
                  [onesf[:], cnt[:, 1, :]], [cnt[:, 2, :]])
            P.tt(cnt[:, 3, :], cnt[:, 2, :], cnt[:, 1, :], ALU.subtract)
            for kk in range(2):
                P.tt(big[:], self.EH[kk][:], cnt[:, 3, :].unsqueeze(1).to_broadcast([128, NTT, 32]), ALU.mult)
                P.reduce(df[:, kk, :], big[:], ALU.add)
                P.tt(df[:, kk, :], df[:, kk, :], self.rank[:, :, kk], ALU.add)
            P.copy(dest[:], df[:])
            P.tt(cmp_[:], cnt[:, 2, :].unsqueeze(1).to_broadcast([128, NBLK, 32]), thr[:], ALU.is_le)
            P.reduce(be[:, 0, :], cmp_[:], ALU.add)
            P.ts(be[:, 0, :], be[:, 0, :], float(NEXP - 1), None, ALU.min)
            P.dma(rgu[:], self.cd['rowoff'])
            P.ts(be[:, 1, :], be[:, 0, :], 128.0, rgu[:, 0:1], ALU.mult, ALU.add)
            P.copy(widx[:], be[:, 1, :])
            self.dump('dest', df[:].rearrange('p a b -> p (a b)'))
            self.dump('be', be[:, 0, :])
            self.dump('cnt', cnt[:].rearrange('p a b -> p (a b)'))
            self.sync_phase()
        with ExitStack() as s2:
            T2 = lambda n, s, dt: self.T(s2, n, s, dt)
            hb = [T2('h2b%d' % i, [128, D], BF16) for i in range(3)]
            for Tg in range(NTT):
                h_ = hb[Tg % 3]
                P.dma(h_[:], self.h2_d[Tg * 128:(Tg + 1) * 128, :], q='sp')
                for kk in range(2):
                    ia = dest[:, kk, Tg:Tg + 1]
                    P.add('pool', (lambda h_=h_, ia=ia: (lambda e: e.indirect_dma_start(
                        out=self.xs_d, out_offset=IOA(ap=ia, axis=0), in_=h_[:, :], in_offset=None)))(),
                        [h_[:], ia], [], dma=True)
            self.sync_phase()
        with ExitStack() as s2:
            T2 = lambda n, s, dt: self.T(s2, n, s, dt)
            wg = [T2('wg%d' % i, [128, 8, FF], BF16) for i in range(2)]
            wu = [T2('wu%d' % i, [128, 8, FF], BF16) for i in range(2)]
            wd = [T2('wd%d' % i, [128, 4, D], BF16) for i in range(2)]
            xsb = [T2('xsb%d' % i, [128, 2, D], BF16) for i in range(2)]
            xT = [T2('xT%d' % i, [128, 8, MB], BF16) for i in range(2)]
            sg = [T2('sg%d' % i, [128, 4, MB], F32) for i in range(2)]
            hidT = [T2('hidT%d' % i, [128, 4, MB], BF16) for i in range(2)]
            yb = [T2('yb%d' % i, [128, 2, D], BF16) for i in range(2)]

            def load_w(blk, which):
                i2 = blk % 2
                ia = widx[:, blk:blk + 1]
                lst = ((wg[i2], self.wg_l), (wu[i2], self.wu_l)) if which == 0 else ((wd[i2], self.wd_l),)
                for (wt, src) in lst:
                    P.add('pool', (lambda wt=wt, src=src, ia=ia: (lambda e: e.indirect_dma_start(
                        out=wt[:].rearrange('p k f -> p (k f)'), out_offset=None, in_=src, in_offset=IOA(ap=ia, axis=0))))(),
                        [ia], [wt[:]], dma=True)

            def m0(blk):
                r0 = blk * MB
                P.dma(xsb[blk % 2][:], self.xs_d[r0:r0 + MB, :].rearrange('(t p) d -> p t d', p=128), q='act')

            def m1(blk):
                load_w(blk, 0)
                for t2 in range(2):
                    pb = ps[t2].bitcast(BF16)
                    for k in range(8):
                        P.tr(pb[:, k * 128:(k + 1) * 128], xsb[blk % 2][:, t2, k * 128:(k + 1) * 128], self.identb[:])

            def m2(blk):
                for t2 in range(2):
                    pb = ps[t2].bitcast(BF16)
                    P.copy(xT[blk % 2][:, :, t2 * 128:(t2 + 1) * 128], pb[:, :].rearrange('p (k t) -> p k t', t=128),
                           eng=('act' if t2 == 0 else 'dve'))

            def m3(blk):
                i2 = blk % 2
                load_w(blk, 1)
                for f in range(4):
                    pg_ = ps[2 + f // 2]
                    pu_ = ps[4 + f // 2]
                    cs = slice((f % 2) * MB, (f % 2 + 1) * MB)
                    for k in range(8):
                        P.mm(pg_[:, cs], wg[i2][:, k, f * 128:(f + 1) * 128], xT[i2][:, k, :], start=(k == 0 and f % 2 == 0),
                             stop=(k == 7), skip_group_check=True)
                    for k in range(8):
                        P.mm(pu_[:, cs], wu[i2][:, k, f * 128:(f + 1) * 128], xT[i2][:, k, :], start=(k == 0 and f % 2 == 0),
                             stop=(k == 7), skip_group_check=True)

            def m4(blk):
                i2 = blk % 2
                for f2 in range(2):
                    P.act(sg[i2][:, 2 * f2:2 * f2 + 2, :], ps[2 + f2][:, :].rearrange('p (f t) -> p f t', t=MB), AF.Silu)
                    P.tt(hidT[i2][:, 2 * f2:2 * f2 + 2, :], sg[i2][:, 2 * f2:2 * f2 + 2, :],
                         ps[4 + f2][:, :].rearrange('p (f t) -> p f t', t=MB), ALU.mult)

            def m5(blk):
                i2 = blk % 2
                for t2 in range(2):
                    for hf in range(2):
                        py = ps[6 + hf]
                        for f in range(4):
                            P.mm(py[:, :], hidT[i2][:, f, t2 * 128:(t2 + 1) * 128], wd[i2][:, f, hf * 512:(hf + 1) * 512],
                                 start=(f == 0), stop=(f == 3))
                        P.copy(yb[i2][:, t2, hf * 512:(hf + 1) * 512], py[:, :], eng=('act' if hf == 0 else 'dve'))

            def m6(blk):
                r0 = blk * MB
                P.dma(self.ys_d[r0:r0 + MB, :].rearrange('(t p) d -> p t d', p=128), yb[blk % 2][:], q='act')

            pipeline(NBLK, [m0, m1, m2, m3, m4, m5, m6], reverse=True)
            self.sync_phase()
        with ExitStack() as s2:
            T2 = lambda n, s, dt: self.T(s2, n, s, dt)
            ND = 3
            y0 = [T2('y0_%d' % i, [128, D], BF16) for i in range(ND)]
            y1 = [T2('y1_%d' % i, [128, D], BF16) for i in range(ND)]
            yo = [T2('yo_%d' % i, [128, D], F32) for i in range(ND)]
            x1 = [T2('x1f%d' % i, [128, D], F32) for i in range(ND)]
            gt2 = [T2('gt2_%d' % i, [128, D], F32) for i in range(2)]
            for Tg in range(NTT):
                i2 = Tg % ND
                b = Tg // NT
                if Tg % NT == 0:
                    P.dma(gt2[b % 2][:], self.mod_d[b:b + 1, 5 * D:6 * D].to_broadcast([128, D]))
                for kk, yt in ((0, y0[i2]), (1, y1[i2])):
                    ia = dest[:, kk, Tg:Tg + 1]
                    P.add('pool', (lambda yt=yt, ia=ia: (lambda e: e.indirect_dma_start(
                        out=yt[:, :], out_offset=None, in_=self.ys_d, in_offset=IOA(ap=ia, axis=0))))(),
                        [ia, self.ys_d], [yt[:]], dma=True)
                P.dma(x1[i2][:], self.x1_d[Tg * 128:(Tg + 1) * 128, :], q='sp')
                P.ts(yo[i2][:], y0[i2][:], self.wts[:, Tg, 0:1], None, ALU.mult)
                P.stt(yo[i2][:], y1[i2][:], self.wts[:, Tg, 1:2], yo[i2][:], ALU.mult, ALU.add)
                P.tt(yo[i2][:], yo[i2][:], gt2[b % 2][:], ALU.mult)
                P.tt(yo[i2][:], yo[i2][:], x1[i2][:], ALU.add)
                P.dma(self.out[Tg * 128:(Tg + 1) * 128, :], yo[i2][:], q='act')
            self.sync_phase()


KB.phase4 = _kb_phase4


N_CORES = 8
_CACHE = {}

_WEIGHT_KEYS = ['w_ada', 'b_ada', 'norm1_g', 'norm2_g', 'w_in', 'nsa_q_norm', 'nsa_k_norm', 'cmp_pe_k', 'cmp_w1_k',
                'cmp_w2_k', 'cmp_pe_v', 'cmp_w1_v', 'cmp_w2_v', 'dil_q_norm', 'dil_k_norm', 'w_up_a', 'w_up_b', 'w_out',
                'w_group', 'b_group', 'w_router', 'b_router', 'w_e_gate', 'w_e_up', 'w_e_down']


def kernel(**inputs):
    x = np.asarray(inputs['x'], dtype=np.float32)
    c = np.asarray(inputs['c'], dtype=np.float32)
    pos = np.asarray(inputs['positions'], dtype=np.int32)
    B = x.shape[0]
    nseq = B // N_CORES
    if 'kb' not in _CACHE:
        kb = KB(nseq)
        kb.build()
        _CACHE['kb'] = kb
    kb = _CACHE['kb']
    w = {k: np.ascontiguousarray(np.asarray(inputs[k], dtype=np.float32)) for k in _WEIGHT_KEYS}
    in_maps = []
    for i in range(N_CORES):
        m = dict(w)
        m['x'] = np.ascontiguousarray(x[i * nseq:(i + 1) * nseq].reshape(nseq * S, D))
        m['c'] = np.ascontiguousarray(c[i * nseq:(i + 1) * nseq])
        m['positions'] = np.ascontiguousarray(pos[i * nseq:(i + 1) * nseq])
        for k, v in kb.consts.items():
            m['k_' + k] = v
        in_maps.append(m)
    res = run_bass_kernel_spmd(kb.nc, in_maps, core_ids=list(range(N_CORES)))
    out = np.concatenate([np.asarray(r['out']).reshape(nseq, S, D) for r in res.results], axis=0)
    return out.astype(np.float32)
```

```python
import numpy as np
import concourse.bass as bass
import concourse.mybir as mybir

F32 = mybir.dt.float32
BF16 = mybir.dt.bfloat16
I32 = mybir.dt.int32
U32 = mybir.dt.uint32
ALU = mybir.AluOpType
AF = mybir.ActivationFunctionType
AX = mybir.AxisListType

ENG_ATTR = {'pe': 'tensor', 'act': 'scalar', 'dve': 'vector', 'pool': 'gpsimd', 'sp': 'sync'}
ENGS = ['pe', 'act', 'dve', 'pool', 'sp']
DMA_RING = 6
_ESZ = {}


def esz(dt):
    k = str(dt)
    if k not in _ESZ:
        _ESZ[k] = mybir.dt.size(dt) if hasattr(mybir.dt, 'size') else np.dtype(mybir.dt.np(dt)).itemsize
    return _ESZ[k]


def box_of(ap):
    t = ap.tensor
    name = t.name
    pat = ap.ap
    e = esz(ap.dtype)
    space = str(ap.space)
    if 'DRAM' in space.upper() or 'HBM' in space.upper():
        lo = ap.offset
        hi = lo
        for st, n in pat:
            if st >= 0:
                hi += st * (n - 1)
            else:
                lo += st * (n - 1)
        return (name, 'D', 0, 1, lo * e, (hi + 1) * e)
    pstep, pn = pat[0]
    p0 = ap.start_partition()
    p1 = p0 + ap.partition_size()
    off = ap.offset - p0 * pstep if pstep else ap.offset
    lo = off
    hi = off
    for st, n in pat[1:]:
        if st >= 0:
            hi += st * (n - 1)
        else:
            lo += st * (n - 1)
    sp = 'P' if 'PSUM' in space.upper() else 'S'
    return (name, sp, p0, p1, lo * e, (hi + 1) * e)


class Op:
    __slots__ = ('eng', 'chan', 'pos', 'emit', 'waits', 'snap', 'inc', 'dma')


class Prog:
    def __init__(self, nc):
        self.nc = nc
        self.sems = {}
        self.semcnt = {}
        self.ops = {e: [] for e in ENGS}
        self.chan_ops = {}
        self.chan_base = {}
        self.vc = {e: {} for e in ENGS}
        self.trk = {}
        self.psum_last = {}
        self.dma_n = {e: 0 for e in ENGS}
        self._oldvals = {}
        self.n_ops = 0

    def sem(self, chan):
        if chan not in self.sems:
            nm = 's_' + (chan if isinstance(chan, str) else '%s%d' % chan)
            h = self.nc.alloc_semaphore(name=nm)
            self.sems[chan] = h
            self.semcnt[chan] = 0
        return self.sems[chan]

    def _known(self, eng, chan, pos):
        return self.vc[eng].get(chan, -1) >= pos

    def _learn(self, eng, chan, pos):
        vc = self.vc[eng]
        op = self.chan_ops[chan][pos - self.chan_base.get(chan, 0)] if pos >= self.chan_base.get(chan, 0) else None
        if op is not None and op.snap:
            for c, p in op.snap.items():
                if vc.get(c, -1) < p:
                    vc[c] = p
        if vc.get(chan, -1) < pos:
            vc[chan] = pos

    def _deps_for(self, eng, reads, writes):
        deps = set()
        for ap in reads:
            bx = box_of(ap)
            name, sp = bx[0], bx[1]
            if sp == 'P':
                self._psum_deps(eng, name, deps)
                continue
            t = self.trk.get(name)
            if t is None:
                continue
            for (wb, c, p) in t['w']:
                if wb[2] < bx[3] and bx[2] < wb[3] and wb[4] < bx[5] and bx[4] < wb[5]:
                    deps.add((c, p))
        for ap in writes:
            bx = box_of(ap)
            name, sp = bx[0], bx[1]
            if sp == 'P':
                self._psum_deps(eng, name, deps)
                continue
            t = self.trk.get(name)
            if t is None:
                continue
            for (wb, c, p) in t['w']:
                if wb[2] < bx[3] and bx[2] < wb[3] and wb[4] < bx[5] and bx[4] < wb[5]:
                    deps.add((c, p))
            for (rb, c), p in t['r'].items():
                if rb[2] < bx[3] and bx[2] < rb[3] and rb[4] < bx[5] and bx[4] < rb[5]:
                    deps.add((c, p))
        return deps

    def _psum_deps(self, eng, name, deps):
        last = self.psum_last.get(name)
        if not last:
            return
        for e, cp in last.items():
            if e == eng and eng == 'pe':
                continue
            deps.add(cp)

    def _record_access(self, eng, chan, pos, reads, writes):
        for ap in reads:
            bx = box_of(ap)
            name, sp = bx[0], bx[1]
            if sp == 'P':
                self.psum_last.setdefault(name, {})[eng] = (chan, pos)
                continue
            t = self.trk.setdefault(name, {'w': [], 'r': {}})
            t['r'][(bx, chan)] = pos
        for ap in writes:
            bx = box_of(ap)
            name, sp = bx[0], bx[1]
            if sp == 'P':
                self.psum_last.setdefault(name, {})[eng] = (chan, pos)
                continue
            t = self.trk.setdefault(name, {'w': [], 'r': {}})
            neww = []
            for ent in t['w']:
                wb = ent[0]
                if bx[2] <= wb[2] and wb[3] <= bx[3] and bx[4] <= wb[4] and wb[5] <= bx[5]:
                    continue
                neww.append(ent)
            neww.append((bx, chan, pos))
            t['w'] = neww
            if t['r']:
                t['r'] = {k: v for k, v in t['r'].items()
                          if not (bx[2] <= k[0][2] and k[0][3] <= bx[3] and bx[4] <= k[0][4] and k[0][5] <= bx[5])}

    def add(self, eng, emit, reads=(), writes=(), dma=False, extra_deps=(), ring=''):
        op = Op()
        op.eng = eng
        op.emit = emit
        op.dma = dma
        op.inc = dma
        deps = self._deps_for(eng, reads, writes)
        deps.update(extra_deps)
        if dma:
            rk = eng + ring
            slot = self.dma_n.get(rk, 0) % (24 if ring == 'bg' else DMA_RING)
            self.dma_n[rk] = self.dma_n.get(rk, 0) + 1
            chan = (rk, slot)
            lst = self.chan_ops.setdefault(chan, [])
            base = self.chan_base.get(chan, 0)
            if lst or base:
                deps.add((chan, base + len(lst) - 1))
        else:
            chan = eng
            lst = self.chan_ops.setdefault(chan, [])
        self.sem(chan)
        pos = self.chan_base.get(chan, 0) + len(lst)
        waits = []
        for (c, p) in sorted(deps, key=lambda cp: (str(cp[0]), cp[1])):
            if c == eng and eng == 'pe':
                continue
            if self._known(eng, c, p):
                continue
            waits.append((c, p))
        best = {}
        for c, p in waits:
            if best.get(c, -1) < p:
                best[c] = p
        op.waits = list(best.items())
        for c, p in op.waits:
            cb = self.chan_base.get(c, 0)
            if p >= cb:
                self.chan_ops[c][p - cb].inc = True
            self._learn(eng, c, p)
        op.snap = dict(self.vc[eng])
        op.chan = chan
        op.pos = pos
        lst.append(op)
        self.ops[eng].append(op)
        self._record_access(eng, chan, pos, reads, writes)
        self.n_ops += 1
        return (chan, pos)

    def barrier(self):
        lasts = []
        for chan, lst in self.chan_ops.items():
            if lst and not (isinstance(chan, tuple) and chan[0].endswith('bg') and not getattr(self, 'final_barrier', False)):
                lst[-1].inc = True
                lasts.append((chan, self.chan_base.get(chan, 0) + len(lst) - 1))
        for e in ENGS:
            op = Op()
            op.eng = e
            op.emit = None
            op.dma = False
            op.inc = False
            op.chan = None
            op.pos = -1
            waits = []
            for (c, p) in lasts:
                if c == e and e == 'pe':
                    continue
                if self._known(e, c, p):
                    continue
                waits.append((c, p))
            op.waits = waits
            for c, p in waits:
                self._learn(e, c, p)
            op.snap = None
            self.ops[e].append(op)

    def flush(self):
        nc = self.nc
        semval = {}
        for chan, lst in self.chan_ops.items():
            v = self.semcnt[chan]
            base = self.chan_base.get(chan, 0)
            for i, op in enumerate(lst):
                if op.inc:
                    v += 16 if op.dma else 1
                semval[(chan, base + i)] = v if op.inc else None
            self.semcnt[chan] = v
        old = self._oldvals
        old.update({k: v for k, v in semval.items() if v is not None})
        ops = self.ops
        sems = self.sems

        def run(engname):
            def f(eng):
                for op in ops[engname]:
                    for (c, p) in op.waits:
                        v = old.get((c, p))
                        assert v is not None, (engname, c, p)
                        eng.wait_ge(sems[c], v)
                    if op.emit is None:
                        continue
                    inst = op.emit(eng)
                    if op.inc:
                        inst.then_inc(sems[op.chan], 16 if op.dma else 1)
            return f

        with nc.Block() as block:
            block.tensor(run('pe'))
            block.scalar(run('act'))
            block.vector(run('dve'))
            block.gpsimd(run('pool'))
            block.sync(run('sp'))
        for chan, lst in self.chan_ops.items():
            self.chan_base[chan] = self.chan_base.get(chan, 0) + len(lst)
            self.chan_ops[chan] = []
        self.ops = {e: [] for e in ENGS}

    def dma(self, out, in_, q='sp', bg=False, **kw):
        return self.add(q, lambda e: e.dma_start(out=out, in_=in_, **kw), [in_], [out], dma=True, ring=('bg' if bg else ''))

    def mm(self, out, lhsT, rhs, start=True, stop=True, **kw):
        return self.add('pe', lambda e: e.matmul(out, lhsT, rhs, start=start, stop=stop, **kw),
                        [lhsT, rhs], [out])

    def tr(self, out, in_, ident):
        return self.add('pe', lambda e: e.transpose(out, in_, ident), [in_, ident], [out])

    def act(self, out, in_, func, bias=None, scale=None, accum_out=None, eng='act'):
        kw = {}
        rd = [in_]
        wr = [out]
        if bias is not None:
            kw['bias'] = bias
            if not isinstance(bias, (int, float)):
                rd.append(bias)
        if scale is not None:
            kw['scale'] = scale
            if not isinstance(scale, (int, float)):
                rd.append(scale)
        if accum_out is not None:
            kw['accum_out'] = accum_out
            wr.append(accum_out)
        return self.add('act', lambda e: e.activation(out, in_, func, **kw), rd, wr)

    def tt(self, out, in0, in1, op, eng='dve'):
        return self.add(eng, lambda e: e.tensor_tensor(out, in0, in1, op), [in0, in1], [out])

    def ts(self, out, in0, s1, s2, op0, op1=None, eng='dve', accum_out=None):
        rd = [in0]
        if not isinstance(s1, (int, float)) and s1 is not None:
            rd.append(s1)
        if not isinstance(s2, (int, float)) and s2 is not None:
            rd.append(s2)
        kw = {}
        wr = [out]
        if op1 is not None:
            kw['op1'] = op1
        if accum_out is not None:
            kw['accum_out'] = accum_out
            wr.append(accum_out)
        return self.add(eng, lambda e: e.tensor_scalar(out, in0, s1, s2, op0, **kw), rd, wr)

    def stt(self, out, in0, scalar, in1, op0, op1, eng='dve'):
        rd = [in0, in1]
        if not isinstance(scalar, (int, float)):
            rd.append(scalar)
        return self.add(eng, lambda e: e.scalar_tensor_tensor(out, in0, scalar, in1, op0, op1), rd, [out])

    def copy(self, out, in_, eng='dve'):
        if eng == 'act':
            return self.add('act', lambda e: e.copy(out, in_), [in_], [out])
        return self.add(eng, lambda e: e.tensor_copy(out, in_), [in_], [out])

    def reduce(self, out, in_, op, axis=AX.X, eng='dve'):
        return self.add(eng, lambda e: e.tensor_reduce(out, in_, axis, op), [in_], [out])

    def memset(self, ap, val, eng='dve'):
        return self.add(eng, lambda e: e.memset(ap, val), [], [ap])

    def recip(self, out, in_, eng='dve'):
        return self.add(eng, lambda e: e.reciprocal(out, in_), [in_], [out])

    def max8(self, out, in_):
        return self.add('dve', lambda e: e.max(out, in_), [in_], [out])
from concourse.bass_utils import run_bass_kernel_spmd
from contextlib import ExitStack

S = 2048
D = 1024
DH = 64
NT = S // 128
IN_COLS = 4504
EPS = 1e-6
BIG = 30000.0
MAGIC = 12582912.0
TWO_PI = 6.283185307179586
NEXP = 32
FF = 512
MB = 256


def host_consts():
    c = {}
    c['identf'] = np.eye(128, dtype=np.float32)
    inv = (10000.0 ** (-np.arange(0, 64, 2, dtype=np.float32) / 64)).astype(np.float32)
    c['invf'] = np.tile(inv[None, :], (128, 1)).astype(np.float32)
    k = np.arange(128)[:, None]
    q = np.arange(128)[None, :]
    tri = (k <= q).astype(np.float32)
    anti = (k >= q).astype(np.float32)
    c['tri'] = tri
    c['anti'] = anti
    c['trianti'] = np.concatenate([tri, anti], axis=1)
    c['tri4'] = np.tile(tri, (1, 4))
    t = np.arange(S)
    cv = np.zeros((128, S), np.float32)
    cv[:127] = ((np.arange(127) * 16 + 31)[:, None] <= t[None, :])
    c['cmpvalid'] = cv
    c['blkoh'] = (np.arange(32)[:, None] == (t // 64)[None, :]).astype(np.float32)
    cs = np.arange(127) * 16
    ss = np.arange(32) * 64
    ov = np.clip(np.minimum(cs[:, None] + 32, ss[None, :] + 64) - np.maximum(cs[:, None], ss[None, :]), 0, None) / 32.0
    ovp = np.zeros((128, 32), np.float32)
    ovp[:127] = ov
    c['overlap'] = ovp
    b = (t // 64)[:, None]
    s = np.arange(32)[None, :]
    cand = ((s >= 1) & (s <= b - 2)).astype(np.float32)
    forced = (((s == 0) | (s == b) | (s == b - 1)) & (s <= b)).astype(np.float32)
    tm = lambda a: np.ascontiguousarray(a.reshape(NT, 128, 32).transpose(1, 0, 2)).astype(np.float32)
    c['cand'] = tm(cand)
    c['candm1'] = tm(cand - 1.0)
    c['forced'] = tm(forced)
    c['lstrict'] = (k < q).astype(np.float32)
    c['ones'] = np.ones((128, 128), np.float32)
    return c


class KB:
    def __init__(self, NSEQ, dbg=None):
        self.NSEQ = NSEQ
        self.NTOK = NSEQ * S
        self.NTT = NSEQ * NT
        self.NBLK = (self.NTOK * 2) // MB + NEXP
        self.NSLOT = self.NBLK * MB
        self.dbg = dbg or {}
        self.nc = bass.Bass("TRN2", target_bir_lowering=False)
        self.P = Prog(self.nc)
        self.din = {}
        self.consts = host_consts()
        blk = np.arange(self.NBLK, dtype=np.float32)[:, None] * MB
        self.consts['thr'] = np.tile(np.tile(blk, (1, 32)).reshape(1, -1), (128, 1)).astype(np.float32)
        p = np.arange(128, dtype=np.float32)[:, None]
        self.consts['rowoff'] = np.concatenate([np.arange(8)[None, :] * 128 + p, np.arange(4)[None, :] * 128 + p], axis=1).astype(np.float32)

    def dram_in(self, name, shape, dt=F32):
        h = self.nc.dram_tensor(name, list(shape), dt, kind="ExternalInput")
        self.din[name] = h
        return h.ap()

    def declare(self):
        NSEQ = self.NSEQ
        nc = self.nc
        d = self.dram_in
        self.x = d('x', [self.NTOK, D])
        self.c = d('c', [NSEQ, D])
        self.pos = d('positions', [NSEQ, S], I32)
        self.w_ada = d('w_ada', [1, D, 6 * D])
        self.b_ada = d('b_ada', [1, 6 * D])
        self.norm1_g = d('norm1_g', [1, D])
        self.norm2_g = d('norm2_g', [1, D])
        self.w_in = d('w_in', [1, D, IN_COLS])
        self.nsa_q_norm = d('nsa_q_norm', [1, DH])
        self.nsa_k_norm = d('nsa_k_norm', [1, DH])
        self.cmp_pe_k = d('cmp_pe_k', [1, 32, DH])
        self.cmp_w1_k = d('cmp_w1_k', [1, 2048, 256])
        self.cmp_w2_k = d('cmp_w2_k', [1, 256, DH])
        self.cmp_pe_v = d('cmp_pe_v', [1, 32, DH])
        self.cmp_w1_v = d('cmp_w1_v', [1, 2048, 256])
        self.cmp_w2_v = d('cmp_w2_v', [1, 256, DH])
        self.dil_q_norm = d('dil_q_norm', [1, DH])
        self.dil_k_norm = d('dil_k_norm', [1, DH])
        self.w_up_a = d('w_up_a', [1, 512, D])
        self.w_up_b = d('w_up_b', [1, 384, D])
        self.w_out = d('w_out', [1, D, D])
        self.w_group = d('w_group', [1, D, 4])
        self.b_group = d('b_group', [1, 4])
        self.w_router = d('w_router', [1, 4, D, 8])
        self.b_router = d('b_router', [1, 4, 8])
        self.w_e_gate = d('w_e_gate', [1, NEXP, D, FF])
        self.w_e_up = d('w_e_up', [1, NEXP, D, FF])
        self.w_e_down = d('w_e_down', [1, NEXP, FF, D])
        self.cd = {}
        for k, v in self.consts.items():
            self.cd[k] = d('k_' + k, v.shape)
        self.out = nc.dram_tensor('out', [self.NTOK, D], F32, kind="ExternalOutput").ap()
        sc = lambda n, shp, dt: nc.dram_tensor(n, list(shp), dt, kind="Internal").ap()
        self.mod_d = sc('mod_d', [NSEQ, 6 * D], F32)
        self.x1_d = sc('x1_d', [self.NTOK, D], F32)
        self.h2_d = sc('h2_d', [self.NTOK, D], BF16)
        self.xs_d = sc('xs_d', [self.NSLOT, D], BF16)
        self.ys_d = sc('ys_d', [self.NSLOT, D], BF16)
        self.wg_l = nc.dram_tensor('wg_l', [NEXP * 128, 8 * FF], BF16, kind="Internal").ap()
        self.wu_l = nc.dram_tensor('wu_l', [NEXP * 128, 8 * FF], BF16, kind="Internal").ap()
        self.wd_l = nc.dram_tensor('wd_l', [NEXP * 128, 4 * D], BF16, kind="Internal").ap()
        self.conv_list = []
        for e_ in range(NEXP):
            rows = slice(e_ * 128, (e_ + 1) * 128)
            self.conv_list.append((self.wg_l[rows, :].rearrange('p (k f) -> p k f', f=FF),
                                   self.w_e_gate[0, e_].rearrange('(k p) f -> p k f', p=128)))
            self.conv_list.append((self.wu_l[rows, :].rearrange('p (k f) -> p k f', f=FF),
                                   self.w_e_up[0, e_].rearrange('(k p) f -> p k f', p=128)))
            self.conv_list.append((self.wd_l[rows, :].rearrange('p (k f) -> p k f', f=D),
                                   self.w_e_down[0, e_].rearrange('(k p) f -> p k f', p=128)))
        self.conv_pos = 0
        self.w_nsa_d = sc('w_nsa_d', [128, 8 * 1304], BF16)
        self.w_dil_d = sc('w_dil_d', [128, 8 * 1152], BF16)
        self.w_gm_d = sc('w_gm_d', [128, 8 * 2048], BF16)
        self.w_upa_d = sc('w_upa_d', [128, 4 * D], BF16)
        self.w_upb_d = sc('w_upb_d', [128, 3 * D], BF16)
        self.w_o_d = sc('w_o_d', [128, 8 * D], BF16)
        self.dbg_out = {}
        for k, shp in self.dbg.items():
            self.dbg_out[k] = nc.dram_tensor('dbg_' + k, list(shp), F32, kind="ExternalOutput").ap()

    def T(self, st, name, shape, dt):
        self._uid = getattr(self, '_uid', 0) + 1
        return st.enter_context(self.nc.sbuf_tensor('%s_%d' % (name, self._uid), list(shape), dt))

    def drain_pending(self, n=None):
        if not hasattr(self, 'pending'):
            return
        k = len(self.pending) if n is None else min(n, len(self.pending))
        for _ in range(k):
            self.pending.pop(0)()

    def emit_fill(self, n):
        if not hasattr(self, 'zrow'):
            return
        for _ in range(n):
            if self.fill_pos >= self.NSLOT // 128:
                return
            r0 = self.fill_pos * 128
            self.fill_pos += 1
            self.P.dma(self.xs_d[r0:r0 + 128, :], self.zrow[:], q='sp')

    def emit_conv(self, n):
        for _ in range(n):
            if self.conv_pos >= len(self.conv_list):
                return
            dst, src = self.conv_list[self.conv_pos]
            self.conv_pos += 1
            self.P.dma(dst, src, q='pool')

    def dump(self, key, ap):
        if key in self.dbg_out:
            self.P.dma(self.dbg_out[key], ap, q='pool')

    def sync_phase(self, name=None):
        self.P.barrier()
        if name is None:
            import inspect
            fr = inspect.stack()[1]
            name = '%s_%d' % (fr.function.replace('_kb_', ''), fr.lineno)
        with self.nc.named_scope(name):
            self.P.flush()

    def phase0(self, prep=False):
        nc, P, NSEQ = self.nc, self.P, self.NSEQ
        ps = self.ps
        with ExitStack() as st:
            T = lambda n, s, dt: self.T(st, n, s, dt)
            if prep:
                self.prep_weights(st)
            cs = T('cs', [4, D], F32)
            csT = T('csT', [128, 8, 4], F32)
            wa = [T('wa%d' % i, [128, 8, 512], F32) for i in range(2)]
            modrows = T('modrows', [4, 6 * D], F32)
            bada = T('bada', [4, 6 * D], F32)
            g1b = T('g1b', [4, D], F32)
            g2b = T('g2b', [4, D], F32)
            P.dma(cs[0:NSEQ, :], self.c)
            P.dma(bada[0:NSEQ, :], self.b_ada.to_broadcast([NSEQ, 6 * D]))
            P.dma(g1b[0:NSEQ, :], self.norm1_g.to_broadcast([NSEQ, D]))
            P.dma(g2b[0:NSEQ, :], self.norm2_g.to_broadcast([NSEQ, D]))
            P.act(cs[0:NSEQ, :], cs[0:NSEQ, :], AF.Silu)
            for k in range(8):
                P.tr(ps[0][:, k * 4:k * 4 + NSEQ], cs[0:NSEQ, k * 128:(k + 1) * 128], self.identf[0:NSEQ, 0:NSEQ])
            P.copy(csT[:, :, 0:NSEQ], ps[0][:, 0:32].rearrange('p (k b) -> p k b', b=4)[:, :, 0:NSEQ])
            wv = self.w_ada[0].rearrange('(k p) c -> p k c', p=128)
            for cc in range(12):
                w = wa[cc % 2]
                P.dma(w[:], wv[:, :, cc * 512:(cc + 1) * 512], q=('sp' if cc % 2 == 0 else 'act'))
                pb = ps[1 + cc % 2]
                for k in range(8):
                    P.mm(pb[0:NSEQ, :], csT[:, k, 0:NSEQ], w[:, k, :], start=(k == 0), stop=(k == 7))
                P.tt(modrows[0:NSEQ, cc * 512:(cc + 1) * 512], pb[0:NSEQ, :], bada[0:NSEQ, cc * 512:(cc + 1) * 512], ALU.add)
            P.stt(modrows[0:NSEQ, D:2 * D], modrows[0:NSEQ, D:2 * D], 1.0, g1b[0:NSEQ, :], ALU.add, ALU.mult)
            P.stt(modrows[0:NSEQ, 4 * D:5 * D], modrows[0:NSEQ, 4 * D:5 * D], 1.0, g2b[0:NSEQ, :], ALU.add, ALU.mult)
            P.dma(self.mod_d, modrows[0:NSEQ, :])
            for ch in range(16):
                P.tr(ps[3][:, ch * 4:ch * 4 + NSEQ], modrows[0:NSEQ, ch * 128:(ch + 1) * 128], self.identf[0:NSEQ, 0:NSEQ])
            P.copy(self.modT1[:, :, 0:NSEQ], ps[3][:, 0:64].rearrange('p (k b) -> p k b', b=4)[:, :, 0:NSEQ])
            self.dump('modrows', modrows[0:NSEQ, :])
            self.sync_phase()

    def phase1(self, b):
        nc, P = self.nc, self.P
        ps = self.ps
        with ExitStack() as st:
            T = lambda n, s, dt: self.T(st, n, s, dt)
            if b == 0 and hasattr(self, 'w_dil_d'):
                self.prep_bg()
            xt = [T('xt%d' % i, [128, D], F32) for i in range(3)]
            junk = T('p1junk', [128, D], F32)
            xn = [T('xn%d' % i, [128, D], F32) for i in range(2)]
            ss = [T('p1ss%d' % i, [128, 4], F32) for i in range(2)]
            def p0(tt):
                r0 = b * S + tt * 128
                P.dma(xt[tt % 3][:], self.x[r0:r0 + 128, :], q=('sp' if tt % 2 == 0 else 'act'))

            def p1(tt):
                s_ = ss[tt % 2]
                P.act(junk[:], xt[tt % 3][:], AF.Square, accum_out=s_[:, 0:1])
                P.act(s_[:, 1:2], s_[:, 0:1], AF.Ln, scale=1.0 / D, bias=EPS)
                P.act(s_[:, 2:3], s_[:, 1:2], AF.Exp, scale=-0.5)

            npend = -(-len(getattr(self, 'pending', [])) // NT)

            def p2(tt):
                P.ts(xn[tt % 2][:], xt[tt % 3][:], ss[tt % 2][:, 2:3], None, ALU.mult)
                self.drain_pending(npend)

            def p3(tt):
                for k in range(8):
                    pb = ps[(tt % 2) * 2 + k // 4]
                    P.tr(pb[:, (k % 4) * 128:(k % 4 + 1) * 128], xn[tt % 2][:, k * 128:(k + 1) * 128], self.identf[:])

            def p4(tt):
                for k in range(8):
                    pb = ps[(tt % 2) * 2 + k // 4]
                    src = pb[:, (k % 4) * 128:(k % 4 + 1) * 128]
                    dst = self.hT[:, k, tt * 128:(tt + 1) * 128]
                    if k < 4:
                        P.act(dst, src, AF.Identity, scale=self.modT1[:, 8 + k, b:b + 1], bias=self.modT1[:, k, b:b + 1])
                    else:
                        P.ts(dst, src, self.modT1[:, 8 + k, b:b + 1], self.modT1[:, k, b:b + 1], ALU.mult, ALU.add)

            pipeline(NT, [p0, p1, p2, p3, p4], reverse=True)
            self.drain_pending()
            if b == 0:
                for k in range(8):
                    if ('hT%d' % k) in self.dbg_out:
                        self.dump('hT%d' % k, self.hT[:, k, :])
            self.sync_phase()


PI_SAFE = 3.1415925


def _kb_rope_tables(self, st, posf, n, cos_out, sin_out, tag):
    P = self.P
    T = lambda nm, s, dt: self.T(st, tag + nm, s, dt)
    ang = T('ang', [128, n, 32], F32)
    a2 = T('a2', [128, n, 32], F32)
    kk = T('kk', [128, n, 32], F32)
    P.tt(ang[:], self.invf[:, :].unsqueeze(1).to_broadcast([128, n, 32]),
         posf.unsqueeze(2).to_broadcast([128, n, 32]), ALU.mult)
    for off, outp in ((0.0, sin_out), (np.pi / 2, cos_out)):
        if off == 0.0:
            a = ang
        else:
            P.ts(a2[:], ang[:], float(off), None, ALU.add)
            a = a2
        P.ts(kk[:], a[:], 1.0 / TWO_PI, MAGIC, ALU.mult, ALU.add)
        P.ts(kk[:], kk[:], MAGIC, None, ALU.subtract)
        P.stt(kk[:], kk[:], -TWO_PI, a[:], ALU.mult, ALU.add)
        P.ts(kk[:], kk[:], PI_SAFE, -PI_SAFE, ALU.min, ALU.max)
        P.act(outp, kk[:], AF.Sin)


KB.rope_tables = _kb_rope_tables


def _kb_setup_nsa_consts(self):
    P, top = self.P, self.top
    T = lambda n, s, dt: self.T(top, n, s, dt)
    self.kslc = T('kslc', [96, S], BF16)
    P.dma(self.kslc[64:96, :], self.cd['blkoh'], q='pool')
    self.v2 = T('v2', [128, NT, 2, 65], BF16)
    P.memset(self.v2[:].rearrange('p a b c -> p (a b c)'), 1.0)
    self.vcaug = T('vcaug', [128, 97], BF16)
    P.memset(self.vcaug[:, 64:65], 1.0)
    P.dma(self.vcaug[:, 65:97], self.cd['overlap'], q='pool')
    self.w1kv = T('w1kv', [128, 32, 256], BF16)
    P.dma(self.w1kv[0:64], self.cmp_w1_k[0].rearrange('(l d) h -> d l h', d=64), q='pool')
    P.dma(self.w1kv[64:128], self.cmp_w1_v[0].rearrange('(l d) h -> d l h', d=64), q='pool')
    self.w2kv = T('w2kv', [128, 2, 2, 64], BF16)
    P.dma(self.w2kv[:, 0], self.cmp_w2_k[0].rearrange('(c p) d -> p c d', p=128), q='pool')
    P.dma(self.w2kv[:, 1], self.cmp_w2_v[0].rearrange('(c p) d -> p c d', p=128), q='pool')
    self.ckv = T('ckv', [128, 2, 2], F32)
    self.gfull = T('gfull', [128, 6, 64], F32)
    self.gk = T('gk', [128, 64], F32)
    self.gqb = T('gqb', [128, 64], F32)
    self.gkb = T('gkb', [128, 64], F32)
    for hh in range(4):
        P.dma(self.gfull[:, hh, :], self.nsa_q_norm.to_broadcast([128, 64]))
    for hh in range(4, 6):
        P.dma(self.gfull[:, hh, :], self.nsa_k_norm.to_broadcast([128, 64]))
    P.ts(self.gfull[:, 0:4, :], self.gfull[:, 0:4, :], 0.125, None, ALU.mult)
    P.dma(self.gk[:], self.nsa_k_norm.to_broadcast([128, 64]))
    P.dma(self.gqb[:], self.dil_q_norm.to_broadcast([128, 64]))
    P.ts(self.gqb[:], self.gqb[:], 0.125, None, ALU.mult)
    P.dma(self.gkb[:], self.dil_k_norm.to_broadcast([128, 64]))
    with ExitStack() as st:
        T2 = lambda n, s, dt: self.T(st, n, s, dt)
        pekv = T2('pekv', [32, 128], F32)
        peT = T2('peT', [128, 32], BF16)
        P.dma(pekv[:, 0:64], self.cmp_pe_k[0])
        P.dma(pekv[:, 64:128], self.cmp_pe_v[0])
        P.tr(self.ps[0][:, 0:32], pekv[:, :], self.identf[0:32, 0:32])
        P.copy(peT[:], self.ps[0][:, 0:32])
        for kv in range(2):
            base = 64 * kv
            for hc in range(2):
                for l in range(32):
                    P.mm(self.ps[1 + kv][:, hc:hc + 1], self.w1kv[base:base + 64, l, hc * 128:(hc + 1) * 128],
                         peT[base:base + 64, l:l + 1], start=(hc == 0 and l == 0), stop=(l == 31), skip_group_check=True)
            P.copy(self.ckv[:, kv, :], self.ps[1 + kv][:, 0:2])
        self.sync_phase()


KB.setup_nsa_consts = _kb_setup_nsa_consts


def _kb_seq_prologue(self, st, b):
    P = self.P
    T = lambda n, s, dt: self.T(st, n, s, dt)
    self.cosT = T('cosT', [128, NT, 32], F32)
    self.sinT = T('sinT', [128, NT, 32], F32)
    self.cosC = T('cosC', [128, 1, 32], F32)
    self.sinC = T('sinC', [128, 1, 32], F32)
    with ExitStack() as s2:
        T2 = lambda n, s, dt: self.T(s2, n, s, dt)
        posi = T2('posi', [128, 2], I32)
        posi16 = T2('posi16', [16, 128], I32)
        posf16 = T2('posf16', [16, 128], F32)
        posf = T2('posf', [128, NT + 1], F32)
        P.memset(posi[:], 0)
        P.dma(posi16[:], self.pos[b].rearrange('(t p) -> t p', p=128))
        P.copy(posf16[:], posi16[:])
        P.tr(self.ps[0][:, 0:NT], posf16[:], self.identf[0:16, 0:16])
        P.copy(posf[:, 0:NT], self.ps[0][:, 0:NT])
        P.dma(posi[0:127, 0:1], self.pos[b, 31:31 + 16 * 126 + 1:16].unsqueeze(1), allow_slow_non_contiguous=True)
        P.copy(posf[:, NT:NT + 1], posi[:, 0:1])
        self.rope_tables(s2, posf[:, 0:NT], NT, self.cosT[:], self.sinT[:], 'rt')
        self.rope_tables(s2, posf[:, NT:NT + 1], 1, self.cosC[:], self.sinC[:], 'rc')
        self.sync_phase()


KB.seq_prologue = _kb_seq_prologue


class BankRound:
    def __init__(self):
        self.started = {}

    def reset(self, bank):
        self.started[bank.name] = False

    def start(self, bank):
        s = not self.started.get(bank.name, False)
        self.started[bank.name] = True
        return s


def pipeline(n, stages, delays=None, reverse=False):
    if delays is None:
        delays = list(range(len(stages)))
    order = list(range(len(stages)))
    if reverse:
        order = order[::-1]
    for step in range(n + max(delays)):
        for j in order:
            i = step - delays[j]
            if 0 <= i < n:
                stages[j](i)


def _kb_phase2_nsa(self, b, st_seq):
    nc, P, ps = self.nc, self.P, self.ps
    BR = self.br
    wv = self.w_in[0].rearrange('(k p) c -> p k c', p=128)
    with ExitStack() as st:
        T = lambda n, s, dt: self.T(st, n, s, dt)
        w_nsa = T('w_nsa', [128, 8, 1304], BF16)
        P.dma(w_nsa[:].rearrange('p k c -> p (k c)'), self.w_nsa_d)
        gates = T('gates', [128, NT, 24], F32)
        for g in range(2):
            with ExitStack() as sg:
                self.nsa_group(b, g, sg, w_nsa, gates)
                self.sync_phase()


def _kb_nsa_group(self, b, g, st, w_nsa, gates):
    nc, P, ps = self.nc, self.P, self.ps
    BR = self.br
    T = lambda n, s, dt: self.T(st, n, s, dt)
    qaug = T('qaug', [96, 4, S], BF16)
    kwin = T('kwin', [64, S], BF16)
    kvcT = T('kvcT', [128, S], BF16)
    kcT = T('kcT', [64, 128], BF16)
    kslc, v2, vcaug = self.kslc, self.v2, self.vcaug
    with ExitStack() as s2:
        T2 = lambda n, s, dt: self.T(s2, n, s, dt)
        NP = NT // 2
        sq = [T2('sq%d' % i, [128, 2, 6, 64], F32) for i in range(1)] * 2
        rc = [T2('rc%d' % i, [128, 2, 6, 64], F32) for i in range(3)]
        rn = [T2('rn%d' % i, [128, 2, 6, 64], F32) for i in range(1)] * 2
        tmp = [T2('rtmp%d' % i, [128, 4, 2, 6, 32], F32) for i in range(1)] * 2
        rr = [T2('rr%d' % i, [128, 2, 6, 64], BF16) for i in range(2)]
        st6 = [T2('st6%d' % i, [128, 3, 12], F32) for i in range(2)]
        for tc in range(4):
            pc = ps[6 + tc % 2]
            for k in range(8):
                P.mm(pc[:, :], w_nsa[:, k, 1024 + 128 * g:1024 + 128 * g + 128], self.hT[:, k, tc * 512:(tc + 1) * 512],
                     start=(k == 0), stop=(k == 7))
            P.copy(kvcT[:, tc * 512:(tc + 1) * 512], pc[:, :], eng='act')
        tokf = lambda tt: slice(tt * 128, (tt + 1) * 128)

        def f0(i):
            for u in range(2):
                tt = 2 * i + u
                pa = ps[(i % 2) * 2 + u]
                for k in range(8):
                    P.mm(pa[:, :], self.hT[:, k, tokf(tt)], w_nsa[:, k, g * 512:(g + 1) * 512], start=(k == 0), stop=(k == 7))
                if g == 0:
                    for k in range(8):
                        P.mm(ps[6][:, u * 24:(u + 1) * 24], self.hT[:, k, tokf(tt)], w_nsa[:, k, 1280:1304],
                             start=(k == 0 and u == 0), stop=(k == 7), skip_group_check=True)

        def f1(i):
            for u in range(2):
                tt = 2 * i + u
                pa = ps[(i % 2) * 2 + u]
                R = pa[:, 0:384].rearrange('p (h d) -> p h d', d=64)
                P.act(sq[i % 2][:, u], R, AF.Square)
                P.copy(rc[i % 3][:, u], R, eng='act')
                P.copy(v2[:, tt, :, 0:64], pa[:, 384:512].rearrange('p (a d) -> p a d', d=64), eng='act')
            if g == 0:
                P.copy(gates[:, 2 * i:2 * i + 2, :], ps[6][:, 0:48].rearrange('p (u c) -> p u c', c=24))

        def f2(i):
            P.reduce(st6[i % 2][:, 0, :], sq[i % 2][:].rearrange('p u h d -> p (u h) d'), ALU.add)

        def f3(i):
            P.act(st6[i % 2][:, 1, :], st6[i % 2][:, 0, :], AF.Ln, scale=1.0 / DH, bias=EPS)
            P.act(st6[i % 2][:, 2, :], st6[i % 2][:, 1, :], AF.Exp, scale=-0.5)

        def f4(i):
            i2 = i % 2
            rnv = rn[i2][:].rearrange('p u h d -> p (u h) d')
            P.tt(rnv, rc[i % 3][:].rearrange('p u h d -> p (u h) d'),
                 st6[i2][:, 2, :].unsqueeze(2).to_broadcast([128, 12, 64]), ALU.mult)
            P.tt(rn[i2][:], rn[i2][:], self.gfull[:].unsqueeze(1).to_broadcast([128, 2, 6, 64]), ALU.mult)
            cosb = self.cosT[:, 2 * i:2 * i + 2, :].unsqueeze(2).to_broadcast([128, 2, 6, 32])
            sinb = self.sinT[:, 2 * i:2 * i + 2, :].unsqueeze(2).to_broadcast([128, 2, 6, 32])
            x1 = rn[i2][:, :, :, 0:32]
            x2 = rn[i2][:, :, :, 32:64]
            tm = tmp[i2]
            P.tt(tm[:, 0], x1, cosb, ALU.mult)
            P.tt(tm[:, 1], x2, sinb, ALU.mult)
            P.tt(tm[:, 2], x1, sinb, ALU.mult, eng='pool')
            P.tt(tm[:, 3], x2, cosb, ALU.mult, eng='pool')
            P.tt(rr[i2][:, :, :, 0:32], tm[:, 0], tm[:, 1], ALU.subtract)
            P.tt(rr[i2][:, :, :, 32:64], tm[:, 2], tm[:, 3], ALU.add, eng='pool')

        def f5(i):
            for u in range(2):
                pt_ = ps[4 + u].bitcast(BF16)
                for hh in range(6):
                    P.tr(pt_[0:64, hh * 128:(hh + 1) * 128], rr[i % 2][:, u, hh, :], self.identb[:])

        def f6(i):
            for u in range(2):
                tt = 2 * i + u
                pt_ = ps[4 + u].bitcast(BF16)
                tok = tokf(tt)
                P.copy(qaug[0:64, :, tok], pt_[0:64, 0:512].rearrange('p (h t) -> p h t', t=128), eng='act')
                P.copy(kslc[0:64, tok], pt_[0:64, 512:640], eng='act')
                P.copy(kwin[0:64, tok], pt_[0:64, 640:768], eng='act')

        pipeline(NP, [f0, f1, f2, f3, f4, f5, f6], reverse=True)
        if g == 0:
            P.act(gates[:].rearrange('p a b -> p (a b)'), gates[:].rearrange('p a b -> p (a b)'), AF.Sigmoid)
        hid = T2('hid', [128, 2, 2, 128], BF16)
        kc4 = T2('kc4', [128, 8, 64], F32)
        kst = T2('kst', [128, 4], F32)
        kcr = T2('kcr', [128, 64], BF16)
        ktm = T2('ktm', [128, 4, 32], F32)
        for kv in range(2):
            base = 64 * kv
            pz = ps[5 + kv]
            BR.reset(pz)
            for hc in range(2):
                for l in range(32):
                    P.mm(pz[:, hc * 128:hc * 128 + 127], self.w1kv[base:base + 64, l, hc * 128:(hc + 1) * 128],
                         kvcT[base:base + 64, l:l + 16 * 126 + 1:16], start=BR.start(pz), stop=(l == 31),
                         skip_group_check=True)
            for hc in range(2):
                P.act(hid[:, kv, hc, 0:127], pz[:, hc * 128:hc * 128 + 127], AF.Silu, bias=self.ckv[:, kv, hc:hc + 1])
        p2 = ps[7]
        BR.reset(p2)
        for kv in range(2):
            for hc in range(2):
                P.mm(p2[0:127, kv * 64:(kv + 1) * 64], hid[:, kv, hc, 0:127], self.w2kv[:, kv, hc, :],
                     start=BR.start(p2), stop=(hc == 1), skip_group_check=True)
        P.copy(vcaug[0:127, 0:64], p2[0:127, 64:128], eng='act')
        P.act(kc4[0:127, 0, :], p2[0:127, 0:64], AF.Square)
        P.reduce(kst[0:127, 0:1], kc4[0:127, 0, :], ALU.add)
        P.act(kst[0:127, 1:2], kst[0:127, 0:1], AF.Ln, scale=1.0 / DH, bias=EPS)
        P.act(kst[0:127, 2:3], kst[0:127, 1:2], AF.Exp, scale=-0.5)
        P.stt(kc4[0:127, 1, :], p2[0:127, 0:64], kst[0:127, 2:3], self.gk[0:127, :], ALU.mult, ALU.mult)
        x1 = kc4[0:127, 1, 0:32]
        x2 = kc4[0:127, 1, 32:64]
        cC = self.cosC[0:127, 0, :]
        sC = self.sinC[0:127, 0, :]
        P.tt(ktm[0:127, 0], x1, cC, ALU.mult)
        P.tt(ktm[0:127, 1], x2, sC, ALU.mult)
        P.tt(ktm[0:127, 2], x1, sC, ALU.mult)
        P.tt(ktm[0:127, 3], x2, cC, ALU.mult)
        P.tt(kcr[0:127, 0:32], ktm[0:127, 0], ktm[0:127, 1], ALU.subtract)
        P.tt(kcr[0:127, 32:64], ktm[0:127, 2], ktm[0:127, 3], ALU.add)
        pk = ps[3].bitcast(BF16)
        P.tr(pk[0:64, 0:127], kcr[0:127, :], self.identb[0:127, 0:127])
        P.copy(kcT[:, 0:127], pk[0:64, 0:127])
        if b == 0:
            self.dump('qaug%d' % g, qaug[0:64].rearrange('p h s -> p (h s)'))
            self.dump('kslc%d' % g, kslc[0:64, :])
            self.dump('kwin%d' % g, kwin[:, :])
            self.dump('kcT%d' % g, kcT[:, :])
            self.dump('vc%d' % g, vcaug[:, 0:64])
        self.sync_phase()
    self.nsa_attention(b, g, st, qaug, kwin, kcT, gates)


KB.phase2_nsa = _kb_phase2_nsa
KB.nsa_group = _kb_nsa_group


def _kb_nsa_attention(self, b, g, st, qaug, kwin, kcT, gates):
    nc, P, ps = self.nc, self.P, self.ps
    BR = self.br
    self.emit_conv(-(-len(self.conv_list) // (2 * self.NSEQ)))
    self.emit_fill(-(-(self.NSLOT // 128) // (2 * self.NSEQ)))
    kslc, v2, vcaug = self.kslc, self.v2, self.vcaug
    with ExitStack() as s3:
        T = lambda n, s, dt: self.T(s3, n, s, dt)
        ptile = [T('ptile%d' % i, [128, 512], BF16) for i in range(4)]
        oacc = T('oacc', [128, NT, 4, 64], F32)
        impacc = T('impacc', [128, NT, 32], F32)
        rz = [T('rz%d' % i, [128, 2, 4], F32) for i in range(3)]
        otmp = [T('otmp%d' % i, [128, 4, 64], F32) for i in range(2)]
        itmp = [T('itmp%d' % i, [128, 4, 32], F32) for i in range(2)]
        scw = [T('scw%d' % i, [128, 4, 32], F32) for i in range(2)]
        slw = [T('slw%d' % i, [128, 4, 32], F32) for i in range(2)]
        m8 = [T('m8%d' % i, [128, 4, 8], F32) for i in range(2)]
        biasb = [T('biasb%d' % i, [128, 4, 32], BF16) for i in range(2)]
        ob = [T('ob%d' % i, [128, 256], BF16) for i in range(2)]
        sbank = [ps[0], ps[1], ps[2]]
        pvbank = [ps[3], ps[4]]
        misc = ps[5]
        otb = [ps[6], ps[7]]
        cnt = {'s': 0, 'pv': 0, 'fin': 0}

        def finalize(pvb, hl, br, qc, ncol, first, want_imp, first_imp):
            h = 4 * g + hl
            k_ = cnt['fin']
            cnt['fin'] += 1
            r = rz[k_ % 3]
            pv3 = pvb[:, 0:4 * ncol].rearrange('p (q c) -> p q c', c=ncol)
            P.ts(r[:, 0, :], pv3[:, :, 64], 1e-30, None, ALU.max)
            P.recip(r[:, 0, :], r[:, 0, :])
            P.tt(r[:, 1, :], r[:, 0, :], gates[:, qc * 4:(qc + 1) * 4, br * 8 + h], ALU.mult)
            tgt = oacc[:, qc * 4:(qc + 1) * 4, hl, :]
            sb_ = r[:, 1, :].unsqueeze(2).to_broadcast([128, 4, 64])
            if first:
                P.tt(tgt, pv3[:, :, 0:64], sb_, ALU.mult)
            else:
                ot = otmp[k_ % 2]
                P.tt(ot[:], pv3[:, :, 0:64], sb_, ALU.mult)
                P.tt(tgt, tgt, ot[:], ALU.add)
            if want_imp:
                itg = impacc[:, qc * 4:(qc + 1) * 4, :]
                rb_ = r[:, 0, :].unsqueeze(2).to_broadcast([128, 4, 32])
                if first_imp:
                    P.tt(itg, pv3[:, :, 65:97], rb_, ALU.mult)
                else:
                    it = itmp[k_ % 2]
                    P.tt(it[:], pv3[:, :, 65:97], rb_, ALU.mult)
                    P.tt(itg, itg, it[:], ALU.add)

        def selection(qc):
            sc, sl, m_, bb = scw[qc % 2], slw[qc % 2], m8[qc % 2], biasb[qc % 2]
            tq = slice(qc * 4, (qc + 1) * 4)
            P.tt(sc[:], impacc[:, tq, :], self.cand[:, tq, :], ALU.mult)
            P.tt(sc[:], sc[:], self.candm1[:, tq, :], ALU.add)
            for qt in range(4):
                P.max8(m_[:, qt, :], sc[:, qt, :])
            P.tt(sl[:], sc[:], m_[:, :, 4].unsqueeze(2).to_broadcast([128, 4, 32]), ALU.is_ge)
            P.tt(sl[:], sl[:], self.forced[:, tq, :], ALU.max)
            P.ts(bb[:], sl[:], 1.0, BIG, ALU.subtract, ALU.mult)
            mb = misc.bitcast(BF16)
            for qt in range(4):
                P.tr(mb[0:32, qt * 128:(qt + 1) * 128], bb[:, qt, :], self.identb[:])
            P.copy(qaug[64:96, :, qc * 512:(qc + 1) * 512],
                   mb[0:32, 0:512].unsqueeze(1).to_broadcast([32, 4, 512]), eng='act')
            if b == 0:
                for qt in range(4):
                    self.dump('sel%d_%d' % (g, qc * 4 + qt), sl[:, qt, :])

        astate = {}

        def c0(i):
            qc, hl = i // 4, i % 4
            k_ = cnt['s']
            cnt['s'] += 1
            astate[i] = (sbank[k_ % 3], ptile[k_ % 3])
            P.mm(astate[i][0][0:127, :], kcT[0:64, 0:127], qaug[0:64, hl, qc * 512:(qc + 1) * 512], start=True, stop=True)

        def c1(i):
            sb, pt = astate[i]
            P.act(pt[0:127, :], sb[0:127, :], AF.Exp)

        def c2(i):
            qc = i // 4
            sb, pt = astate[i]
            P.tt(pt[0:127, :], pt[0:127, :], self.cmpvalid[0:127, qc * 512:(qc + 1) * 512], ALU.mult)

        def c3(i):
            sb, pt = astate[i]
            pvb = pvbank[cnt['pv'] % 2]
            cnt['pv'] += 1
            BR.reset(pvb)
            for qt in range(4):
                P.mm(pvb[:, qt * 97:(qt + 1) * 97], pt[0:127, qt * 128:(qt + 1) * 128], vcaug[0:127, :],
                     start=BR.start(pvb), stop=True, skip_group_check=True)
            astate[i] = pvb

        def c4(i):
            qc, hl = i // 4, i % 4
            finalize(astate.pop(i), hl, 0, qc, 97, True, True, hl == 0)
            if hl == 3:
                selection(qc)

        pipeline(16, [c0, c1, c2, c3, c4])

        steps = []
        for qc in range(4):
            for hl in range(4):
                kbs = list(range(max(0, 4 * qc - 4), 4 * qc + 4))
                for kb in kbs:
                    if kb < 4 * qc:
                        j = kb - (4 * qc - 4)
                        c0_, c1_, mask = 0, 128 * (j + 1), ('anti', 128 * j)
                    else:
                        j = kb - 4 * qc
                        c0_, c1_, mask = 128 * j, 512, ('tri', 128 * j)
                    steps.append(dict(br=2, hl=hl, kb=kb, c0=c0_, c1=c1_, mask=mask, qc=qc, firsth=(kb == kbs[0]), last=(kb == kbs[-1])))
            for hl in range(4):
                kbs = list(range(0, 4 * qc + 4))
                for kb in kbs:
                    if kb < 4 * qc:
                        c0_, c1_, mask = 0, 512, None
                    else:
                        j = kb - 4 * qc
                        c0_, c1_, mask = 128 * j, 512, ('tri', 128 * j)
                    steps.append(dict(br=1, hl=hl, kb=kb, c0=c0_, c1=c1_, mask=mask, qc=qc, firsth=(kb == kbs[0]), last=(kb == kbs[-1])))
        n = len(steps)
        state = {}

        def qk(i):
            s_ = steps[i]
            k_ = cnt['s']
            cnt['s'] += 1
            sb = sbank[k_ % 3]
            pt = ptile[k_ % 4]
            hl, kb, c0_, c1_, br = s_['hl'], s_['kb'], s_['c0'], s_['c1'], s_['br']
            q0 = s_['qc'] * 512
            if br == 1:
                lhsT = kslc[0:96, kb * 128:(kb + 1) * 128]
                rhs = qaug[0:96, hl, q0 + c0_:q0 + c1_]
            else:
                lhsT = kwin[0:64, kb * 128:(kb + 1) * 128]
                rhs = qaug[0:64, hl, q0 + c0_:q0 + c1_]
            P.mm(sb[:, c0_:c1_], lhsT, rhs, start=True, stop=True)
            P.act(pt[:, c0_:c1_], sb[:, c0_:c1_], AF.Exp)
            if s_['mask'] is not None:
                kind, col = s_['mask']
                mk = self.trib if kind == 'tri' else self.antib
                P.tt(pt[:, col:col + 128], pt[:, col:col + 128], mk[:], ALU.mult)
            state[i] = pt

        def pv(i):
            s_ = steps[i]
            pt = state.pop(i)
            hl, kb, c0_, c1_, br = s_['hl'], s_['kb'], s_['c0'], s_['c1'], s_['br']
            if s_['firsth']:
                state['pvb'] = pvbank[cnt['pv'] % 2]
                cnt['pv'] += 1
                BR.reset(state['pvb'])
            pvb = state['pvb']
            for qt in range(c0_ // 128, c1_ // 128):
                P.mm(pvb[:, qt * 65:(qt + 1) * 65], pt[:, qt * 128:(qt + 1) * 128], v2[:, kb, br - 1, :],
                     start=BR.start(pvb), stop=True, skip_group_check=True)
            if s_['last']:
                finalize(pvb, hl, br, s_['qc'], 65, False, False, False)

        AHEAD = 3
        for i in range(min(AHEAD, n)):
            qk(i)
        for i in range(n):
            if i + AHEAD < n:
                qk(i + AHEAD)
            pv(i)

        def o0(tg):
            P.copy(ob[tg % 2][:], oacc[:, tg].rearrange('p h d -> p (h d)'), eng='act')

        def o1(tg):
            pb = otb[tg % 2].bitcast(BF16)
            for pr in range(2):
                P.tr(pb[:, pr * 128:(pr + 1) * 128], ob[tg % 2][:, pr * 128:(pr + 1) * 128], self.identb[:])

        def o2(tg):
            pb = otb[tg % 2].bitcast(BF16)
            P.copy(self.o_aT[:, 2 * g:2 * g + 2, tg * 128:(tg + 1) * 128],
                   pb[:, 0:256].rearrange('p (c t) -> p c t', t=128), eng='dve')

        pipeline(NT, [o0, o1, o2])


KB.nsa_attention = _kb_nsa_attention


def _kb_prep_weights(self, st):
    P = self.P
    wv = self.w_in[0].rearrange('(k p) c -> p k c', p=128)
    T = lambda n, s, dt: self.T(st, n, s, dt)
    stg = [T('pw_stage%d' % i, [128, 8 * 1304], BF16) for i in range(1)]
    w_nsa = stg[0][:, 0:8 * 1304].rearrange('p (k c) -> p k c', c=1304)
    for g in range(2):
        o = g * 512
        for (dst, src, n) in ((0, 256 * g, 256), (256, 768 + 64 * g, 64), (320, 1024 + 64 * g, 64),
                              (384, 896 + 64 * g, 64), (448, 1152 + 64 * g, 64)):
            P.dma(w_nsa[:, :, o + dst:o + dst + n], wv[:, :, src:src + n], q='pool')
        P.dma(w_nsa[:, :, 1024 + 128 * g:1024 + 128 * g + 64], wv[:, :, 512 + 64 * g:512 + 64 * g + 64], q='pool')
        P.dma(w_nsa[:, :, 1024 + 128 * g + 64:1024 + 128 * g + 128], wv[:, :, 640 + 64 * g:640 + 64 * g + 64], q='pool')
    P.dma(w_nsa[:, :, 1280:1304], wv[:, :, 1280:1304], q='pool')
    P.dma(self.w_nsa_d, stg[0][:, 0:8 * 1304])


def _kb_prep_bg(self):
    P = self.P
    wv = self.w_in[0].rearrange('(k p) c -> p k c', p=128)
    dil_d = self.w_dil_d.rearrange('p (k c) -> p k c', c=1152)
    for gi in range(3):
        for pi, base in enumerate((1304, 1688, 2072)):
            P.dma(dil_d[:, :, gi * 384 + pi * 128:gi * 384 + (pi + 1) * 128],
                  wv[:, :, base + 128 * gi:base + 128 * gi + 128], q='pool', bg=True)
    gm_d = self.w_gm_d.rearrange('p (k c) -> p k c', c=2048)
    for q4 in range(4):
        P.dma(gm_d[:, :, q4 * 512:(q4 + 1) * 512], wv[:, :, 2456 + q4 * 512:2456 + (q4 + 1) * 512], q='pool', bg=True)
    P.dma(self.w_upa_d.rearrange('p (c n) -> p c n', n=D), self.w_up_a[0].rearrange('(c p) n -> p c n', p=128), q='pool', bg=True)
    P.dma(self.w_upb_d.rearrange('p (c n) -> p c n', n=D), self.w_up_b[0].rearrange('(c p) n -> p c n', p=128), q='pool', bg=True)
    od = self.w_o_d.rearrange('p (k n) -> p k n', n=D)
    ov = self.w_out[0].rearrange('(k p) n -> p k n', p=128)
    for hk in range(2):
        P.dma(od[:, hk * 4:(hk + 1) * 4, :], ov[:, hk * 4:(hk + 1) * 4, :], q='pool', bg=True)


KB.prep_bg = _kb_prep_bg
KB.prep_weights = _kb_prep_weights


def _kb_build(self, upto=99):
    nc, P = self.nc, self.P
    self.declare()
    self.br = BankRound()
    with ExitStack() as top:
        self.top = top
        T = lambda n, s, dt: self.T(top, n, s, dt)
        self.ps = [top.enter_context(nc.psum_tensor("ps%d" % i, [128, 512], F32)) for i in range(8)]
        self.identf = T('identf', [128, 128], F32)
        self.identb = T('identb', [128, 128], BF16)
        self.invf = T('invf', [128, 32], F32)
        self.trib = T('trib', [128, 128], BF16)
        self.antib = T('antib', [128, 128], BF16)
        self.triantib = T('triantib', [128, 256], BF16)
        self.cmpvalid = T('cmpvalid', [128, S], BF16)
        self.cand = T('cand', [128, NT, 32], BF16)
        self.candm1 = T('candm1', [128, NT, 32], BF16)
        self.forced = T('forced', [128, NT, 32], BF16)
        self.lstrict = T('lstrict', [128, 128], BF16)
        self.onesb = T('onesb', [128, 128], BF16)
        P.dma(self.identf[:], self.cd['identf'])
        P.dma(self.identb[:], self.cd['identf'], q='pool')
        P.dma(self.invf[:], self.cd['invf'])
        P.dma(self.trib[:], self.cd['tri'], q='pool')
        P.dma(self.antib[:], self.cd['anti'], q='pool')
        P.dma(self.triantib[:], self.cd['trianti'], q='pool')
        P.dma(self.cmpvalid[:], self.cd['cmpvalid'], q='pool')
        P.dma(self.cand[:], self.cd['cand'], q='pool')
        P.dma(self.candm1[:], self.cd['candm1'], q='pool')
        P.dma(self.forced[:], self.cd['forced'], q='pool')
        P.dma(self.lstrict[:], self.cd['lstrict'], q='pool')
        P.dma(self.onesb[:], self.cd['ones'], q='pool')
        self.modT1 = T('modT1', [128, 16, 4], F32)
        if upto >= 4:
            self.setup_moe_consts()
        self.phase0(prep=(upto >= 2))
        with ExitStack() as mix:
            self.top = mix
            Tm = lambda n, s, dt: self.T(mix, n, s, dt)
            self.hT = Tm('hT', [128, 8, S], BF16)
            self.o_aT = Tm('o_aT', [128, 4, S], BF16)
            self.o_bT = Tm('o_bT', [128, 3, S], BF16)
            if upto >= 2:
                self.setup_nsa_consts()
            for b in range(self.NSEQ):
                if upto >= 1:
                    self.phase1(b)
                if upto >= 2:
                    with ExitStack() as st_seq:
                        self.seq_prologue(st_seq, b)
                        self.phase2_nsa(b, st_seq)
                        if b == 0:
                            for c in range(4):
                                self.dump('oaT%d' % c, self.o_aT[:, c, :])
                        if upto >= 3:
                            self.phase2_dil(b, st_seq)
                            if b == 0:
                                for c in range(3):
                                    self.dump('obT%d' % c, self.o_bT[:, c, :])
                        if upto >= 4:
                            self.phase3(b, st_seq)
                        self.sync_phase()
            self.sync_phase()
        self.top = top
        if upto >= 5:
            self.phase4()
        P.final_barrier = True
        P.barrier()
        P.flush()
    return nc


KB.build = _kb_build


DIL_D = (1, 4, 16)


def _kb_phase2_dil(self, b, st_seq):
    nc, P, ps = self.nc, self.P, self.ps
    BR = self.br
    wv = self.w_in[0].rearrange('(k p) c -> p k c', p=128)
    with ExitStack() as st:
        T = lambda n, s, dt: self.T(st, n, s, dt)
        w_dil = T('w_dil', [128, 8, 1152], BF16)
        P.dma(w_dil[:].rearrange('p k c -> p (k c)'), self.w_dil_d, q='act')
        us = T('us', [128, 3, S], BF16)
        ztot = T('ztot', [128, S], F32)
        gfb = T('gfb', [128, 4, 64], F32)
        for hh in range(2):
            P.copy(gfb[:, hh, :], self.gqb[:])
            P.copy(gfb[:, 2 + hh, :], self.gkb[:])
        for gi in range(3):
            d = DIL_D[gi]
            with ExitStack() as sg:
                T2 = lambda n, s, dt: self.T(sg, n, s, dt)
                qbT = T2('qbT', [64, 2, S], BF16)
                kbT = T2('kbT', [64, 2, S], BF16)
                vb = T2('vb', [128, 16, 128], BF16)
                sq = [T2('dsq%d' % i, [128, 2, 4, 64], F32) for i in range(1)] * 2
                rc = [T2('drc%d' % i, [128, 2, 4, 64], F32) for i in range(3)]
                rn = T2('drn', [128, 2, 4, 64], F32)
                tmp = T2('dtmp', [128, 4, 2, 4, 32], F32)
                rr = [T2('drr%d' % i, [128, 2, 4, 64], BF16) for i in range(2)]
                st4 = [T2('dst%d' % i, [128, 3, 8], F32) for i in range(2)]
                ptile = [T2('dpt%d' % i, [128, 512], BF16) for i in range(3)]
                tokf = lambda tt: slice(tt * 128, (tt + 1) * 128)

                def d0(i):
                    for u in range(2):
                        tt = 2 * i + u
                        pa = ps[(i % 2) * 2 + u]
                        for k in range(8):
                            P.mm(pa[:, 0:256], self.hT[:, k, tokf(tt)], w_dil[:, k, gi * 384:gi * 384 + 256], start=(k == 0), stop=(k == 7))

                def d1(i):
                    for u in range(2):
                        R = ps[(i % 2) * 2 + u][:, 0:256].rearrange('p (h d) -> p h d', d=64)
                        P.act(sq[i % 2][:, u], R, AF.Square)
                        P.copy(rc[i % 3][:, u], R, eng='act')

                def d2(i):
                    P.reduce(st4[i % 2][:, 0, :], sq[i % 2][:].rearrange('p u h d -> p (u h) d'), ALU.add)

                def d3(i):
                    P.act(st4[i % 2][:, 1, :], st4[i % 2][:, 0, :], AF.Ln, scale=1.0 / DH, bias=EPS)
                    P.act(st4[i % 2][:, 2, :], st4[i % 2][:, 1, :], AF.Exp, scale=-0.5)

                def d4(i):
                    P.tt(rn[:].rearrange('p u h d -> p (u h) d'), rc[i % 3][:].rearrange('p u h d -> p (u h) d'),
                         st4[i % 2][:, 2, :].unsqueeze(2).to_broadcast([128, 8, 64]), ALU.mult)
                    P.tt(rn[:], rn[:], gfb[:].unsqueeze(1).to_broadcast([128, 2, 4, 64]), ALU.mult)
                    cosb = self.cosT[:, 2 * i:2 * i + 2, :].unsqueeze(2).to_broadcast([128, 2, 4, 32])
                    sinb = self.sinT[:, 2 * i:2 * i + 2, :].unsqueeze(2).to_broadcast([128, 2, 4, 32])
                    x1 = rn[:, :, :, 0:32]
                    x2 = rn[:, :, :, 32:64]
                    P.tt(tmp[:, 0], x1, cosb, ALU.mult)
                    P.tt(tmp[:, 1], x2, sinb, ALU.mult)
                    P.tt(tmp[:, 2], x1, sinb, ALU.mult, eng='pool')
                    P.tt(tmp[:, 3], x2, cosb, ALU.mult, eng='pool')
                    P.tt(rr[i % 2][:, :, :, 0:32], tmp[:, 0], tmp[:, 1], ALU.subtract)
                    P.tt(rr[i % 2][:, :, :, 32:64], tmp[:, 2], tmp[:, 3], ALU.add, eng='pool')

                def d5(i):
                    pt_ = ps[4].bitcast(BF16)
                    for u in range(2):
                        for hh in range(4):
                            P.tr(pt_[0:64, (u * 4 + hh) * 128:(u * 4 + hh + 1) * 128], rr[i % 2][:, u, hh, :], self.identb[:])

                def d6(i):
                    pt_ = ps[4].bitcast(BF16)
                    for u in range(2):
                        tt = 2 * i + u
                        P.copy(qbT[:, :, tokf(tt)], pt_[0:64, u * 512:u * 512 + 256].rearrange('p (h t) -> p h t', t=128), eng='act')
                        P.copy(kbT[:, :, tokf(tt)], pt_[0:64, u * 512 + 256:u * 512 + 512].rearrange('p (h t) -> p h t', t=128), eng='act')

                pipeline(NT // 2, [d0, d1, d2, d3, d4, d5, d6], reverse=True)
                nkb = 16 // d
                for r in range(d):
                    for kbs in range(nkb):
                        bi = r * nkb + kbs
                        pvp = ps[4 + bi % 2]
                        s0 = 128 * kbs * d + r
                        for k in range(8):
                            P.mm(pvp[:, 0:128], self.hT[:, k, s0:s0 + 127 * d + 1:d], w_dil[:, k, gi * 384 + 256:gi * 384 + 384],
                                 start=(k == 0), stop=(k == 7))
                        P.copy(vb[:, bi, :], pvp[:, 0:128], eng='act')
                rounds = []
                if d == 1:
                    for Rn_ in range(4):
                        steps = []
                        for kbs in range(max(0, 4 * Rn_ - 1), 4 * Rn_ + 4):
                            if kbs < 4 * Rn_:
                                steps.append(dict(r=0, kbs=kbs, q0=4 * Rn_, nq=1, slot=0, mask='anti'))
                            elif kbs < 4 * Rn_ + 3:
                                steps.append(dict(r=0, kbs=kbs, q0=kbs, nq=2, slot=kbs - 4 * Rn_, mask='trianti'))
                            else:
                                steps.append(dict(r=0, kbs=kbs, q0=kbs, nq=1, slot=3, mask='tri'))
                        rounds.append(dict(steps=steps, out=('contig', 512 * Rn_)))
                elif d == 4:
                    for r in range(4):
                        steps = []
                        for kbs in range(4):
                            if kbs < 3:
                                steps.append(dict(r=r, kbs=kbs, q0=kbs, nq=2, slot=kbs, mask='trianti'))
                            else:
                                steps.append(dict(r=r, kbs=kbs, q0=kbs, nq=1, slot=3, mask='tri'))
                        rounds.append(dict(steps=steps, out=('strided', r)))
                else:
                    for Rr in range(4):
                        steps = [dict(r=4 * Rr + i, kbs=0, q0=0, nq=1, slot=i, mask='tri') for i in range(4)]
                        rounds.append(dict(steps=steps, out=('res16', 4 * Rr)))
                items = []
                for rd_i, rd in enumerate(rounds):
                    for si, s_ in enumerate(rd['steps']):
                        for j in range(2):
                            items.append((rd_i, s_, j, si == len(rd['steps']) - 1 and j == 1))
                dstate = {}
                dcnt = {'s': 0}
                dstarted = {}

                def dqk(ii):
                    rd_i, s_, j, _ = items[ii]
                    r, kbs, q0, nq = s_['r'], s_['kbs'], s_['q0'], s_['nq']
                    k0 = 128 * kbs * d + r
                    qs0 = 128 * q0 * d + r
                    ncol = 128 * nq
                    sb = ps[dcnt['s'] % 4]
                    pt = ptile[dcnt['s'] % 3]
                    dcnt['s'] += 1
                    P.mm(sb[:, 0:ncol], kbT[0:64, j, k0:k0 + 127 * d + 1:d],
                         qbT[0:64, j, qs0:qs0 + (ncol - 1) * d + 1:d], start=True, stop=True)
                    P.act(pt[:, 0:ncol], sb[:, 0:ncol], AF.Exp)
                    mk = {'tri': self.trib[:], 'anti': self.antib[:], 'trianti': self.triantib[:]}[s_['mask']]
                    P.tt(pt[:, 0:ncol], pt[:, 0:ncol], mk, ALU.mult)
                    dstate[ii] = pt

                def dpv(ii):
                    rd_i, s_, j, last = items[ii]
                    rd = rounds[rd_i]
                    pt = dstate.pop(ii)
                    r, kbs, nq, slot = s_['r'], s_['kbs'], s_['nq'], s_['slot']
                    bi = r * nkb + kbs
                    ncol = 128 * nq
                    c0 = slot * 128
                    pu = ps[4 + (rd_i % 2) * 2]
                    pz = ps[5 + (rd_i % 2) * 2]
                    first = not dstarted.get((rd_i, j), False)
                    dstarted[(rd_i, j)] = True
                    P.mm(pu[64 * j:64 * j + 64, c0:c0 + ncol], vb[:, bi, 64 * j:64 * j + 64], pt[:, 0:ncol],
                         start=first, stop=True, skip_group_check=True)
                    P.mm(pz[64 * j:64 * j + 64, c0:c0 + ncol], self.onesb[:, 0:64], pt[:, 0:ncol],
                         start=first, stop=True, skip_group_check=True)
                    if last:
                        kind, o0 = rd['out']
                        if kind == 'contig':
                            uo = us[:, gi, o0:o0 + 512]
                            zo = ztot[:, o0:o0 + 512]
                            pui, pzi = pu[:, :], pz[:, :]
                        elif kind == 'strided':
                            uo = us[:, gi, o0:o0 + 511 * 4 + 1:4]
                            zo = ztot[:, o0:o0 + 511 * 4 + 1:4]
                            pui, pzi = pu[:, :], pz[:, :]
                        else:
                            uo = us[:, gi, :].rearrange('p (k r) -> p r k', r=16)[:, o0:o0 + 4, :]
                            zo = ztot[:, :].rearrange('p (k r) -> p r k', r=16)[:, o0:o0 + 4, :]
                            pui = pu[:, :].rearrange('p (s k) -> p s k', k=128)
                            pzi = pz[:, :].rearrange('p (s k) -> p s k', k=128)
                        P.copy(uo, pui, eng='act')
                        if gi == 0:
                            P.copy(zo, pzi)
                        else:
                            P.tt(zo, pzi, zo, ALU.add)

                AH = 2
                nit = len(items)
                for ii in range(min(AH, nit)):
                    dqk(ii)
                for ii in range(nit):
                    if ii + AH < nit:
                        dqk(ii + AH)
                    dpv(ii)
                self.sync_phase()
        P.act(ztot[:], ztot[:], AF.Ln)
        P.act(ztot[:], ztot[:], AF.Exp, scale=-1.0)
        for gi in range(3):
            P.tt(self.o_bT[:, gi, :], us[:, gi, :], ztot[:], ALU.mult)
        self.sync_phase()


KB.phase2_dil = _kb_phase2_dil


def _kb_setup_moe_consts(self):
    P, top = self.P, self.top
    T = lambda n, s, dt: self.T(top, n, s, dt)
    NTT = self.NTT
    self.wr = T('wr', [128, 8, 36], F32)
    with self.nc.allow_non_contiguous_dma(reason="tiny router weight rows"):
        P.dma(self.wr[:, :, 0:4], self.w_group[0].rearrange('(k p) g -> p k g', p=128))
        for gg in range(4):
            P.dma(self.wr[:, :, 4 + 8 * gg:12 + 8 * gg], self.w_router[0, gg].rearrange('(k p) e -> p k e', p=128))
    self.brow = T('brow', [128, 36], F32)
    P.dma(self.brow[:, 0:4], self.b_group.to_broadcast([128, 4]))
    P.dma(self.brow[:, 4:36], self.b_router[0:1].rearrange('o g e -> o (g e)').to_broadcast([128, 32]))
    self.EH = [T('EH%d' % k, [128, NTT, 32], BF16) for k in range(2)]
    self.rank = T('rank', [128, NTT, 2], F32)
    self.wts = T('wts', [128, NTT, 2], F32)
    self.lgall = [T('lgall%d' % i, [128, NT, 36], F32) for i in range(2)]
    self.rt_tiles = dict(rt=T('rt', [128, NT, 16], F32), gw=T('gw', [128, 6, NT], F32), sel4=T('sel4', [128, NT, 4, 8], F32),
                         sel=T('selr', [128, NT, 8], F32), m8a=T('m8a', [128, NT, 8], F32), oh=T('ohr', [128, 2, NT, 8], F32),
                         eh12=T('eh12a', [128, NT, 32], BF16), pre=T('pre', [128, NT, 32], F32), big=T('bigr', [128, NT, 32], F32))
    self.pending = []
    self.zrow = T('zrow', [128, D], BF16)
    P.memset(self.zrow[:], 0.0)
    self.fill_pos = 0
    self.carry = T('carry', [128, 32], F32)
    P.memset(self.carry[:], 0.0)


KB.setup_moe_consts = _kb_setup_moe_consts


def _kb_phase3(self, b, st_seq):
    nc, P, ps = self.nc, self.P, self.ps
    with ExitStack() as st:
        T = lambda n, s, dt: self.T(st, n, s, dt)
        w_gm = T('w_gm', [128, 8, 2048], BF16)
        w_upa = T('w_upa', [128, 4, D], BF16)
        w_upb = T('w_upb', [128, 3, D], BF16)
        P.dma(w_upa[:].rearrange('p k c -> p (k c)'), self.w_upa_d, q='act')
        P.dma(w_upb[:].rearrange('p k c -> p (k c)'), self.w_upb_d, q='act')
        P.dma(w_gm[:].rearrange('p k c -> p (k c)'), self.w_gm_d)
        gm = [T('gm%d' % i, [128, 2, 512], F32) for i in range(2)]
        ybf = [T('ybf%d' % i, [128, 512], BF16) for i in range(2)]
        tokf = lambda tt: slice(tt * 128, (tt + 1) * 128)

        def a0(i):
            tt, hf = i // 2, i % 2
            pa_, pb_ = (ps[0], ps[1]) if i % 2 == 0 else (ps[5], ps[6])
            cs = slice(hf * 512, (hf + 1) * 512)
            for c in range(4):
                P.mm(pa_[:, :], self.o_aT[:, c, tokf(tt)], w_upa[:, c, cs], start=(c == 0), stop=(c == 3))
            for c in range(3):
                P.mm(pb_[:, :], self.o_bT[:, c, tokf(tt)], w_upb[:, c, cs], start=(c == 0), stop=(c == 2))
            for q2 in range(2):
                cg = slice(q2 * 1024 + hf * 512, q2 * 1024 + (hf + 1) * 512)
                for k in range(8):
                    P.mm(ps[2 + q2][:, :], self.hT[:, k, tokf(tt)], w_gm[:, k, cg], start=(k == 0), stop=(k == 7))

        def a1(i):
            for q2 in range(2):
                P.act(gm[i % 2][:, q2, :], ps[2 + q2][:, :], AF.Sigmoid)

        def a2(i):
            pa_, pb_ = (ps[0], ps[1]) if i % 2 == 0 else (ps[5], ps[6])
            g_ = gm[i % 2]
            P.tt(g_[:, 0, :], g_[:, 0, :], pa_[:, :], ALU.mult)
            P.tt(g_[:, 1, :], g_[:, 1, :], pb_[:, :], ALU.mult)
            P.tt(ybf[i % 2][:], g_[:, 0, :], g_[:, 1, :], ALU.add)

        def a3(i):
            pb = ps[4].bitcast(BF16)
            for k in range(4):
                P.tr(pb[:, k * 128:(k + 1) * 128], ybf[i % 2][:, k * 128:(k + 1) * 128], self.identb[:])

        def a4(i):
            tt, hf = i // 2, i % 2
            pb = ps[4].bitcast(BF16)
            P.copy(self.hT[:, 4 * hf:4 * hf + 4, tokf(tt)], pb[:, 0:512].rearrange('p (k t) -> p k t', t=128), eng='act')

        pipeline(2 * NT, [a0, a1, a2, a3, a4], reverse=True)
        self.sync_phase()
    with ExitStack() as st:
        T = lambda n, s, dt: self.T(st, n, s, dt)
        w_o = T('w_o', [128, 8, D], BF16)
        P.dma(w_o[:].rearrange('p k c -> p (k c)'), self.w_o_d)
        gt1 = T('gt1', [128, D], F32)
        A2 = T('A2', [128, D], F32)
        sh2 = T('sh2', [128, D], F32)
        P.dma(gt1[:], self.mod_d[b:b + 1, 2 * D:3 * D].to_broadcast([128, D]), q='act')
        P.dma(sh2[:], self.mod_d[b:b + 1, 3 * D:4 * D].to_broadcast([128, D]), q='act')
        P.dma(A2[:], self.mod_d[b:b + 1, 4 * D:5 * D].to_broadcast([128, D]), q='act')
        xt = [T('x3t%d' % i, [128, D], F32) for i in range(2)]
        x1 = [T('x1t%d' % i, [128, D], F32) for i in range(2)]
        h2 = [T('h2t%d' % i, [128, D], F32) for i in range(2)]
        junk = T('p3junk', [128, D], F32)
        h2T = [T('h2T%d' % i, [128, 8, 128], F32) for i in range(2)]
        sm = [T('sm%d' % i, [128, 4], F32) for i in range(2)]
        lgall = self.lgall[b % 2]
        tokf = lambda tt: slice(tt * 128, (tt + 1) * 128)

        def b0(tt):
            r0 = b * S + tt * 128
            P.dma(xt[tt % 2][:], self.x[r0:r0 + 128, :], q=('sp' if tt % 2 == 0 else 'act'))

        def b1(tt):
            for hf in range(2):
                for k in range(8):
                    P.mm(ps[hf][:, :], self.hT[:, k, tokf(tt)], w_o[:, k, hf * 512:(hf + 1) * 512], start=(k == 0), stop=(k == 7))

        def b2(tt):
            i2 = tt % 2
            r0 = b * S + tt * 128
            for hf in range(2):
                sl = slice(hf * 512, (hf + 1) * 512)
                P.tt(x1[i2][:, sl], ps[hf][:, :], gt1[:, sl], ALU.mult)
                P.tt(x1[i2][:, sl], x1[i2][:, sl], xt[i2][:, sl], ALU.add)
            P.dma(self.x1_d[r0:r0 + 128, :], x1[i2][:])

        def b3(tt):
            s_ = sm[tt % 2]
            P.act(junk[:], x1[tt % 2][:], AF.Square, accum_out=s_[:, 0:1])
            P.act(s_[:, 1:2], s_[:, 0:1], AF.Ln, scale=1.0 / D, bias=EPS)
            P.act(s_[:, 2:3], s_[:, 1:2], AF.Exp, scale=-0.5)

        def b4(tt):
            i2 = tt % 2
            r0 = b * S + tt * 128
            P.stt(h2[i2][:], x1[i2][:], sm[i2][:, 2:3], A2[:], ALU.mult, ALU.mult)
            P.tt(h2[i2][:], h2[i2][:], sh2[:], ALU.add)
            P.dma(self.h2_d[r0:r0 + 128, :], h2[i2][:], q='pool')

        def b5(tt):
            for k in range(8):
                pb = ps[2 + k // 4]
                P.tr(pb[:, (k % 4) * 128:(k % 4 + 1) * 128], h2[tt % 2][:, k * 128:(k + 1) * 128], self.identf[:])

        def b6(tt):
            for hf in range(2):
                P.copy(h2T[tt % 2][:, hf * 4:(hf + 1) * 4, :], ps[2 + hf][:, :].rearrange('p (k t) -> p k t', t=128),
                       eng=('act' if hf == 0 else 'dve'))

        def b7(tt):
            for k in range(8):
                P.mm(ps[4][:, 0:36], h2T[tt % 2][:, k, :], self.wr[:, k, :], start=(k == 0), stop=(k == 7))

        def b8(tt):
            P.tt(lgall[:, tt, :], ps[4][:, 0:36], self.brow[:], ALU.add)

        pipeline(NT, [b0, b1, b2, b3, b4, b5, b6, b7, b8], reverse=True)
        T0 = b * NT
        R_ = self.rt_tiles
        rt, gw, sel4, sel, m8a, oh, eh12, pre, big = (R_['rt'], R_['gw'], R_['sel4'], R_['sel'], R_['m8a'], R_['oh'],
                                                      R_['eh12'], R_['pre'], R_['big'])
        lgs = self.lgall[b % 2]
        th = []
        A = th.append
        G4 = lgs[:, :, 0:4]
        A(lambda: P.reduce(gw[:, 0, :], G4, ALU.max))
        A(lambda: P.tt(rt[:, :, 0:4], G4, gw[:, 0, :].unsqueeze(2).to_broadcast([128, NT, 4]), ALU.subtract))
        A(lambda: P.act(rt[:, :, 4:8], rt[:, :, 0:4], AF.Exp))
        A(lambda: P.reduce(gw[:, 1, :], rt[:, :, 4:8], ALU.add))
        A(lambda: P.recip(gw[:, 1, :], gw[:, 1, :]))
        A(lambda: P.tt(rt[:, :, 8:12], G4, gw[:, 0, :].unsqueeze(2).to_broadcast([128, NT, 4]), ALU.is_equal))
        goh = rt[:, :, 8:12]
        A(lambda: P.tt(sel4[:], lgs[:, :, 4:36].rearrange('p t (g e) -> p t g e', e=8),
                       goh.unsqueeze(3).to_broadcast([128, NT, 4, 8]), ALU.mult))
        A(lambda: P.reduce(sel[:], sel4[:].rearrange('p t g e -> p t e g'), ALU.add))
        for t in range(NT):
            A(lambda t=t: P.max8(m8a[:, t, :], sel[:, t, :]))
        A(lambda: P.tt(gw[:, 2, :], m8a[:, :, 1], m8a[:, :, 0], ALU.subtract))
        A(lambda: P.act(gw[:, 3, :], gw[:, 2, :], AF.Exp))
        A(lambda: P.ts(gw[:, 4, :], gw[:, 3, :], 1.0, None, ALU.add))
        A(lambda: P.recip(gw[:, 4, :], gw[:, 4, :]))
        A(lambda: P.tt(gw[:, 5, :], gw[:, 3, :], gw[:, 4, :], ALU.mult))
        A(lambda: P.tt(self.wts[:, T0:T0 + NT, 0], gw[:, 4, :], gw[:, 1, :], ALU.mult))
        A(lambda: P.tt(self.wts[:, T0:T0 + NT, 1], gw[:, 5, :], gw[:, 1, :], ALU.mult))
        for kk in range(2):
            A(lambda kk=kk: P.tt(oh[:, kk], sel[:], m8a[:, :, kk].unsqueeze(2).to_broadcast([128, NT, 8]), ALU.is_equal))
            A(lambda kk=kk: P.tt(self.EH[kk][:, T0:T0 + NT, :].rearrange('p t (g e) -> p t g e', e=8),
                                 goh.unsqueeze(3).to_broadcast([128, NT, 4, 8]),
                                 oh[:, kk].unsqueeze(2).to_broadcast([128, NT, 4, 8]), ALU.mult))
        A(lambda: P.tt(eh12[:], self.EH[0][:, T0:T0 + NT, :], self.EH[1][:, T0:T0 + NT, :], ALU.add))
        A(lambda: P.mm(ps[7][:, :], self.lstrict[:], eh12[:].rearrange('p t e -> p (t e)'), start=True, stop=True))
        A(lambda: P.copy(pre[:].rearrange('p t e -> p (t e)'), ps[7][:, :]))
        A(lambda: P.mm(ps[7][:, :], self.onesb[:], eh12[:].rearrange('p t e -> p (t e)'), start=True, stop=True))
        A(lambda: P.copy(big[:].rearrange('p t e -> p (t e)'), ps[7][:, :]))
        for t in range(NT):
            A(lambda t=t: P.tt(pre[:, t, :], pre[:, t, :], self.carry[:], ALU.add))
            A(lambda t=t: P.tt(self.carry[:], self.carry[:], big[:, t, :], ALU.add))
        for kk in range(2):
            A(lambda kk=kk: P.tt(big[:], self.EH[kk][:, T0:T0 + NT, :], pre[:], ALU.mult))
            A(lambda kk=kk: P.reduce(self.rank[:, T0:T0 + NT, kk], big[:], ALU.add))
        self.pending = th
        if b == 0:
            self.dump('lg0', lgall[:, 0, :])
        self.sync_phase()


KB.phase3 = _kb_phase3


def _kb_phase4(self):
    nc, P, ps = self.nc, self.P, self.ps
    NTT, NBLK = self.NTT, self.NBLK
    wg_v = self.w_e_gate[0].rearrange('e r f -> (e r) f')
    wu_v = self.w_e_up[0].rearrange('e r f -> (e r) f')
    wd_v = self.w_e_down[0].rearrange('e r f -> (e r) f')
    IOA = bass.IndirectOffsetOnAxis
    with ExitStack() as st:
        T = lambda n, s, dt: self.T(st, n, s, dt)
        self.drain_pending()
        dest = T('dest', [128, 2, NTT], I32)
        widx = T('widx', [128, NBLK], I32)
        with ExitStack() as s2:
            T2 = lambda n, s, dt: self.T(s2, n, s, dt)
            cnt = T2('cnt', [128, 6, 32], F32)
            onesf = T2('onesf', [128, 32], F32)
            big = T2('bigtmp', [128, NTT, 32], F32)
            thr = T2('thr', [128, NBLK, 32], F32)
            cmp_ = T2('cmpb', [128, NBLK, 32], F32)
            be = T2('be', [128, 2, NBLK], F32)
            rgu = T2('rgu', [128, 12], F32)
            df = T2('df', [128, 2, NTT], F32)
            P.dma(thr[:].rearrange('p a b -> p (a b)'), self.cd['thr'])
            P.memset(onesf[:], 1.0)
            P.copy(cnt[:, 0, :], self.carry[:])
            P.ts(cnt[:, 1, :], cnt[:, 0, :], float(MB - 1), 1.0 / MB, ALU.add, ALU.mult)
            P.ts(cnt[:, 1, :], cnt[:, 1, :], -0.498, MAGIC, ALU.add, ALU.add)
            P.ts(cnt[:, 1, :], cnt[:, 1, :], MAGIC, float(MB), ALU.subtract, ALU.mult)
            P.add('dve', lambda e: e.tensor_tensor_scan(cnt[:, 2, :], onesf[:], cnt[:, 1, :], 0.0, ALU.mult, ALU.add),
                  [onesf[:], cnt[:, 1, :]], [cnt[:, 2, :]])
            P.tt(cnt[:, 3, :], cnt[:, 2, :], cnt[:, 1, :], ALU.subtract)
            for kk in range(2):
                P.tt(big[:], self.EH[kk][:], cnt[:, 3, :].unsqueeze(1).to_broadcast([128, NTT, 32]), ALU.mult)
                P.reduce(df[:, kk, :], big[:], ALU.add)
                P.tt(df[:, kk, :], df[:, kk, :], self.rank[:, :, kk], ALU.add)
            P.copy(dest[:], df[:])
            P.tt(cmp_[:], cnt[:, 2, :].unsqueeze(1).to_broadcast([128, NBLK, 32]), thr[:], ALU.is_le)
            P.reduce(be[:, 0, :], cmp_[:], ALU.add)
            P.ts(be[:, 0, :], be[:, 0, :], float(NEXP - 1), None, ALU.min)
            P.dma(rgu[:], self.cd['rowoff'])
            P.ts(be[:, 1, :], be[:, 0, :], 128.0, rgu[:, 0:1], ALU.mult, ALU.add)
            P.copy(widx[:], be[:, 1, :])
            self.dump('dest', df[:].rearrange('p a b -> p (a b)'))
            self.dump('be', be[:, 0, :])
            self.dump('cnt', cnt[:].rearrange('p a b -> p (a b)'))
            self.sync_phase()
        with ExitStack() as s2:
            T2 = lambda n, s, dt: self.T(s2, n, s, dt)
            hb = [T2('h2b%d' % i, [128, D], BF16) for i in range(3)]
            for Tg in range(NTT):
                h_ = hb[Tg % 3]
                P.dma(h_[:], self.h2_d[Tg * 128:(Tg + 1) * 128, :], q='sp')
                for kk in range(2):
                    ia = dest[:, kk, Tg:Tg + 1]
                    P.add('pool', (lambda h_=h_, ia=ia: (lambda e: e.indirect_dma_start(
                        out=self.xs_d, out_offset=IOA(ap=ia, axis=0), in_=h_[:, :], in_offset=None)))(),
                        [h_[:], ia], [], dma=True)
            self.sync_phase()
        with ExitStack() as s2:
            T2 = lambda n, s, dt: self.T(s2, n, s, dt)
            wg = [T2('wg%d' % i, [128, 8, FF], BF16) for i in range(2)]
            wu = [T2('wu%d' % i, [128, 8, FF], BF16) for i in range(2)]
            wd = [T2('wd%d' % i, [128, 4, D], BF16) for i in range(2)]
            xsb = [T2('xsb%d' % i, [128, 2, D], BF16) for i in range(2)]
            xT = [T2('xT%d' % i, [128, 8, MB], BF16) for i in range(2)]
            sg = [T2('sg%d' % i, [128, 4, MB], F32) for i in range(2)]
            hidT = [T2('hidT%d' % i, [128, 4, MB], BF16) for i in range(2)]
            yb = [T2('yb%d' % i, [128, 2, D], BF16) for i in range(2)]

            def load_w(blk, which):
                i2 = blk % 2
                ia = widx[:, blk:blk + 1]
                lst = ((wg[i2], self.wg_l), (wu[i2], self.wu_l)) if which == 0 else ((wd[i2], self.wd_l),)
                for (wt, src) in lst:
                    P.add('pool', (lambda wt=wt, src=src, ia=ia: (lambda e: e.indirect_dma_start(
                        out=wt[:].rearrange('p k f -> p (k f)'), out_offset=None, in_=src, in_offset=IOA(ap=ia, axis=0))))(),
                        [ia], [wt[:]], dma=True)

            def m0(blk):
                r0 = blk * MB
                P.dma(xsb[blk % 2][:], self.xs_d[r0:r0 + MB, :].rearrange('(t p) d -> p t d', p=128), q='act')

            def m1(blk):
                load_w(blk, 0)
                for t2 in range(2):
                    pb = ps[t2].bitcast(BF16)
                    for k in range(8):
                        P.tr(pb[:, k * 128:(k + 1) * 128], xsb[blk % 2][:, t2, k * 128:(k + 1) * 128], self.identb[:])

            def m2(blk):
                for t2 in range(2):
                    pb = ps[t2].bitcast(BF16)
                    P.copy(xT[blk % 2][:, :, t2 * 128:(t2 + 1) * 128], pb[:, :].rearrange('p (k t) -> p k t', t=128),
                           eng=('act' if t2 == 0 else 'dve'))

            def m3(blk):
                i2 = blk % 2
                load_w(blk, 1)
                for f in range(4):
                    pg_ = ps[2 + f // 2]
                    pu_ = ps[4 + f // 2]
                    cs = slice((f % 2) * MB, (f % 2 + 1) * MB)
                    for k in range(8):
                        P.mm(pg_[:, cs], wg[i2][:, k, f * 128:(f + 1) * 128], xT[i2][:, k, :], start=(k == 0 and f % 2 == 0),
                             stop=(k == 7), skip_group_check=True)
                    for k in range(8):
                        P.mm(pu_[:, cs], wu[i2][:, k, f * 128:(f + 1) * 128], xT[i2][:, k, :], start=(k == 0 and f % 2 == 0),
                             stop=(k == 7), skip_group_check=True)

            def m4(blk):
                i2 = blk % 2
                for f2 in range(2):
                    P.act(sg[i2][:, 2 * f2:2 * f2 + 2, :], ps[2 + f2][:, :].rearrange('p (f t) -> p f t', t=MB), AF.Silu)
                    P.tt(hidT[i2][:, 2 * f2:2 * f2 + 2, :], sg[i2][:, 2 * f2:2 * f2 + 2, :],
                         ps[4 + f2][:, :].rearrange('p (f t) -> p f t', t=MB), ALU.mult)

            def m5(blk):
                i2 = blk % 2
                for t2 in range(2):
                    for hf in range(2):
                        py = ps[6 + hf]
                        for f in range(4):
                            P.mm(py[:, :], hidT[i2][:, f, t2 * 128:(t2 + 1) * 128], wd[i2][:, f, hf * 512:(hf + 1) * 512],
                                 start=(f == 0), stop=(f == 3))
                        P.copy(yb[i2][:, t2, hf * 512:(hf + 1) * 512], py[:, :], eng=('act' if hf == 0 else 'dve'))

            def m6(blk):
                r0 = blk * MB
                P.dma(self.ys_d[r0:r0 + MB, :].rearrange('(t p) d -> p t d', p=128), yb[blk % 2][:], q='act')

            pipeline(NBLK, [m0, m1, m2, m3, m4, m5, m6], reverse=True)
            self.sync_phase()
        with ExitStack() as s2:
            T2 = lambda n, s, dt: self.T(s2, n, s, dt)
            ND = 3
            y0 = [T2('y0_%d' % i, [128, D], BF16) for i in range(ND)]
            y1 = [T2('y1_%d' % i, [128, D], BF16) for i in range(ND)]
            yo = [T2('yo_%d' % i, [128, D], F32) for i in range(ND)]
            x1 = [T2('x1f%d' % i, [128, D], F32) for i in range(ND)]
            gt2 = [T2('gt2_%d' % i, [128, D], F32) for i in range(2)]
            for Tg in range(NTT):
                i2 = Tg % ND
                b = Tg // NT
                if Tg % NT == 0:
                    P.dma(gt2[b % 2][:], self.mod_d[b:b + 1, 5 * D:6 * D].to_broadcast([128, D]))
                for kk, yt in ((0, y0[i2]), (1, y1[i2])):
                    ia = dest[:, kk, Tg:Tg + 1]
                    P.add('pool', (lambda yt=yt, ia=ia: (lambda e: e.indirect_dma_start(
                        out=yt[:, :], out_offset=None, in_=self.ys_d, in_offset=IOA(ap=ia, axis=0))))(),
                        [ia, self.ys_d], [yt[:]], dma=True)
                P.dma(x1[i2][:], self.x1_d[Tg * 128:(Tg + 1) * 128, :], q='sp')
                P.ts(yo[i2][:], y0[i2][:], self.wts[:, Tg, 0:1], None, ALU.mult)
                P.stt(yo[i2][:], y1[i2][:], self.wts[:, Tg, 1:2], yo[i2][:], ALU.mult, ALU.add)
                P.tt(yo[i2][:], yo[i2][:], gt2[b % 2][:], ALU.mult)
                P.tt(yo[i2][:], yo[i2][:], x1[i2][:], ALU.add)
                P.dma(self.out[Tg * 128:(Tg + 1) * 128, :], yo[i2][:], q='act')
            self.sync_phase()


KB.phase4 = _kb_phase4


N_CORES = 8
_CACHE = {}

_WEIGHT_KEYS = ['w_ada', 'b_ada', 'norm1_g', 'norm2_g', 'w_in', 'nsa_q_norm', 'nsa_k_norm', 'cmp_pe_k', 'cmp_w1_k',
                'cmp_w2_k', 'cmp_pe_v', 'cmp_w1_v', 'cmp_w2_v', 'dil_q_norm', 'dil_k_norm', 'w_up_a', 'w_up_b', 'w_out',
                'w_group', 'b_group', 'w_router', 'b_router', 'w_e_gate', 'w_e_up', 'w_e_down']


def kernel(**inputs):
    x = np.asarray(inputs['x'], dtype=np.float32)
    c = np.asarray(inputs['c'], dtype=np.float32)
    pos = np.asarray(inputs['positions'], dtype=np.int32)
    B = x.shape[0]
    nseq = B // N_CORES
    if 'kb' not in _CACHE:
        kb = KB(nseq)
        kb.build()
        _CACHE['kb'] = kb
    kb = _CACHE['kb']
    w = {k: np.ascontiguousarray(np.asarray(inputs[k], dtype=np.float32)) for k in _WEIGHT_KEYS}
    in_maps = []
    for i in range(N_CORES):
        m = dict(w)
        m['x'] = np.ascontiguousarray(x[i * nseq:(i + 1) * nseq].reshape(nseq * S, D))
        m['c'] = np.ascontiguousarray(c[i * nseq:(i + 1) * nseq])
        m['positions'] = np.ascontiguousarray(pos[i * nseq:(i + 1) * nseq])
        for k, v in kb.consts.items():
            m['k_' + k] = v
        in_maps.append(m)
    res = run_bass_kernel_spmd(kb.nc, in_maps, core_ids=list(range(N_CORES)))
    out = np.concatenate([np.asarray(r['out']).reshape(nseq, S, D) for r in res.results], axis=0)
    return out.astype(np.float32)
```
